# Optimizing a Trainium2 kernel written in Bass

```python
import math
import jax
import jax.numpy as jnp
from jax import lax
import numpy as np

D_MODEL = 1024
BATCH = 2
SEQ = 8192
DEPTH = 4

CTX_LEN = 256
GRID_W = 64
EPS = 1e-6
ROPE_BASE = 10000.0
Q_BLOCK = 128
N_MOD = 6
F32 = jnp.float32

GROUP_WIDTH = D_MODEL // 4
MIX_WIDTH = 4 * GROUP_WIDTH

MLA_V = 64
MLA_HEADS = GROUP_WIDTH // MLA_V
MLA_NOPE = 64
MLA_ROPE = 32
MLA_QK = MLA_NOPE + MLA_ROPE
MLA_KV_RANK = 128

SSD_HEAD_DIM = 64
SSD_INNER = GROUP_WIDTH
SSD_HEADS = SSD_INNER // SSD_HEAD_DIM
SSD_GROUPS = 2
SSD_STATE = 64
SSD_CONV = 3
SSD_CHUNK = 128
SSD_GN = SSD_GROUPS * SSD_STATE
SSD_XBC = SSD_INNER + 2 * SSD_GN

S5_WIDTH = GROUP_WIDTH
S5_GROUP = 16
S5_GROUPS = S5_WIDTH // S5_GROUP
S5_STATE = 64

DIFF_V = 64
DIFF_HEADS = GROUP_WIDTH // DIFF_V
DIFF_HEAD = DIFF_V // 2

MLA_COLS = MLA_HEADS * MLA_QK + MLA_KV_RANK + MLA_ROPE
SSD_COLS = SSD_INNER + SSD_XBC + 2 * SSD_HEADS
S5_COLS = S5_WIDTH
DIFF_COLS = 2 * DIFF_HEADS * 2 * DIFF_HEAD + DIFF_HEADS * DIFF_V
IN_COLS = MLA_COLS + SSD_COLS + S5_COLS + DIFF_COLS

MOE_GROUPS = 4
MOE_PER_GROUP = 8
N_EXPERTS = MOE_GROUPS * MOE_PER_GROUP
MOE_TOP_K = 2
EXPERT_HIDDEN = D_MODEL // 4

kernel_name = 'hybrid_mla_ssd_s5_diffattn_hmoe_dit'


def rmsnorm(x, g):
    xf = x.astype(F32)
    y = xf * lax.rsqrt(jnp.mean(xf * xf, axis=-1, keepdims=True) + EPS)
    return (y * g.astype(F32)).astype(x.dtype)


def modulate(x, g, shift, scale):
    return rmsnorm(x, g) * (1.0 + scale) + shift


def _angles(pos, dim):
    inv = ROPE_BASE ** (-jnp.arange(0, dim, 2, dtype=F32) / dim)
    return pos.astype(F32)[:, None] * inv[None, :]


def _rotate_half(x, ang):
    m = ang.shape[-1]
    cos = jnp.cos(ang)[None, :, None, :].astype(x.dtype)
    sin = jnp.sin(ang)[None, :, None, :].astype(x.dtype)
    x1, x2 = x[..., :m], x[..., m:]
    return jnp.concatenate([x1 * cos - x2 * sin, x1 * sin + x2 * cos], axis=-1)


def axial_rope(x, rows, cols):
    half = x.shape[-1] // 2
    return jnp.concatenate([_rotate_half(x[..., :half], _angles(rows, half)),
                            _rotate_half(x[..., half:], _angles(cols, half))], axis=-1)


def blocked_attention(q, k, v, scale):
    b, lq, h, dk = q.shape
    nb = lq // Q_BLOCK
    qb = q.reshape(b, nb, Q_BLOCK, h, dk).transpose(1, 0, 2, 3, 4)

    def one(qblk):
        s = jnp.einsum('bqhd,bkhd->bhqk', qblk, k).astype(F32) * scale
        p = jax.nn.softmax(s, axis=-1)
        return jnp.einsum('bhqk,bkhd->bqhd', p.astype(v.dtype), v)

    o = lax.map(one, qb)
    return o.transpose(1, 0, 2, 3, 4).reshape(b, lq, h, v.shape[-1])


def blocked_diff_attention(q1, q2, k1, k2, v, lam, scale):
    b, lq, h, dk = q1.shape
    nb = lq // Q_BLOCK
    to_blocks = lambda t: t.reshape(b, nb, Q_BLOCK, h, dk).transpose(1, 0, 2, 3, 4)

    def one(qs):
        a1, a2 = qs
        p1 = jax.nn.softmax(jnp.einsum('bqhd,bkhd->bhqk', a1, k1).astype(F32) * scale, axis=-1)
        p2 = jax.nn.softmax(jnp.einsum('bqhd,bkhd->bhqk', a2, k2).astype(F32) * scale, axis=-1)
        return jnp.einsum('bhqk,bkhd->bqhd', (p1 - lam * p2).astype(v.dtype), v)

    o = lax.map(one, (to_blocks(q1), to_blocks(q2)))
    return o.transpose(1, 0, 2, 3, 4).reshape(b, lq, h, v.shape[-1])


def centred_depthwise_conv(x, w, b):
    k = w.shape[0]
    y = lax.conv_general_dilated(x, w[:, None, :].astype(x.dtype), window_strides=(1,),
                                 padding=[(k // 2, k - 1 - k // 2)],
                                 dimension_numbers=('NWC', 'WIO', 'NWC'),
                                 feature_group_count=x.shape[-1])
    return y + b


def split_groups(p):
    o = 0
    parts = []
    for n in (MLA_COLS, SSD_COLS, S5_COLS, DIFF_COLS):
        parts.append(p[..., o:o + n])
        o += n
    return parts


def mla_mixer(p_c, p_l, rows, cols, kv_norm, w_uk, w_uv, q_norm, k_norm, need_ctx):
    nq = MLA_HEADS * MLA_QK

    def project(p):
        b, l, _ = p.shape
        q = rmsnorm(p[..., :nq].reshape(b, l, MLA_HEADS, MLA_QK), q_norm)
        ckv = rmsnorm(p[..., nq:nq + MLA_KV_RANK], kv_norm)
        k_rope = jnp.broadcast_to(p[..., nq + MLA_KV_RANK:][:, :, None, :], (b, l, MLA_HEADS, MLA_ROPE))
        k_nope = (ckv @ w_uk).reshape(b, l, MLA_HEADS, MLA_NOPE)
        v = (ckv @ w_uv).reshape(b, l, MLA_HEADS, MLA_V)
        k = rmsnorm(jnp.concatenate([k_nope, k_rope], axis=-1), k_norm)
        return q, k, v

    def rope_tail(t):
        return jnp.concatenate([t[..., :MLA_NOPE], axial_rope(t[..., MLA_NOPE:], rows, cols)], axis=-1)

    qc, kc, vc = project(p_c)
    ql, kl, vl = project(p_l)
    ql, kl = rope_tail(ql), rope_tail(kl)
    scale = MLA_QK ** -0.5
    b, l = p_l.shape[:2]
    o_l = blocked_attention(ql, jnp.concatenate([kc, kl], 1), jnp.concatenate([vc, vl], 1), scale)
    o_l = o_l.reshape(b, l, MLA_HEADS * MLA_V)
    o_c = None
    if need_ctx:
        o_c = blocked_attention(qc, kc, vc, scale).reshape(b, p_c.shape[1], MLA_HEADS * MLA_V)
    return o_c, o_l


def ssd_scan(x, dt, a, bm, cm, h0):
    b, l, h, p = x.shape
    n = bm.shape[-1]
    nc = l // SSD_CHUNK
    xs = (x * dt[..., None]).reshape(b, nc, SSD_CHUNK, h, p)
    bc = bm.reshape(b, nc, SSD_CHUNK, h, n)
    cc = cm.reshape(b, nc, SSD_CHUNK, h, n)
    a_cum = jnp.cumsum((dt * a).reshape(b, nc, SSD_CHUNK, h), axis=2)
    mask = jnp.tril(jnp.ones((SSD_CHUNK, SSD_CHUNK), dtype=bool))[None, None, :, :, None]
    seg = a_cum[:, :, :, None, :] - a_cum[:, :, None, :, :]
    decay = jnp.exp(jnp.where(mask, seg, -jnp.inf))
    scores = jnp.einsum('bcihn,bcjhn->bcijh', cc, bc) * decay
    y_diag = jnp.einsum('bcijh,bcjhp->bcihp', scores, xs)
    decay_to_end = jnp.exp(a_cum[:, :, -1:, :] - a_cum)
    states = jnp.einsum('bclhn,bclhp->bchpn', bc * decay_to_end[..., None], xs)
    chunk_decay = jnp.exp(a_cum[:, :, -1, :])

    def step(hs, inp):
        dec, st = inp
        return hs * dec[:, :, None, None] + st, hs

    h_final, h_prev = lax.scan(step, h0.astype(F32),
                               (chunk_decay.transpose(1, 0, 2), states.transpose(1, 0, 2, 3, 4)))
    h_prev = h_prev.transpose(1, 0, 2, 3, 4)
    y_off = jnp.einsum('bclhn,bchpn->bclhp', cc * jnp.exp(a_cum)[..., None], h_prev)
    return (y_diag + y_off).reshape(b, l, h, p), h_final


def _ssd_prep(p, conv_w, conv_b, dt_bias):
    b, l, _ = p.shape
    z = p[..., :SSD_INNER]
    xbc = jax.nn.silu(centred_depthwise_conv(p[..., SSD_INNER:SSD_INNER + SSD_XBC], conv_w, conv_b))
    xs = xbc[..., :SSD_INNER].reshape(b, l, SSD_HEADS, SSD_HEAD_DIM)
    rep = SSD_HEADS // SSD_GROUPS
    bm = jnp.repeat(xbc[..., SSD_INNER:SSD_INNER + SSD_GN].reshape(b, l, SSD_GROUPS, SSD_STATE), rep, axis=2)
    cm = jnp.repeat(xbc[..., SSD_INNER + SSD_GN:].reshape(b, l, SSD_GROUPS, SSD_STATE), rep, axis=2)
    dt = jax.nn.softplus(p[..., SSD_INNER + SSD_XBC:].astype(F32).reshape(b, l, 2, SSD_HEADS)
                         + dt_bias.astype(F32))
    return z, xs, bm, cm, dt


def _ssd_out(y, xs, z, d_skip, norm_g):
    b, l = z.shape[:2]
    y = y + xs * d_skip[:, None]
    y = y.reshape(b, l, SSD_INNER) * jax.nn.silu(z)
    return rmsnorm(y, norm_g).astype(z.dtype)


def ssd_mixer(p_c, p_l, conv_w, conv_b, a_log, dt_bias, d_skip, norm_g, need_ctx):
    zc, xc, bc, cc, dtc = _ssd_prep(p_c, conv_w, conv_b, dt_bias)
    zl, xl, bl, cl, dtl = _ssd_prep(p_l, conv_w, conv_b, dt_bias)
    a = -jnp.exp(a_log.astype(F32))
    flip = lambda t: jnp.flip(t, axis=1)
    h0 = jnp.zeros((p_l.shape[0], SSD_HEADS, SSD_HEAD_DIM, SSD_STATE), F32)
    yc_f, hc_f = ssd_scan(xc, dtc[:, :, 0], a[0], bc, cc, h0)
    yl_f, _ = ssd_scan(xl, dtl[:, :, 0], a[0], bl, cl, hc_f)
    yc_b, hc_b = ssd_scan(flip(xc), flip(dtc[:, :, 1]), a[1], flip(bc), flip(cc), h0)
    yl_b, _ = ssd_scan(flip(xl), flip(dtl[:, :, 1]), a[1], flip(bl), flip(cl), hc_b)
    o_l = _ssd_out(yl_f + flip(yl_b), xl, zl, d_skip, norm_g)
    o_c = _ssd_out(yc_f + flip(yc_b), xc, zc, d_skip, norm_g) if need_ctx else None
    return o_c, o_l


def s5_discretise(lam_re, lam_im, log_step, b_re, b_im):
    step = jnp.exp(log_step)[:, None]
    er = jnp.exp(lam_re * step)
    ar = er * jnp.cos(lam_im * step)
    ai = er * jnp.sin(lam_im * step)
    nr, ni = ar - 1.0, ai
    den = lam_re * lam_re + lam_im * lam_im
    fr = (nr * lam_re + ni * lam_im) / den
    fi = (ni * lam_re - nr * lam_im) / den
    br = fr[..., None] * b_re - fi[..., None] * b_im
    bi = fr[..., None] * b_im + fi[..., None] * b_re
    return ar, ai, br, bi


def s5_scan(u, ar, ai, br, bi, s0_re, s0_im):
    l = u.shape[0]
    bu_re = jnp.einsum('gpj,lbgj->lbgp', br, u)
    bu_im = jnp.einsum('gpj,lbgj->lbgp', bi, u)
    bu_re = bu_re.at[0].add(ar * s0_re - ai * s0_im)
    bu_im = bu_im.at[0].add(ar * s0_im + ai * s0_re)
    a_re = jnp.broadcast_to(ar[None, None], (l, 1) + ar.shape)
    a_im = jnp.broadcast_to(ai[None, None], (l, 1) + ai.shape)

    def combine(e1, e2):
        a1r, a1i, b1r, b1i = e1
        a2r, a2i, b2r, b2i = e2
        return (a2r * a1r - a2i * a1i, a2r * a1i + a2i * a1r,
                a2r * b1r - a2i * b1i + b2r, a2r * b1i + a2i * b1r + b2i)

    _, _, s_re, s_im = lax.associative_scan(combine, (a_re, a_im, bu_re, bu_im), axis=0)
    return s_re, s_im


def s5_readout(s_re, s_im, c_re, c_im):
    return jnp.einsum('gjp,lbgp->lbgj', c_re, s_re) - jnp.einsum('gjp,lbgp->lbgj', c_im, s_im)


def s5_mixer(u_c, u_l, lam_re, lam_im, log_step, b_re, b_im, c_re, c_im, d_skip, w_glu, b_glu, need_ctx):
    lam_re, lam_im, log_step = lam_re.astype(F32), lam_im.astype(F32), log_step.astype(F32)
    b_re, b_im, c_re, c_im = b_re.astype(F32), b_im.astype(F32), c_re.astype(F32), c_im.astype(F32)

    def to_lbgj(u):
        b, l, _ = u.shape
        return u.astype(F32).reshape(b, l, S5_GROUPS, S5_GROUP).transpose(1, 0, 2, 3)

    uc, ul = to_lbgj(u_c), to_lbgj(u_l)
    zero = jnp.zeros((u_l.shape[0], S5_GROUPS, S5_STATE), F32)
    yc = jnp.zeros_like(uc)
    yl = jnp.zeros_like(ul)
    for d in range(2):
        rev = (lambda t: t[::-1]) if d == 1 else (lambda t: t)
        ar, ai, br, bi = s5_discretise(lam_re[d], lam_im[d], log_step[d], b_re, b_im)
        sc_re, sc_im = s5_scan(rev(uc), ar, ai, br, bi, zero, zero)
        sl_re, sl_im = s5_scan(rev(ul), ar, ai, br, bi, sc_re[-1], sc_im[-1])
        yl = yl + rev(s5_readout(sl_re, sl_im, c_re[d], c_im[d]))
        if need_ctx:
            yc = yc + rev(s5_readout(sc_re, sc_im, c_re[d], c_im[d]))

    def finish(y, u_in):
        b, l, _ = u_in.shape
        y = y.transpose(1, 0, 2, 3).reshape(b, l, S5_WIDTH) + d_skip * u_in.astype(F32)
        g = jax.nn.gelu(y)
        return (g * jax.nn.sigmoid(g @ w_glu + b_glu)).astype(u_in.dtype)

    o_l = finish(yl, u_l)
    o_c = finish(yc, u_c) if need_ctx else None
    return o_c, o_l


def diff_mixer(p_c, p_l, rows, cols, q_norm, k_norm, lq1, lk1, lq2, lk2, subln, lam_init, need_ctx):
    nq = DIFF_HEADS * 2 * DIFF_HEAD

    def project(p):
        b, l, _ = p.shape
        q = rmsnorm(p[..., :nq].reshape(b, l, 2 * DIFF_HEADS, DIFF_HEAD), q_norm)
        k = rmsnorm(p[..., nq:2 * nq].reshape(b, l, 2 * DIFF_HEADS, DIFF_HEAD), k_norm)
        v = p[..., 2 * nq:].reshape(b, l, DIFF_HEADS, DIFF_V)
        return q, k, v

    def pair(t):
        b, l = t.shape[:2]
        t = t.reshape(b, l, DIFF_HEADS, 2, DIFF_HEAD)
        return t[:, :, :, 0], t[:, :, :, 1]

    qc, kc, vc = project(p_c)
    ql, kl, vl = project(p_l)
    ql, kl = axial_rope(ql, rows, cols), axial_rope(kl, rows, cols)
    lam = (jnp.exp(jnp.sum(lq1.astype(F32) * lk1.astype(F32)))
           - jnp.exp(jnp.sum(lq2.astype(F32) * lk2.astype(F32))) + lam_init)
    scale = DIFF_HEAD ** -0.5

    def attend(q, k, v):
        q1, q2 = pair(q)
        k1, k2 = pair(k)
        o = blocked_diff_attention(q1, q2, k1, k2, v, lam, scale)
        b, l = o.shape[:2]
        return (rmsnorm(o, subln) * (1.0 - lam_init)).reshape(b, l, DIFF_HEADS * DIFF_V)

    o_l = attend(ql, jnp.concatenate([kc, kl], 1), jnp.concatenate([vc, vl], 1))
    o_c = attend(qc, kc, vc) if need_ctx else None
    return o_c, o_l


def hier_moe(h, w_group, b_group, w_expert, b_expert, w_gate, w_up, w_down):
    t = h.shape[0]
    lg = (h @ w_group + b_group).astype(F32)
    g_idx = jnp.argmax(lg, axis=-1)
    g_hot = jax.nn.one_hot(g_idx, MOE_GROUPS, dtype=F32)
    p_group = jnp.sum(jax.nn.softmax(lg, axis=-1) * g_hot, axis=-1, keepdims=True)
    le = (jnp.einsum('td,gde->tge', h, w_expert) + b_expert).astype(F32)
    le = jnp.einsum('tg,tge->te', g_hot, le)
    top_v, top_i = lax.top_k(le, MOE_TOP_K)
    w_top = jax.nn.softmax(top_v, axis=-1) * p_group
    inner = jnp.einsum('tk,tke->te', w_top, jax.nn.one_hot(top_i, MOE_PER_GROUP, dtype=F32))
    gates = (g_hot[:, :, None] * inner[:, None, :]).reshape(t, N_EXPERTS).astype(h.dtype)
    out = jnp.zeros_like(h)
    for e in range(N_EXPERTS):
        hid = jax.nn.silu(h @ w_gate[e]) * (h @ w_up[e])
        out = out + gates[:, e:e + 1] * (hid @ w_down[e])
    return out


def setup_inputs(seed: int = 0) -> dict:
    key = jax.random.key(seed)
    keys = list(jax.random.split(key, 64))
    nk = lambda: keys.pop()
    nrm = lambda shape, s: jax.random.normal(nk(), shape, F32) * s
    gain = lambda shape: 1.0 + 0.05 * jax.random.normal(nk(), shape, F32)
    unif = lambda shape, lo, hi: jax.random.uniform(nk(), shape, F32, lo, hi)
    dm = D_MODEL
    inp = {}
    inp['x'] = nrm((BATCH, SEQ, dm), 1.0)
    inp['c'] = nrm((BATCH, dm), 1.0)
    inp['ctx'] = nrm((BATCH, CTX_LEN, dm), 1.0)
    inp['c_ctx'] = nrm((dm,), 1.0)
    inp['w_ada'] = nrm((DEPTH, dm, N_MOD * dm), 0.5 * dm ** -0.5)
    inp['b_ada'] = nrm((DEPTH, N_MOD * dm), 0.02)
    inp['norm1'] = gain((DEPTH, dm))
    inp['norm2'] = gain((DEPTH, dm))
    inp['w_in'] = nrm((DEPTH, dm, IN_COLS), dm ** -0.5)
    inp['w_out'] = nrm((DEPTH, MIX_WIDTH, dm), MIX_WIDTH ** -0.5)
    inp['mla_kv_norm'] = gain((DEPTH, MLA_KV_RANK))
    inp['mla_w_uk'] = nrm((DEPTH, MLA_KV_RANK, MLA_HEADS * MLA_NOPE), MLA_KV_RANK ** -0.5)
    inp['mla_w_uv'] = nrm((DEPTH, MLA_KV_RANK, MLA_HEADS * MLA_V), MLA_KV_RANK ** -0.5)
    inp['mla_q_norm'] = gain((DEPTH, MLA_QK))
    inp['mla_k_norm'] = gain((DEPTH, MLA_QK))
    inp['ssd_conv_w'] = nrm((DEPTH, SSD_CONV, SSD_XBC), SSD_CONV ** -0.5)
    inp['ssd_conv_b'] = nrm((DEPTH, SSD_XBC), 0.02)
    inp['ssd_a_log'] = jnp.log(unif((DEPTH, 2, SSD_HEADS), 1.0, 16.0))
    dt0 = jnp.exp(unif((DEPTH, 2, SSD_HEADS), math.log(1e-3), math.log(1e-1)))
    inp['ssd_dt_bias'] = dt0 + jnp.log(-jnp.expm1(-dt0))
    inp['ssd_d'] = gain((DEPTH, SSD_HEADS))
    inp['ssd_norm'] = gain((DEPTH, SSD_INNER))
    n_idx = jnp.arange(S5_STATE, dtype=F32)
    inp['s5_lam_re'] = -0.5 + 0.01 * jax.random.normal(nk(), (DEPTH, 2, S5_GROUPS, S5_STATE), F32)
    inp['s5_lam_im'] = math.pi * n_idx + 0.01 * jax.random.normal(nk(), (DEPTH, 2, S5_GROUPS, S5_STATE), F32)
    inp['s5_log_step'] = unif((DEPTH, 2, S5_GROUPS), math.log(1e-3), math.log(1e-1))
    inp['s5_b_re'] = nrm((DEPTH, S5_GROUPS, S5_STATE, S5_GROUP), (2 * S5_GROUP) ** -0.5)
    inp['s5_b_im'] = nrm((DEPTH, S5_GROUPS, S5_STATE, S5_GROUP), (2 * S5_GROUP) ** -0.5)
    inp['s5_c_re'] = nrm((DEPTH, 2, S5_GROUPS, S5_GROUP, S5_STATE), S5_STATE ** -0.5)
    inp['s5_c_im'] = nrm((DEPTH, 2, S5_GROUPS, S5_GROUP, S5_STATE), S5_STATE ** -0.5)
    inp['s5_d'] = nrm((DEPTH, S5_WIDTH), 1.0)
    inp['s5_w_glu'] = nrm((DEPTH, S5_WIDTH, S5_WIDTH), S5_WIDTH ** -0.5)
    inp['s5_b_glu'] = nrm((DEPTH, S5_WIDTH), 0.02)
    inp['diff_q_norm'] = gain((DEPTH, DIFF_HEAD))
    inp['diff_k_norm'] = gain((DEPTH, DIFF_HEAD))
    inp['diff_lq1'] = nrm((DEPTH, DIFF_HEAD), 0.1)
    inp['diff_lk1'] = nrm((DEPTH, DIFF_HEAD), 0.1)
    inp['diff_lq2'] = nrm((DEPTH, DIFF_HEAD), 0.1)
    inp['diff_lk2'] = nrm((DEPTH, DIFF_HEAD), 0.1)
    inp['diff_subln'] = gain((DEPTH, DIFF_V))
    inp['moe_w_group'] = nrm((DEPTH, dm, MOE_GROUPS), dm ** -0.5)
    inp['moe_b_group'] = nrm((DEPTH, MOE_GROUPS), 0.01)
    inp['moe_w_expert'] = nrm((DEPTH, MOE_GROUPS, dm, MOE_PER_GROUP), dm ** -0.5)
    inp['moe_b_expert'] = nrm((DEPTH, MOE_GROUPS, MOE_PER_GROUP), 0.01)
    inp['moe_w_gate'] = nrm((DEPTH, N_EXPERTS, dm, EXPERT_HIDDEN), dm ** -0.5)
    inp['moe_w_up'] = nrm((DEPTH, N_EXPERTS, dm, EXPERT_HIDDEN), dm ** -0.5)
    inp['moe_w_down'] = nrm((DEPTH, N_EXPERTS, EXPERT_HIDDEN, dm), EXPERT_HIDDEN ** -0.5)
    return inp


def reference(x, c, ctx, c_ctx, w_ada, b_ada, norm1, norm2, w_in, w_out,
              mla_kv_norm, mla_w_uk, mla_w_uv, mla_q_norm, mla_k_norm,
              ssd_conv_w, ssd_conv_b, ssd_a_log, ssd_dt_bias, ssd_d, ssd_norm,
              s5_lam_re, s5_lam_im, s5_log_step, s5_b_re, s5_b_im, s5_c_re, s5_c_im,
              s5_d, s5_w_glu, s5_b_glu,
              diff_q_norm, diff_k_norm, diff_lq1, diff_lk1, diff_lq2, diff_lk2, diff_subln,
              moe_w_group, moe_b_group, moe_w_expert, moe_b_expert, moe_w_gate, moe_w_up, moe_w_down):
    bsz, seq, d = x.shape
    n_rows = seq // GRID_W
    rows = jnp.repeat(jnp.arange(n_rows, dtype=jnp.int32), GRID_W, total_repeat_length=seq)
    cols = jnp.broadcast_to(jnp.arange(GRID_W, dtype=jnp.int32)[None, :], (n_rows, GRID_W)).reshape(seq)
    x_lat, x_ctx = x, ctx
    for i in range(DEPTH):
        need_ctx = i < DEPTH - 1
        mod_l = (jax.nn.silu(c) @ w_ada[i] + b_ada[i]).reshape(bsz, N_MOD, 1, d)
        mod_c = (jax.nn.silu(c_ctx) @ w_ada[i] + b_ada[i]).reshape(N_MOD, d)
        p_l = modulate(x_lat, norm1[i], mod_l[:, 0], mod_l[:, 1]) @ w_in[i]
        p_c = modulate(x_ctx, norm1[i], mod_c[0], mod_c[1]) @ w_in[i]
        mla_l, ssd_l, s5_l, diff_l = split_groups(p_l)
        mla_c, ssd_c, s5_c, diff_c = split_groups(p_c)
        a_c, a_l = mla_mixer(mla_c, mla_l, rows, cols, mla_kv_norm[i], mla_w_uk[i], mla_w_uv[i],
                             mla_q_norm[i], mla_k_norm[i], need_ctx)
        b_c, b_l = ssd_mixer(ssd_c, ssd_l, ssd_conv_w[i], ssd_conv_b[i], ssd_a_log[i], ssd_dt_bias[i],
                             ssd_d[i], ssd_norm[i], need_ctx)
        s_c, s_l = s5_mixer(s5_c, s5_l, s5_lam_re[i], s5_lam_im[i], s5_log_step[i], s5_b_re[i], s5_b_im[i],
                            s5_c_re[i], s5_c_im[i], s5_d[i], s5_w_glu[i], s5_b_glu[i], need_ctx)
        lam_init = 0.8 - 0.6 * math.exp(-0.3 * i)
        f_c, f_l = diff_mixer(diff_c, diff_l, rows, cols, diff_q_norm[i], diff_k_norm[i], diff_lq1[i],
                              diff_lk1[i], diff_lq2[i], diff_lk2[i], diff_subln[i], lam_init, need_ctx)
        x_lat = x_lat + mod_l[:, 2] * (jnp.concatenate([a_l, b_l, s_l, f_l], axis=-1) @ w_out[i])
        h_l = modulate(x_lat, norm2[i], mod_l[:, 3], mod_l[:, 4]).reshape(-1, d)
        moe_p = (moe_w_group[i], moe_b_group[i], moe_w_expert[i], moe_b_expert[i],
                 moe_w_gate[i], moe_w_up[i], moe_w_down[i])
        if need_ctx:
            x_ctx = x_ctx + mod_c[2] * (jnp.concatenate([a_c, b_c, s_c, f_c], axis=-1) @ w_out[i])
            h_c = modulate(x_ctx, norm2[i], mod_c[3], mod_c[4]).reshape(-1, d)
            y = hier_moe(jnp.concatenate([h_l, h_c], axis=0), *moe_p)
            x_ctx = x_ctx + mod_c[5] * y[h_l.shape[0]:].reshape(x_ctx.shape)
            y_l = y[:h_l.shape[0]]
        else:
            y_l = hier_moe(h_l, *moe_p)
        x_lat = x_lat + mod_l[:, 5] * y_l.reshape(x_lat.shape)
    return x_lat
```

```python
import contextlib
import math
import numpy as np
import concourse.bass as bass
import concourse.mybir as mybir
from concourse.bass_utils import run_bass_kernel_spmd

F32 = mybir.dt.float32
BF16 = mybir.dt.bfloat16
I32 = mybir.dt.int32
AF = mybir.ActivationFunctionType
ALU = mybir.AluOpType
AX = mybir.AxisListType

SAME_ENGINE_SYNC = True


class Buf:
    _n = 0

    def __init__(self, t, name):
        self.t = t
        self.name = name
        Buf._n += 1
        self.id = Buf._n
        self.w = {}
        self.r = {}
        self.dcnt = {}

    def __getitem__(self, idx):
        return self.t[idx]


class Prog:
    ENG = ("pe", "act", "dve", "pool", "sp")

    def __init__(self, nc):
        self.nc = nc
        self.stack = contextlib.ExitStack()
        self.q = {e: [] for e in self.ENG}
        self.cnt = {e: 0 for e in self.ENG}
        self.seen = {e: {} for e in self.ENG}
        self.semh = {}
        self.out_events = []
        self.ninstr = 0
        self.dma_latest = {}

    def sb(self, name, shape, dt=F32):
        t = self.stack.enter_context(self.nc.sbuf_tensor(name, list(shape), dt))
        return Buf(t, name)

    def ps(self, name, shape, dt=F32):
        t = self.stack.enter_context(self.nc.psum_tensor(name, list(shape), dt))
        return Buf(t, name)

    def view(self, b, name=None):
        return Buf(b.t, name or (b.name + "_v"))

    def dram(self, name, shape, dt, kind):
        t = self.nc.dram_tensor(name, list(shape), dt, kind=kind)
        b = Buf(t, name)
        b.ap = t.ap()
        return b

    def sem(self, key):
        if key not in self.semh:
            nm = "s_" + "_".join(str(k) for k in (key if isinstance(key, tuple) else (key,)))
            self.semh[key] = self.stack.enter_context(self.nc.semaphore(nm))
        return self.semh[key]

    def push(self):
        self._saved = getattr(self, "_saved", [])
        self._saved.append(self.stack)
        self.stack = contextlib.ExitStack()

    def pop(self):
        self.barrier()
        self._deferred = getattr(self, "_deferred", [])
        self._deferred.append(self.stack)
        self.stack = self._saved.pop()

    def barrier(self):
        evs = {}
        for e in self.ENG:
            if self.cnt[e] > 0:
                evs[e] = self.cnt[e]
        for k, v in self.dma_latest.items():
            evs[k] = v
        for e in self.ENG:
            waits = []
            for k, v in evs.items():
                if k == e:
                    continue
                if self.seen[e].get(k, 0) >= v:
                    continue
                self.seen[e][k] = v
                waits.append((k, v))
            if waits:
                self.q[e].append((None, waits, None))

    def _deps(self, eng, reads, writes, own_key):
        deps = {}

        def add(k, v):
            if deps.get(k, 0) < v:
                deps[k] = v

        for b in reads:
            for k, v in b.w.items():
                add(k, v)
        for b in writes:
            for k, v in b.w.items():
                add(k, v)
            for k, v in b.r.items():
                add(k, v)
        waits = []
        for k, v in deps.items():
            if k == own_key and (k == "pe" or not SAME_ENGINE_SYNC or isinstance(k, tuple)):
                continue
            if self.seen[eng].get(k, 0) >= v:
                continue
            self.seen[eng][k] = v
            waits.append((k, v))
        return waits

    def _commit(self, reads, writes, key, val):
        for b in reads:
            if b.r.get(key, 0) < val:
                b.r[key] = val
        for b in writes:
            b.w = {key: val}
            b.r = {}

    def op(self, eng, fn, reads=(), writes=(), inc=True):
        waits = self._deps(eng, reads, writes, eng)
        if inc:
            self.cnt[eng] += 1
            val = self.cnt[eng]
        else:
            assert eng == "pe"
            val = self.cnt[eng] + 1
        self.q[eng].append((fn, waits, (eng, 1) if inc else None))
        self._commit(reads, writes, eng, val)
        self.ninstr += 1

    def dma(self, eng, out_ap, in_ap, reads=(), writes=(), is_output=False, **kw):
        prim = None
        for b in list(writes) + list(reads):
            if not hasattr(b, "ap"):
                prim = b
                break
        if prim is None:
            prim = (list(writes) + list(reads))[0]
        key = ("d", prim.id, eng)
        waits = self._deps(eng, reads, writes, key)
        prim.dcnt[key] = prim.dcnt.get(key, 0) + 16
        val = prim.dcnt[key]
        fn = lambda e, o=out_ap, i=in_ap, kw=kw: e.dma_start(out=o, in_=i, **kw)
        self.q[eng].append((fn, waits, (key, 16)))
        self._commit(reads, writes, key, val)
        self.dma_latest[key] = val
        if is_output:
            self.out_events.append((key, val))
        self.ninstr += 1

    def emit(self, final_eng="sp"):
        nc = self.nc
        fw = {}
        for k, v in self.out_events:
            fw[k] = max(fw.get(k, 0), v)
        final_waits = list(fw.items())
        for e in self.ENG:
            self.sem(e)
        for e in self.ENG:
            for fn, waits, inc in self.q[e]:
                for k, v in waits:
                    self.sem(k)
                if fn is None:
                    continue
                if inc:
                    self.sem(inc[0])
        for k, v in final_waits:
            self.sem(k)
        qs = self.q
        semh = self.semh

        def run(engobj, e):
            for fn, waits, inc in qs[e]:
                for k, v in waits:
                    engobj.wait_ge(semh[k], v)
                if fn is None:
                    continue
                ins = fn(engobj)
                if inc:
                    ins.then_inc(semh[inc[0]], inc[1])
            if e == final_eng:
                for k, v in final_waits:
                    engobj.wait_ge(semh[k], v)

        with nc.Block() as block:
            @block.tensor
            def _(eng):
                run(eng, "pe")

            @block.scalar
            def _(eng):
                run(eng, "act")

            @block.vector
            def _(eng):
                run(eng, "dve")

            @block.gpsimd
            def _(eng):
                run(eng, "pool")

            @block.sync
            def _(eng):
                run(eng, "sp")
        self.stack.close()


D = 1024
KT = 8
NTOK = 2176
IN_COLS = 2344
EPS = 1e-6
BLOCKS = [(0, 512), (512, 512), (1024, 512), (1536, 512), (2048, 128)]
NT_IN = [(0, 512), (512, 512), (1024, 512), (1536, 512), (2048, 296)]

_CACHE = {}


def _run(key, builder, in_maps):
    if key not in _CACHE:
        _CACHE[key] = builder()
    nc = _CACHE[key]
    res = run_bass_kernel_spmd(nc, in_maps, core_ids=list(range(8)))
    return res.results


def build_stage_a():
    nc = bass.Bass("TRN2", target_bir_lowering=False)
    P = Prog(nc)
    xT = P.dram("xT", [128, KT, NTOK], F32, "ExternalInput")
    cvec = P.dram("cvec", [128, KT, 2], F32, "ExternalInput")
    w_ada = P.dram("w_ada", [D, 6 * D], F32, "ExternalInput")
    b_adaT = P.dram("b_adaT", [128, 48], F32, "ExternalInput")
    norm1T = P.dram("norm1T", [128, KT], F32, "ExternalInput")
    w_in = P.dram("w_in", [D, IN_COLS], F32, "ExternalInput")
    p_out = P.dram("p", [NTOK, IN_COLS], F32, "ExternalOutput")
    mod_out = P.dram("modT", [128, 48, 2], F32, "ExternalOutput")

    ones_bf = P.sb("ones_bf", [128, 128], BF16)
    P.op("dve", lambda e: e.memset(ones_bf[:], 1.0), writes=[ones_bf])
    cv = P.sb("cv", [128, KT, 2])
    sc = P.sb("sc", [128, KT, 2])
    P.dma("sp", cv[:], cvec.ap[:, :, :], writes=[cv])
    P.op("act", lambda e: e.activation(out=sc[:], in_=cv[:], func=AF.Silu), reads=[cv], writes=[sc])
    bada = P.sb("bada", [128, 48])
    n1 = P.sb("n1", [128, KT])
    P.dma("sp", bada[:], b_adaT.ap[:, :], writes=[bada])
    P.dma("sp", n1[:], norm1T.ap[:, :], writes=[n1])

    win = [P.sb(f"win{kt}", [128, IN_COLS], BF16) for kt in range(KT)]
    for kt in range(KT):
        P.dma("pool", win[kt][:], w_in.ap[kt * 128:(kt + 1) * 128, :], writes=[win[kt]])

    modps = P.ps("modps", [128, 48, 2])
    wab = [P.sb(f"wab{i}", [128, KT, 512]) for i in range(2)]
    wv = w_ada.ap.rearrange("(kt p) n -> p kt n", p=128)
    for j in range(12):
        wb = wab[j % 2]
        P.dma("sp", wb[:], wv[:, :, j * 512:(j + 1) * 512], writes=[wb])
        for cl in range(4):
            c = j * 4 + cl
            for kt in range(KT):
                P.op("pe", lambda e, wb=wb, cl=cl, kt=kt, c=c: e.matmul(
                    modps[:, c, :], lhsT=wb[:, kt, cl * 128:(cl + 1) * 128], rhs=sc[:, kt, :],
                    start=(kt == 0), stop=(kt == KT - 1)), reads=[wb, sc], writes=[modps], inc=(kt == KT - 1))
    modT = P.sb("modT_sb", [128, 48, 2])
    for r in range(2):
        P.op("dve", lambda e, r=r: e.tensor_tensor(out=modT[:, :, r], in0=modps[:, :, r], in1=bada[:, :], op=ALU.add),
             reads=[modps, bada], writes=[modT])
    P.dma("sp", mod_out.ap[:, :, :], modT[:], reads=[modT], is_output=True)
    G = P.sb("G", [128, KT, 2])
    for r in range(2):
        P.op("dve", lambda e, r=r: e.scalar_tensor_tensor(out=G[:, :, r], in0=modT[:, 8:16, r], scalar=1.0, in1=n1[:, :],
                                                          op0=ALU.add, op1=ALU.mult), reads=[modT, n1], writes=[G])

    xb = [P.sb(f"xb{i}", [128, KT, 512]) for i in range(2)]
    sq = P.sb("sq", [128, KT, 512], BF16)
    ss_ps = P.ps("ss_ps", [128, 512])
    rs = P.sb("rs", [128, 512])
    rstd = P.sb("rstd", [128, 512])
    tmp = P.sb("tmp", [128, KT, 512])
    xn = [P.sb(f"xn{i}", [128, KT, 512], BF16) for i in range(2)]
    pp = [P.ps(f"pp{i}", [128, 512]) for i in range(3)]
    psb = [P.sb(f"psb{i}", [128, IN_COLS]) for i in range(2)]
    ppi = 0
    sti = 0
    for bi, (t0, T) in enumerate(BLOCKS):
        r = 1 if bi == 4 else 0
        x = xb[bi % 2]
        xnb = xn[bi % 2]
        P.dma("sp", x[:, :, 0:T], xT.ap[:, :, t0:t0 + T], writes=[x])
        P.op("act", lambda e, x=x, T=T: e.activation(out=sq[:, :, 0:T], in_=x[:, :, 0:T], func=AF.Square), reads=[x], writes=[sq])
        for kt in range(KT):
            P.op("pe", lambda e, kt=kt, T=T: e.matmul(ss_ps[:, 0:T], lhsT=ones_bf[:, :], rhs=sq[:, kt, 0:T],
                                                      start=(kt == 0), stop=(kt == KT - 1)),
                 reads=[ones_bf, sq], writes=[ss_ps], inc=(kt == KT - 1))
        P.op("act", lambda e, T=T: e.activation(out=rs[:, 0:T], in_=ss_ps[:, 0:T], func=AF.Sqrt, scale=1.0 / D, bias=EPS),
             reads=[ss_ps], writes=[rs])
        P.op("dve", lambda e, T=T: e.reciprocal(out=rstd[:, 0:T], in_=rs[:, 0:T]), reads=[rs], writes=[rstd])
        for kt in range(KT):
            P.op("dve", lambda e, x=x, kt=kt, T=T: e.tensor_tensor(out=tmp[:, kt, 0:T], in0=x[:, kt, 0:T], in1=rstd[:, 0:T], op=ALU.mult),
                 reads=[x, rstd], writes=[tmp])
        for kt in range(KT):
            P.op("act", lambda e, xnb=xnb, kt=kt, T=T, r=r: e.activation(
                out=xnb[:, kt, 0:T], in_=tmp[:, kt, 0:T], func=AF.Identity, scale=G[:, kt, r:r + 1], bias=modT[:, kt, r:r + 1]),
                reads=[tmp, G, modT], writes=[xnb])
        for s in range(T // 128):
            pb = psb[sti % 2]
            sti += 1
            for ni, (n0, nw) in enumerate(NT_IN):
                ps = pp[ppi % 3]
                ppi += 1
                for kt in range(KT):
                    P.op("pe", lambda e, ps=ps, xnb=xnb, kt=kt, s=s, n0=n0, nw=nw: e.matmul(
                        ps[:, 0:nw], lhsT=xnb[:, kt, s * 128:(s + 1) * 128], rhs=win[kt][:, n0:n0 + nw],
                        start=(kt == 0), stop=(kt == KT - 1)), reads=[xnb, win[kt]], writes=[ps], inc=(kt == KT - 1))
                if ni % 2 == 0:
                    P.op("act", lambda e, ps=ps, pb=pb, n0=n0, nw=nw: e.activation(out=pb[:, n0:n0 + nw], in_=ps[:, 0:nw], func=AF.Copy),
                         reads=[ps], writes=[pb])
                else:
                    P.op("dve", lambda e, ps=ps, pb=pb, n0=n0, nw=nw: e.tensor_copy(out=pb[:, n0:n0 + nw], in_=ps[:, 0:nw]),
                         reads=[ps], writes=[pb])
            tok = t0 + s * 128
            P.dma("sp", p_out.ap[tok:tok + 128, :], pb[:], reads=[pb], is_output=True)
    P.emit()
    return nc


def tok_to_feat(x_tok):
    T = x_tok.shape[0]
    return np.ascontiguousarray(x_tok.reshape(T, KT, 128).transpose(2, 1, 0))


def vecT(v, n=KT):
    return np.ascontiguousarray(v.reshape(n, 128).T)


def core_tokens(x_lat_b, x_ctx_b, q):
    pad = np.zeros((64, x_lat_b.shape[1]), x_lat_b.dtype)
    return np.concatenate([x_lat_b[q * 2048:(q + 1) * 2048], x_ctx_b[q * 64:(q + 1) * 64], pad], axis=0)


def stage_a_inputs(xT_cores, c, c_ctx, w_ada_i, b_ada_i, norm1_i, w_in_i):
    maps = []
    for core in range(8):
        b = core // 4
        cvec = np.stack([vecT(c[b]), vecT(c_ctx)], axis=-1)
        maps.append({"xT": xT_cores[core], "cvec": np.ascontiguousarray(cvec), "w_ada": w_ada_i,
                     "b_adaT": vecT(b_ada_i, 48), "norm1T": vecT(norm1_i), "w_in": w_in_i})
    return maps


TS = 8448
NTILE = 66
SBLK = [(0, 256)] + [(256 + 512 * i, 512) for i in range(16)]


def bview(P, ap, name):
    b = Buf(ap, name)
    return b


def build_stage_attn():
    nc = bass.Bass("TRN2", target_bir_lowering=False)
    P = Prog(nc)
    dr = {}
    for nm, shp in [("mq", [96, TS]), ("mckv", [128, TS]), ("mkr", [32, TS]), ("dq", [64, TS]), ("dk", [64, TS]),
                    ("dv", [128, NTILE, 64]), ("c96", [96, TS]), ("s96", [96, TS]), ("c64", [64, TS]), ("s64", [64, TS]),
                    ("ones96", [96, 96]), ("ones128", [128, 128]), ("bones64", [64, 64]), ("perm96", [96, 96]), ("perm64", [64, 64]),
                    ("wuk", [128, 64]), ("wuv", [128, 64]),
                    ("gq", [96, 1]), ("gk", [96, 1]), ("gkv", [128, 1]), ("gdq", [64, 1]), ("gdk", [64, 1]),
                    ("lamv", [128, 4, 32]), ("laminit", [128, 2]), ("gsub", [128, 64])]:
        dr[nm] = P.dram(nm, shp, F32, "ExternalInput")
    o_m = P.dram("o_m", [128, NTILE, 64], F32, "ExternalOutput")
    o_d = P.dram("o_d", [128, NTILE, 64], F32, "ExternalOutput")

    banks = [P.ps(f"bank{i}", [128, 512]) for i in range(8)]

    def cload(nm, shp, dt=BF16):
        t = P.sb(nm + "_sb", shp, dt)
        P.dma("pool", t[:], dr[nm].ap, writes=[t])
        return t
    ones96 = cload("ones96", [96, 96]); ones128 = cload("ones128", [128, 128]); bones64 = cload("bones64", [64, 64])
    perm96 = cload("perm96", [96, 96]); perm64 = cload("perm64", [64, 64])
    wuk = cload("wuk", [128, 64]); wuv = cload("wuv", [128, 64])
    gq = cload("gq", [96, 1], F32); gk = cload("gk", [96, 1], F32); gkv = cload("gkv", [128, 1], F32)
    gdq = cload("gdq", [64, 1], F32); gdk = cload("gdk", [64, 1], F32)
    lamv = cload("lamv", [128, 4, 32], F32); laminit = cload("laminit", [128, 2], F32); gsub = cload("gsub", [128, 64], F32)

    QTm = P.sb("QTm", [96, TS], BF16); KTm = P.sb("KTm", [96, TS], BF16); Vm = P.sb("Vm", [128, NTILE, 65], BF16)
    QTd = P.sb("QTd", [64, TS], BF16); KTd = P.sb("KTd", [64, TS], BF16); Vd = P.sb("Vd", [128, NTILE, 65], BF16)
    P.op("pool", lambda e: e.memset(Vm[:], 1.0), writes=[Vm])
    P.op("pool", lambda e: e.memset(Vd[:], 1.0), writes=[Vd])

    lprod = P.sb("lprod", [128, 2, 32]); lsum = P.sb("lsum", [128, 2]); lexp = P.sb("lexp", [128, 2]); lam = P.sb("lam", [128, 1]); nlam = P.sb("nlam", [128, 1])
    P.op("dve", lambda e: e.tensor_tensor(out=lprod[:, 0, :], in0=lamv[:, 0, :], in1=lamv[:, 1, :], op=ALU.mult), reads=[lamv], writes=[lprod])
    P.op("dve", lambda e: e.tensor_tensor(out=lprod[:, 1, :], in0=lamv[:, 2, :], in1=lamv[:, 3, :], op=ALU.mult), reads=[lamv], writes=[lprod])
    P.op("dve", lambda e: e.tensor_reduce(out=lsum[:, :], in_=lprod[:, :, :], axis=AX.X, op=ALU.add), reads=[lprod], writes=[lsum])
    P.op("act", lambda e: e.activation(out=lexp[:], in_=lsum[:], func=AF.Exp), reads=[lsum], writes=[lexp])
    P.op("dve", lambda e: e.tensor_tensor(out=lam[:], in0=lexp[:, 0:1], in1=lexp[:, 1:2], op=ALU.subtract), reads=[lexp], writes=[lam])
    P.op("dve", lambda e: e.tensor_tensor(out=lam[:], in0=lam[:], in1=laminit[:, 0:1], op=ALU.add), reads=[lam, laminit], writes=[lam])
    P.op("dve", lambda e: e.tensor_scalar(out=nlam[:], in0=lam[:], scalar1=-1.0, scalar2=None, op0=ALU.mult), reads=[lam], writes=[nlam])

    ss_ps = bview(P, banks[0][:, :], "ss_ps"); sw_ps = bview(P, banks[1][:, :], "sw_ps")
    kn_ps = bview(P, banks[2][:, :], "kn_ps"); v_ps = bview(P, banks[3][:, :], "v_ps")
    ss2_ps = bview(P, banks[4][:, :], "ss2_ps"); sw2_ps = bview(P, banks[5][:, :], "sw2_ps")
    src = [P.sb(f"src{i}", [128, 512]) for i in range(2)]
    sq = P.sb("sq_a", [128, 512], BF16); rs = P.sb("rs_a", [128, 512]); rstd = P.sb("rstd_a", [128, 512])
    xnrm = P.sb("xnrm", [128, 512]); xg = P.sb("xg", [128, 512], BF16)
    cb = P.sb("cb", [128, 512]); sbk = P.sb("sbk", [128, 512]); t1 = P.sb("t1", [128, 512]); t2 = P.sb("t2", [128, 512])
    ckvn = P.sb("ckvn", [128, 512], BF16); kfull = P.sb("kfull", [96, 512])
    dvs = P.sb("dvs", [128, NTILE, 64])
    P.dma("sp", dvs[:], dr["dv"].ap, writes=[dvs])
    P.op("dve", lambda e: e.tensor_copy(out=Vd[:, :, 0:64], in_=dvs[:]), reads=[dvs], writes=[Vd])
    cnt = [0]

    def norm_rope(srcb, R, T, t0, ones_m, ngrp, gain, perm, cdr, sdr, dst, ssb, swb, rope=True):
        P.op("act", lambda e: e.activation(out=sq[0:R, 0:T], in_=srcb[0:R, 0:T], func=AF.Square), reads=[srcb], writes=[sq])
        P.op("pe", lambda e: e.matmul(ssb[0:R, 0:T], lhsT=ones_m[:, :], rhs=sq[0:R, 0:T], start=True, stop=True), reads=[ones_m, sq], writes=[ssb])
        P.op("act", lambda e: e.activation(out=rs[0:R, 0:T], in_=ssb[0:R, 0:T], func=AF.Sqrt, scale=1.0 / ngrp, bias=EPS), reads=[ssb], writes=[rs])
        P.op("dve", lambda e: e.reciprocal(out=rstd[0:R, 0:T], in_=rs[0:R, 0:T]), reads=[rs], writes=[rstd])
        P.op("dve", lambda e: e.tensor_tensor(out=xnrm[0:R, 0:T], in0=srcb[0:R, 0:T], in1=rstd[0:R, 0:T], op=ALU.mult), reads=[srcb, rstd], writes=[xnrm])
        if not rope:
            P.op("act", lambda e: e.activation(out=dst[0:R, t0:t0 + T], in_=xnrm[0:R, 0:T], func=AF.Identity, scale=gain[:, 0:1]), reads=[xnrm, gain], writes=[dst])
            return
        P.op("act", lambda e: e.activation(out=xg[0:R, 0:T], in_=xnrm[0:R, 0:T], func=AF.Identity, scale=gain[:, 0:1]), reads=[xnrm, gain], writes=[xg])
        P.op("pe", lambda e: e.matmul(swb[0:R, 0:T], lhsT=perm[:, :], rhs=xg[0:R, 0:T], start=True, stop=True), reads=[perm, xg], writes=[swb])
        P.dma("sp", cb[0:R, 0:T], cdr.ap[:, t0:t0 + T], writes=[cb])
        P.dma("sp", sbk[0:R, 0:T], sdr.ap[:, t0:t0 + T], writes=[sbk])
        P.op("dve", lambda e: e.tensor_tensor(out=t1[0:R, 0:T], in0=xg[0:R, 0:T], in1=cb[0:R, 0:T], op=ALU.mult), reads=[xg, cb], writes=[t1])
        P.op("dve", lambda e: e.tensor_tensor(out=t2[0:R, 0:T], in0=swb[0:R, 0:T], in1=sbk[0:R, 0:T], op=ALU.mult), reads=[swb, sbk], writes=[t2])
        P.op("pool", lambda e: e.tensor_tensor(out=dst[0:R, t0:t0 + T], in0=t1[0:R, 0:T], in1=t2[0:R, 0:T], op=ALU.add), reads=[t1, t2], writes=[dst])

    for (t0, T) in SBLK:
        s0 = src[cnt[0] % 2]; cnt[0] += 1
        P.dma("sp", s0[0:96, 0:T], dr["mq"].ap[:, t0:t0 + T], writes=[s0])
        norm_rope(s0, 96, T, t0, ones96, 96, gq, perm96, dr["c96"], dr["s96"], QTm, ss_ps, sw_ps)
        s1 = src[cnt[0] % 2]; cnt[0] += 1
        P.dma("sp", s1[0:128, 0:T], dr["mckv"].ap[:, t0:t0 + T], writes=[s1])
        norm_rope(s1, 128, T, 0, ones128, 128, gkv, None, None, None, ckvn, ss2_ps, None, rope=False)
        P.op("pe", lambda e, T=T: e.matmul(kn_ps[0:64, 0:T], lhsT=wuk[:, :], rhs=ckvn[:, 0:T], start=True, stop=True), reads=[wuk, ckvn], writes=[kn_ps])
        P.op("act", lambda e, T=T: e.activation(out=kfull[0:64, 0:T], in_=kn_ps[0:64, 0:T], func=AF.Copy), reads=[kn_ps], writes=[kfull])
        P.dma("sp", kfull[64:96, 0:T], dr["mkr"].ap[:, t0:t0 + T], writes=[kfull])
        for s in range(T // 128):
            tile = t0 // 128 + s
            P.op("pe", lambda e, s=s: e.matmul(v_ps[:, s * 64:(s + 1) * 64], lhsT=ckvn[:, s * 128:(s + 1) * 128], rhs=wuv[:, :], start=True, stop=True),
                 reads=[ckvn, wuv], writes=[v_ps])
            P.op("dve", lambda e, s=s, tile=tile: e.tensor_copy(out=Vm[:, tile, 0:64], in_=v_ps[:, s * 64:(s + 1) * 64]), reads=[v_ps], writes=[Vm])
        norm_rope(kfull, 96, T, t0, ones96, 96, gk, perm96, dr["c96"], dr["s96"], KTm, ss_ps, sw_ps)
        s2 = src[cnt[0] % 2]; cnt[0] += 1
        P.dma("sp", s2[0:64, 0:T], dr["dq"].ap[:, t0:t0 + T], writes=[s2])
        norm_rope(s2, 64, T, t0, bones64, 32, gdq, perm64, dr["c64"], dr["s64"], QTd, ss2_ps, sw2_ps)
        s3 = src[cnt[0] % 2]; cnt[0] += 1
        P.dma("sp", s3[0:64, 0:T], dr["dk"].ap[:, t0:t0 + T], writes=[s3])
        norm_rope(s3, 64, T, t0, bones64, 32, gdk, perm64, dr["c64"], dr["s64"], KTd, ss_ps, sw_ps)
    P.barrier()

    def attention(kind):
        if kind == "m":
            sps = [bview(P, banks[i][:, :], f"sps_m{i}") for i in range(3)]
            po = [bview(P, banks[3][:, 0:260].rearrange("p (s c) -> p s c", c=65), "po_m")]
            QT, KT_, V, dk, scale, outd = [QTm], [KTm], Vm, 96, 96 ** -0.5, o_m
            QTs = [(QTm, 0, 96)]
        else:
            sps = [bview(P, banks[i][:, :], f"sps_d{i}") for i in range(4)]
            po = [bview(P, banks[4][:, 0:260].rearrange("p (s c) -> p s c", c=65), "po_d1"),
                  bview(P, banks[5][:, 0:260].rearrange("p (s c) -> p s c", c=65), "po_d2")]
            V, scale, outd = Vd, 32 ** -0.5, o_d
            QTs = [(QTd, 0, 32), (QTd, 32, 64)]
        KTs = [KTm] if kind == "m" else [KTd, KTd]
        npt = len(QTs)
        pts = [P.sb(f"pt_{kind}{i}", [128, 512], BF16) for i in range(2 * npt)]
        ob = [P.sb(f"ob_{kind}{i}", [128, 4, 64]) for i in range(2)]
        rec = P.sb(f"rec_{kind}", [128, 2, 4]); a_sb = P.sb(f"a_{kind}", [128, 4, 64])
        ssq = P.sb(f"ssq_{kind}", [128, 4]); junk = P.sb(f"junk_{kind}", [128, 64]); rr = P.sb(f"rr_{kind}", [128, 4])
        spi = 0; pti = 0
        for qb, (q0, Tq) in enumerate(SBLK):
            nk = 2 if qb == 0 else NTILE
            nsub = Tq // 128
            for kt in range(nk):
                for j in range(npt):
                    QTb, r0, r1 = QTs[j]
                    KTb = KTs[j]
                    sp_ = sps[spi % len(sps)]; spi += 1
                    pt = pts[pti % len(pts)]; pti += 1
                    P.op("pe", lambda e, sp_=sp_, KTb=KTb, QTb=QTb, r0=r0, r1=r1, kt=kt, q0=q0, Tq=Tq: e.matmul(
                        sp_[:, 0:Tq], lhsT=KTb[r0:r1, kt * 128:(kt + 1) * 128], rhs=QTb[r0:r1, q0:q0 + Tq], start=True, stop=True),
                        reads=[KTb, QTb], writes=[sp_])
                    P.op("act", lambda e, sp_=sp_, pt=pt, Tq=Tq: e.activation(out=pt[:, 0:Tq], in_=sp_[:, 0:Tq], func=AF.Exp, scale=scale),
                         reads=[sp_], writes=[pt])
                    for s in range(nsub):
                        P.op("pe", lambda e, pt=pt, j=j, s=s, kt=kt, nk=nk: e.matmul(
                            po[j][:, s, :], lhsT=pt[:, s * 128:(s + 1) * 128], rhs=V[:, kt, :], start=(kt == 0 and s == 0), stop=(kt == nk - 1), skip_group_check=True),
                            reads=[pt, V], writes=[po[j]], inc=(s == nsub - 1))
            o = ob[qb % 2]
            P.op("dve", lambda e: e.memset(ssq[:], 0.0), writes=[ssq])
            for j in range(npt):
                P.op("dve", lambda e, j=j, nsub=nsub: e.reciprocal(out=rec[:, j, 0:nsub], in_=po[j][:, 0:nsub, 64]), reads=[po[j]], writes=[rec])
            if kind == "m":
                for s in range(nsub):
                    P.op("dve", lambda e, s=s, o=o: e.tensor_scalar(out=o[:, s, :], in0=po[0][:, s, 0:64], scalar1=rec[:, 0, s:s + 1], scalar2=None, op0=ALU.mult),
                         reads=[po[0], rec], writes=[o])
            else:
                P.op("dve", lambda e, nsub=nsub: e.tensor_scalar(out=rec[:, 1, 0:nsub], in0=rec[:, 1, 0:nsub], scalar1=nlam[:, 0:1], scalar2=None, op0=ALU.mult),
                     reads=[rec, nlam], writes=[rec])
                for s in range(nsub):
                    P.op("dve", lambda e, s=s: e.tensor_scalar(out=a_sb[:, s, :], in0=po[0][:, s, 0:64], scalar1=rec[:, 0, s:s + 1], scalar2=None, op0=ALU.mult),
                         reads=[po[0], rec], writes=[a_sb])
                    P.op("dve", lambda e, s=s: e.scalar_tensor_tensor(out=a_sb[:, s, :], in0=po[1][:, s, 0:64], scalar=rec[:, 1, s:s + 1], in1=a_sb[:, s, :],
                                                                      op0=ALU.mult, op1=ALU.add), reads=[po[1], rec, a_sb], writes=[a_sb])
                    P.op("act", lambda e, s=s: e.activation(out=junk[:, :], in_=a_sb[:, s, :], func=AF.Square, accum_out=ssq[:, s:s + 1]),
                         reads=[a_sb], writes=[junk, ssq])
                P.op("act", lambda e, nsub=nsub: e.activation(out=rr[:, 0:nsub], in_=ssq[:, 0:nsub], func=AF.Sqrt, scale=1.0 / 64, bias=EPS), reads=[ssq], writes=[rr])
                P.op("dve", lambda e, nsub=nsub: e.reciprocal(out=rr[:, 0:nsub], in_=rr[:, 0:nsub]), reads=[rr], writes=[rr])
                P.op("dve", lambda e, nsub=nsub: e.tensor_scalar(out=rr[:, 0:nsub], in0=rr[:, 0:nsub], scalar1=laminit[:, 1:2], scalar2=None, op0=ALU.mult),
                     reads=[rr, laminit], writes=[rr])
                for s in range(nsub):
                    P.op("dve", lambda e, s=s, o=o: e.scalar_tensor_tensor(out=o[:, s, :], in0=a_sb[:, s, :], scalar=rr[:, s:s + 1], in1=gsub[:, :],
                                                                           op0=ALU.mult, op1=ALU.mult), reads=[a_sb, rr, gsub], writes=[o])
            tl = q0 // 128
            P.dma("sp", outd.ap[:, tl:tl + nsub, :], o[:, 0:nsub, :], reads=[o], is_output=True)

    attention("m")
    P.barrier()
    attention("d")
    P.emit()
    return nc


def rope_tables():
    inv = (10000.0 ** (-np.arange(8, dtype=np.float32) / 8.0)).astype(np.float32)
    t = np.arange(8192)
    rows = (t // 64).astype(np.float32); cols = (t % 64).astype(np.float32)
    ang = np.zeros((32, TS), np.float32)
    for a in range(16):
        ang[a, 256:] = rows * inv[a % 8]
        ang[16 + a, 256:] = cols * inv[a % 8]
    C = np.cos(ang).astype(np.float32); S = np.sin(ang).astype(np.float32)
    C[:, :256] = 1.0; S[:, :256] = 0.0
    return C, S


def perm_matrix(n, offsets):
    Pm = np.zeros((n, n), np.float32)
    for o in offsets:
        for a in range(8):
            Pm[o + a + 8, o + a] = -1.0
            Pm[o + a, o + a + 8] = 1.0
    return Pm


_CONST = {}


def attn_consts():
    if "attn" not in _CONST:
        C, S = rope_tables()
        c96 = np.concatenate([np.ones((64, TS), np.float32), C], 0); s96 = np.concatenate([np.zeros((64, TS), np.float32), S], 0)
        c64 = np.concatenate([C, C], 0); s64 = np.concatenate([S, S], 0)
        b64 = np.zeros((64, 64), np.float32); b64[:32, :32] = 1; b64[32:, 32:] = 1
        _CONST["attn"] = dict(c96=c96, s96=s96, c64=c64, s64=s64, ones96=np.ones((96, 96), np.float32), ones128=np.ones((128, 128), np.float32),
                              bones64=b64, perm96=perm_matrix(96, [64, 80]), perm64=perm_matrix(64, [0, 16, 32, 48]))
    return _CONST["attn"]


def rep(v, n=128):
    return np.ascontiguousarray(np.broadcast_to(np.asarray(v, np.float32).reshape(1, -1), (n, np.asarray(v).size)))


def col(v):
    return np.ascontiguousarray(np.asarray(v, np.float32).reshape(-1, 1))


def stage_attn_inputs(PS, inp, i):
    cst = attn_consts()
    lam_init = 0.8 - 0.6 * math.exp(-0.3 * i)
    maps = []
    for core in range(8):
        b, h = core // 4, core % 4
        p = PS[b]
        dof = 1576
        m = dict(cst)
        m["mq"] = np.ascontiguousarray(p[:, h * 96:(h + 1) * 96].T)
        m["mckv"] = np.ascontiguousarray(p[:, 384:512].T)
        m["mkr"] = np.ascontiguousarray(p[:, 512:544].T)
        m["dq"] = np.ascontiguousarray(p[:, dof + h * 64:dof + (h + 1) * 64].T)
        m["dk"] = np.ascontiguousarray(p[:, dof + 256 + h * 64:dof + 256 + (h + 1) * 64].T)
        m["dv"] = np.ascontiguousarray(p[:, dof + 512 + h * 64:dof + 512 + (h + 1) * 64].reshape(NTILE, 128, 64).transpose(1, 0, 2))
        m["wuk"] = np.ascontiguousarray(inp["mla_w_uk"][i][:, h * 64:(h + 1) * 64])
        m["wuv"] = np.ascontiguousarray(inp["mla_w_uv"][i][:, h * 64:(h + 1) * 64])
        m["gq"] = col(inp["mla_q_norm"][i]); m["gk"] = col(inp["mla_k_norm"][i]); m["gkv"] = col(inp["mla_kv_norm"][i])
        m["gdq"] = col(np.tile(inp["diff_q_norm"][i], 2)); m["gdk"] = col(np.tile(inp["diff_k_norm"][i], 2))
        lv = np.stack([inp["diff_lq1"][i], inp["diff_lk1"][i], inp["diff_lq2"][i], inp["diff_lk2"][i]], 0)
        m["lamv"] = np.ascontiguousarray(np.broadcast_to(lv[None], (128, 4, 32))).astype(np.float32)
        m["laminit"] = rep(np.array([lam_init, 1.0 - lam_init], np.float32))
        m["gsub"] = rep(inp["diff_subln"][i])
        maps.append(m)
    return maps


def untile(o):
    return np.ascontiguousarray(o.transpose(1, 0, 2).reshape(TS, o.shape[2]))


TPAD = TS + 3
ORDER_F = list(range(NTILE))
ORDER_B = [1, 0] + list(range(NTILE - 1, 1, -1))
NEG = -30000.0


def pcol(s):
    return 1 + s if s < 256 else 2 + s


def build_stage_ssd():
    nc = bass.Bass("TRN2", target_bir_lowering=False)
    P = Prog(nc)
    dr = {}
    for nm, shp in [("xp", [64, TPAD]), ("bp", [64, TPAD]), ("cp", [64, TPAD]), ("cw", [64, 3, 3]), ("cbias", [64, 3]),
                    ("z", [128, NTILE, 64]), ("dtr", [128, NTILE, 2]), ("dtb", [128, 2]), ("alog", [128, 2]), ("dsk", [128, 1]),
                    ("triu", [128, 128]), ("tril", [128, 128]), ("ntriu", [128, 128]), ("ntril", [128, 128]),
                    ("mneg_f", [128, 128]), ("mneg_b", [128, 128]), ("ident", [128, 128]), ("ones", [128, 128]),
                    ("slo", [128, 128]), ("sup", [128, 128])]:
        dr[nm] = P.dram(nm, shp, F32, "ExternalInput")
    o_s = P.dram("o_s", [128, NTILE, 64], F32, "ExternalOutput")
    banks = [P.ps(f"bank{i}", [128, 512]) for i in range(8)]

    def cload(nm, shp, dt=F32):
        t = P.sb(nm + "_sb", shp, dt)
        P.dma("pool", t[:], dr[nm].ap, writes=[t])
        return t
    cw = cload("cw", [64, 3, 3]); cbias = cload("cbias", [64, 3])
    dtr = cload("dtr", [128, NTILE, 2]); dtb = cload("dtb", [128, 2]); alog = cload("alog", [128, 2]); dsk = cload("dsk", [128, 1])
    triu = cload("triu", [128, 128]); tril = cload("tril", [128, 128]); ntriu = cload("ntriu", [128, 128]); ntril = cload("ntril", [128, 128])
    mneg_f = cload("mneg_f", [128, 128]); mneg_b = cload("mneg_b", [128, 128]); ident = cload("ident", [128, 128]); ones = cload("ones", [128, 128])
    slo = cload("slo", [128, 128]); sup = cload("sup", [128, 128])
    zs = cload("z", [128, NTILE, 64])
    P.op("act", lambda e: e.activation(out=zs[:], in_=zs[:], func=AF.Silu), reads=[zs], writes=[zs])

    BT = P.sb("BT", [64, TS], BF16); CT = P.sb("CT", [64, TS], BF16)
    x_tok = P.sb("x_tok", [128, NTILE, 64]); B_tok = P.sb("B_tok", [128, NTILE, 64]); y_acc = P.sb("y_acc", [128, NTILE, 64])
    dt = P.sb("dt", [128, NTILE, 2]); dta = P.sb("dta", [128, NTILE, 2]); aneg = P.sb("aneg", [128, 2])
    spx = P.sb("spx", [128, NTILE, 2]); spu = P.sb("spu", [128, NTILE, 2]); spw = P.sb("spw", [128, NTILE, 2])
    spw2 = P.sb("spw2", [128, NTILE, 2]); spq = P.sb("spq", [128, NTILE, 2])
    for d in range(2):
        P.op("dve", lambda e, d=d: e.tensor_scalar(out=spx[:, :, d], in0=dtr[:, :, d], scalar1=dtb[:, d:d + 1], scalar2=None, op0=ALU.add), reads=[dtr, dtb], writes=[spx])
    P.op("act", lambda e: e.activation(out=spu[:], in_=spx[:], func=AF.Abs), reads=[spx], writes=[spu])
    P.op("act", lambda e: e.activation(out=spu[:], in_=spu[:], func=AF.Exp, scale=-1.0), reads=[spu], writes=[spu])
    P.op("dve", lambda e: e.tensor_scalar(out=spw[:], in0=spu[:], scalar1=2.0, scalar2=None, op0=ALU.add), reads=[spu], writes=[spw])
    P.op("dve", lambda e: e.reciprocal(out=spw[:], in_=spw[:]), reads=[spw], writes=[spw])
    P.op("dve", lambda e: e.tensor_tensor(out=spw[:], in0=spw[:], in1=spu[:], op=ALU.mult), reads=[spw, spu], writes=[spw])
    P.op("dve", lambda e: e.tensor_tensor(out=spw2[:], in0=spw[:], in1=spw[:], op=ALU.mult), reads=[spw], writes=[spw2])
    P.op("dve", lambda e: e.tensor_scalar(out=spq[:], in0=spw2[:], scalar1=1.0 / 13.0, scalar2=None, op0=ALU.mult), reads=[spw2], writes=[spq])
    for cst_ in (1.0 / 11.0, 1.0 / 9.0, 1.0 / 7.0, 1.0 / 5.0, 1.0 / 3.0):
        P.op("dve", lambda e, cst_=cst_: e.scalar_tensor_tensor(out=spq[:], in0=spq[:], scalar=cst_, in1=spw2[:], op0=ALU.add, op1=ALU.mult), reads=[spq, spw2], writes=[spq])
    P.op("dve", lambda e: e.scalar_tensor_tensor(out=spq[:], in0=spq[:], scalar=1.0, in1=spw[:], op0=ALU.add, op1=ALU.mult), reads=[spq, spw], writes=[spq])
    P.op("dve", lambda e: e.tensor_scalar(out=spx[:], in0=spx[:], scalar1=0.0, scalar2=None, op0=ALU.max), reads=[spx], writes=[spx])
    P.op("dve", lambda e: e.scalar_tensor_tensor(out=dt[:], in0=spq[:], scalar=2.0, in1=spx[:], op0=ALU.mult, op1=ALU.add), reads=[spq, spx], writes=[dt])
    P.op("act", lambda e: e.activation(out=aneg[:], in_=alog[:], func=AF.Exp), reads=[alog], writes=[aneg])
    P.op("dve", lambda e: e.tensor_scalar(out=aneg[:], in0=aneg[:], scalar1=-1.0, scalar2=None, op0=ALU.mult), reads=[aneg], writes=[aneg])
    for d in range(2):
        P.op("dve", lambda e, d=d: e.tensor_scalar(out=dta[:, :, d], in0=dt[:, :, d], scalar1=aneg[:, d:d + 1], scalar2=None, op0=ALU.mult),
             reads=[dt, aneg], writes=[dta])

    tp_ps = [bview(P, banks[i][:, :], f"tp_ps{i}") for i in range(2)]
    xin = [P.sb(f"xin{i}", [64, 514]) for i in range(2)]
    acc = P.sb("cacc", [64, 512]); xc = P.sb("xc", [64, 512])
    segs = [(0, 256)] + [(256 + 512 * k, 512) for k in range(16)]
    ci = 0
    for (s0, T) in segs:
        c0 = pcol(s0) - 1
        for wi, (nm, dstT) in enumerate([("xp", None), ("bp", BT), ("cp", CT)]):
            xi = xin[ci % 2]; ci += 1
            P.dma("sp", xi[:, 0:T + 2], dr[nm].ap[:, c0:c0 + T + 2], writes=[xi])
            P.op("act", lambda e, xi=xi, T=T, wi=wi: e.activation(out=acc[:, 0:T], in_=xi[:, 1:T + 1], func=AF.Identity, scale=cw[:, wi, 1:2], bias=cbias[:, wi:wi + 1]),
                 reads=[xi, cw, cbias], writes=[acc])
            P.op("dve", lambda e, xi=xi, T=T, wi=wi: e.scalar_tensor_tensor(out=acc[:, 0:T], in0=xi[:, 0:T], scalar=cw[:, wi, 0:1], in1=acc[:, 0:T], op0=ALU.mult, op1=ALU.add),
                 reads=[xi, cw, acc], writes=[acc])
            P.op("dve", lambda e, xi=xi, T=T, wi=wi: e.scalar_tensor_tensor(out=acc[:, 0:T], in0=xi[:, 2:T + 2], scalar=cw[:, wi, 2:3], in1=acc[:, 0:T], op0=ALU.mult, op1=ALU.add),
                 reads=[xi, cw, acc], writes=[acc])
            P.op("act", lambda e, T=T: e.activation(out=xc[:, 0:T], in_=acc[:, 0:T], func=AF.Silu), reads=[acc], writes=[xc])
            if dstT is not None:
                P.op("pool", lambda e, dstT=dstT, s0=s0, T=T: e.tensor_copy(out=dstT[:, s0:s0 + T], in_=xc[:, 0:T]), reads=[xc], writes=[dstT])
            if nm in ("xp", "bp"):
                dtok = x_tok if nm == "xp" else B_tok
                tp = tp_ps[wi % 2]
                for j in range(T // 128):
                    P.op("pe", lambda e, tp=tp, j=j: e.transpose(tp[:, j * 64:(j + 1) * 64], xc[0:64, j * 128:(j + 1) * 128], ident[0:64, 0:64]),
                         reads=[xc, ident], writes=[tp])
                tl = s0 // 128
                nj = T // 128
                P.op("dve", lambda e, tp=tp, dtok=dtok, tl=tl, nj=nj: e.tensor_copy(out=dtok[:, tl:tl + nj, :], in_=tp[:, 0:nj * 64].rearrange("p (j c) -> p j c", c=64)),
                     reads=[tp], writes=[dtok])
    P.barrier()

    rowbc = [bview(P, banks[i][:, 0:128], f"rowbc{i}") for i in range(2)]
    colp = bview(P, banks[2][:, 0:2], "colp")
    STp = [bview(P, banks[3 + i][:, 0:128], f"STp{i}") for i in range(2)]
    ydp = bview(P, banks[5][:, 0:64], "ydp"); yop = bview(P, banks[6][:, 0:64], "yop"); stp = bview(P, banks[7][0:64, 0:64], "stp")
    dbc = [P.sb(f"dbc{i}", [128, 128]) for i in range(2)]
    cols = [P.sb(f"cols{i}", [128, 2]) for i in range(2)]
    Dm = [P.sb(f"Dm{i}", [128, 128]) for i in range(2)]
    STm = [P.sb(f"STm{i}", [128, 128], BF16) for i in range(2)]
    xs = [P.sb(f"xs{i}", [128, 64], BF16) for i in range(2)]
    ea = [P.sb(f"ea{i}", [128, 1]) for i in range(2)]
    yos = [P.sb(f"yos{i}", [128, 64]) for i in range(2)]
    Bdec = [P.sb(f"Bdec{i}", [128, 64], BF16) for i in range(2)]
    cd = [P.sb(f"cd{i}", [64, 1]) for i in range(2)]
    h = P.sb("h", [64, 64]); hb = P.sb("hb", [64, 64], BF16)
    tfin = P.sb("tfin", [128, 64]); ofin = [P.sb(f"ofin{i}", [128, 64]) for i in range(2)]
    it = 0
    for d in range(2):
        tri, ntri, mneg, strict = (triu, ntriu, mneg_f, slo) if d == 0 else (tril, ntril, mneg_b, sup)
        dcol = 127 if d == 0 else 0
        P.op("dve", lambda e: e.memset(h[:], 0.0), writes=[h])
        P.op("dve", lambda e: e.memset(hb[:], 0.0), writes=[hb])
        for c in (ORDER_F if d == 0 else ORDER_B):
            k = it % 2; it += 1
            rb, stq, db, cl, dm, sm, xsb, eab, yob, bd, cdb = rowbc[k], STp[k], dbc[k], cols[k], Dm[k], STm[k], xs[k], ea[k], yos[k], Bdec[k], cd[k]
            ch = slice(c * 128, (c + 1) * 128)
            P.op("dve", lambda e, db=db, c=c, d=d, strict=strict: e.tensor_scalar(out=db[:], in0=strict[:], scalar1=dta[:, c, d:d + 1], scalar2=None, op0=ALU.mult),
                 reads=[strict, dta], writes=[db])
            P.op("pe", lambda e, rb=rb, db=db, tri=tri: e.matmul(rb[:, :], lhsT=db[:, :], rhs=tri[:, :], start=True, stop=False), reads=[db, tri], writes=[rb], inc=False)
            P.op("pe", lambda e, rb=rb, mneg=mneg: e.matmul(rb[:, :], lhsT=ident[:, :], rhs=mneg[:, :], start=False, stop=True), reads=[ident, mneg], writes=[rb])
            P.op("pe", lambda e, ntri=ntri, c=c, d=d: e.matmul(colp[:, 0:1], lhsT=ntri[:, :], rhs=dta[:, c, d:d + 1], start=True, stop=True), reads=[ntri, dta], writes=[colp], inc=False)
            P.op("pe", lambda e, c=c, d=d: e.matmul(colp[:, 1:2], lhsT=ones[:, :], rhs=dta[:, c, d:d + 1], start=True, stop=True), reads=[ones, dta], writes=[colp])
            P.op("dve", lambda e, cl=cl: e.tensor_copy(out=cl[:], in_=colp[:, :]), reads=[colp], writes=[cl])
            P.op("act", lambda e, dm=dm, rb=rb: e.activation(out=dm[:], in_=rb[:, :], func=AF.Exp), reads=[rb], writes=[dm])
            P.op("pe", lambda e, stq=stq, ch=ch: e.matmul(stq[:, :], lhsT=BT[:, ch], rhs=CT[:, ch], start=True, stop=True), reads=[BT, CT], writes=[stq])
            P.op("dve", lambda e, sm=sm, stq=stq, dm=dm: e.tensor_tensor(out=sm[:], in0=stq[:, :], in1=dm[:], op=ALU.mult), reads=[stq, dm], writes=[sm])
            P.op("pool", lambda e, xsb=xsb, c=c, d=d: e.tensor_scalar(out=xsb[:], in0=x_tok[:, c, :], scalar1=dt[:, c, d:d + 1], scalar2=None, op0=ALU.mult),
                 reads=[x_tok, dt], writes=[xsb])
            P.op("pe", lambda e, sm=sm, xsb=xsb: e.matmul(ydp[:, :], lhsT=sm[:, :], rhs=xsb[:, :], start=True, stop=True), reads=[sm, xsb], writes=[ydp])
            P.op("act", lambda e, eab=eab, cl=cl: e.activation(out=eab[:], in_=cl[:, 0:1], func=AF.Exp, scale=-1.0), reads=[cl], writes=[eab])
            P.op("pe", lambda e, ch=ch: e.matmul(yop[:, :], lhsT=CT[:, ch], rhs=hb[:, :], start=True, stop=True), reads=[CT, hb], writes=[yop])
            P.op("act", lambda e, yob=yob, eab=eab: e.activation(out=yob[:], in_=yop[:, :], func=AF.Identity, scale=eab[:, 0:1]), reads=[yop, eab], writes=[yob])
            if d == 0:
                P.op("dve", lambda e, c=c, yob=yob: e.tensor_tensor(out=y_acc[:, c, :], in0=ydp[:, :], in1=yob[:], op=ALU.add), reads=[ydp, yob], writes=[y_acc])
            else:
                of = ofin[it % 2]
                P.op("dve", lambda e, yob=yob: e.tensor_tensor(out=tfin[:], in0=ydp[:, :], in1=yob[:], op=ALU.add), reads=[ydp, yob], writes=[tfin])
                P.op("dve", lambda e, c=c: e.tensor_tensor(out=tfin[:], in0=tfin[:], in1=y_acc[:, c, :], op=ALU.add), reads=[tfin, y_acc], writes=[tfin])
                P.op("dve", lambda e, c=c: e.scalar_tensor_tensor(out=tfin[:], in0=x_tok[:, c, :], scalar=dsk[:, 0:1], in1=tfin[:], op0=ALU.mult, op1=ALU.add),
                     reads=[x_tok, dsk, tfin], writes=[tfin])
                P.op("dve", lambda e, c=c, of=of: e.tensor_tensor(out=of[:], in0=tfin[:], in1=zs[:, c, :], op=ALU.mult), reads=[tfin, zs], writes=[of])
                P.dma("sp", o_s.ap[:, c, :], of[:], reads=[of], is_output=True)
            P.op("dve", lambda e, bd=bd, c=c, dm=dm, dcol=dcol: e.tensor_scalar(out=bd[:], in0=B_tok[:, c, :], scalar1=dm[:, dcol:dcol + 1], scalar2=None, op0=ALU.mult),
                 reads=[B_tok, dm], writes=[bd])
            P.op("pe", lambda e, bd=bd, xsb=xsb: e.matmul(stp[:, :], lhsT=bd[:, :], rhs=xsb[:, :], start=True, stop=True), reads=[bd, xsb], writes=[stp])
            P.op("act", lambda e, cdb=cdb, cl=cl: e.activation(out=cdb[:], in_=cl[0:64, 1:2], func=AF.Exp), reads=[cl], writes=[cdb])
            P.op("dve", lambda e, cdb=cdb: e.scalar_tensor_tensor(out=h[:], in0=h[:], scalar=cdb[:, 0:1], in1=stp[:, :], op0=ALU.mult, op1=ALU.add),
                 reads=[h, cdb, stp], writes=[h])
            P.op("act", lambda e: e.activation(out=hb[:], in_=h[:], func=AF.Copy), reads=[h], writes=[hb])
    P.emit()
    return nc


def ssd_consts():
    if "ssd" not in _CONST:
        tu = np.triu(np.ones((128, 128), np.float32)); tl = np.tril(np.ones((128, 128), np.float32))
        _CONST["ssd"] = dict(triu=tu, tril=tl, ntriu=-tu, ntril=-tl, mneg_f=NEG * (1 - tu), mneg_b=NEG * (1 - tl),
                             ident=np.eye(128, dtype=np.float32), ones=np.ones((128, 128), np.float32),
                             slo=np.tril(np.ones((128, 128), np.float32), -1), sup=np.triu(np.ones((128, 128), np.float32), 1))
    return _CONST["ssd"]


def pad_stream(aT):
    C = aT.shape[0]
    z = np.zeros((C, 1), aT.dtype)
    return np.ascontiguousarray(np.concatenate([z, aT[:, :256], z, aT[:, 256:], z], axis=1))


def tile_tok(a):
    return np.ascontiguousarray(a.reshape(NTILE, 128, a.shape[1]).transpose(1, 0, 2))


def stage_ssd_inputs(PS, inp, i):
    cst = ssd_consts()
    maps = []
    so = 544
    for core in range(8):
        b, h = core // 4, core % 4
        g = h // 2
        p = PS[b]
        m = dict(cst)
        xcols = slice(so + 256 + h * 64, so + 256 + (h + 1) * 64)
        bcols = slice(so + 512 + g * 64, so + 512 + (g + 1) * 64)
        ccols = slice(so + 640 + g * 64, so + 640 + (g + 1) * 64)
        m["xp"] = pad_stream(p[:, xcols].T); m["bp"] = pad_stream(p[:, bcols].T); m["cp"] = pad_stream(p[:, ccols].T)
        cwf = inp["ssd_conv_w"][i]; cbf = inp["ssd_conv_b"][i]
        chx = slice(h * 64, (h + 1) * 64); chb = slice(256 + g * 64, 256 + (g + 1) * 64); chc = slice(384 + g * 64, 384 + (g + 1) * 64)
        m["cw"] = np.ascontiguousarray(np.stack([cwf[:, chx].T, cwf[:, chb].T, cwf[:, chc].T], axis=1)).astype(np.float32)
        m["cbias"] = np.ascontiguousarray(np.stack([cbf[chx], cbf[chb], cbf[chc]], axis=1)).astype(np.float32)
        m["z"] = tile_tok(p[:, so + h * 64: so + (h + 1) * 64])
        m["dtr"] = tile_tok(p[:, [so + 768 + h, so + 768 + 4 + h]])
        m["dtb"] = rep(inp["ssd_dt_bias"][i][:, h]); m["alog"] = rep(inp["ssd_a_log"][i][:, h]); m["dsk"] = rep(inp["ssd_d"][i][h:h + 1])
        maps.append(m)
    return maps


S5OFF = 1320


def build_stage_s5():
    nc = bass.Bass("TRN2", target_bir_lowering=False)
    P = Prog(nc)
    uT = P.dram("uT", [64, TS], F32, "ExternalInput")
    prm = P.dram("prm", [128, 4, 3], F32, "ExternalInput")
    bT = P.dram("bT", [2, 64, 128], F32, "ExternalInput")
    cT = P.dram("cT", [2, 4, 128, 64], F32, "ExternalInput")
    dskd = P.dram("dsk", [64, 1], F32, "ExternalInput")
    yT = P.dram("yT", [64, TS], F32, "ExternalOutput")

    u = P.sb("u_all", [64, TS]); y = P.sb("y_all", [64, TS])
    for (t0, L) in SBLK:
        P.dma("sp", u[:, t0:t0 + L], uT.ap[:, t0:t0 + L], writes=[u])
    pr = P.sb("pr", [128, 4, 3]); P.dma("sp", pr[:], prm.ap, writes=[pr])
    bre = P.sb("bre", [64, 128]); bim = P.sb("bim", [64, 128])
    P.dma("sp", bre[:], bT.ap[0], writes=[bre]); P.dma("sp", bim[:], bT.ap[1], writes=[bim])
    cre = P.sb("cre", [128, 4, 64]); cim = P.sb("cim", [128, 4, 64]); ncre = P.sb("ncre", [128, 4, 64]); ncim = P.sb("ncim", [128, 4, 64])
    P.dma("sp", cre[:], cT.ap[0].rearrange("q p m -> p q m"), writes=[cre])
    P.dma("sp", cim[:], cT.ap[1].rearrange("q p m -> p q m"), writes=[cim])
    P.op("dve", lambda e: e.tensor_scalar(out=ncre[:], in0=cre[:], scalar1=-1.0, scalar2=None, op0=ALU.mult), reads=[cre], writes=[ncre])
    P.op("dve", lambda e: e.tensor_scalar(out=ncim[:], in0=cim[:], scalar1=-1.0, scalar2=None, op0=ALU.mult), reads=[cim], writes=[ncim])
    dsk = P.sb("dsk_sb", [64, 1]); P.dma("sp", dsk[:], dskd.ap, writes=[dsk])

    def T4(nm):
        return P.sb(nm, [128, 4])
    step = T4("step"); er = T4("er"); th = T4("th"); cc = T4("cc"); ss = T4("ss"); t1 = T4("t1"); t2 = T4("t2"); t3 = T4("t3")
    halfpi = P.sb("halfpi", [128, 1]); P.op("dve", lambda e: e.memset(halfpi[:], math.pi / 2), writes=[halfpi])
    P.op("act", lambda e: e.activation(out=step[:], in_=pr[:, :, 2], func=AF.Exp), reads=[pr], writes=[step])
    P.op("dve", lambda e: e.tensor_tensor(out=t1[:], in0=pr[:, :, 0], in1=step[:], op=ALU.mult), reads=[pr, step], writes=[t1])
    P.op("act", lambda e: e.activation(out=er[:], in_=t1[:], func=AF.Exp), reads=[t1], writes=[er])
    P.op("dve", lambda e: e.tensor_tensor(out=th[:], in0=pr[:, :, 1], in1=step[:], op=ALU.mult), reads=[pr, step], writes=[th])
    P.op("act", lambda e: e.activation(out=ss[:], in_=th[:], func=AF.Sin, scale=1.0 / 16), reads=[th], writes=[ss])
    P.op("act", lambda e: e.activation(out=cc[:], in_=th[:], func=AF.Sin, scale=1.0 / 16, bias=halfpi[:, 0:1]), reads=[th, halfpi], writes=[cc])

    def square(cin, sin_, cout, sout):
        P.op("dve", lambda e: e.tensor_tensor(out=t1[:], in0=cin[:], in1=cin[:], op=ALU.mult), reads=[cin], writes=[t1])
        P.op("dve", lambda e: e.tensor_tensor(out=t2[:], in0=sin_[:], in1=sin_[:], op=ALU.mult), reads=[sin_], writes=[t2])
        P.op("dve", lambda e: e.tensor_tensor(out=t3[:], in0=cin[:], in1=sin_[:], op=ALU.mult), reads=[cin, sin_], writes=[t3])
        P.op("dve", lambda e: e.tensor_tensor(out=cout[:], in0=t1[:], in1=t2[:], op=ALU.subtract), reads=[t1, t2], writes=[cout])
        P.op("dve", lambda e: e.tensor_scalar(out=sout[:], in0=t3[:], scalar1=2.0, scalar2=None, op0=ALU.mult), reads=[t3], writes=[sout])
    for _ in range(4):
        square(cc, ss, cc, ss)
    Mre = [cc]; Mim = [ss]
    for l in range(1, 10):
        a = T4(f"Mre{l}"); b = T4(f"Mim{l}")
        square(Mre[-1], Mim[-1], a, b)
        Mre.append(a); Mim.append(b)
    ar = T4("ar"); ai = T4("ai"); den = T4("den"); fr = T4("fr"); fi = T4("fi"); t4 = T4("t4")
    P.op("dve", lambda e: e.tensor_tensor(out=ar[:], in0=er[:], in1=Mre[0][:], op=ALU.mult), reads=[er, Mre[0]], writes=[ar])
    P.op("dve", lambda e: e.tensor_tensor(out=ai[:], in0=er[:], in1=Mim[0][:], op=ALU.mult), reads=[er, Mim[0]], writes=[ai])
    P.op("dve", lambda e: e.tensor_scalar(out=ar[:], in0=ar[:], scalar1=-1.0, scalar2=None, op0=ALU.add), reads=[ar], writes=[ar])
    P.op("dve", lambda e: e.tensor_tensor(out=t1[:], in0=pr[:, :, 0], in1=pr[:, :, 0], op=ALU.mult), reads=[pr], writes=[t1])
    P.op("dve", lambda e: e.tensor_tensor(out=t2[:], in0=pr[:, :, 1], in1=pr[:, :, 1], op=ALU.mult), reads=[pr], writes=[t2])
    P.op("dve", lambda e: e.tensor_tensor(out=den[:], in0=t1[:], in1=t2[:], op=ALU.add), reads=[t1, t2], writes=[den])
    P.op("dve", lambda e: e.reciprocal(out=den[:], in_=den[:]), reads=[den], writes=[den])
    P.op("dve", lambda e: e.tensor_tensor(out=t1[:], in0=ar[:], in1=pr[:, :, 0], op=ALU.mult), reads=[ar, pr], writes=[t1])
    P.op("dve", lambda e: e.tensor_tensor(out=t2[:], in0=ai[:], in1=pr[:, :, 1], op=ALU.mult), reads=[ai, pr], writes=[t2])
    P.op("dve", lambda e: e.tensor_tensor(out=t3[:], in0=t1[:], in1=t2[:], op=ALU.add), reads=[t1, t2], writes=[t3])
    P.op("dve", lambda e: e.tensor_tensor(out=fr[:], in0=t3[:], in1=den[:], op=ALU.mult), reads=[t3, den], writes=[fr])
    P.op("dve", lambda e: e.tensor_tensor(out=t1[:], in0=ai[:], in1=pr[:, :, 0], op=ALU.mult), reads=[ai, pr], writes=[t1])
    P.op("dve", lambda e: e.tensor_tensor(out=t2[:], in0=ar[:], in1=pr[:, :, 1], op=ALU.mult), reads=[ar, pr], writes=[t2])
    P.op("dve", lambda e: e.tensor_tensor(out=t4[:], in0=t1[:], in1=t2[:], op=ALU.subtract), reads=[t1, t2], writes=[t4])
    P.op("dve", lambda e: e.tensor_tensor(out=fi[:], in0=t4[:], in1=den[:], op=ALU.mult), reads=[t4, den], writes=[fi])

    Ere = [P.sb(f"Ere{q}", [128, 512]) for q in range(4)]; Eim = [P.sb(f"Eim{q}", [128, 512]) for q in range(4)]
    Wre = [P.sb(f"Wre{q}", [128, 512]) for q in range(4)]; Wim = [P.sb(f"Wim{q}", [128, 512]) for q in range(4)]
    tt = P.sb("tt", [128, 512])
    for q in range(4):
        d = q // 2
        P.op("dve", lambda e, q=q: e.memset(Ere[q][:, 0:1], 1.0), writes=[Ere[q]])
        P.op("dve", lambda e, q=q: e.memset(Eim[q][:, 0:1], 0.0), writes=[Eim[q]])
        for l in range(9):
            n = 1 << l
            mr = Mre[l]; mi = Mim[l]
            P.op("dve", lambda e, q=q, n=n, mi=mi: e.tensor_scalar(out=tt[:, 0:n], in0=Eim[q][:, 0:n], scalar1=mi[:, q:q + 1], scalar2=None, op0=ALU.mult),
                 reads=[Eim[q], mi], writes=[tt])
            P.op("dve", lambda e, q=q, n=n, mr=mr: e.scalar_tensor_tensor(out=Ere[q][:, n:2 * n], in0=Ere[q][:, 0:n], scalar=mr[:, q:q + 1], in1=tt[:, 0:n],
                                                                         op0=ALU.mult, op1=ALU.subtract), reads=[Ere[q], mr, tt], writes=[Ere[q]])
            P.op("dve", lambda e, q=q, n=n, mi=mi: e.tensor_scalar(out=tt[:, 0:n], in0=Ere[q][:, 0:n], scalar1=mi[:, q:q + 1], scalar2=None, op0=ALU.mult),
                 reads=[Ere[q], mi], writes=[tt])
            P.op("dve", lambda e, q=q, n=n, mr=mr: e.scalar_tensor_tensor(out=Eim[q][:, n:2 * n], in0=Eim[q][:, 0:n], scalar=mr[:, q:q + 1], in1=tt[:, 0:n],
                                                                         op0=ALU.mult, op1=ALU.add), reads=[Eim[q], mr, tt], writes=[Eim[q]])
        opa = ALU.add if d == 0 else ALU.subtract
        opb = ALU.subtract if d == 0 else ALU.add
        P.op("dve", lambda e, q=q: e.tensor_scalar(out=tt[:], in0=Eim[q][:], scalar1=fi[:, q:q + 1], scalar2=None, op0=ALU.mult), reads=[Eim[q], fi], writes=[tt])
        P.op("dve", lambda e, q=q, opa=opa: e.scalar_tensor_tensor(out=Wre[q][:], in0=Ere[q][:], scalar=fr[:, q:q + 1], in1=tt[:], op0=ALU.mult, op1=opa),
             reads=[Ere[q], fr, tt], writes=[Wre[q]])
        P.op("dve", lambda e, q=q: e.tensor_scalar(out=tt[:], in0=Eim[q][:], scalar1=fr[:, q:q + 1], scalar2=None, op0=ALU.mult), reads=[Eim[q], fr], writes=[tt])
        P.op("dve", lambda e, q=q, opb=opb: e.scalar_tensor_tensor(out=Wim[q][:], in0=Ere[q][:], scalar=fi[:, q:q + 1], in1=tt[:], op0=ALU.mult, op1=opb),
             reads=[Ere[q], fi, tt], writes=[Wim[q]])

    Aps = [P.ps(f"Aps{i}", [128, 512]) for i in range(2)]; Bps = [P.ps(f"Bps{i}", [128, 512]) for i in range(2)]
    yps = [P.ps(f"yps{i}", [64, 512]) for i in range(2)]

    def dbl(nm):
        return [P.sb(f"{nm}{i}", [128, 512]) for i in range(2)]
    p1 = dbl("p1"); p2 = dbl("p2"); p3 = dbl("p3"); p4 = dbl("p4"); bur = dbl("bur"); bui = dbl("bui"); sre = dbl("sre"); sim = dbl("sim")
    m1 = dbl("m1"); m2 = dbl("m2"); m3 = dbl("m3"); m4 = dbl("m4")
    ini_re = [P.sb(f"ini_re{i}", [128, 1]) for i in range(2)]; ini_im = [P.sb(f"ini_im{i}", [128, 1]) for i in range(2)]
    ta = P.sb("ta", [128, 1])
    it = 0
    for q in range(4):
        d, k = q // 2, q % 2
        order = list(range(17)) if d == 0 else [0] + list(range(16, 0, -1))
        rq = er[:, q:q + 1]
        for ci, c in enumerate(order):
            t0, L = SBLK[c]
            z = it % 2; it += 1
            A = Aps[z]; Bp = Bps[z]
            P.op("pe", lambda e, A=A, k=k, t0=t0, L=L: e.matmul(A[:, 0:L], lhsT=bre[32 * k:32 * k + 32, :], rhs=u[32 * k:32 * k + 32, t0:t0 + L], start=True, stop=True),
                 reads=[bre, u], writes=[A])
            P.op("pe", lambda e, Bp=Bp, k=k, t0=t0, L=L: e.matmul(Bp[:, 0:L], lhsT=bim[32 * k:32 * k + 32, :], rhs=u[32 * k:32 * k + 32, t0:t0 + L], start=True, stop=True),
                 reads=[bim, u], writes=[Bp])
            wr_, wi_ = Wre[q], Wim[q]
            P.op("dve", lambda e, z=z, A=A, L=L, wr_=wr_: e.tensor_tensor(out=p1[z][:, 0:L], in0=wr_[:, 0:L], in1=A[:, 0:L], op=ALU.mult), reads=[wr_, A], writes=[p1[z]])
            P.op("dve", lambda e, z=z, Bp=Bp, L=L, wi_=wi_: e.tensor_tensor(out=p2[z][:, 0:L], in0=wi_[:, 0:L], in1=Bp[:, 0:L], op=ALU.mult), reads=[wi_, Bp], writes=[p2[z]])
            P.op("dve", lambda e, z=z, Bp=Bp, L=L, wr_=wr_: e.tensor_tensor(out=p3[z][:, 0:L], in0=wr_[:, 0:L], in1=Bp[:, 0:L], op=ALU.mult), reads=[wr_, Bp], writes=[p3[z]])
            P.op("dve", lambda e, z=z, A=A, L=L, wi_=wi_: e.tensor_tensor(out=p4[z][:, 0:L], in0=wi_[:, 0:L], in1=A[:, 0:L], op=ALU.mult), reads=[wi_, A], writes=[p4[z]])
            P.op("pool", lambda e, z=z, L=L: e.tensor_tensor(out=bur[z][:, 0:L], in0=p1[z][:, 0:L], in1=p2[z][:, 0:L], op=ALU.subtract), reads=[p1[z], p2[z]], writes=[bur[z]])
            P.op("pool", lambda e, z=z, L=L: e.tensor_tensor(out=bui[z][:, 0:L], in0=p3[z][:, 0:L], in1=p4[z][:, 0:L], op=ALU.add), reads=[p3[z], p4[z]], writes=[bui[z]])
            for (sx, bx, inix) in ((sre, bur, ini_re), (sim, bui, ini_im)):
                if ci == 0:
                    init = 0.0; rd = [bx[z], er]
                else:
                    init = inix[(ci - 1) % 2][:, 0:1]; rd = [bx[z], er, inix[(ci - 1) % 2]]
                if d == 0:
                    P.op("dve", lambda e, sx=sx, bx=bx, z=z, L=L, init=init, rq=rq: e.tensor_tensor_scan(
                        out=sx[z][:, 0:L], data0=rq.to_broadcast([128, L]), data1=bx[z][:, 0:L], initial=init, op0=ALU.mult, op1=ALU.add), reads=rd, writes=[sx[z]])
                else:
                    P.op("dve", lambda e, sx=sx, bx=bx, z=z, L=L, init=init, rq=rq: e.tensor_tensor_scan(
                        out=sx[z][:, 0:L][:, ::-1], data0=rq.to_broadcast([128, L]), data1=bx[z][:, 0:L][:, ::-1], initial=init, op0=ALU.mult, op1=ALU.add), reads=rd, writes=[sx[z]])
            if ci < len(order) - 1:
                if d == 0:
                    lvl = 8 if L == 256 else 9; col = L - 1
                else:
                    lvl = 9; col = 0
                elr = Mre[lvl][:, q:q + 1]; eli = Mim[lvl][:, q:q + 1]
                w = ci % 2
                P.op("dve", lambda e, z=z, col=col, eli=eli: e.tensor_tensor(out=ta[:], in0=sim[z][:, col:col + 1], in1=eli, op=ALU.mult), reads=[sim[z], Mim[lvl]], writes=[ta])
                P.op("dve", lambda e, z=z, col=col, elr=elr, w=w: e.scalar_tensor_tensor(out=ini_re[w][:], in0=sre[z][:, col:col + 1], scalar=elr, in1=ta[:], op0=ALU.mult, op1=ALU.subtract),
                     reads=[sre[z], Mre[lvl], ta], writes=[ini_re[w]])
                P.op("dve", lambda e, z=z, col=col, eli=eli: e.tensor_tensor(out=ta[:], in0=sre[z][:, col:col + 1], in1=eli, op=ALU.mult), reads=[sre[z], Mim[lvl]], writes=[ta])
                P.op("dve", lambda e, z=z, col=col, elr=elr, w=w: e.scalar_tensor_tensor(out=ini_im[w][:], in0=sim[z][:, col:col + 1], scalar=elr, in1=ta[:], op0=ALU.mult, op1=ALU.add),
                     reads=[sim[z], Mre[lvl], ta], writes=[ini_im[w]])
            er_, ei_ = Ere[q], Eim[q]
            P.op("pool", lambda e, z=z, L=L, er_=er_: e.tensor_tensor(out=m1[z][:, 0:L], in0=er_[:, 0:L], in1=sre[z][:, 0:L], op=ALU.mult), reads=[er_, sre[z]], writes=[m1[z]])
            P.op("pool", lambda e, z=z, L=L, ei_=ei_: e.tensor_tensor(out=m2[z][:, 0:L], in0=ei_[:, 0:L], in1=sim[z][:, 0:L], op=ALU.mult), reads=[ei_, sim[z]], writes=[m2[z]])
            P.op("pool", lambda e, z=z, L=L, er_=er_: e.tensor_tensor(out=m3[z][:, 0:L], in0=er_[:, 0:L], in1=sim[z][:, 0:L], op=ALU.mult), reads=[er_, sim[z]], writes=[m3[z]])
            P.op("pool", lambda e, z=z, L=L, ei_=ei_: e.tensor_tensor(out=m4[z][:, 0:L], in0=ei_[:, 0:L], in1=sre[z][:, 0:L], op=ALU.mult), reads=[ei_, sre[z]], writes=[m4[z]])
            yp = yps[z]
            c2 = ncre if d == 0 else cre
            c4 = ncim if d == 0 else cim
            for mi_, (cm, mm) in enumerate(((cre, m1), (c2, m2), (ncim, m3), (c4, m4))):
                P.op("pe", lambda e, yp=yp, cm=cm, mm=mm, z=z, L=L, q=q, mi_=mi_: e.matmul(yp[:, 0:L], lhsT=cm[:, q, :], rhs=mm[z][:, 0:L], start=(mi_ == 0), stop=(mi_ == 3)),
                     reads=[cm, mm[z]], writes=[yp], inc=(mi_ == 3))
            if q == 0:
                P.op("act", lambda e, yp=yp, t0=t0, L=L: e.activation(out=y[:, t0:t0 + L], in_=yp[:, 0:L], func=AF.Copy), reads=[yp], writes=[y])
            else:
                P.op("dve", lambda e, yp=yp, t0=t0, L=L: e.tensor_tensor(out=y[:, t0:t0 + L], in0=yp[:, 0:L], in1=y[:, t0:t0 + L], op=ALU.add), reads=[yp, y], writes=[y])
    for (t0, L) in SBLK:
        P.op("dve", lambda e, t0=t0, L=L: e.scalar_tensor_tensor(out=y[:, t0:t0 + L], in0=u[:, t0:t0 + L], scalar=dsk[:, 0:1], in1=y[:, t0:t0 + L], op0=ALU.mult, op1=ALU.add),
             reads=[u, dsk, y], writes=[y])
    P.dma("sp", yT.ap, y[:], reads=[y], is_output=True)
    P.emit()
    return nc


def stage_s5_inputs(PS, inp, i):
    maps = []
    lre, lim, lst = inp["s5_lam_re"][i], inp["s5_lam_im"][i], inp["s5_log_step"][i]
    for core in range(8):
        b, gq = core // 4, core % 4
        m = {}
        m["uT"] = np.ascontiguousarray(PS[b][:, S5OFF + 64 * gq:S5OFF + 64 * gq + 64].T)
        prm = np.zeros((128, 4, 3), np.float32)
        bT = np.zeros((2, 64, 128), np.float32)
        cT = np.zeros((2, 4, 128, 64), np.float32)
        for k in range(2):
            for gl in range(2):
                g = 4 * gq + 2 * k + gl
                rows = slice(32 * k + 16 * gl, 32 * k + 16 * gl + 16)
                cols = slice(64 * gl, 64 * gl + 64)
                bT[0, rows, cols] = inp["s5_b_re"][i][g].T
                bT[1, rows, cols] = inp["s5_b_im"][i][g].T
                for d in range(2):
                    q = 2 * d + k
                    prm[cols, q, 0] = lre[d, g]; prm[cols, q, 1] = lim[d, g]; prm[cols, q, 2] = lst[d, g]
                    cT[0, q, cols, rows] = inp["s5_c_re"][i][d, g].T
                    cT[1, q, cols, rows] = inp["s5_c_im"][i][d, g].T
        m["prm"] = prm; m["bT"] = bT; m["cT"] = cT
        m["dsk"] = col(inp["s5_d"][i][64 * gq:64 * gq + 64])
        maps.append(m)
    return maps


def build_stage_c1():
    nc = bass.Bass("TRN2", target_bir_lowering=False)
    P = Prog(nc)
    catT = P.dram("catT", [128, KT, NTOK], F32, "ExternalInput")
    xT = P.dram("xT", [128, KT, NTOK], F32, "ExternalInput")
    modTd = P.dram("modT", [128, 48, 2], F32, "ExternalInput")
    w_out = P.dram("w_out", [D, D], F32, "ExternalInput")
    w_glu = P.dram("w_glu", [256, 256], F32, "ExternalInput")
    bgluT = P.dram("bgluT", [128, 2], F32, "ExternalInput")
    gssdT = P.dram("gssdT", [128, 2], F32, "ExternalInput")
    norm2T = P.dram("norm2T", [128, KT], F32, "ExternalInput")
    wr_d = P.dram("wr", [D, 36], F32, "ExternalInput")
    br_d = P.dram("br", [128, 36], F32, "ExternalInput")
    ident_d = P.dram("ident", [128, 128], F32, "ExternalInput")
    xmid = P.dram("xmid", [128, KT, NTOK], F32, "ExternalOutput")
    hT = P.dram("hT", [128, KT, NTOK], F32, "ExternalOutput")
    gatesT = P.dram("gatesT", [32, NTOK], F32, "ExternalOutput")

    ones_bf = P.sb("ones_bf", [128, 128], BF16)
    P.op("dve", lambda e: e.memset(ones_bf[:], 1.0), writes=[ones_bf])
    modT = P.sb("modT_sb", [128, 48, 2]); P.dma("sp", modT[:], modTd.ap, writes=[modT])
    bglu = P.sb("bglu", [128, 2]); P.dma("sp", bglu[:], bgluT.ap, writes=[bglu])
    gssd = P.sb("gssd", [128, 2]); P.dma("sp", gssd[:], gssdT.ap, writes=[gssd])
    n2 = P.sb("n2", [128, KT]); P.dma("sp", n2[:], norm2T.ap, writes=[n2])
    wr = P.sb("wr_sb", [128, KT, 36]); P.dma("sp", wr[:], wr_d.ap.rearrange("(kt p) n -> p kt n", p=128), writes=[wr])
    br = P.sb("br_sb", [128, 36]); P.dma("sp", br[:], br_d.ap, writes=[br])
    ident = P.sb("ident_sb", [128, 128]); P.dma("sp", ident[:], ident_d.ap, writes=[ident])
    wout = P.sb("wout_bf", [128, KT, D], BF16)
    P.dma("pool", wout[:], w_out.ap.rearrange("(kt p) n -> p kt n", p=128), writes=[wout])
    wglu = P.sb("wglu_bf", [128, 2, 256], BF16)
    P.dma("pool", wglu[:], w_glu.ap.rearrange("(kt p) n -> p kt n", p=128), writes=[wglu])
    G2 = P.sb("G2", [128, KT, 2])
    for r in range(2):
        P.op("dve", lambda e, r=r: e.scalar_tensor_tensor(out=G2[:, :, r], in0=modT[:, 32:40, r], scalar=1.0, in1=n2[:, :], op0=ALU.add, op1=ALU.mult),
             reads=[modT, n2], writes=[G2])

    cat = P.sb("cat", [128, KT, 512]); xm = P.sb("xm", [128, KT, 512]); tmp = P.sb("tmp", [128, KT, 512]); hh = P.sb("hh", [128, KT, 512])
    sq = P.sb("sq", [128, KT, 512], BF16); catb = P.sb("catb", [128, KT, 512], BF16)
    gg = P.sb("gg", [128, 2, 512]); ggb = P.sb("ggb", [128, 2, 512], BF16); sig = P.sb("sig", [128, 512])
    rs = P.sb("rs", [128, 512]); rstd = P.sb("rstd", [128, 512])
    gT = P.sb("gT", [32, NTOK])
    ss_ps = P.ps("ss_ps", [128, 512]); lin_ps = P.ps("lin_ps", [128, 512])
    mix_ps = [P.ps(f"mix_ps{i}", [128, 512]) for i in range(2)]
    lg_ps = [P.ps(f"lg_ps{i}", [128, 36]) for i in range(2)]
    tr_ps = P.ps("tr_ps", [32, 128])
    Lg = P.sb("Lg", [128, 36]); gmax = P.sb("gmax", [128, 1]); ngmax = P.sb("ngmax", [128, 1]); ghot = P.sb("ghot", [128, 4]); eg = P.sb("eg", [128, 4])
    sume = P.sb("sume", [128, 1]); pg = P.sb("pg", [128, 1]); les = P.sb("les", [128, 8]); m1 = P.sb("m1", [128, 1]); hot1 = P.sb("hot1", [128, 8])
    le2 = P.sb("le2", [128, 8]); m2 = P.sb("m2", [128, 1]); hot2 = P.sb("hot2", [128, 8]); d21 = P.sb("d21", [128, 1]); e21 = P.sb("e21", [128, 1])
    w1 = P.sb("w1", [128, 1]); w2 = P.sb("w2", [128, 1]); inner = P.sb("inner", [128, 8]); gates = P.sb("gates", [128, 32])
    mi = 0
    for bi, (t0, T) in enumerate(BLOCKS):
        r = 1 if bi == 4 else 0
        P.dma("sp", cat[:, :, 0:T], catT.ap[:, :, t0:t0 + T], writes=[cat])
        P.dma("sp", xm[:, :, 0:T], xT.ap[:, :, t0:t0 + T], writes=[xm])
        P.op("act", lambda e, T=T: e.activation(out=sq[:, 2:4, 0:T], in_=cat[:, 2:4, 0:T], func=AF.Square), reads=[cat], writes=[sq])
        for j, kt in enumerate((2, 3)):
            P.op("pe", lambda e, kt=kt, j=j, T=T: e.matmul(ss_ps[:, 0:T], lhsT=ones_bf[:, :], rhs=sq[:, kt, 0:T], start=(j == 0), stop=(j == 1)),
                 reads=[ones_bf, sq], writes=[ss_ps], inc=(j == 1))
        P.op("act", lambda e, T=T: e.activation(out=rs[:, 0:T], in_=ss_ps[:, 0:T], func=AF.Sqrt, scale=1.0 / 256, bias=EPS), reads=[ss_ps], writes=[rs])
        P.op("dve", lambda e, T=T: e.reciprocal(out=rstd[:, 0:T], in_=rs[:, 0:T]), reads=[rs], writes=[rstd])
        for j, kt in enumerate((2, 3)):
            P.op("dve", lambda e, kt=kt, T=T: e.tensor_tensor(out=tmp[:, kt, 0:T], in0=cat[:, kt, 0:T], in1=rstd[:, 0:T], op=ALU.mult), reads=[cat, rstd], writes=[tmp])
            P.op("act", lambda e, kt=kt, j=j, T=T: e.activation(out=catb[:, kt, 0:T], in_=tmp[:, kt, 0:T], func=AF.Identity, scale=gssd[:, j:j + 1]),
                 reads=[tmp, gssd], writes=[catb])
        P.op("act", lambda e, T=T: e.activation(out=gg[:, :, 0:T], in_=cat[:, 4:6, 0:T], func=AF.Gelu_apprx_tanh), reads=[cat], writes=[gg])
        P.op("pool", lambda e, T=T: e.tensor_copy(out=ggb[:, :, 0:T], in_=gg[:, :, 0:T]), reads=[gg], writes=[ggb])
        for j in range(2):
            for kt in range(2):
                P.op("pe", lambda e, j=j, kt=kt, T=T: e.matmul(lin_ps[:, 0:T], lhsT=wglu[:, kt, j * 128:(j + 1) * 128], rhs=ggb[:, kt, 0:T], start=(kt == 0), stop=(kt == 1)),
                     reads=[wglu, ggb], writes=[lin_ps], inc=(kt == 1))
            P.op("act", lambda e, j=j, T=T: e.activation(out=sig[:, 0:T], in_=lin_ps[:, 0:T], func=AF.Sigmoid, bias=bglu[:, j:j + 1]), reads=[lin_ps, bglu], writes=[sig])
            P.op("dve", lambda e, j=j, T=T: e.tensor_tensor(out=catb[:, 4 + j, 0:T], in0=gg[:, j, 0:T], in1=sig[:, 0:T], op=ALU.mult), reads=[gg, sig], writes=[catb])
        P.op("pool", lambda e, T=T: e.tensor_copy(out=catb[:, 0:2, 0:T], in_=cat[:, 0:2, 0:T]), reads=[cat], writes=[catb])
        P.op("pool", lambda e, T=T: e.tensor_copy(out=catb[:, 6:8, 0:T], in_=cat[:, 6:8, 0:T]), reads=[cat], writes=[catb])
        for nt in range(KT):
            mp = mix_ps[mi % 2]; mi += 1
            for kt in range(KT):
                P.op("pe", lambda e, mp=mp, nt=nt, kt=kt, T=T: e.matmul(mp[:, 0:T], lhsT=wout[:, kt, nt * 128:(nt + 1) * 128], rhs=catb[:, kt, 0:T], start=(kt == 0), stop=(kt == KT - 1)),
                     reads=[wout, catb], writes=[mp], inc=(kt == KT - 1))
            P.op("dve", lambda e, mp=mp, nt=nt, T=T, r=r: e.scalar_tensor_tensor(out=xm[:, nt, 0:T], in0=mp[:, 0:T], scalar=modT[:, 16 + nt, r:r + 1], in1=xm[:, nt, 0:T],
                                                                             op0=ALU.mult, op1=ALU.add), reads=[mp, modT, xm], writes=[xm])
        P.dma("sp", xmid.ap[:, :, t0:t0 + T], xm[:, :, 0:T], reads=[xm], is_output=True)
        P.op("act", lambda e, T=T: e.activation(out=sq[:, :, 0:T], in_=xm[:, :, 0:T], func=AF.Square), reads=[xm], writes=[sq])
        for kt in range(KT):
            P.op("pe", lambda e, kt=kt, T=T: e.matmul(ss_ps[:, 0:T], lhsT=ones_bf[:, :], rhs=sq[:, kt, 0:T], start=(kt == 0), stop=(kt == KT - 1)),
                 reads=[ones_bf, sq], writes=[ss_ps], inc=(kt == KT - 1))
        P.op("act", lambda e, T=T: e.activation(out=rs[:, 0:T], in_=ss_ps[:, 0:T], func=AF.Sqrt, scale=1.0 / D, bias=EPS), reads=[ss_ps], writes=[rs])
        P.op("dve", lambda e, T=T: e.reciprocal(out=rstd[:, 0:T], in_=rs[:, 0:T]), reads=[rs], writes=[rstd])
        for kt in range(KT):
            P.op("dve", lambda e, kt=kt, T=T: e.tensor_tensor(out=tmp[:, kt, 0:T], in0=xm[:, kt, 0:T], in1=rstd[:, 0:T], op=ALU.mult), reads=[xm, rstd], writes=[tmp])
            P.op("act", lambda e, kt=kt, T=T, r=r: e.activation(out=hh[:, kt, 0:T], in_=tmp[:, kt, 0:T], func=AF.Identity, scale=G2[:, kt, r:r + 1], bias=modT[:, 24 + kt, r:r + 1]),
                 reads=[tmp, G2, modT], writes=[hh])
        P.dma("sp", hT.ap[:, :, t0:t0 + T], hh[:, :, 0:T], reads=[hh], is_output=True)
        for s in range(T // 128):
            lp = lg_ps[s % 2]
            for kt in range(KT):
                P.op("pe", lambda e, lp=lp, kt=kt, s=s: e.matmul(lp[:, :], lhsT=hh[:, kt, s * 128:(s + 1) * 128], rhs=wr[:, kt, :], start=(kt == 0), stop=(kt == KT - 1)),
                     reads=[hh, wr], writes=[lp], inc=(kt == KT - 1))
            P.op("dve", lambda e, lp=lp: e.tensor_tensor(out=Lg[:], in0=lp[:, :], in1=br[:], op=ALU.add), reads=[lp, br], writes=[Lg])
            P.op("dve", lambda e: e.tensor_reduce(out=gmax[:], in_=Lg[:, 0:4], axis=AX.X, op=ALU.max), reads=[Lg], writes=[gmax])
            P.op("dve", lambda e: e.tensor_scalar(out=ghot[:], in0=Lg[:, 0:4], scalar1=gmax[:, 0:1], scalar2=None, op0=ALU.is_ge), reads=[Lg, gmax], writes=[ghot])
            P.op("dve", lambda e: e.tensor_scalar(out=ngmax[:], in0=gmax[:], scalar1=-1.0, scalar2=None, op0=ALU.mult), reads=[gmax], writes=[ngmax])
            P.op("act", lambda e: e.activation(out=eg[:], in_=Lg[:, 0:4], func=AF.Exp, bias=ngmax[:, 0:1]), reads=[Lg, ngmax], writes=[eg])
            P.op("dve", lambda e: e.tensor_reduce(out=sume[:], in_=eg[:], axis=AX.X, op=ALU.add), reads=[eg], writes=[sume])
            P.op("dve", lambda e: e.reciprocal(out=pg[:], in_=sume[:]), reads=[sume], writes=[pg])
            P.op("dve", lambda e: e.tensor_scalar(out=les[:], in0=Lg[:, 4:12], scalar1=ghot[:, 0:1], scalar2=None, op0=ALU.mult), reads=[Lg, ghot], writes=[les])
            for g in range(1, 4):
                P.op("dve", lambda e, g=g: e.scalar_tensor_tensor(out=les[:], in0=Lg[:, 4 + 8 * g:12 + 8 * g], scalar=ghot[:, g:g + 1], in1=les[:], op0=ALU.mult, op1=ALU.add),
                     reads=[Lg, ghot, les], writes=[les])
            P.op("dve", lambda e: e.tensor_reduce(out=m1[:], in_=les[:], axis=AX.X, op=ALU.max), reads=[les], writes=[m1])
            P.op("dve", lambda e: e.tensor_scalar(out=hot1[:], in0=les[:], scalar1=m1[:, 0:1], scalar2=None, op0=ALU.is_ge), reads=[les, m1], writes=[hot1])
            P.op("dve", lambda e: e.scalar_tensor_tensor(out=le2[:], in0=hot1[:], scalar=-1e30, in1=les[:], op0=ALU.mult, op1=ALU.add), reads=[hot1, les], writes=[le2])
            P.op("dve", lambda e: e.tensor_reduce(out=m2[:], in_=le2[:], axis=AX.X, op=ALU.max), reads=[le2], writes=[m2])
            P.op("dve", lambda e: e.tensor_scalar(out=hot2[:], in0=le2[:], scalar1=m2[:, 0:1], scalar2=None, op0=ALU.is_ge), reads=[le2, m2], writes=[hot2])
            P.op("dve", lambda e: e.tensor_tensor(out=d21[:], in0=m2[:], in1=m1[:], op=ALU.subtract), reads=[m1, m2], writes=[d21])
            P.op("act", lambda e: e.activation(out=e21[:], in_=d21[:], func=AF.Exp), reads=[d21], writes=[e21])
            P.op("dve", lambda e: e.tensor_scalar(out=w1[:], in0=e21[:], scalar1=1.0, scalar2=None, op0=ALU.add), reads=[e21], writes=[w1])
            P.op("dve", lambda e: e.reciprocal(out=w1[:], in_=w1[:]), reads=[w1], writes=[w1])
            P.op("dve", lambda e: e.tensor_tensor(out=w1[:], in0=w1[:], in1=pg[:], op=ALU.mult), reads=[w1, pg], writes=[w1])
            P.op("dve", lambda e: e.tensor_tensor(out=w2[:], in0=w1[:], in1=e21[:], op=ALU.mult), reads=[w1, e21], writes=[w2])
            P.op("dve", lambda e: e.tensor_scalar(out=inner[:], in0=hot1[:], scalar1=w1[:, 0:1], scalar2=None, op0=ALU.mult), reads=[hot1, w1], writes=[inner])
            P.op("dve", lambda e: e.scalar_tensor_tensor(out=inner[:], in0=hot2[:], scalar=w2[:, 0:1], in1=inner[:], op0=ALU.mult, op1=ALU.add), reads=[hot2, w2, inner], writes=[inner])
            for g in range(4):
                P.op("dve", lambda e, g=g: e.tensor_scalar(out=gates[:, 8 * g:8 * g + 8], in0=inner[:], scalar1=ghot[:, g:g + 1], scalar2=None, op0=ALU.mult),
                     reads=[inner, ghot], writes=[gates])
            P.op("pe", lambda e: e.transpose(tr_ps[:, :], gates[:, :], ident[:, :]), reads=[gates, ident], writes=[tr_ps])
            tk = t0 + s * 128
            P.op("act", lambda e, tk=tk: e.activation(out=gT[:, tk:tk + 128], in_=tr_ps[:, :], func=AF.Copy), reads=[tr_ps], writes=[gT])
    P.dma("sp", gatesT.ap, gT[:], reads=[gT], is_output=True)
    P.emit()
    return nc


def build_stage_c2():
    nc = bass.Bass("TRN2", target_bir_lowering=False)
    P = Prog(nc)
    hTd = P.dram("hT", [128, KT, NTOK], F32, "ExternalInput")
    xmid = P.dram("xmid", [128, KT, NTOK], F32, "ExternalInput")
    gatesT = P.dram("gatesT", [32, NTOK], F32, "ExternalInput")
    modTd = P.dram("modT", [128, 48, 2], F32, "ExternalInput")
    seld = P.dram("sel", [32, 32, 128], F32, "ExternalInput")
    w_gate = P.dram("w_gate", [32, D, 256], F32, "ExternalInput")
    w_up = P.dram("w_up", [32, D, 256], F32, "ExternalInput")
    w_down = P.dram("w_down", [32, 256, D], F32, "ExternalInput")
    xout = P.dram("xout", [128, KT, NTOK], F32, "ExternalOutput")

    hb = P.sb("hb", [128, KT, NTOK], BF16)
    xa = P.sb("xa", [128, KT, NTOK])
    for kt in range(KT):
        P.dma("pool", hb[:, kt, :], hTd.ap[:, kt, :], writes=[hb])
    for kt in range(KT):
        P.dma("sp", xa[:, kt, :], xmid.ap[:, kt, :], writes=[xa])
    gT = P.sb("gT", [32, NTOK]); P.dma("sp", gT[:], gatesT.ap, writes=[gT])
    modT = P.sb("modT_sb", [128, 48, 2]); P.dma("sp", modT[:], modTd.ap, writes=[modT])
    sel = P.sb("sel_sb", [32, 32, 128]); P.dma("sp", sel[:], seld.ap, writes=[sel])
    wg = [P.sb(f"wg{i}", [128, KT, 256], BF16) for i in range(2)]
    wu = [P.sb(f"wu{i}", [128, KT, 256], BF16) for i in range(2)]
    wd = [P.sb(f"wd{i}", [128, 2, D], BF16) for i in range(2)]
    gb_ps = P.ps("gb_ps", [128, 512])
    G_ps = [P.ps(f"G_ps{i}", [128, 512]) for i in range(2)]; U_ps = [P.ps(f"U_ps{i}", [128, 512]) for i in range(2)]
    O_ps = [P.ps(f"O_ps{i}", [128, 512]) for i in range(2)]
    gbs = [P.sb(f"gbs{i}", [128, 512]) for i in range(2)]
    sg = [P.sb(f"sg{i}", [128, 512]) for i in range(2)]; su = [P.sb(f"su{i}", [128, 512]) for i in range(2)]
    hid = [P.sb(f"hid{i}", [128, 2, 512], BF16) for i in range(2)]
    it = 0; oi = 0; ji = 0
    for ex in range(32):
        z = ex % 2
        P.dma("pool", wg[z][:], w_gate.ap[ex].rearrange("(kt p) n -> p kt n", p=128), writes=[wg[z]])
        P.dma("pool", wu[z][:], w_up.ap[ex].rearrange("(kt p) n -> p kt n", p=128), writes=[wu[z]])
        P.dma("pool", wd[z][:], w_down.ap[ex].rearrange("(kt p) n -> p kt n", p=128), writes=[wd[z]])
        for bi, (t0, T) in enumerate(BLOCKS):
            r = 1 if bi == 4 else 0
            b2 = it % 2; it += 1
            P.op("pe", lambda e, ex=ex, t0=t0, T=T: e.matmul(gb_ps[:, 0:T], lhsT=sel[:, ex, :], rhs=gT[:, t0:t0 + T], start=True, stop=True), reads=[sel, gT], writes=[gb_ps])
            P.op("act", lambda e, b2=b2, T=T: e.activation(out=gbs[b2][:, 0:T], in_=gb_ps[:, 0:T], func=AF.Copy), reads=[gb_ps], writes=[gbs[b2]])
            for j in range(2):
                j2 = ji % 2; ji += 1
                Gp = G_ps[j2]; Up = U_ps[j2]
                for kt in range(KT):
                    P.op("pe", lambda e, Gp=Gp, z=z, kt=kt, j=j, t0=t0, T=T: e.matmul(Gp[:, 0:T], lhsT=wg[z][:, kt, j * 128:(j + 1) * 128], rhs=hb[:, kt, t0:t0 + T],
                                                                                   start=(kt == 0), stop=(kt == KT - 1)), reads=[wg[z], hb], writes=[Gp], inc=(kt == KT - 1))
                for kt in range(KT):
                    P.op("pe", lambda e, Up=Up, z=z, kt=kt, j=j, t0=t0, T=T: e.matmul(Up[:, 0:T], lhsT=wu[z][:, kt, j * 128:(j + 1) * 128], rhs=hb[:, kt, t0:t0 + T],
                                                                                   start=(kt == 0), stop=(kt == KT - 1)), reads=[wu[z], hb], writes=[Up], inc=(kt == KT - 1))
                P.op("act", lambda e, Gp=Gp, j2=j2, T=T: e.activation(out=sg[j2][:, 0:T], in_=Gp[:, 0:T], func=AF.Silu), reads=[Gp], writes=[sg[j2]])
                P.op("dve", lambda e, Up=Up, j2=j2, T=T: e.tensor_tensor(out=su[j2][:, 0:T], in0=sg[j2][:, 0:T], in1=Up[:, 0:T], op=ALU.mult), reads=[sg[j2], Up], writes=[su[j2]])
                P.op("pool", lambda e, j2=j2, b2=b2, j=j, T=T: e.tensor_tensor(out=hid[b2][:, j, 0:T], in0=su[j2][:, 0:T], in1=gbs[b2][:, 0:T], op=ALU.mult),
                     reads=[su[j2], gbs[b2]], writes=[hid[b2]])
            for nt in range(KT):
                Op = O_ps[oi % 2]; oi += 1
                for j in range(2):
                    P.op("pe", lambda e, Op=Op, z=z, j=j, nt=nt, b2=b2, T=T: e.matmul(Op[:, 0:T], lhsT=wd[z][:, j, nt * 128:(nt + 1) * 128], rhs=hid[b2][:, j, 0:T],
                                                                                   start=(j == 0), stop=(j == 1)), reads=[wd[z], hid[b2]], writes=[Op], inc=(j == 1))
                P.op("dve", lambda e, Op=Op, nt=nt, t0=t0, T=T, r=r: e.scalar_tensor_tensor(out=xa[:, nt, t0:t0 + T], in0=Op[:, 0:T], scalar=modT[:, 40 + nt, r:r + 1],
                                                                                          in1=xa[:, nt, t0:t0 + T], op0=ALU.mult, op1=ALU.add), reads=[Op, modT, xa], writes=[xa])
    for kt in range(KT):
        P.dma("sp", xout.ap[:, kt, :], xa[:, kt, :], reads=[xa], is_output=True)
    P.emit()
    return nc


def feat_to_tok(xt):
    return np.ascontiguousarray(xt.transpose(2, 1, 0).reshape(xt.shape[2], D))


def c_consts():
    if "c" not in _CONST:
        sel = np.zeros((32, 32, 128), np.float32)
        for e in range(32):
            sel[e, e, :] = 1.0
        _CONST["c"] = dict(sel=sel, ident=np.eye(128, dtype=np.float32))
    return _CONST["c"]


def run_layer(i, xT_cores, inp, dbg=None):
    f32 = lambda a: np.ascontiguousarray(np.asarray(a, np.float32))
    resA = _run("A", build_stage_a, stage_a_inputs(xT_cores, inp["c"], inp["c_ctx"], f32(inp["w_ada"][i]), inp["b_ada"][i], inp["norm1"][i], f32(inp["w_in"][i])))
    PS = []
    for b in range(2):
        lat = np.concatenate([resA[4 * b + q]["p"][0:2048] for q in range(4)], 0)
        cx = np.concatenate([resA[4 * b + q]["p"][2048:2112] for q in range(4)], 0)
        PS.append(np.concatenate([cx, lat], 0))
    resB1 = _run("B1", build_stage_attn, stage_attn_inputs(PS, inp, i))
    resB2 = _run("B2", build_stage_ssd, stage_ssd_inputs(PS, inp, i))
    resB3 = _run("B3", build_stage_s5, stage_s5_inputs(PS, inp, i))
    cst = c_consts()
    wr = f32(np.concatenate([inp["moe_w_group"][i], inp["moe_w_expert"][i].transpose(1, 0, 2).reshape(D, 32)], axis=1))
    br = rep(np.concatenate([inp["moe_b_group"][i], inp["moe_b_expert"][i].reshape(32)]))
    mapsC1 = []
    cats = []
    for b in range(2):
        a = np.concatenate([untile(resB1[4 * b + h]["o_m"]) for h in range(4)], 1)
        sd = np.concatenate([untile(resB2[4 * b + h]["o_s"]) for h in range(4)], 1)
        s5 = np.concatenate([resB3[4 * b + g]["yT"].T for g in range(4)], 1)
        df = np.concatenate([untile(resB1[4 * b + h]["o_d"]) for h in range(4)], 1)
        cats.append(np.concatenate([a, sd, s5, df], 1))
    if dbg is not None:
        dbg["PS"] = PS; dbg["cats"] = cats
    for core in range(8):
        b, q = core // 4, core % 4
        cs = cats[b]
        ct = tok_to_feat(core_tokens(cs[256:], cs[:256], q))
        mapsC1.append({"catT": ct, "xT": xT_cores[core], "modT": resA[core]["modT"], "w_out": f32(inp["w_out"][i]), "w_glu": f32(inp["s5_w_glu"][i]),
                       "bgluT": vecT(inp["s5_b_glu"][i], 2), "gssdT": vecT(inp["ssd_norm"][i], 2), "norm2T": vecT(inp["norm2"][i]),
                       "wr": wr, "br": br, "ident": cst["ident"]})
    resC1 = _run("C1", build_stage_c1, mapsC1)
    if dbg is not None:
        dbg["C1"] = resC1
    wg, wu, wd = f32(inp["moe_w_gate"][i]), f32(inp["moe_w_up"][i]), f32(inp["moe_w_down"][i])
    mapsC2 = [{"hT": resC1[c]["hT"], "xmid": resC1[c]["xmid"], "gatesT": resC1[c]["gatesT"], "modT": resA[c]["modT"], "sel": cst["sel"],
               "w_gate": wg, "w_up": wu, "w_down": wd} for c in range(8)]
    resC2 = _run("C2", build_stage_c2, mapsC2)
    return [resC2[c]["xout"] for c in range(8)]


def kernel(**inputs):
    inp = {k: np.asarray(v) for k, v in inputs.items()}
    x = np.asarray(inp["x"], np.float32); ctx = np.asarray(inp["ctx"], np.float32)
    xT_cores = [tok_to_feat(core_tokens(x[c // 4], ctx[c // 4], c % 4)) for c in range(8)]
    for i in range(4):
        xT_cores = run_layer(i, xT_cores, inp)
    out = np.zeros((2, 8192, D), np.float32)
    for c in range(8):
        b, q = c // 4, c % 4
        out[b, q * 2048:(q + 1) * 2048] = feat_to_tok(xT_cores[c])[:2048]
    return out
```

```python
import contextlib
import math
import numpy as np
import concourse.bass as bass
import concourse.mybir as mybir
from concourse.bass_utils import run_bass_kernel_spmd

F32 = mybir.dt.float32
BF16 = mybir.dt.bfloat16
I32 = mybir.dt.int32
AF = mybir.ActivationFunctionType
ALU = mybir.AluOpType
AX = mybir.AxisListType

SAME_ENGINE_SYNC = True


class Buf:
    _n = 0

    def __init__(self, t, name):
        self.t = t
        self.name = name
        Buf._n += 1
        self.id = Buf._n
        self.w = {}
        self.r = {}
        self.dcnt = {}

    def __getitem__(self, idx):
        return self.t[idx]


class Prog:
    ENG = ("pe", "act", "dve", "pool", "sp")

    def __init__(self, nc):
        self.nc = nc
        self.stack = contextlib.ExitStack()
        self.q = {e: [] for e in self.ENG}
        self.cnt = {e: 0 for e in self.ENG}
        self.seen = {e: {} for e in self.ENG}
        self.semh = {}
        self.out_events = []
        self.ninstr = 0
        self.dma_latest = {}

    def sb(self, name, shape, dt=F32):
        t = self.stack.enter_context(self.nc.sbuf_tensor(name, list(shape), dt))
        return Buf(t, name)

    def ps(self, name, shape, dt=F32):
        t = self.stack.enter_context(self.nc.psum_tensor(name, list(shape), dt))
        return Buf(t, name)

    def view(self, b, name=None):
        return Buf(b.t, name or (b.name + "_v"))

    def dram(self, name, shape, dt, kind):
        t = self.nc.dram_tensor(name, list(shape), dt, kind=kind)
        b = Buf(t, name)
        b.ap = t.ap()
        return b

    def sem(self, key):
        if key not in self.semh:
            nm = "s_" + "_".join(str(k) for k in (key if isinstance(key, tuple) else (key,)))
            self.semh[key] = self.stack.enter_context(self.nc.semaphore(nm))
        return self.semh[key]

    def push(self):
        self._saved = getattr(self, "_saved", [])
        self._saved.append(self.stack)
        self.stack = contextlib.ExitStack()

    def pop(self):
        self.barrier()
        self._deferred = getattr(self, "_deferred", [])
        self._deferred.append(self.stack)
        self.stack = self._saved.pop()

    def barrier(self):
        evs = {}
        for e in self.ENG:
            if self.cnt[e] > 0:
                evs[e] = self.cnt[e]
        for k, v in self.dma_latest.items():
            evs[k] = v
        for e in self.ENG:
            waits = []
            for k, v in evs.items():
                if k == e:
                    continue
                if self.seen[e].get(k, 0) >= v:
                    continue
                self.seen[e][k] = v
                waits.append((k, v))
            if waits:
                self.q[e].append((None, waits, None))

    def _deps(self, eng, reads, writes, own_key):
        deps = {}

        def add(k, v):
            if deps.get(k, 0) < v:
                deps[k] = v

        for b in reads:
            for k, v in b.w.items():
                add(k, v)
        for b in writes:
            for k, v in b.w.items():
                add(k, v)
            for k, v in b.r.items():
                add(k, v)
        waits = []
        for k, v in deps.items():
            if k == own_key and (k == "pe" or not SAME_ENGINE_SYNC or isinstance(k, tuple)):
                continue
            if self.seen[eng].get(k, 0) >= v:
                continue
            self.seen[eng][k] = v
            waits.append((k, v))
        return waits

    def _commit(self, reads, writes, key, val):
        for b in reads:
            if b.r.get(key, 0) < val:
                b.r[key] = val
        for b in writes:
            b.w = {key: val}
            b.r = {}

    def op(self, eng, fn, reads=(), writes=(), inc=True):
        waits = self._deps(eng, reads, writes, eng)
        if inc:
            self.cnt[eng] += 1
            val = self.cnt[eng]
        else:
            assert eng == "pe"
            val = self.cnt[eng] + 1
        self.q[eng].append((fn, waits, (eng, 1) if inc else None))
        self._commit(reads, writes, eng, val)
        self.ninstr += 1

    def dma(self, eng, out_ap, in_ap, reads=(), writes=(), is_output=False, **kw):
        prim = None
        for b in list(writes) + list(reads):
            if not hasattr(b, "ap"):
                prim = b
                break
        if prim is None:
            prim = (list(writes) + list(reads))[0]
        key = ("d", prim.id, eng)
        waits = self._deps(eng, reads, writes, key)
        prim.dcnt[key] = prim.dcnt.get(key, 0) + 16
        val = prim.dcnt[key]
        fn = lambda e, o=out_ap, i=in_ap, kw=kw: e.dma_start(out=o, in_=i, **kw)
        self.q[eng].append((fn, waits, (key, 16)))
        self._commit(reads, writes, key, val)
        self.dma_latest[key] = val
        if is_output:
            self.out_events.append((key, val))
        self.ninstr += 1

    def emit(self, final_eng="sp"):
        nc = self.nc
        fw = {}
        for k, v in self.out_events:
            fw[k] = max(fw.get(k, 0), v)
        final_waits = list(fw.items())
        for e in self.ENG:
            self.sem(e)
        for e in self.ENG:
            for fn, waits, inc in self.q[e]:
                for k, v in waits:
                    self.sem(k)
                if fn is None:
                    continue
                if inc:
                    self.sem(inc[0])
        for k, v in final_waits:
            self.sem(k)
        qs = self.q
        semh = self.semh

        def run(engobj, e):
            for fn, waits, inc in qs[e]:
                for k, v in waits:
                    engobj.wait_ge(semh[k], v)
                if fn is None:
                    continue
                ins = fn(engobj)
                if inc:
                    ins.then_inc(semh[inc[0]], inc[1])
            if e == final_eng:
                for k, v in final_waits:
                    engobj.wait_ge(semh[k], v)

        with nc.Block() as block:
            @block.tensor
            def _(eng):
                run(eng, "pe")

            @block.scalar
            def _(eng):
                run(eng, "act")

            @block.vector
            def _(eng):
                run(eng, "dve")

            @block.gpsimd
            def _(eng):
                run(eng, "pool")

            @block.sync
            def _(eng):
                run(eng, "sp")
        self.stack.close()


D = 1024
KT = 8
NTOK = 2176
IN_COLS = 2344
EPS = 1e-6
BLOCKS = [(0, 512), (512, 512), (1024, 512), (1536, 512), (2048, 128)]
NT_IN = [(0, 512), (512, 512), (1024, 512), (1536, 512), (2048, 296)]

_CACHE = {}


def _run(key, builder, in_maps):
    if key not in _CACHE:
        _CACHE[key] = builder()
    nc = _CACHE[key]
    res = run_bass_kernel_spmd(nc, in_maps, core_ids=list(range(8)))
    return res.results


def build_stage_a():
    nc = bass.Bass("TRN2", target_bir_lowering=False)
    P = Prog(nc)
    xT = P.dram("xT", [128, KT, NTOK], F32, "ExternalInput")
    cvec = P.dram("cvec", [128, KT, 2], F32, "ExternalInput")
    w_ada = P.dram("w_ada", [D, 6 * D], F32, "ExternalInput")
    b_adaT = P.dram("b_adaT", [128, 48], F32, "ExternalInput")
    norm1T = P.dram("norm1T", [128, KT], F32, "ExternalInput")
    w_in = P.dram("w_in", [D, IN_COLS], F32, "ExternalInput")
    p_out = P.dram("p", [NTOK, IN_COLS], F32, "ExternalOutput")
    mod_out = P.dram("modT", [128, 48, 2], F32, "ExternalOutput")

    ones_bf = P.sb("ones_bf", [128, 128], BF16)
    P.op("dve", lambda e: e.memset(ones_bf[:], 1.0), writes=[ones_bf])
    cv = P.sb("cv", [128, KT, 2])
    sc = P.sb("sc", [128, KT, 2])
    P.dma("sp", cv[:], cvec.ap[:, :, :], writes=[cv])
    P.op("act", lambda e: e.activation(out=sc[:], in_=cv[:], func=AF.Silu), reads=[cv], writes=[sc])
    bada = P.sb("bada", [128, 48])
    n1 = P.sb("n1", [128, KT])
    P.dma("sp", bada[:], b_adaT.ap[:, :], writes=[bada])
    P.dma("sp", n1[:], norm1T.ap[:, :], writes=[n1])

    win = [P.sb(f"win{kt}", [128, IN_COLS], BF16) for kt in range(KT)]
    for kt in range(KT):
        P.dma("pool", win[kt][:], w_in.ap[kt * 128:(kt + 1) * 128, :], writes=[win[kt]])

    modps = P.ps("modps", [128, 48, 2])
    wab = [P.sb(f"wab{i}", [128, KT, 512]) for i in range(2)]
    wv = w_ada.ap.rearrange("(kt p) n -> p kt n", p=128)
    for j in range(12):
        wb = wab[j % 2]
        P.dma("sp", wb[:], wv[:, :, j * 512:(j + 1) * 512], writes=[wb])
        for cl in range(4):
            c = j * 4 + cl
            for kt in range(KT):
                P.op("pe", lambda e, wb=wb, cl=cl, kt=kt, c=c: e.matmul(
                    modps[:, c, :], lhsT=wb[:, kt, cl * 128:(cl + 1) * 128], rhs=sc[:, kt, :],
                    start=(kt == 0), stop=(kt == KT - 1)), reads=[wb, sc], writes=[modps], inc=(kt == KT - 1))
    modT = P.sb("modT_sb", [128, 48, 2])
    for r in range(2):
        P.op("dve", lambda e, r=r: e.tensor_tensor(out=modT[:, :, r], in0=modps[:, :, r], in1=bada[:, :], op=ALU.add),
             reads=[modps, bada], writes=[modT])
    P.dma("sp", mod_out.ap[:, :, :], modT[:], reads=[modT], is_output=True)
    G = P.sb("G", [128, KT, 2])
    for r in range(2):
        P.op("dve", lambda e, r=r: e.scalar_tensor_tensor(out=G[:, :, r], in0=modT[:, 8:16, r], scalar=1.0, in1=n1[:, :],
                                                          op0=ALU.add, op1=ALU.mult), reads=[modT, n1], writes=[G])

    xb = [P.sb(f"xb{i}", [128, KT, 512]) for i in range(2)]
    sq = P.sb("sq", [128, KT, 512], BF16)
    ss_ps = P.ps("ss_ps", [128, 512])
    rs = P.sb("rs", [128, 512])
    rstd = P.sb("rstd", [128, 512])
    tmp = P.sb("tmp", [128, KT, 512])
    xn = [P.sb(f"xn{i}", [128, KT, 512], BF16) for i in range(2)]
    pp = [P.ps(f"pp{i}", [128, 512]) for i in range(3)]
    psb = [P.sb(f"psb{i}", [128, IN_COLS]) for i in range(2)]
    ppi = 0
    sti = 0
    for bi, (t0, T) in enumerate(BLOCKS):
        r = 1 if bi == 4 else 0
        x = xb[bi % 2]
        xnb = xn[bi % 2]
        P.dma("sp", x[:, :, 0:T], xT.ap[:, :, t0:t0 + T], writes=[x])
        P.op("act", lambda e, x=x, T=T: e.activation(out=sq[:, :, 0:T], in_=x[:, :, 0:T], func=AF.Square), reads=[x], writes=[sq])
        for kt in range(KT):
            P.op("pe", lambda e, kt=kt, T=T: e.matmul(ss_ps[:, 0:T], lhsT=ones_bf[:, :], rhs=sq[:, kt, 0:T],
                                                      start=(kt == 0), stop=(kt == KT - 1)),
                 reads=[ones_bf, sq], writes=[ss_ps], inc=(kt == KT - 1))
        P.op("act", lambda e, T=T: e.activation(out=rs[:, 0:T], in_=ss_ps[:, 0:T], func=AF.Sqrt, scale=1.0 / D, bias=EPS),
             reads=[ss_ps], writes=[rs])
        P.op("dve", lambda e, T=T: e.reciprocal(out=rstd[:, 0:T], in_=rs[:, 0:T]), reads=[rs], writes=[rstd])
        for kt in range(KT):
            P.op("dve", lambda e, x=x, kt=kt, T=T: e.tensor_tensor(out=tmp[:, kt, 0:T], in0=x[:, kt, 0:T], in1=rstd[:, 0:T], op=ALU.mult),
                 reads=[x, rstd], writes=[tmp])
        for kt in range(KT):
            P.op("act", lambda e, xnb=xnb, kt=kt, T=T, r=r: e.activation(
                out=xnb[:, kt, 0:T], in_=tmp[:, kt, 0:T], func=AF.Identity, scale=G[:, kt, r:r + 1], bias=modT[:, kt, r:r + 1]),
                reads=[tmp, G, modT], writes=[xnb])
        for s in range(T // 128):
            pb = psb[sti % 2]
            sti += 1
            for ni, (n0, nw) in enumerate(NT_IN):
                ps = pp[ppi % 3]
                ppi += 1
                for kt in range(KT):
                    P.op("pe", lambda e, ps=ps, xnb=xnb, kt=kt, s=s, n0=n0, nw=nw: e.matmul(
                        ps[:, 0:nw], lhsT=xnb[:, kt, s * 128:(s + 1) * 128], rhs=win[kt][:, n0:n0 + nw],
                        start=(kt == 0), stop=(kt == KT - 1)), reads=[xnb, win[kt]], writes=[ps], inc=(kt == KT - 1))
                if ni % 2 == 0:
                    P.op("act", lambda e, ps=ps, pb=pb, n0=n0, nw=nw: e.activation(out=pb[:, n0:n0 + nw], in_=ps[:, 0:nw], func=AF.Copy),
                         reads=[ps], writes=[pb])
                else:
                    P.op("dve", lambda e, ps=ps, pb=pb, n0=n0, nw=nw: e.tensor_copy(out=pb[:, n0:n0 + nw], in_=ps[:, 0:nw]),
                         reads=[ps], writes=[pb])
            tok = t0 + s * 128
            P.dma("sp", p_out.ap[tok:tok + 128, :], pb[:], reads=[pb], is_output=True)
    P.emit()
    return nc


def tok_to_feat(x_tok):
    T = x_tok.shape[0]
    return np.ascontiguousarray(x_tok.reshape(T, KT, 128).transpose(2, 1, 0))


def vecT(v, n=KT):
    return np.ascontiguousarray(v.reshape(n, 128).T)


def core_tokens(x_lat_b, x_ctx_b, q):
    pad = np.zeros((64, x_lat_b.shape[1]), x_lat_b.dtype)
    return np.concatenate([x_lat_b[q * 2048:(q + 1) * 2048], x_ctx_b[q * 64:(q + 1) * 64], pad], axis=0)


def stage_a_inputs(xT_cores, c, c_ctx, w_ada_i, b_ada_i, norm1_i, w_in_i):
    maps = []
    for core in range(8):
        b = core // 4
        cvec = np.stack([vecT(c[b]), vecT(c_ctx)], axis=-1)
        maps.append({"xT": xT_cores[core], "cvec": np.ascontiguousarray(cvec), "w_ada": w_ada_i,
                     "b_adaT": vecT(b_ada_i, 48), "norm1T": vecT(norm1_i), "w_in": w_in_i})
    return maps


TS = 8448
NTILE = 66
SBLK = [(0, 256)] + [(256 + 512 * i, 512) for i in range(16)]


def bview(P, ap, name):
    b = Buf(ap, name)
    return b


def build_stage_attn():
    nc = bass.Bass("TRN2", target_bir_lowering=False)
    P = Prog(nc)
    dr = {}
    for nm, shp in [("mq", [96, TS]), ("mckv", [128, TS]), ("mkr", [32, TS]), ("dq", [64, TS]), ("dk", [64, TS]),
                    ("dv", [128, NTILE, 64]), ("c96", [96, TS]), ("s96", [96, TS]), ("c64", [64, TS]), ("s64", [64, TS]),
                    ("ones96", [96, 96]), ("ones128", [128, 128]), ("bones64", [64, 64]), ("perm96", [96, 96]), ("perm64", [64, 64]),
                    ("wuk", [128, 64]), ("wuv", [128, 64]),
                    ("gq", [96, 1]), ("gk", [96, 1]), ("gkv", [128, 1]), ("gdq", [64, 1]), ("gdk", [64, 1]),
                    ("lamv", [128, 4, 32]), ("laminit", [128, 2]), ("gsub", [128, 64])]:
        dr[nm] = P.dram(nm, shp, F32, "ExternalInput")
    o_m = P.dram("o_m", [128, NTILE, 64], F32, "ExternalOutput")
    o_d = P.dram("o_d", [128, NTILE, 64], F32, "ExternalOutput")

    banks = [P.ps(f"bank{i}", [128, 512]) for i in range(8)]

    def cload(nm, shp, dt=BF16):
        t = P.sb(nm + "_sb", shp, dt)
        P.dma("pool", t[:], dr[nm].ap, writes=[t])
        return t
    ones96 = cload("ones96", [96, 96]); ones128 = cload("ones128", [128, 128]); bones64 = cload("bones64", [64, 64])
    perm96 = cload("perm96", [96, 96]); perm64 = cload("perm64", [64, 64])
    wuk = cload("wuk", [128, 64]); wuv = cload("wuv", [128, 64])
    gq = cload("gq", [96, 1], F32); gk = cload("gk", [96, 1], F32); gkv = cload("gkv", [128, 1], F32)
    gdq = cload("gdq", [64, 1], F32); gdk = cload("gdk", [64, 1], F32)
    lamv = cload("lamv", [128, 4, 32], F32); laminit = cload("laminit", [128, 2], F32); gsub = cload("gsub", [128, 64], F32)

    QTm = P.sb("QTm", [96, TS], BF16); KTm = P.sb("KTm", [96, TS], BF16); Vm = P.sb("Vm", [128, NTILE, 65], BF16)
    QTd = P.sb("QTd", [64, TS], BF16); KTd = P.sb("KTd", [64, TS], BF16); Vd = P.sb("Vd", [128, NTILE, 65], BF16)
    P.op("pool", lambda e: e.memset(Vm[:], 1.0), writes=[Vm])
    P.op("pool", lambda e: e.memset(Vd[:], 1.0), writes=[Vd])

    lprod = P.sb("lprod", [128, 2, 32]); lsum = P.sb("lsum", [128, 2]); lexp = P.sb("lexp", [128, 2]); lam = P.sb("lam", [128, 1]); nlam = P.sb("nlam", [128, 1])
    P.op("dve", lambda e: e.tensor_tensor(out=lprod[:, 0, :], in0=lamv[:, 0, :], in1=lamv[:, 1, :], op=ALU.mult), reads=[lamv], writes=[lprod])
    P.op("dve", lambda e: e.tensor_tensor(out=lprod[:, 1, :], in0=lamv[:, 2, :], in1=lamv[:, 3, :], op=ALU.mult), reads=[lamv], writes=[lprod])
    P.op("dve", lambda e: e.tensor_reduce(out=lsum[:, :], in_=lprod[:, :, :], axis=AX.X, op=ALU.add), reads=[lprod], writes=[lsum])
    P.op("act", lambda e: e.activation(out=lexp[:], in_=lsum[:], func=AF.Exp), reads=[lsum], writes=[lexp])
    P.op("dve", lambda e: e.tensor_tensor(out=lam[:], in0=lexp[:, 0:1], in1=lexp[:, 1:2], op=ALU.subtract), reads=[lexp], writes=[lam])
    P.op("dve", lambda e: e.tensor_tensor(out=lam[:], in0=lam[:], in1=laminit[:, 0:1], op=ALU.add), reads=[lam, laminit], writes=[lam])
    P.op("dve", lambda e: e.tensor_scalar(out=nlam[:], in0=lam[:], scalar1=-1.0, scalar2=None, op0=ALU.mult), reads=[lam], writes=[nlam])

    ss_ps = bview(P, banks[0][:, :], "ss_ps"); sw_ps = bview(P, banks[1][:, :], "sw_ps")
    kn_ps = bview(P, banks[2][:, :], "kn_ps"); v_ps = bview(P, banks[3][:, :], "v_ps")
    ss2_ps = bview(P, banks[4][:, :], "ss2_ps"); sw2_ps = bview(P, banks[5][:, :], "sw2_ps")
    src = [P.sb(f"src{i}", [128, 512]) for i in range(2)]
    sq_l = [P.sb(f"sq_a{i}", [128, 512], BF16) for i in range(2)]; rs_l = [P.sb(f"rs_a{i}", [128, 512]) for i in range(2)]
    rstd_l = [P.sb(f"rstd_a{i}", [128, 512]) for i in range(2)]
    xnrm_l = [P.sb(f"xnrm{i}", [128, 512]) for i in range(2)]; xg_l = [P.sb(f"xg{i}", [128, 512], BF16) for i in range(2)]
    cb_l = [P.sb(f"cb{i}", [128, 512]) for i in range(2)]; sbk_l = [P.sb(f"sbk{i}", [128, 512]) for i in range(2)]
    t1_l = [P.sb(f"t1{i}", [128, 512]) for i in range(2)]; t2_l = [P.sb(f"t2{i}", [128, 512]) for i in range(2)]
    ncall = [0]
    ckvn_l = [P.sb(f"ckvn{i}", [128, 512], BF16) for i in range(2)]; kfull_l = [P.sb(f"kfull{i}", [96, 512]) for i in range(2)]
    dvs = P.sb("dvs", [128, NTILE, 64])
    P.dma("sp", dvs[:], dr["dv"].ap, writes=[dvs])
    P.op("dve", lambda e: e.tensor_copy(out=Vd[:, :, 0:64], in_=dvs[:]), reads=[dvs], writes=[Vd])
    cnt = [0]

    def norm_rope(srcb, R, T, t0, ones_m, ngrp, gain, perm, cdr, sdr, dst, ssb, swb, rope=True):
        zz = ncall[0] % 2; ncall[0] += 1
        sq, rs, rstd, xnrm, xg, cb, sbk, t1, t2 = sq_l[zz], rs_l[zz], rstd_l[zz], xnrm_l[zz], xg_l[zz], cb_l[zz], sbk_l[zz], t1_l[zz], t2_l[zz]
        P.op("act", lambda e: e.activation(out=sq[0:R, 0:T], in_=srcb[0:R, 0:T], func=AF.Square), reads=[srcb], writes=[sq])
        P.op("pe", lambda e: e.matmul(ssb[0:R, 0:T], lhsT=ones_m[:, :], rhs=sq[0:R, 0:T], start=True, stop=True), reads=[ones_m, sq], writes=[ssb])
        P.op("act", lambda e: e.activation(out=rs[0:R, 0:T], in_=ssb[0:R, 0:T], func=AF.Sqrt, scale=1.0 / ngrp, bias=EPS), reads=[ssb], writes=[rs])
        P.op("dve", lambda e: e.reciprocal(out=rstd[0:R, 0:T], in_=rs[0:R, 0:T]), reads=[rs], writes=[rstd])
        P.op("dve", lambda e: e.tensor_tensor(out=xnrm[0:R, 0:T], in0=srcb[0:R, 0:T], in1=rstd[0:R, 0:T], op=ALU.mult), reads=[srcb, rstd], writes=[xnrm])
        if not rope:
            P.op("act", lambda e: e.activation(out=dst[0:R, t0:t0 + T], in_=xnrm[0:R, 0:T], func=AF.Identity, scale=gain[:, 0:1]), reads=[xnrm, gain], writes=[dst])
            return
        P.op("act", lambda e: e.activation(out=xg[0:R, 0:T], in_=xnrm[0:R, 0:T], func=AF.Identity, scale=gain[:, 0:1]), reads=[xnrm, gain], writes=[xg])
        P.op("pe", lambda e: e.matmul(swb[0:R, 0:T], lhsT=perm[:, :], rhs=xg[0:R, 0:T], start=True, stop=True), reads=[perm, xg], writes=[swb])
        P.dma("sp", cb[0:R, 0:T], cdr.ap[:, t0:t0 + T], writes=[cb])
        P.dma("sp", sbk[0:R, 0:T], sdr.ap[:, t0:t0 + T], writes=[sbk])
        P.op("dve", lambda e: e.tensor_tensor(out=t1[0:R, 0:T], in0=xg[0:R, 0:T], in1=cb[0:R, 0:T], op=ALU.mult), reads=[xg, cb], writes=[t1])
        P.op("dve", lambda e: e.tensor_tensor(out=t2[0:R, 0:T], in0=swb[0:R, 0:T], in1=sbk[0:R, 0:T], op=ALU.mult), reads=[swb, sbk], writes=[t2])
        P.op("pool", lambda e: e.tensor_tensor(out=dst[0:R, t0:t0 + T], in0=t1[0:R, 0:T], in1=t2[0:R, 0:T], op=ALU.add), reads=[t1, t2], writes=[dst])

    for bi_, (t0, T) in enumerate(SBLK):
        ckvn = ckvn_l[bi_ % 2]; kfull = kfull_l[bi_ % 2]
        s0 = src[cnt[0] % 2]; cnt[0] += 1
        P.dma("sp", s0[0:96, 0:T], dr["mq"].ap[:, t0:t0 + T], writes=[s0])
        norm_rope(s0, 96, T, t0, ones96, 96, gq, perm96, dr["c96"], dr["s96"], QTm, ss_ps, sw_ps)
        s1 = src[cnt[0] % 2]; cnt[0] += 1
        P.dma("sp", s1[0:128, 0:T], dr["mckv"].ap[:, t0:t0 + T], writes=[s1])
        norm_rope(s1, 128, T, 0, ones128, 128, gkv, None, None, None, ckvn, ss2_ps, None, rope=False)
        P.op("pe", lambda e, T=T, ckvn=ckvn: e.matmul(kn_ps[0:64, 0:T], lhsT=wuk[:, :], rhs=ckvn[:, 0:T], start=True, stop=True), reads=[wuk, ckvn], writes=[kn_ps])
        P.op("act", lambda e, T=T, kfull=kfull: e.activation(out=kfull[0:64, 0:T], in_=kn_ps[0:64, 0:T], func=AF.Copy), reads=[kn_ps], writes=[kfull])
        P.dma("sp", kfull[64:96, 0:T], dr["mkr"].ap[:, t0:t0 + T], writes=[kfull])
        for s in range(T // 128):
            tile = t0 // 128 + s
            P.op("pe", lambda e, s=s, ckvn=ckvn: e.matmul(v_ps[:, s * 64:(s + 1) * 64], lhsT=ckvn[:, s * 128:(s + 1) * 128], rhs=wuv[:, :], start=True, stop=True),
                 reads=[ckvn, wuv], writes=[v_ps])
            P.op("dve", lambda e, s=s, tile=tile: e.tensor_copy(out=Vm[:, tile, 0:64], in_=v_ps[:, s * 64:(s + 1) * 64]), reads=[v_ps], writes=[Vm])
        norm_rope(kfull, 96, T, t0, ones96, 96, gk, perm96, dr["c96"], dr["s96"], KTm, ss_ps, sw_ps)
        s2 = src[cnt[0] % 2]; cnt[0] += 1
        P.dma("sp", s2[0:64, 0:T], dr["dq"].ap[:, t0:t0 + T], writes=[s2])
        norm_rope(s2, 64, T, t0, bones64, 32, gdq, perm64, dr["c64"], dr["s64"], QTd, ss2_ps, sw2_ps)
        s3 = src[cnt[0] % 2]; cnt[0] += 1
        P.dma("sp", s3[0:64, 0:T], dr["dk"].ap[:, t0:t0 + T], writes=[s3])
        norm_rope(s3, 64, T, t0, bones64, 32, gdk, perm64, dr["c64"], dr["s64"], KTd, ss_ps, sw_ps)
    P.barrier()

    def attention(kind):
        if kind == "m":
            sps = [bview(P, banks[i][:, :], f"sps_m{i}") for i in range(3)]
            po = [bview(P, banks[3][:, 0:260].rearrange("p (s c) -> p s c", c=65), "po_m")]
            QT, KT_, V, dk, scale, outd = [QTm], [KTm], Vm, 96, 96 ** -0.5, o_m
            QTs = [(QTm, 0, 96)]
        else:
            sps = [bview(P, banks[i][:, :], f"sps_d{i}") for i in range(4)]
            po = [bview(P, banks[4][:, 0:260].rearrange("p (s c) -> p s c", c=65), "po_d1"),
                  bview(P, banks[5][:, 0:260].rearrange("p (s c) -> p s c", c=65), "po_d2")]
            V, scale, outd = Vd, 32 ** -0.5, o_d
            QTs = [(QTd, 0, 32), (QTd, 32, 64)]
        KTs = [KTm] if kind == "m" else [KTd, KTd]
        npt = len(QTs)
        pts = [P.sb(f"pt_{kind}{i}", [128, 512], BF16) for i in range(2 * npt)]
        ob = [P.sb(f"ob_{kind}{i}", [128, 4, 64]) for i in range(2)]
        rec = P.sb(f"rec_{kind}", [128, 2, 4]); a_sb = P.sb(f"a_{kind}", [128, 4, 64])
        ssq = P.sb(f"ssq_{kind}", [128, 4]); junk = P.sb(f"junk_{kind}", [128, 64]); rr = P.sb(f"rr_{kind}", [128, 4])
        items = []
        for qb, (q0, Tq) in enumerate(SBLK):
            nk = 2 if qb == 0 else NTILE
            for kt in range(nk):
                for j in range(npt):
                    items.append((qb, q0, Tq, nk, kt, j))
        NI = len(items)
        LOOK = 2

        def emit_qk(n):
            qb, q0, Tq, nk, kt, j = items[n]
            QTb, r0, r1 = QTs[j]
            KTb = KTs[j]
            sp_ = sps[n % len(sps)]
            P.op("pe", lambda e, sp_=sp_, KTb=KTb, QTb=QTb, r0=r0, r1=r1, kt=kt, q0=q0, Tq=Tq: e.matmul(
                sp_[:, 0:Tq], lhsT=KTb[r0:r1, kt * 128:(kt + 1) * 128], rhs=QTb[r0:r1, q0:q0 + Tq], start=True, stop=True),
                reads=[KTb, QTb], writes=[sp_])

        def emit_exp(n):
            qb, q0, Tq, nk, kt, j = items[n]
            sp_ = sps[n % len(sps)]; pt = pts[n % len(pts)]
            P.op("act", lambda e, sp_=sp_, pt=pt, Tq=Tq: e.activation(out=pt[:, 0:Tq], in_=sp_[:, 0:Tq], func=AF.Exp, scale=scale),
                 reads=[sp_], writes=[pt])

        def emit_pv(n):
            qb, q0, Tq, nk, kt, j = items[n]
            pt = pts[n % len(pts)]
            nsub = Tq // 128
            for s in range(nsub):
                P.op("pe", lambda e, pt=pt, j=j, s=s, kt=kt, nk=nk: e.matmul(
                    po[j][:, s, :], lhsT=pt[:, s * 128:(s + 1) * 128], rhs=V[:, kt, :], start=(kt == 0 and s == 0), stop=(kt == nk - 1), skip_group_check=True),
                    reads=[pt, V], writes=[po[j]], inc=(s == nsub - 1))

        for n in range(min(LOOK, NI)):
            emit_qk(n)
        for n in range(NI):
            qb, q0, Tq, nk, kt, j = items[n]
            nsub = Tq // 128
            emit_exp(n)
            if n + LOOK < NI:
                emit_qk(n + LOOK)
            emit_pv(n)
            if not (kt == nk - 1 and j == npt - 1):
                continue
            o = ob[qb % 2]
            P.op("dve", lambda e: e.memset(ssq[:], 0.0), writes=[ssq])
            for j in range(npt):
                P.op("dve", lambda e, j=j, nsub=nsub: e.reciprocal(out=rec[:, j, 0:nsub], in_=po[j][:, 0:nsub, 64]), reads=[po[j]], writes=[rec])
            if kind == "m":
                for s in range(nsub):
                    P.op("dve", lambda e, s=s, o=o: e.tensor_scalar(out=o[:, s, :], in0=po[0][:, s, 0:64], scalar1=rec[:, 0, s:s + 1], scalar2=None, op0=ALU.mult),
                         reads=[po[0], rec], writes=[o])
            else:
                P.op("dve", lambda e, nsub=nsub: e.tensor_scalar(out=rec[:, 1, 0:nsub], in0=rec[:, 1, 0:nsub], scalar1=nlam[:, 0:1], scalar2=None, op0=ALU.mult),
                     reads=[rec, nlam], writes=[rec])
                for s in range(nsub):
                    P.op("dve", lambda e, s=s: e.tensor_scalar(out=a_sb[:, s, :], in0=po[0][:, s, 0:64], scalar1=rec[:, 0, s:s + 1], scalar2=None, op0=ALU.mult),
                         reads=[po[0], rec], writes=[a_sb])
                    P.op("dve", lambda e, s=s: e.scalar_tensor_tensor(out=a_sb[:, s, :], in0=po[1][:, s, 0:64], scalar=rec[:, 1, s:s + 1], in1=a_sb[:, s, :],
                                                                      op0=ALU.mult, op1=ALU.add), reads=[po[1], rec, a_sb], writes=[a_sb])
                    P.op("act", lambda e, s=s: e.activation(out=junk[:, :], in_=a_sb[:, s, :], func=AF.Square, accum_out=ssq[:, s:s + 1]),
                         reads=[a_sb], writes=[junk, ssq])
                P.op("act", lambda e, nsub=nsub: e.activation(out=rr[:, 0:nsub], in_=ssq[:, 0:nsub], func=AF.Sqrt, scale=1.0 / 64, bias=EPS), reads=[ssq], writes=[rr])
                P.op("dve", lambda e, nsub=nsub: e.reciprocal(out=rr[:, 0:nsub], in_=rr[:, 0:nsub]), reads=[rr], writes=[rr])
                P.op("dve", lambda e, nsub=nsub: e.tensor_scalar(out=rr[:, 0:nsub], in0=rr[:, 0:nsub], scalar1=laminit[:, 1:2], scalar2=None, op0=ALU.mult),
                     reads=[rr, laminit], writes=[rr])
                for s in range(nsub):
                    P.op("dve", lambda e, s=s, o=o: e.scalar_tensor_tensor(out=o[:, s, :], in0=a_sb[:, s, :], scalar=rr[:, s:s + 1], in1=gsub[:, :],
                                                                           op0=ALU.mult, op1=ALU.mult), reads=[a_sb, rr, gsub], writes=[o])
            tl = q0 // 128
            P.dma("sp", outd.ap[:, tl:tl + nsub, :], o[:, 0:nsub, :], reads=[o], is_output=True)

    attention("m")
    P.barrier()
    attention("d")
    P.emit()
    return nc


def rope_tables():
    inv = (10000.0 ** (-np.arange(8, dtype=np.float32) / 8.0)).astype(np.float32)
    t = np.arange(8192)
    rows = (t // 64).astype(np.float32); cols = (t % 64).astype(np.float32)
    ang = np.zeros((32, TS), np.float32)
    for a in range(16):
        ang[a, 256:] = rows * inv[a % 8]
        ang[16 + a, 256:] = cols * inv[a % 8]
    C = np.cos(ang).astype(np.float32); S = np.sin(ang).astype(np.float32)
    C[:, :256] = 1.0; S[:, :256] = 0.0
    return C, S


def perm_matrix(n, offsets):
    Pm = np.zeros((n, n), np.float32)
    for o in offsets:
        for a in range(8):
            Pm[o + a + 8, o + a] = -1.0
            Pm[o + a, o + a + 8] = 1.0
    return Pm


_CONST = {}


def attn_consts():
    if "attn" not in _CONST:
        C, S = rope_tables()
        c96 = np.concatenate([np.ones((64, TS), np.float32), C], 0); s96 = np.concatenate([np.zeros((64, TS), np.float32), S], 0)
        c64 = np.concatenate([C, C], 0); s64 = np.concatenate([S, S], 0)
        b64 = np.zeros((64, 64), np.float32); b64[:32, :32] = 1; b64[32:, 32:] = 1
        _CONST["attn"] = dict(c96=c96, s96=s96, c64=c64, s64=s64, ones96=np.ones((96, 96), np.float32), ones128=np.ones((128, 128), np.float32),
                              bones64=b64, perm96=perm_matrix(96, [64, 80]), perm64=perm_matrix(64, [0, 16, 32, 48]))
    return _CONST["attn"]


def rep(v, n=128):
    return np.ascontiguousarray(np.broadcast_to(np.asarray(v, np.float32).reshape(1, -1), (n, np.asarray(v).size)))


def col(v):
    return np.ascontiguousarray(np.asarray(v, np.float32).reshape(-1, 1))


def stage_attn_inputs(PS, inp, i):
    cst = attn_consts()
    lam_init = 0.8 - 0.6 * math.exp(-0.3 * i)
    maps = []
    for core in range(8):
        b, h = core // 4, core % 4
        p = PS[b]
        dof = 1576
        m = dict(cst)
        m["mq"] = np.ascontiguousarray(p[:, h * 96:(h + 1) * 96].T)
        m["mckv"] = np.ascontiguousarray(p[:, 384:512].T)
        m["mkr"] = np.ascontiguousarray(p[:, 512:544].T)
        m["dq"] = np.ascontiguousarray(p[:, dof + h * 64:dof + (h + 1) * 64].T)
        m["dk"] = np.ascontiguousarray(p[:, dof + 256 + h * 64:dof + 256 + (h + 1) * 64].T)
        m["dv"] = np.ascontiguousarray(p[:, dof + 512 + h * 64:dof + 512 + (h + 1) * 64].reshape(NTILE, 128, 64).transpose(1, 0, 2))
        m["wuk"] = np.ascontiguousarray(inp["mla_w_uk"][i][:, h * 64:(h + 1) * 64])
        m["wuv"] = np.ascontiguousarray(inp["mla_w_uv"][i][:, h * 64:(h + 1) * 64])
        m["gq"] = col(inp["mla_q_norm"][i]); m["gk"] = col(inp["mla_k_norm"][i]); m["gkv"] = col(inp["mla_kv_norm"][i])
        m["gdq"] = col(np.tile(inp["diff_q_norm"][i], 2)); m["gdk"] = col(np.tile(inp["diff_k_norm"][i], 2))
        lv = np.stack([inp["diff_lq1"][i], inp["diff_lk1"][i], inp["diff_lq2"][i], inp["diff_lk2"][i]], 0)
        m["lamv"] = np.ascontiguousarray(np.broadcast_to(lv[None], (128, 4, 32))).astype(np.float32)
        m["laminit"] = rep(np.array([lam_init, 1.0 - lam_init], np.float32))
        m["gsub"] = rep(inp["diff_subln"][i])
        maps.append(m)
    return maps


def untile(o):
    return np.ascontiguousarray(o.transpose(1, 0, 2).reshape(TS, o.shape[2]))


TPAD = TS + 3
ORDER_F = list(range(NTILE))
ORDER_B = [1, 0] + list(range(NTILE - 1, 1, -1))
NEG = -30000.0


def pcol(s):
    return 1 + s if s < 256 else 2 + s


def build_stage_ssd():
    nc = bass.Bass("TRN2", target_bir_lowering=False)
    P = Prog(nc)
    dr = {}
    for nm, shp in [("xp", [64, TPAD]), ("bp", [64, TPAD]), ("cp", [64, TPAD]), ("cw", [64, 3, 3]), ("cbias", [64, 3]),
                    ("z", [128, NTILE, 64]), ("dtr", [128, NTILE, 2]), ("dtb", [128, 2]), ("alog", [128, 2]), ("dsk", [128, 1]),
                    ("triu", [128, 128]), ("tril", [128, 128]), ("ntriu", [128, 128]), ("ntril", [128, 128]),
                    ("mneg_f", [128, 128]), ("mneg_b", [128, 128]), ("ident", [128, 128]), ("ones", [128, 128]),
                    ("slo", [128, 128]), ("sup", [128, 128])]:
        dr[nm] = P.dram(nm, shp, F32, "ExternalInput")
    o_s = P.dram("o_s", [128, NTILE, 64], F32, "ExternalOutput")
    banks = [P.ps(f"bank{i}", [128, 512]) for i in range(8)]

    def cload(nm, shp, dt=F32):
        t = P.sb(nm + "_sb", shp, dt)
        P.dma("pool", t[:], dr[nm].ap, writes=[t])
        return t
    cw = cload("cw", [64, 3, 3]); cbias = cload("cbias", [64, 3])
    dtr = cload("dtr", [128, NTILE, 2]); dtb = cload("dtb", [128, 2]); alog = cload("alog", [128, 2]); dsk = cload("dsk", [128, 1])
    triu = cload("triu", [128, 128]); tril = cload("tril", [128, 128]); ntriu = cload("ntriu", [128, 128]); ntril = cload("ntril", [128, 128])
    mneg_f = cload("mneg_f", [128, 128]); mneg_b = cload("mneg_b", [128, 128]); ident = cload("ident", [128, 128]); ones = cload("ones", [128, 128])
    slo = cload("slo", [128, 128]); sup = cload("sup", [128, 128])
    zs = cload("z", [128, NTILE, 64])
    P.op("act", lambda e: e.activation(out=zs[:], in_=zs[:], func=AF.Silu), reads=[zs], writes=[zs])

    BT = P.sb("BT", [64, TS], BF16); CT = P.sb("CT", [64, TS], BF16)
    x_tok = P.sb("x_tok", [128, NTILE, 64]); B_tok = P.sb("B_tok", [128, NTILE, 64]); y_acc = P.sb("y_acc", [128, NTILE, 64])
    dt = P.sb("dt", [128, NTILE, 2]); dta = P.sb("dta", [128, NTILE, 2]); aneg = P.sb("aneg", [128, 2])
    spx = P.sb("spx", [128, NTILE, 2]); spu = P.sb("spu", [128, NTILE, 2]); spw = P.sb("spw", [128, NTILE, 2])
    spw2 = P.sb("spw2", [128, NTILE, 2]); spq = P.sb("spq", [128, NTILE, 2])
    for d in range(2):
        P.op("dve", lambda e, d=d: e.tensor_scalar(out=spx[:, :, d], in0=dtr[:, :, d], scalar1=dtb[:, d:d + 1], scalar2=None, op0=ALU.add), reads=[dtr, dtb], writes=[spx])
    P.op("act", lambda e: e.activation(out=spu[:], in_=spx[:], func=AF.Abs), reads=[spx], writes=[spu])
    P.op("act", lambda e: e.activation(out=spu[:], in_=spu[:], func=AF.Exp, scale=-1.0), reads=[spu], writes=[spu])
    P.op("dve", lambda e: e.tensor_scalar(out=spw[:], in0=spu[:], scalar1=2.0, scalar2=None, op0=ALU.add), reads=[spu], writes=[spw])
    P.op("dve", lambda e: e.reciprocal(out=spw[:], in_=spw[:]), reads=[spw], writes=[spw])
    P.op("dve", lambda e: e.tensor_tensor(out=spw[:], in0=spw[:], in1=spu[:], op=ALU.mult), reads=[spw, spu], writes=[spw])
    P.op("dve", lambda e: e.tensor_tensor(out=spw2[:], in0=spw[:], in1=spw[:], op=ALU.mult), reads=[spw], writes=[spw2])
    P.op("dve", lambda e: e.tensor_scalar(out=spq[:], in0=spw2[:], scalar1=1.0 / 13.0, scalar2=None, op0=ALU.mult), reads=[spw2], writes=[spq])
    for cst_ in (1.0 / 11.0, 1.0 / 9.0, 1.0 / 7.0, 1.0 / 5.0, 1.0 / 3.0):
        P.op("dve", lambda e, cst_=cst_: e.scalar_tensor_tensor(out=spq[:], in0=spq[:], scalar=cst_, in1=spw2[:], op0=ALU.add, op1=ALU.mult), reads=[spq, spw2], writes=[spq])
    P.op("dve", lambda e: e.scalar_tensor_tensor(out=spq[:], in0=spq[:], scalar=1.0, in1=spw[:], op0=ALU.add, op1=ALU.mult), reads=[spq, spw], writes=[spq])
    P.op("dve", lambda e: e.tensor_scalar(out=spx[:], in0=spx[:], scalar1=0.0, scalar2=None, op0=ALU.max), reads=[spx], writes=[spx])
    P.op("dve", lambda e: e.scalar_tensor_tensor(out=dt[:], in0=spq[:], scalar=2.0, in1=spx[:], op0=ALU.mult, op1=ALU.add), reads=[spq, spx], writes=[dt])
    P.op("act", lambda e: e.activation(out=aneg[:], in_=alog[:], func=AF.Exp), reads=[alog], writes=[aneg])
    P.op("dve", lambda e: e.tensor_scalar(out=aneg[:], in0=aneg[:], scalar1=-1.0, scalar2=None, op0=ALU.mult), reads=[aneg], writes=[aneg])
    for d in range(2):
        P.op("dve", lambda e, d=d: e.tensor_scalar(out=dta[:, :, d], in0=dt[:, :, d], scalar1=aneg[:, d:d + 1], scalar2=None, op0=ALU.mult),
             reads=[dt, aneg], writes=[dta])

    tp_ps = [bview(P, banks[i][:, :], f"tp_ps{i}") for i in range(2)]
    xin = [P.sb(f"xin{i}", [64, 514]) for i in range(2)]
    acc = P.sb("cacc", [64, 512]); xc = P.sb("xc", [64, 512])
    segs = [(0, 256)] + [(256 + 512 * k, 512) for k in range(16)]
    ci = 0
    for (s0, T) in segs:
        c0 = pcol(s0) - 1
        for wi, (nm, dstT) in enumerate([("xp", None), ("bp", BT), ("cp", CT)]):
            xi = xin[ci % 2]; ci += 1
            P.dma("sp", xi[:, 0:T + 2], dr[nm].ap[:, c0:c0 + T + 2], writes=[xi])
            P.op("act", lambda e, xi=xi, T=T, wi=wi: e.activation(out=acc[:, 0:T], in_=xi[:, 1:T + 1], func=AF.Identity, scale=cw[:, wi, 1:2], bias=cbias[:, wi:wi + 1]),
                 reads=[xi, cw, cbias], writes=[acc])
            P.op("dve", lambda e, xi=xi, T=T, wi=wi: e.scalar_tensor_tensor(out=acc[:, 0:T], in0=xi[:, 0:T], scalar=cw[:, wi, 0:1], in1=acc[:, 0:T], op0=ALU.mult, op1=ALU.add),
                 reads=[xi, cw, acc], writes=[acc])
            P.op("dve", lambda e, xi=xi, T=T, wi=wi: e.scalar_tensor_tensor(out=acc[:, 0:T], in0=xi[:, 2:T + 2], scalar=cw[:, wi, 2:3], in1=acc[:, 0:T], op0=ALU.mult, op1=ALU.add),
                 reads=[xi, cw, acc], writes=[acc])
            P.op("act", lambda e, T=T: e.activation(out=xc[:, 0:T], in_=acc[:, 0:T], func=AF.Silu), reads=[acc], writes=[xc])
            if dstT is not None:
                P.op("pool", lambda e, dstT=dstT, s0=s0, T=T: e.tensor_copy(out=dstT[:, s0:s0 + T], in_=xc[:, 0:T]), reads=[xc], writes=[dstT])
            if nm in ("xp", "bp"):
                dtok = x_tok if nm == "xp" else B_tok
                tp = tp_ps[wi % 2]
                for j in range(T // 128):
                    P.op("pe", lambda e, tp=tp, j=j: e.transpose(tp[:, j * 64:(j + 1) * 64], xc[0:64, j * 128:(j + 1) * 128], ident[0:64, 0:64]),
                         reads=[xc, ident], writes=[tp])
                tl = s0 // 128
                nj = T // 128
                P.op("dve", lambda e, tp=tp, dtok=dtok, tl=tl, nj=nj: e.tensor_copy(out=dtok[:, tl:tl + nj, :], in_=tp[:, 0:nj * 64].rearrange("p (j c) -> p j c", c=64)),
                     reads=[tp], writes=[dtok])
    P.barrier()

    rowbc = [bview(P, banks[i][:, 0:128], f"rowbc{i}") for i in range(2)]
    colp = bview(P, banks[2][:, 0:2], "colp")
    STp = [bview(P, banks[3 + i][:, 0:128], f"STp{i}") for i in range(2)]
    ydp = bview(P, banks[5][:, 0:64], "ydp"); yop = bview(P, banks[6][:, 0:64], "yop"); stp = bview(P, banks[7][0:64, 0:64], "stp")
    dbc = [P.sb(f"dbc{i}", [128, 128]) for i in range(2)]
    cols = [P.sb(f"cols{i}", [128, 2]) for i in range(2)]
    Dm = [P.sb(f"Dm{i}", [128, 128]) for i in range(2)]
    STm = [P.sb(f"STm{i}", [128, 128], BF16) for i in range(2)]
    xs = [P.sb(f"xs{i}", [128, 64], BF16) for i in range(2)]
    ea = [P.sb(f"ea{i}", [128, 1]) for i in range(2)]
    yos = [P.sb(f"yos{i}", [128, 64]) for i in range(2)]
    Bdec = [P.sb(f"Bdec{i}", [128, 64], BF16) for i in range(2)]
    cd = [P.sb(f"cd{i}", [64, 1]) for i in range(2)]
    h = P.sb("h", [64, 64]); hbs = [P.sb(f"hb{i}", [64, 64], BF16) for i in range(2)]
    tfin = P.sb("tfin", [128, 64]); ofin = [P.sb(f"ofin{i}", [128, 64]) for i in range(2)]
    items = [(0, c) for c in ORDER_F] + [(1, c) for c in ORDER_B]
    NI = len(items)

    def bufs(n):
        k = n % 2
        return rowbc[k], STp[k], dbc[k], cols[k], Dm[k], STm[k], xs[k], ea[k], yos[k], Bdec[k], cd[k]

    def S1(n):
        d, c = items[n]
        tri, ntri, mneg, strict = (triu, ntriu, mneg_f, slo) if d == 0 else (tril, ntril, mneg_b, sup)
        rb, stq, db, cl, dm, sm, xsb, eab, yob, bd, cdb = bufs(n)
        ch = slice(c * 128, (c + 1) * 128)
        P.op("dve", lambda e, db=db, c=c, d=d, strict=strict: e.tensor_scalar(out=db[:], in0=strict[:], scalar1=dta[:, c, d:d + 1], scalar2=None, op0=ALU.mult),
             reads=[strict, dta], writes=[db])
        P.op("pe", lambda e, rb=rb, db=db, tri=tri: e.matmul(rb[:, :], lhsT=db[:, :], rhs=tri[:, :], start=True, stop=False), reads=[db, tri], writes=[rb], inc=False)
        P.op("pe", lambda e, rb=rb, mneg=mneg: e.matmul(rb[:, :], lhsT=ident[:, :], rhs=mneg[:, :], start=False, stop=True), reads=[ident, mneg], writes=[rb])
        P.op("pe", lambda e, ntri=ntri, c=c, d=d: e.matmul(colp[:, 0:1], lhsT=ntri[:, :], rhs=dta[:, c, d:d + 1], start=True, stop=True), reads=[ntri, dta], writes=[colp], inc=False)
        P.op("pe", lambda e, c=c, d=d: e.matmul(colp[:, 1:2], lhsT=ones[:, :], rhs=dta[:, c, d:d + 1], start=True, stop=True), reads=[ones, dta], writes=[colp])
        P.op("dve", lambda e, cl=cl: e.tensor_copy(out=cl[:], in_=colp[:, :]), reads=[colp], writes=[cl])
        P.op("act", lambda e, dm=dm, rb=rb: e.activation(out=dm[:], in_=rb[:, :], func=AF.Exp), reads=[rb], writes=[dm])
        P.op("pe", lambda e, stq=stq, ch=ch: e.matmul(stq[:, :], lhsT=BT[:, ch], rhs=CT[:, ch], start=True, stop=True), reads=[BT, CT], writes=[stq])
        P.op("pool", lambda e, xsb=xsb, c=c, d=d: e.tensor_scalar(out=xsb[:], in0=x_tok[:, c, :], scalar1=dt[:, c, d:d + 1], scalar2=None, op0=ALU.mult),
             reads=[x_tok, dt], writes=[xsb])
        P.op("act", lambda e, eab=eab, cl=cl: e.activation(out=eab[:], in_=cl[:, 0:1], func=AF.Exp, scale=-1.0), reads=[cl], writes=[eab])
        P.op("act", lambda e, cdb=cdb, cl=cl: e.activation(out=cdb[:], in_=cl[0:64, 1:2], func=AF.Exp), reads=[cl], writes=[cdb])

    def S2(n):
        d, c = items[n]
        dcol = 127 if d == 0 else 0
        rb, stq, db, cl, dm, sm, xsb, eab, yob, bd, cdb = bufs(n)
        ch = slice(c * 128, (c + 1) * 128)
        if n == 0 or n == NTILE:
            P.op("dve", lambda e: e.memset(h[:], 0.0), writes=[h])
            P.op("dve", lambda e, n=n: e.memset(hbs[(n - 1) % 2][:], 0.0), writes=[hbs[(n - 1) % 2]])
        hprev = hbs[(n - 1) % 2]; hnew = hbs[n % 2]
        P.op("dve", lambda e, sm=sm, stq=stq, dm=dm: e.tensor_tensor(out=sm[:], in0=stq[:, :], in1=dm[:], op=ALU.mult), reads=[stq, dm], writes=[sm])
        P.op("dve", lambda e, bd=bd, c=c, dm=dm, dcol=dcol: e.tensor_scalar(out=bd[:], in0=B_tok[:, c, :], scalar1=dm[:, dcol:dcol + 1], scalar2=None, op0=ALU.mult),
             reads=[B_tok, dm], writes=[bd])
        P.op("pe", lambda e, sm=sm, xsb=xsb: e.matmul(ydp[:, :], lhsT=sm[:, :], rhs=xsb[:, :], start=True, stop=True), reads=[sm, xsb], writes=[ydp])
        P.op("pe", lambda e, bd=bd, xsb=xsb: e.matmul(stp[:, :], lhsT=bd[:, :], rhs=xsb[:, :], start=True, stop=True), reads=[bd, xsb], writes=[stp])
        P.op("pe", lambda e, ch=ch, hprev=hprev: e.matmul(yop[:, :], lhsT=CT[:, ch], rhs=hprev[:, :], start=True, stop=True), reads=[CT, hprev], writes=[yop])
        P.op("dve", lambda e, cdb=cdb: e.scalar_tensor_tensor(out=h[:], in0=h[:], scalar=cdb[:, 0:1], in1=stp[:, :], op0=ALU.mult, op1=ALU.add),
             reads=[h, cdb, stp], writes=[h])
        P.op("act", lambda e, hnew=hnew: e.activation(out=hnew[:], in_=h[:], func=AF.Copy), reads=[h], writes=[hnew])
        P.op("act", lambda e, yob=yob, eab=eab: e.activation(out=yob[:], in_=yop[:, :], func=AF.Identity, scale=eab[:, 0:1]), reads=[yop, eab], writes=[yob])
        if d == 0:
            P.op("dve", lambda e, c=c, yob=yob: e.tensor_tensor(out=y_acc[:, c, :], in0=ydp[:, :], in1=yob[:], op=ALU.add), reads=[ydp, yob], writes=[y_acc])
        else:
            of = ofin[n % 2]
            P.op("dve", lambda e, yob=yob: e.tensor_tensor(out=tfin[:], in0=ydp[:, :], in1=yob[:], op=ALU.add), reads=[ydp, yob], writes=[tfin])
            P.op("dve", lambda e, c=c: e.tensor_tensor(out=tfin[:], in0=tfin[:], in1=y_acc[:, c, :], op=ALU.add), reads=[tfin, y_acc], writes=[tfin])
            P.op("dve", lambda e, c=c: e.scalar_tensor_tensor(out=tfin[:], in0=x_tok[:, c, :], scalar=dsk[:, 0:1], in1=tfin[:], op0=ALU.mult, op1=ALU.add),
                 reads=[x_tok, dsk, tfin], writes=[tfin])
            P.op("dve", lambda e, c=c, of=of: e.tensor_tensor(out=of[:], in0=tfin[:], in1=zs[:, c, :], op=ALU.mult), reads=[tfin, zs], writes=[of])
            P.dma("sp", o_s.ap[:, c, :], of[:], reads=[of], is_output=True)

    S1(0)
    for n in range(NI):
        if n + 1 < NI:
            S1(n + 1)
        S2(n)
    P.emit()
    return nc


def ssd_consts():
    if "ssd" not in _CONST:
        tu = np.triu(np.ones((128, 128), np.float32)); tl = np.tril(np.ones((128, 128), np.float32))
        _CONST["ssd"] = dict(triu=tu, tril=tl, ntriu=-tu, ntril=-tl, mneg_f=NEG * (1 - tu), mneg_b=NEG * (1 - tl),
                             ident=np.eye(128, dtype=np.float32), ones=np.ones((128, 128), np.float32),
                             slo=np.tril(np.ones((128, 128), np.float32), -1), sup=np.triu(np.ones((128, 128), np.float32), 1))
    return _CONST["ssd"]


def pad_stream(aT):
    C = aT.shape[0]
    z = np.zeros((C, 1), aT.dtype)
    return np.ascontiguousarray(np.concatenate([z, aT[:, :256], z, aT[:, 256:], z], axis=1))


def tile_tok(a):
    return np.ascontiguousarray(a.reshape(NTILE, 128, a.shape[1]).transpose(1, 0, 2))


def stage_ssd_inputs(PS, inp, i):
    cst = ssd_consts()
    maps = []
    so = 544
    for core in range(8):
        b, h = core // 4, core % 4
        g = h // 2
        p = PS[b]
        m = dict(cst)
        xcols = slice(so + 256 + h * 64, so + 256 + (h + 1) * 64)
        bcols = slice(so + 512 + g * 64, so + 512 + (g + 1) * 64)
        ccols = slice(so + 640 + g * 64, so + 640 + (g + 1) * 64)
        m["xp"] = pad_stream(p[:, xcols].T); m["bp"] = pad_stream(p[:, bcols].T); m["cp"] = pad_stream(p[:, ccols].T)
        cwf = inp["ssd_conv_w"][i]; cbf = inp["ssd_conv_b"][i]
        chx = slice(h * 64, (h + 1) * 64); chb = slice(256 + g * 64, 256 + (g + 1) * 64); chc = slice(384 + g * 64, 384 + (g + 1) * 64)
        m["cw"] = np.ascontiguousarray(np.stack([cwf[:, chx].T, cwf[:, chb].T, cwf[:, chc].T], axis=1)).astype(np.float32)
        m["cbias"] = np.ascontiguousarray(np.stack([cbf[chx], cbf[chb], cbf[chc]], axis=1)).astype(np.float32)
        m["z"] = tile_tok(p[:, so + h * 64: so + (h + 1) * 64])
        m["dtr"] = tile_tok(p[:, [so + 768 + h, so + 768 + 4 + h]])
        m["dtb"] = rep(inp["ssd_dt_bias"][i][:, h]); m["alog"] = rep(inp["ssd_a_log"][i][:, h]); m["dsk"] = rep(inp["ssd_d"][i][h:h + 1])
        maps.append(m)
    return maps


S5OFF = 1320


def build_stage_s5():
    nc = bass.Bass("TRN2", target_bir_lowering=False)
    P = Prog(nc)
    uT = P.dram("uT", [64, TS], F32, "ExternalInput")
    prm = P.dram("prm", [128, 4, 3], F32, "ExternalInput")
    bT = P.dram("bT", [2, 64, 128], F32, "ExternalInput")
    cT = P.dram("cT", [2, 4, 128, 64], F32, "ExternalInput")
    dskd = P.dram("dsk", [64, 1], F32, "ExternalInput")
    yT = P.dram("yT", [64, TS], F32, "ExternalOutput")

    u = P.sb("u_all", [64, TS]); y = P.sb("y_all", [64, TS])
    for (t0, L) in SBLK:
        P.dma("sp", u[:, t0:t0 + L], uT.ap[:, t0:t0 + L], writes=[u])
    pr = P.sb("pr", [128, 4, 3]); P.dma("sp", pr[:], prm.ap, writes=[pr])
    ub = P.sb("u_bf", [64, TS], BF16)
    for (t0, L) in SBLK:
        P.dma("pool", ub[:, t0:t0 + L], uT.ap[:, t0:t0 + L], writes=[ub])
    bre = P.sb("bre", [64, 128], BF16); bim = P.sb("bim", [64, 128], BF16)
    P.dma("pool", bre[:], bT.ap[0], writes=[bre]); P.dma("pool", bim[:], bT.ap[1], writes=[bim])
    cre32 = P.sb("cre32", [128, 4, 64]); cim32 = P.sb("cim32", [128, 4, 64])
    cre = P.sb("cre", [128, 4, 64], BF16); cim = P.sb("cim", [128, 4, 64], BF16); ncre = P.sb("ncre", [128, 4, 64], BF16); ncim = P.sb("ncim", [128, 4, 64], BF16)
    P.dma("sp", cre32[:], cT.ap[0].rearrange("q p m -> p q m"), writes=[cre32])
    P.dma("sp", cim32[:], cT.ap[1].rearrange("q p m -> p q m"), writes=[cim32])
    P.op("dve", lambda e: e.tensor_copy(out=cre[:], in_=cre32[:]), reads=[cre32], writes=[cre])
    P.op("dve", lambda e: e.tensor_copy(out=cim[:], in_=cim32[:]), reads=[cim32], writes=[cim])
    P.op("dve", lambda e: e.tensor_scalar(out=ncre[:], in0=cre32[:], scalar1=-1.0, scalar2=None, op0=ALU.mult), reads=[cre32], writes=[ncre])
    P.op("dve", lambda e: e.tensor_scalar(out=ncim[:], in0=cim32[:], scalar1=-1.0, scalar2=None, op0=ALU.mult), reads=[cim32], writes=[ncim])
    dsk = P.sb("dsk_sb", [64, 1]); P.dma("sp", dsk[:], dskd.ap, writes=[dsk])

    def T4(nm):
        return P.sb(nm, [128, 4])
    step = T4("step"); er = T4("er"); th = T4("th"); cc = T4("cc"); ss = T4("ss"); t1 = T4("t1"); t2 = T4("t2"); t3 = T4("t3")
    halfpi = P.sb("halfpi", [128, 1]); P.op("dve", lambda e: e.memset(halfpi[:], math.pi / 2), writes=[halfpi])
    P.op("act", lambda e: e.activation(out=step[:], in_=pr[:, :, 2], func=AF.Exp), reads=[pr], writes=[step])
    P.op("dve", lambda e: e.tensor_tensor(out=t1[:], in0=pr[:, :, 0], in1=step[:], op=ALU.mult), reads=[pr, step], writes=[t1])
    P.op("act", lambda e: e.activation(out=er[:], in_=t1[:], func=AF.Exp), reads=[t1], writes=[er])
    P.op("dve", lambda e: e.tensor_tensor(out=th[:], in0=pr[:, :, 1], in1=step[:], op=ALU.mult), reads=[pr, step], writes=[th])
    P.op("act", lambda e: e.activation(out=ss[:], in_=th[:], func=AF.Sin, scale=1.0 / 16), reads=[th], writes=[ss])
    P.op("act", lambda e: e.activation(out=cc[:], in_=th[:], func=AF.Sin, scale=1.0 / 16, bias=halfpi[:, 0:1]), reads=[th, halfpi], writes=[cc])

    def square(cin, sin_, cout, sout):
        P.op("dve", lambda e: e.tensor_tensor(out=t1[:], in0=cin[:], in1=cin[:], op=ALU.mult), reads=[cin], writes=[t1])
        P.op("dve", lambda e: e.tensor_tensor(out=t2[:], in0=sin_[:], in1=sin_[:], op=ALU.mult), reads=[sin_], writes=[t2])
        P.op("dve", lambda e: e.tensor_tensor(out=t3[:], in0=cin[:], in1=sin_[:], op=ALU.mult), reads=[cin, sin_], writes=[t3])
        P.op("dve", lambda e: e.tensor_tensor(out=cout[:], in0=t1[:], in1=t2[:], op=ALU.subtract), reads=[t1, t2], writes=[cout])
        P.op("dve", lambda e: e.tensor_scalar(out=sout[:], in0=t3[:], scalar1=2.0, scalar2=None, op0=ALU.mult), reads=[t3], writes=[sout])
    for _ in range(4):
        square(cc, ss, cc, ss)
    Mre = [cc]; Mim = [ss]
    for l in range(1, 10):
        a = T4(f"Mre{l}"); b = T4(f"Mim{l}")
        square(Mre[-1], Mim[-1], a, b)
        Mre.append(a); Mim.append(b)
    ar = T4("ar"); ai = T4("ai"); den = T4("den"); fr = T4("fr"); fi = T4("fi"); t4 = T4("t4")
    P.op("dve", lambda e: e.tensor_tensor(out=ar[:], in0=er[:], in1=Mre[0][:], op=ALU.mult), reads=[er, Mre[0]], writes=[ar])
    P.op("dve", lambda e: e.tensor_tensor(out=ai[:], in0=er[:], in1=Mim[0][:], op=ALU.mult), reads=[er, Mim[0]], writes=[ai])
    P.op("dve", lambda e: e.tensor_scalar(out=ar[:], in0=ar[:], scalar1=-1.0, scalar2=None, op0=ALU.add), reads=[ar], writes=[ar])
    P.op("dve", lambda e: e.tensor_tensor(out=t1[:], in0=pr[:, :, 0], in1=pr[:, :, 0], op=ALU.mult), reads=[pr], writes=[t1])
    P.op("dve", lambda e: e.tensor_tensor(out=t2[:], in0=pr[:, :, 1], in1=pr[:, :, 1], op=ALU.mult), reads=[pr], writes=[t2])
    P.op("dve", lambda e: e.tensor_tensor(out=den[:], in0=t1[:], in1=t2[:], op=ALU.add), reads=[t1, t2], writes=[den])
    P.op("dve", lambda e: e.reciprocal(out=den[:], in_=den[:]), reads=[den], writes=[den])
    P.op("dve", lambda e: e.tensor_tensor(out=t1[:], in0=ar[:], in1=pr[:, :, 0], op=ALU.mult), reads=[ar, pr], writes=[t1])
    P.op("dve", lambda e: e.tensor_tensor(out=t2[:], in0=ai[:], in1=pr[:, :, 1], op=ALU.mult), reads=[ai, pr], writes=[t2])
    P.op("dve", lambda e: e.tensor_tensor(out=t3[:], in0=t1[:], in1=t2[:], op=ALU.add), reads=[t1, t2], writes=[t3])
    P.op("dve", lambda e: e.tensor_tensor(out=fr[:], in0=t3[:], in1=den[:], op=ALU.mult), reads=[t3, den], writes=[fr])
    P.op("dve", lambda e: e.tensor_tensor(out=t1[:], in0=ai[:], in1=pr[:, :, 0], op=ALU.mult), reads=[ai, pr], writes=[t1])
    P.op("dve", lambda e: e.tensor_tensor(out=t2[:], in0=ar[:], in1=pr[:, :, 1], op=ALU.mult), reads=[ar, pr], writes=[t2])
    P.op("dve", lambda e: e.tensor_tensor(out=t4[:], in0=t1[:], in1=t2[:], op=ALU.subtract), reads=[t1, t2], writes=[t4])
    P.op("dve", lambda e: e.tensor_tensor(out=fi[:], in0=t4[:], in1=den[:], op=ALU.mult), reads=[t4, den], writes=[fi])

    Ere = [P.sb(f"Ere{q}", [128, 512]) for q in range(4)]; Eim = [P.sb(f"Eim{q}", [128, 512]) for q in range(4)]
    Wre = [P.sb(f"Wre{q}", [128, 512]) for q in range(4)]; Wim = [P.sb(f"Wim{q}", [128, 512]) for q in range(4)]
    tt = P.sb("tt", [128, 512])
    for q in range(4):
        d = q // 2
        P.op("dve", lambda e, q=q: e.memset(Ere[q][:, 0:1], 1.0), writes=[Ere[q]])
        P.op("dve", lambda e, q=q: e.memset(Eim[q][:, 0:1], 0.0), writes=[Eim[q]])
        for l in range(9):
            n = 1 << l
            mr = Mre[l]; mi = Mim[l]
            P.op("dve", lambda e, q=q, n=n, mi=mi: e.tensor_scalar(out=tt[:, 0:n], in0=Eim[q][:, 0:n], scalar1=mi[:, q:q + 1], scalar2=None, op0=ALU.mult),
                 reads=[Eim[q], mi], writes=[tt])
            P.op("dve", lambda e, q=q, n=n, mr=mr: e.scalar_tensor_tensor(out=Ere[q][:, n:2 * n], in0=Ere[q][:, 0:n], scalar=mr[:, q:q + 1], in1=tt[:, 0:n],
                                                                         op0=ALU.mult, op1=ALU.subtract), reads=[Ere[q], mr, tt], writes=[Ere[q]])
            P.op("dve", lambda e, q=q, n=n, mi=mi: e.tensor_scalar(out=tt[:, 0:n], in0=Ere[q][:, 0:n], scalar1=mi[:, q:q + 1], scalar2=None, op0=ALU.mult),
                 reads=[Ere[q], mi], writes=[tt])
            P.op("dve", lambda e, q=q, n=n, mr=mr: e.scalar_tensor_tensor(out=Eim[q][:, n:2 * n], in0=Eim[q][:, 0:n], scalar=mr[:, q:q + 1], in1=tt[:, 0:n],
                                                                         op0=ALU.mult, op1=ALU.add), reads=[Eim[q], mr, tt], writes=[Eim[q]])
        opa = ALU.add if d == 0 else ALU.subtract
        opb = ALU.subtract if d == 0 else ALU.add
        P.op("dve", lambda e, q=q: e.tensor_scalar(out=tt[:], in0=Eim[q][:], scalar1=fi[:, q:q + 1], scalar2=None, op0=ALU.mult), reads=[Eim[q], fi], writes=[tt])
        P.op("dve", lambda e, q=q, opa=opa: e.scalar_tensor_tensor(out=Wre[q][:], in0=Ere[q][:], scalar=fr[:, q:q + 1], in1=tt[:], op0=ALU.mult, op1=opa),
             reads=[Ere[q], fr, tt], writes=[Wre[q]])
        P.op("dve", lambda e, q=q: e.tensor_scalar(out=tt[:], in0=Eim[q][:], scalar1=fr[:, q:q + 1], scalar2=None, op0=ALU.mult), reads=[Eim[q], fr], writes=[tt])
        P.op("dve", lambda e, q=q, opb=opb: e.scalar_tensor_tensor(out=Wim[q][:], in0=Ere[q][:], scalar=fi[:, q:q + 1], in1=tt[:], op0=ALU.mult, op1=opb),
             reads=[Ere[q], fi, tt], writes=[Wim[q]])

    Aps = [P.ps(f"Aps{i}", [128, 512]) for i in range(2)]; Bps = [P.ps(f"Bps{i}", [128, 512]) for i in range(2)]
    yps = [P.ps(f"yps{i}", [64, 512]) for i in range(2)]

    def dbl(nm):
        return [P.sb(f"{nm}{i}", [128, 512]) for i in range(2)]
    p1 = dbl("p1"); p2 = dbl("p2"); p3 = dbl("p3"); p4 = dbl("p4"); bur = dbl("bur"); bui = dbl("bui"); sre = dbl("sre"); sim = dbl("sim")
    def dblb(nm):
        return [P.sb(f"{nm}{i}", [128, 512], BF16) for i in range(2)]
    m1 = dblb("m1"); m2 = dblb("m2"); m3 = dblb("m3"); m4 = dblb("m4")
    ini_re = [P.sb(f"ini_re{i}", [128, 1]) for i in range(2)]; ini_im = [P.sb(f"ini_im{i}", [128, 1]) for i in range(2)]
    ta = P.sb("ta", [128, 1])
    items = []
    for q in range(4):
        d = q // 2
        order = list(range(17)) if d == 0 else [0] + list(range(16, 0, -1))
        for ci, c in enumerate(order):
            items.append((q, ci, c, len(order)))
    NI = len(items)

    def S1(n):
        q, ci, c, no = items[n]
        d, k = q // 2, q % 2
        t0, L = SBLK[c]
        z = n % 2
        A = Aps[z]; Bp = Bps[z]
        P.op("pe", lambda e, A=A, k=k, t0=t0, L=L: e.matmul(A[:, 0:L], lhsT=bre[32 * k:32 * k + 32, :], rhs=ub[32 * k:32 * k + 32, t0:t0 + L], start=True, stop=True),
             reads=[bre, ub], writes=[A])
        P.op("pe", lambda e, Bp=Bp, k=k, t0=t0, L=L: e.matmul(Bp[:, 0:L], lhsT=bim[32 * k:32 * k + 32, :], rhs=ub[32 * k:32 * k + 32, t0:t0 + L], start=True, stop=True),
             reads=[bim, ub], writes=[Bp])
        wr_, wi_ = Wre[q], Wim[q]
        P.op("dve", lambda e, z=z, A=A, L=L, wr_=wr_: e.tensor_tensor(out=p1[z][:, 0:L], in0=wr_[:, 0:L], in1=A[:, 0:L], op=ALU.mult), reads=[wr_, A], writes=[p1[z]])
        P.op("dve", lambda e, z=z, Bp=Bp, L=L, wi_=wi_: e.tensor_tensor(out=p2[z][:, 0:L], in0=wi_[:, 0:L], in1=Bp[:, 0:L], op=ALU.mult), reads=[wi_, Bp], writes=[p2[z]])
        P.op("dve", lambda e, z=z, Bp=Bp, L=L, wr_=wr_: e.tensor_tensor(out=p3[z][:, 0:L], in0=wr_[:, 0:L], in1=Bp[:, 0:L], op=ALU.mult), reads=[wr_, Bp], writes=[p3[z]])
        P.op("dve", lambda e, z=z, A=A, L=L, wi_=wi_: e.tensor_tensor(out=p4[z][:, 0:L], in0=wi_[:, 0:L], in1=A[:, 0:L], op=ALU.mult), reads=[wi_, A], writes=[p4[z]])
        P.op("pool", lambda e, z=z, L=L: e.tensor_tensor(out=bur[z][:, 0:L], in0=p1[z][:, 0:L], in1=p2[z][:, 0:L], op=ALU.subtract), reads=[p1[z], p2[z]], writes=[bur[z]])
        P.op("pool", lambda e, z=z, L=L: e.tensor_tensor(out=bui[z][:, 0:L], in0=p3[z][:, 0:L], in1=p4[z][:, 0:L], op=ALU.add), reads=[p3[z], p4[z]], writes=[bui[z]])

    def S2(n):
        q, ci, c, no = items[n]
        d, k = q // 2, q % 2
        t0, L = SBLK[c]
        z = n % 2
        rq = er[:, q:q + 1]
        for (sx, bx, inix) in ((sre, bur, ini_re), (sim, bui, ini_im)):
            if ci == 0:
                init = 0.0; rd = [bx[z], er]
            else:
                init = inix[(ci - 1) % 2][:, 0:1]; rd = [bx[z], er, inix[(ci - 1) % 2]]
            if d == 0:
                P.op("dve", lambda e, sx=sx, bx=bx, z=z, L=L, init=init, rq=rq: e.tensor_tensor_scan(
                    out=sx[z][:, 0:L], data0=rq.to_broadcast([128, L]), data1=bx[z][:, 0:L], initial=init, op0=ALU.mult, op1=ALU.add), reads=rd, writes=[sx[z]])
            else:
                P.op("dve", lambda e, sx=sx, bx=bx, z=z, L=L, init=init, rq=rq: e.tensor_tensor_scan(
                    out=sx[z][:, 0:L][:, ::-1], data0=rq.to_broadcast([128, L]), data1=bx[z][:, 0:L][:, ::-1], initial=init, op0=ALU.mult, op1=ALU.add), reads=rd, writes=[sx[z]])
        if ci < no - 1:
            if d == 0:
                lvl = 8 if L == 256 else 9; col_ = L - 1
            else:
                lvl = 9; col_ = 0
            elr = Mre[lvl][:, q:q + 1]; eli = Mim[lvl][:, q:q + 1]
            w = ci % 2
            P.op("dve", lambda e, z=z, col_=col_, eli=eli: e.tensor_tensor(out=ta[:], in0=sim[z][:, col_:col_ + 1], in1=eli, op=ALU.mult), reads=[sim[z], Mim[lvl]], writes=[ta])
            P.op("dve", lambda e, z=z, col_=col_, elr=elr, w=w: e.scalar_tensor_tensor(out=ini_re[w][:], in0=sre[z][:, col_:col_ + 1], scalar=elr, in1=ta[:], op0=ALU.mult, op1=ALU.subtract),
                 reads=[sre[z], Mre[lvl], ta], writes=[ini_re[w]])
            P.op("dve", lambda e, z=z, col_=col_, eli=eli: e.tensor_tensor(out=ta[:], in0=sre[z][:, col_:col_ + 1], in1=eli, op=ALU.mult), reads=[sre[z], Mim[lvl]], writes=[ta])
            P.op("dve", lambda e, z=z, col_=col_, elr=elr, w=w: e.scalar_tensor_tensor(out=ini_im[w][:], in0=sim[z][:, col_:col_ + 1], scalar=elr, in1=ta[:], op0=ALU.mult, op1=ALU.add),
                 reads=[sim[z], Mre[lvl], ta], writes=[ini_im[w]])
        er_, ei_ = Ere[q], Eim[q]
        P.op("pool", lambda e, z=z, L=L, er_=er_: e.tensor_tensor(out=m1[z][:, 0:L], in0=er_[:, 0:L], in1=sre[z][:, 0:L], op=ALU.mult), reads=[er_, sre[z]], writes=[m1[z]])
        P.op("pool", lambda e, z=z, L=L, ei_=ei_: e.tensor_tensor(out=m2[z][:, 0:L], in0=ei_[:, 0:L], in1=sim[z][:, 0:L], op=ALU.mult), reads=[ei_, sim[z]], writes=[m2[z]])
        P.op("pool", lambda e, z=z, L=L, er_=er_: e.tensor_tensor(out=m3[z][:, 0:L], in0=er_[:, 0:L], in1=sim[z][:, 0:L], op=ALU.mult), reads=[er_, sim[z]], writes=[m3[z]])
        P.op("dve", lambda e, z=z, L=L, ei_=ei_: e.tensor_tensor(out=m4[z][:, 0:L], in0=ei_[:, 0:L], in1=sre[z][:, 0:L], op=ALU.mult), reads=[ei_, sre[z]], writes=[m4[z]])
        yp = yps[z]
        c2 = ncre if d == 0 else cre
        c4 = ncim if d == 0 else cim
        for mi_, (cm, mm) in enumerate(((cre, m1), (c2, m2), (ncim, m3), (c4, m4))):
            P.op("pe", lambda e, yp=yp, cm=cm, mm=mm, z=z, L=L, q=q, mi_=mi_: e.matmul(yp[:, 0:L], lhsT=cm[:, q, :], rhs=mm[z][:, 0:L], start=(mi_ == 0), stop=(mi_ == 3)),
                 reads=[cm, mm[z]], writes=[yp], inc=(mi_ == 3))
        if q == 0:
            P.op("act", lambda e, yp=yp, t0=t0, L=L: e.activation(out=y[:, t0:t0 + L], in_=yp[:, 0:L], func=AF.Copy), reads=[yp], writes=[y])
        else:
            P.op("dve", lambda e, yp=yp, t0=t0, L=L: e.tensor_tensor(out=y[:, t0:t0 + L], in0=yp[:, 0:L], in1=y[:, t0:t0 + L], op=ALU.add), reads=[yp, y], writes=[y])

    S1(0)
    for n in range(NI):
        if n + 1 < NI:
            S1(n + 1)
        S2(n)
    for (t0, L) in SBLK:
        P.op("dve", lambda e, t0=t0, L=L: e.scalar_tensor_tensor(out=y[:, t0:t0 + L], in0=u[:, t0:t0 + L], scalar=dsk[:, 0:1], in1=y[:, t0:t0 + L], op0=ALU.mult, op1=ALU.add),
             reads=[u, dsk, y], writes=[y])
    P.dma("sp", yT.ap, y[:], reads=[y], is_output=True)
    P.emit()
    return nc


def stage_s5_inputs(PS, inp, i):
    maps = []
    lre, lim, lst = inp["s5_lam_re"][i], inp["s5_lam_im"][i], inp["s5_log_step"][i]
    for core in range(8):
        b, gq = core // 4, core % 4
        m = {}
        m["uT"] = np.ascontiguousarray(PS[b][:, S5OFF + 64 * gq:S5OFF + 64 * gq + 64].T)
        prm = np.zeros((128, 4, 3), np.float32)
        bT = np.zeros((2, 64, 128), np.float32)
        cT = np.zeros((2, 4, 128, 64), np.float32)
        for k in range(2):
            for gl in range(2):
                g = 4 * gq + 2 * k + gl
                rows = slice(32 * k + 16 * gl, 32 * k + 16 * gl + 16)
                cols = slice(64 * gl, 64 * gl + 64)
                bT[0, rows, cols] = inp["s5_b_re"][i][g].T
                bT[1, rows, cols] = inp["s5_b_im"][i][g].T
                for d in range(2):
                    q = 2 * d + k
                    prm[cols, q, 0] = lre[d, g]; prm[cols, q, 1] = lim[d, g]; prm[cols, q, 2] = lst[d, g]
                    cT[0, q, cols, rows] = inp["s5_c_re"][i][d, g].T
                    cT[1, q, cols, rows] = inp["s5_c_im"][i][d, g].T
        m["prm"] = prm; m["bT"] = bT; m["cT"] = cT
        m["dsk"] = col(inp["s5_d"][i][64 * gq:64 * gq + 64])
        maps.append(m)
    return maps


def build_stage_c1():
    nc = bass.Bass("TRN2", target_bir_lowering=False)
    P = Prog(nc)
    catT = P.dram("catT", [128, KT, NTOK], F32, "ExternalInput")
    xT = P.dram("xT", [128, KT, NTOK], F32, "ExternalInput")
    modTd = P.dram("modT", [128, 48, 2], F32, "ExternalInput")
    w_out = P.dram("w_out", [D, D], F32, "ExternalInput")
    w_glu = P.dram("w_glu", [256, 256], F32, "ExternalInput")
    bgluT = P.dram("bgluT", [128, 2], F32, "ExternalInput")
    gssdT = P.dram("gssdT", [128, 2], F32, "ExternalInput")
    norm2T = P.dram("norm2T", [128, KT], F32, "ExternalInput")
    wr_d = P.dram("wr", [D, 36], F32, "ExternalInput")
    br_d = P.dram("br", [128, 36], F32, "ExternalInput")
    ident_d = P.dram("ident", [128, 128], F32, "ExternalInput")
    xmid = P.dram("xmid", [128, KT, NTOK], F32, "ExternalOutput")
    hT = P.dram("hT", [128, KT, NTOK], F32, "ExternalOutput")
    gatesT = P.dram("gatesT", [32, NTOK], F32, "ExternalOutput")

    ones_bf = P.sb("ones_bf", [128, 128], BF16)
    P.op("dve", lambda e: e.memset(ones_bf[:], 1.0), writes=[ones_bf])
    modT = P.sb("modT_sb", [128, 48, 2]); P.dma("sp", modT[:], modTd.ap, writes=[modT])
    bglu = P.sb("bglu", [128, 2]); P.dma("sp", bglu[:], bgluT.ap, writes=[bglu])
    gssd = P.sb("gssd", [128, 2]); P.dma("sp", gssd[:], gssdT.ap, writes=[gssd])
    n2 = P.sb("n2", [128, KT]); P.dma("sp", n2[:], norm2T.ap, writes=[n2])
    wr = P.sb("wr_sb", [128, KT, 36]); P.dma("sp", wr[:], wr_d.ap.rearrange("(kt p) n -> p kt n", p=128), writes=[wr])
    br = P.sb("br_sb", [128, 36]); P.dma("sp", br[:], br_d.ap, writes=[br])
    ident = P.sb("ident_sb", [128, 128]); P.dma("sp", ident[:], ident_d.ap, writes=[ident])
    wout = P.sb("wout_bf", [128, KT, D], BF16)
    P.dma("pool", wout[:], w_out.ap.rearrange("(kt p) n -> p kt n", p=128), writes=[wout])
    wglu = P.sb("wglu_bf", [128, 2, 256], BF16)
    P.dma("pool", wglu[:], w_glu.ap.rearrange("(kt p) n -> p kt n", p=128), writes=[wglu])
    G2 = P.sb("G2", [128, KT, 2])
    for r in range(2):
        P.op("dve", lambda e, r=r: e.scalar_tensor_tensor(out=G2[:, :, r], in0=modT[:, 32:40, r], scalar=1.0, in1=n2[:, :], op0=ALU.add, op1=ALU.mult),
             reads=[modT, n2], writes=[G2])

    cat = P.sb("cat", [128, KT, 512]); xm = P.sb("xm", [128, KT, 512]); tmp = P.sb("tmp", [128, KT, 512]); hh = P.sb("hh", [128, KT, 512])
    sq = P.sb("sq", [128, KT, 512], BF16); catb = P.sb("catb", [128, KT, 512], BF16)
    gg = P.sb("gg", [128, 2, 512]); ggb = P.sb("ggb", [128, 2, 512], BF16); sig = P.sb("sig", [128, 512])
    rs = P.sb("rs", [128, 512]); rstd = P.sb("rstd", [128, 512])
    gT = P.sb("gT", [32, NTOK])
    ss_ps = P.ps("ss_ps", [128, 512]); lin_ps = P.ps("lin_ps", [128, 512])
    mix_ps = [P.ps(f"mix_ps{i}", [128, 512]) for i in range(2)]
    lg_ps = [P.ps(f"lg_ps{i}", [128, 36]) for i in range(2)]
    tr_ps = P.ps("tr_ps", [32, 128])
    Lg = P.sb("Lg", [128, 36]); gmax = P.sb("gmax", [128, 1]); ngmax = P.sb("ngmax", [128, 1]); ghot = P.sb("ghot", [128, 4]); eg = P.sb("eg", [128, 4])
    sume = P.sb("sume", [128, 1]); pg = P.sb("pg", [128, 1]); les = P.sb("les", [128, 8]); m1 = P.sb("m1", [128, 1]); hot1 = P.sb("hot1", [128, 8])
    le2 = P.sb("le2", [128, 8]); m2 = P.sb("m2", [128, 1]); hot2 = P.sb("hot2", [128, 8]); d21 = P.sb("d21", [128, 1]); e21 = P.sb("e21", [128, 1])
    w1 = P.sb("w1", [128, 1]); w2 = P.sb("w2", [128, 1]); inner = P.sb("inner", [128, 8]); gates = P.sb("gates", [128, 32])
    mi = 0
    for bi, (t0, T) in enumerate(BLOCKS):
        r = 1 if bi == 4 else 0
        P.dma("sp", cat[:, :, 0:T], catT.ap[:, :, t0:t0 + T], writes=[cat])
        P.dma("sp", xm[:, :, 0:T], xT.ap[:, :, t0:t0 + T], writes=[xm])
        P.op("act", lambda e, T=T: e.activation(out=sq[:, 2:4, 0:T], in_=cat[:, 2:4, 0:T], func=AF.Square), reads=[cat], writes=[sq])
        for j, kt in enumerate((2, 3)):
            P.op("pe", lambda e, kt=kt, j=j, T=T: e.matmul(ss_ps[:, 0:T], lhsT=ones_bf[:, :], rhs=sq[:, kt, 0:T], start=(j == 0), stop=(j == 1)),
                 reads=[ones_bf, sq], writes=[ss_ps], inc=(j == 1))
        P.op("act", lambda e, T=T: e.activation(out=rs[:, 0:T], in_=ss_ps[:, 0:T], func=AF.Sqrt, scale=1.0 / 256, bias=EPS), reads=[ss_ps], writes=[rs])
        P.op("dve", lambda e, T=T: e.reciprocal(out=rstd[:, 0:T], in_=rs[:, 0:T]), reads=[rs], writes=[rstd])
        for j, kt in enumerate((2, 3)):
            P.op("dve", lambda e, kt=kt, T=T: e.tensor_tensor(out=tmp[:, kt, 0:T], in0=cat[:, kt, 0:T], in1=rstd[:, 0:T], op=ALU.mult), reads=[cat, rstd], writes=[tmp])
            P.op("act", lambda e, kt=kt, j=j, T=T: e.activation(out=catb[:, kt, 0:T], in_=tmp[:, kt, 0:T], func=AF.Identity, scale=gssd[:, j:j + 1]),
                 reads=[tmp, gssd], writes=[catb])
        P.op("act", lambda e, T=T: e.activation(out=gg[:, :, 0:T], in_=cat[:, 4:6, 0:T], func=AF.Gelu_apprx_tanh), reads=[cat], writes=[gg])
        P.op("pool", lambda e, T=T: e.tensor_copy(out=ggb[:, :, 0:T], in_=gg[:, :, 0:T]), reads=[gg], writes=[ggb])
        for j in range(2):
            for kt in range(2):
                P.op("pe", lambda e, j=j, kt=kt, T=T: e.matmul(lin_ps[:, 0:T], lhsT=wglu[:, kt, j * 128:(j + 1) * 128], rhs=ggb[:, kt, 0:T], start=(kt == 0), stop=(kt == 1)),
                     reads=[wglu, ggb], writes=[lin_ps], inc=(kt == 1))
            P.op("act", lambda e, j=j, T=T: e.activation(out=sig[:, 0:T], in_=lin_ps[:, 0:T], func=AF.Sigmoid, bias=bglu[:, j:j + 1]), reads=[lin_ps, bglu], writes=[sig])
            P.op("dve", lambda e, j=j, T=T: e.tensor_tensor(out=catb[:, 4 + j, 0:T], in0=gg[:, j, 0:T], in1=sig[:, 0:T], op=ALU.mult), reads=[gg, sig], writes=[catb])
        P.op("pool", lambda e, T=T: e.tensor_copy(out=catb[:, 0:2, 0:T], in_=cat[:, 0:2, 0:T]), reads=[cat], writes=[catb])
        P.op("pool", lambda e, T=T: e.tensor_copy(out=catb[:, 6:8, 0:T], in_=cat[:, 6:8, 0:T]), reads=[cat], writes=[catb])
        for nt in range(KT):
            mp = mix_ps[mi % 2]; mi += 1
            for kt in range(KT):
                P.op("pe", lambda e, mp=mp, nt=nt, kt=kt, T=T: e.matmul(mp[:, 0:T], lhsT=wout[:, kt, nt * 128:(nt + 1) * 128], rhs=catb[:, kt, 0:T], start=(kt == 0), stop=(kt == KT - 1)),
                     reads=[wout, catb], writes=[mp], inc=(kt == KT - 1))
            P.op("dve", lambda e, mp=mp, nt=nt, T=T, r=r: e.scalar_tensor_tensor(out=xm[:, nt, 0:T], in0=mp[:, 0:T], scalar=modT[:, 16 + nt, r:r + 1], in1=xm[:, nt, 0:T],
                                                                             op0=ALU.mult, op1=ALU.add), reads=[mp, modT, xm], writes=[xm])
        P.dma("sp", xmid.ap[:, :, t0:t0 + T], xm[:, :, 0:T], reads=[xm], is_output=True)
        P.op("act", lambda e, T=T: e.activation(out=sq[:, :, 0:T], in_=xm[:, :, 0:T], func=AF.Square), reads=[xm], writes=[sq])
        for kt in range(KT):
            P.op("pe", lambda e, kt=kt, T=T: e.matmul(ss_ps[:, 0:T], lhsT=ones_bf[:, :], rhs=sq[:, kt, 0:T], start=(kt == 0), stop=(kt == KT - 1)),
                 reads=[ones_bf, sq], writes=[ss_ps], inc=(kt == KT - 1))
        P.op("act", lambda e, T=T: e.activation(out=rs[:, 0:T], in_=ss_ps[:, 0:T], func=AF.Sqrt, scale=1.0 / D, bias=EPS), reads=[ss_ps], writes=[rs])
        P.op("dve", lambda e, T=T: e.reciprocal(out=rstd[:, 0:T], in_=rs[:, 0:T]), reads=[rs], writes=[rstd])
        for kt in range(KT):
            P.op("dve", lambda e, kt=kt, T=T: e.tensor_tensor(out=tmp[:, kt, 0:T], in0=xm[:, kt, 0:T], in1=rstd[:, 0:T], op=ALU.mult), reads=[xm, rstd], writes=[tmp])
            P.op("act", lambda e, kt=kt, T=T, r=r: e.activation(out=hh[:, kt, 0:T], in_=tmp[:, kt, 0:T], func=AF.Identity, scale=G2[:, kt, r:r + 1], bias=modT[:, 24 + kt, r:r + 1]),
                 reads=[tmp, G2, modT], writes=[hh])
        P.dma("sp", hT.ap[:, :, t0:t0 + T], hh[:, :, 0:T], reads=[hh], is_output=True)
        for s in range(T // 128):
            lp = lg_ps[s % 2]
            for kt in range(KT):
                P.op("pe", lambda e, lp=lp, kt=kt, s=s: e.matmul(lp[:, :], lhsT=hh[:, kt, s * 128:(s + 1) * 128], rhs=wr[:, kt, :], start=(kt == 0), stop=(kt == KT - 1)),
                     reads=[hh, wr], writes=[lp], inc=(kt == KT - 1))
            P.op("dve", lambda e, lp=lp: e.tensor_tensor(out=Lg[:], in0=lp[:, :], in1=br[:], op=ALU.add), reads=[lp, br], writes=[Lg])
            P.op("dve", lambda e: e.tensor_reduce(out=gmax[:], in_=Lg[:, 0:4], axis=AX.X, op=ALU.max), reads=[Lg], writes=[gmax])
            P.op("dve", lambda e: e.tensor_scalar(out=ghot[:], in0=Lg[:, 0:4], scalar1=gmax[:, 0:1], scalar2=None, op0=ALU.is_ge), reads=[Lg, gmax], writes=[ghot])
            P.op("dve", lambda e: e.tensor_scalar(out=ngmax[:], in0=gmax[:], scalar1=-1.0, scalar2=None, op0=ALU.mult), reads=[gmax], writes=[ngmax])
            P.op("act", lambda e: e.activation(out=eg[:], in_=Lg[:, 0:4], func=AF.Exp, bias=ngmax[:, 0:1]), reads=[Lg, ngmax], writes=[eg])
            P.op("dve", lambda e: e.tensor_reduce(out=sume[:], in_=eg[:], axis=AX.X, op=ALU.add), reads=[eg], writes=[sume])
            P.op("dve", lambda e: e.reciprocal(out=pg[:], in_=sume[:]), reads=[sume], writes=[pg])
            P.op("dve", lambda e: e.tensor_scalar(out=les[:], in0=Lg[:, 4:12], scalar1=ghot[:, 0:1], scalar2=None, op0=ALU.mult), reads=[Lg, ghot], writes=[les])
            for g in range(1, 4):
                P.op("dve", lambda e, g=g: e.scalar_tensor_tensor(out=les[:], in0=Lg[:, 4 + 8 * g:12 + 8 * g], scalar=ghot[:, g:g + 1], in1=les[:], op0=ALU.mult, op1=ALU.add),
                     reads=[Lg, ghot, les], writes=[les])
            P.op("dve", lambda e: e.tensor_reduce(out=m1[:], in_=les[:], axis=AX.X, op=ALU.max), reads=[les], writes=[m1])
            P.op("dve", lambda e: e.tensor_scalar(out=hot1[:], in0=les[:], scalar1=m1[:, 0:1], scalar2=None, op0=ALU.is_ge), reads=[les, m1], writes=[hot1])
            P.op("dve", lambda e: e.scalar_tensor_tensor(out=le2[:], in0=hot1[:], scalar=-1e30, in1=les[:], op0=ALU.mult, op1=ALU.add), reads=[hot1, les], writes=[le2])
            P.op("dve", lambda e: e.tensor_reduce(out=m2[:], in_=le2[:], axis=AX.X, op=ALU.max), reads=[le2], writes=[m2])
            P.op("dve", lambda e: e.tensor_scalar(out=hot2[:], in0=le2[:], scalar1=m2[:, 0:1], scalar2=None, op0=ALU.is_ge), reads=[le2, m2], writes=[hot2])
            P.op("dve", lambda e: e.tensor_tensor(out=d21[:], in0=m2[:], in1=m1[:], op=ALU.subtract), reads=[m1, m2], writes=[d21])
            P.op("act", lambda e: e.activation(out=e21[:], in_=d21[:], func=AF.Exp), reads=[d21], writes=[e21])
            P.op("dve", lambda e: e.tensor_scalar(out=w1[:], in0=e21[:], scalar1=1.0, scalar2=None, op0=ALU.add), reads=[e21], writes=[w1])
            P.op("dve", lambda e: e.reciprocal(out=w1[:], in_=w1[:]), reads=[w1], writes=[w1])
            P.op("dve", lambda e: e.tensor_tensor(out=w1[:], in0=w1[:], in1=pg[:], op=ALU.mult), reads=[w1, pg], writes=[w1])
            P.op("dve", lambda e: e.tensor_tensor(out=w2[:], in0=w1[:], in1=e21[:], op=ALU.mult), reads=[w1, e21], writes=[w2])
            P.op("dve", lambda e: e.tensor_scalar(out=inner[:], in0=hot1[:], scalar1=w1[:, 0:1], scalar2=None, op0=ALU.mult), reads=[hot1, w1], writes=[inner])
            P.op("dve", lambda e: e.scalar_tensor_tensor(out=inner[:], in0=hot2[:], scalar=w2[:, 0:1], in1=inner[:], op0=ALU.mult, op1=ALU.add), reads=[hot2, w2, inner], writes=[inner])
            for g in range(4):
                P.op("dve", lambda e, g=g: e.tensor_scalar(out=gates[:, 8 * g:8 * g + 8], in0=inner[:], scalar1=ghot[:, g:g + 1], scalar2=None, op0=ALU.mult),
                     reads=[inner, ghot], writes=[gates])
            P.op("pe", lambda e: e.transpose(tr_ps[:, :], gates[:, :], ident[:, :]), reads=[gates, ident], writes=[tr_ps])
            tk = t0 + s * 128
            P.op("act", lambda e, tk=tk: e.activation(out=gT[:, tk:tk + 128], in_=tr_ps[:, :], func=AF.Copy), reads=[tr_ps], writes=[gT])
    P.dma("sp", gatesT.ap, gT[:], reads=[gT], is_output=True)
    P.emit()
    return nc


def build_stage_c2():
    nc = bass.Bass("TRN2", target_bir_lowering=False)
    P = Prog(nc)
    hTd = P.dram("hT", [128, KT, NTOK], F32, "ExternalInput")
    xmid = P.dram("xmid", [128, KT, NTOK], F32, "ExternalInput")
    gatesT = P.dram("gatesT", [32, NTOK], F32, "ExternalInput")
    modTd = P.dram("modT", [128, 48, 2], F32, "ExternalInput")
    seld = P.dram("sel", [32, 32, 128], F32, "ExternalInput")
    w_gate = P.dram("w_gate", [32, D, 256], F32, "ExternalInput")
    w_up = P.dram("w_up", [32, D, 256], F32, "ExternalInput")
    w_down = P.dram("w_down", [32, 256, D], F32, "ExternalInput")
    xout = P.dram("xout", [128, KT, NTOK], F32, "ExternalOutput")

    hb = P.sb("hb", [128, KT, NTOK], BF16)
    xa = P.sb("xa", [128, KT, NTOK])
    for kt in range(KT):
        P.dma("pool", hb[:, kt, :], hTd.ap[:, kt, :], writes=[hb])
    for kt in range(KT):
        P.dma("sp", xa[:, kt, :], xmid.ap[:, kt, :], writes=[xa])
    gT = P.sb("gT", [32, NTOK]); P.dma("sp", gT[:], gatesT.ap, writes=[gT])
    modT = P.sb("modT_sb", [128, 48, 2]); P.dma("sp", modT[:], modTd.ap, writes=[modT])
    sel = P.sb("sel_sb", [32, 32, 128]); P.dma("sp", sel[:], seld.ap, writes=[sel])
    wg = [P.sb(f"wg{i}", [128, KT, 256], BF16) for i in range(2)]
    wu = [P.sb(f"wu{i}", [128, KT, 256], BF16) for i in range(2)]
    wd = [P.sb(f"wd{i}", [128, 2, D], BF16) for i in range(2)]
    gb_ps = P.ps("gb_ps", [128, 512])
    G_ps = [P.ps(f"G_ps{i}", [128, 512]) for i in range(2)]; U_ps = [P.ps(f"U_ps{i}", [128, 512]) for i in range(2)]
    O_ps = [P.ps(f"O_ps{i}", [128, 512]) for i in range(2)]
    gbs = [P.sb(f"gbs{i}", [128, 512]) for i in range(2)]
    sg = [P.sb(f"sg{i}", [128, 512]) for i in range(2)]; su = [P.sb(f"su{i}", [128, 512]) for i in range(2)]
    hid = [P.sb(f"hid{i}", [128, 2, 512], BF16) for i in range(2)]
    cnt = {"oi": 0, "ji": 0}
    items = [(ex, bi) for ex in range(32) for bi in range(len(BLOCKS))]
    NI = len(items)

    def load_w(ex):
        z = ex % 2
        P.dma("pool", wg[z][:], w_gate.ap[ex].rearrange("(kt p) n -> p kt n", p=128), writes=[wg[z]])
        P.dma("pool", wu[z][:], w_up.ap[ex].rearrange("(kt p) n -> p kt n", p=128), writes=[wu[z]])
        P.dma("pool", wd[z][:], w_down.ap[ex].rearrange("(kt p) n -> p kt n", p=128), writes=[wd[z]])

    def emit_front(n):
        ex, bi = items[n]
        t0, T = BLOCKS[bi]
        z = ex % 2; b2 = n % 2
        P.op("pe", lambda e, ex=ex, t0=t0, T=T: e.matmul(gb_ps[:, 0:T], lhsT=sel[:, ex, :], rhs=gT[:, t0:t0 + T], start=True, stop=True), reads=[sel, gT], writes=[gb_ps])
        P.op("act", lambda e, b2=b2, T=T: e.activation(out=gbs[b2][:, 0:T], in_=gb_ps[:, 0:T], func=AF.Copy), reads=[gb_ps], writes=[gbs[b2]])
        for j in range(2):
            j2 = cnt["ji"] % 2; cnt["ji"] += 1
            Gp = G_ps[j2]; Up = U_ps[j2]
            for kt in range(KT):
                P.op("pe", lambda e, Gp=Gp, z=z, kt=kt, j=j, t0=t0, T=T: e.matmul(Gp[:, 0:T], lhsT=wg[z][:, kt, j * 128:(j + 1) * 128], rhs=hb[:, kt, t0:t0 + T],
                                                                               start=(kt == 0), stop=(kt == KT - 1)), reads=[wg[z], hb], writes=[Gp], inc=(kt == KT - 1))
            for kt in range(KT):
                P.op("pe", lambda e, Up=Up, z=z, kt=kt, j=j, t0=t0, T=T: e.matmul(Up[:, 0:T], lhsT=wu[z][:, kt, j * 128:(j + 1) * 128], rhs=hb[:, kt, t0:t0 + T],
                                                                               start=(kt == 0), stop=(kt == KT - 1)), reads=[wu[z], hb], writes=[Up], inc=(kt == KT - 1))
            P.op("act", lambda e, Gp=Gp, j2=j2, T=T: e.activation(out=sg[j2][:, 0:T], in_=Gp[:, 0:T], func=AF.Silu), reads=[Gp], writes=[sg[j2]])
            P.op("dve", lambda e, Up=Up, j2=j2, T=T: e.tensor_tensor(out=su[j2][:, 0:T], in0=sg[j2][:, 0:T], in1=Up[:, 0:T], op=ALU.mult), reads=[sg[j2], Up], writes=[su[j2]])
            P.op("dve", lambda e, j2=j2, b2=b2, j=j, T=T: e.tensor_tensor(out=hid[b2][:, j, 0:T], in0=su[j2][:, 0:T], in1=gbs[b2][:, 0:T], op=ALU.mult),
                 reads=[su[j2], gbs[b2]], writes=[hid[b2]])

    def emit_back(n):
        ex, bi = items[n]
        t0, T = BLOCKS[bi]
        r = 1 if bi == 4 else 0
        z = ex % 2; b2 = n % 2
        for nt in range(KT):
            Op = O_ps[cnt["oi"] % 2]; cnt["oi"] += 1
            for j in range(2):
                P.op("pe", lambda e, Op=Op, z=z, j=j, nt=nt, b2=b2, T=T: e.matmul(Op[:, 0:T], lhsT=wd[z][:, j, nt * 128:(nt + 1) * 128], rhs=hid[b2][:, j, 0:T],
                                                                               start=(j == 0), stop=(j == 1)), reads=[wd[z], hid[b2]], writes=[Op], inc=(j == 1))
            P.op("dve", lambda e, Op=Op, nt=nt, t0=t0, T=T, r=r: e.scalar_tensor_tensor(out=xa[:, nt, t0:t0 + T], in0=Op[:, 0:T], scalar=modT[:, 40 + nt, r:r + 1],
                                                                                      in1=xa[:, nt, t0:t0 + T], op0=ALU.mult, op1=ALU.add), reads=[Op, modT, xa], writes=[xa])

    load_w(0); load_w(1)
    emit_front(0)
    for n in range(NI):
        if n + 1 < NI:
            emit_front(n + 1)
        emit_back(n)
        ex, bi = items[n]
        if bi == len(BLOCKS) - 1 and ex + 2 < 32:
            load_w(ex + 2)
    for kt in range(KT):
        P.dma("sp", xout.ap[:, kt, :], xa[:, kt, :], reads=[xa], is_output=True)
    P.emit()
    return nc


def feat_to_tok(xt):
    return np.ascontiguousarray(xt.transpose(2, 1, 0).reshape(xt.shape[2], D))


def c_consts():
    if "c" not in _CONST:
        sel = np.zeros((32, 32, 128), np.float32)
        for e in range(32):
            sel[e, e, :] = 1.0
        _CONST["c"] = dict(sel=sel, ident=np.eye(128, dtype=np.float32))
    return _CONST["c"]


def run_layer(i, xT_cores, inp, dbg=None):
    f32 = lambda a: np.ascontiguousarray(np.asarray(a, np.float32))
    resA = _run("A", build_stage_a, stage_a_inputs(xT_cores, inp["c"], inp["c_ctx"], f32(inp["w_ada"][i]), inp["b_ada"][i], inp["norm1"][i], f32(inp["w_in"][i])))
    PS = []
    for b in range(2):
        lat = np.concatenate([resA[4 * b + q]["p"][0:2048] for q in range(4)], 0)
        cx = np.concatenate([resA[4 * b + q]["p"][2048:2112] for q in range(4)], 0)
        PS.append(np.concatenate([cx, lat], 0))
    resB1 = _run("B1", build_stage_attn, stage_attn_inputs(PS, inp, i))
    resB2 = _run("B2", build_stage_ssd, stage_ssd_inputs(PS, inp, i))
    resB3 = _run("B3", build_stage_s5, stage_s5_inputs(PS, inp, i))
    cst = c_consts()
    wr = f32(np.concatenate([inp["moe_w_group"][i], inp["moe_w_expert"][i].transpose(1, 0, 2).reshape(D, 32)], axis=1))
    br = rep(np.concatenate([inp["moe_b_group"][i], inp["moe_b_expert"][i].reshape(32)]))
    mapsC1 = []
    cats = []
    for b in range(2):
        a = np.concatenate([untile(resB1[4 * b + h]["o_m"]) for h in range(4)], 1)
        sd = np.concatenate([untile(resB2[4 * b + h]["o_s"]) for h in range(4)], 1)
        s5 = np.concatenate([resB3[4 * b + g]["yT"].T for g in range(4)], 1)
        df = np.concatenate([untile(resB1[4 * b + h]["o_d"]) for h in range(4)], 1)
        cats.append(np.concatenate([a, sd, s5, df], 1))
    if dbg is not None:
        dbg["PS"] = PS; dbg["cats"] = cats
    for core in range(8):
        b, q = core // 4, core % 4
        cs = cats[b]
        ct = tok_to_feat(core_tokens(cs[256:], cs[:256], q))
        mapsC1.append({"catT": ct, "xT": xT_cores[core], "modT": resA[core]["modT"], "w_out": f32(inp["w_out"][i]), "w_glu": f32(inp["s5_w_glu"][i]),
                       "bgluT": vecT(inp["s5_b_glu"][i], 2), "gssdT": vecT(inp["ssd_norm"][i], 2), "norm2T": vecT(inp["norm2"][i]),
                       "wr": wr, "br": br, "ident": cst["ident"]})
    resC1 = _run("C1", build_stage_c1, mapsC1)
    if dbg is not None:
        dbg["C1"] = resC1
    wg, wu, wd = f32(inp["moe_w_gate"][i]), f32(inp["moe_w_up"][i]), f32(inp["moe_w_down"][i])
    mapsC2 = [{"hT": resC1[c]["hT"], "xmid": resC1[c]["xmid"], "gatesT": resC1[c]["gatesT"], "modT": resA[c]["modT"], "sel": cst["sel"],
               "w_gate": wg, "w_up": wu, "w_down": wd} for c in range(8)]
    resC2 = _run("C2", build_stage_c2, mapsC2)
    return [resC2[c]["xout"] for c in range(8)]


def kernel(**inputs):
    inp = {k: np.asarray(v) for k, v in inputs.items()}
    x = np.asarray(inp["x"], np.float32); ctx = np.asarray(inp["ctx"], np.float32)
    xT_cores = [tok_to_feat(core_tokens(x[c // 4], ctx[c // 4], c % 4)) for c in range(8)]
    for i in range(4):
        xT_cores = run_layer(i, xT_cores, inp)
    out = np.zeros((2, 8192, D), np.float32)
    for c in range(8):
        b, q = c // 4, c % 4
        out[b, q * 2048:(q + 1) * 2048] = feat_to_tok(xT_cores[c])[:2048]
    return out
```

```python
import contextlib
import math
import numpy as np
import concourse.bass as bass
import concourse.mybir as mybir
from concourse.bass_utils import run_bass_kernel_spmd

F32 = mybir.dt.float32
BF16 = mybir.dt.bfloat16
I32 = mybir.dt.int32
AF = mybir.ActivationFunctionType
ALU = mybir.AluOpType
AX = mybir.AxisListType

SAME_ENGINE_SYNC = True


class Buf:
    _n = 0

    def __init__(self, t, name):
        self.t = t
        self.name = name
        Buf._n += 1
        self.id = Buf._n
        self.w = {}
        self.r = {}
        self.dcnt = {}

    def __getitem__(self, idx):
        return self.t[idx]


class Prog:
    ENG = ("pe", "act", "dve", "pool", "sp")

    def __init__(self, nc):
        self.nc = nc
        self.stack = contextlib.ExitStack()
        self.q = {e: [] for e in self.ENG}
        self.cnt = {e: 0 for e in self.ENG}
        self.seen = {e: {} for e in self.ENG}
        self.semh = {}
        self.out_events = []
        self.ninstr = 0
        self.dma_latest = {}

    def sb(self, name, shape, dt=F32):
        t = self.stack.enter_context(self.nc.sbuf_tensor(name, list(shape), dt))
        return Buf(t, name)

    def ps(self, name, shape, dt=F32):
        t = self.stack.enter_context(self.nc.psum_tensor(name, list(shape), dt))
        return Buf(t, name)

    def view(self, b, name=None):
        return Buf(b.t, name or (b.name + "_v"))

    def dram(self, name, shape, dt, kind):
        t = self.nc.dram_tensor(name, list(shape), dt, kind=kind)
        b = Buf(t, name)
        b.ap = t.ap()
        return b

    def sem(self, key):
        if key not in self.semh:
            nm = "s_" + "_".join(str(k) for k in (key if isinstance(key, tuple) else (key,)))
            self.semh[key] = self.stack.enter_context(self.nc.semaphore(nm))
        return self.semh[key]

    def push(self):
        self._saved = getattr(self, "_saved", [])
        self._saved.append(self.stack)
        self.stack = contextlib.ExitStack()

    def pop(self):
        self.barrier()
        self._deferred = getattr(self, "_deferred", [])
        self._deferred.append(self.stack)
        self.stack = self._saved.pop()

    def barrier(self):
        evs = {}
        for e in self.ENG:
            if self.cnt[e] > 0:
                evs[e] = self.cnt[e]
        for k, v in self.dma_latest.items():
            evs[k] = v
        for e in self.ENG:
            waits = []
            for k, v in evs.items():
                if k == e:
                    continue
                if self.seen[e].get(k, 0) >= v:
                    continue
                self.seen[e][k] = v
                waits.append((k, v))
            if waits:
                self.q[e].append((None, waits, None))

    def _deps(self, eng, reads, writes, own_key):
        deps = {}

        def add(k, v):
            if deps.get(k, 0) < v:
                deps[k] = v

        for b in reads:
            for k, v in b.w.items():
                add(k, v)
        for b in writes:
            for k, v in b.w.items():
                add(k, v)
            for k, v in b.r.items():
                add(k, v)
        waits = []
        for k, v in deps.items():
            if k == own_key and (k == "pe" or not SAME_ENGINE_SYNC or isinstance(k, tuple)):
                continue
            if self.seen[eng].get(k, 0) >= v:
                continue
            self.seen[eng][k] = v
            waits.append((k, v))
        return waits

    def _commit(self, reads, writes, key, val):
        for b in reads:
            if b.r.get(key, 0) < val:
                b.r[key] = val
        for b in writes:
            b.w = {key: val}
            b.r = {}

    def op(self, eng, fn, reads=(), writes=(), inc=True):
        waits = self._deps(eng, reads, writes, eng)
        if inc:
            self.cnt[eng] += 1
            val = self.cnt[eng]
        else:
            assert eng == "pe"
            val = self.cnt[eng] + 1
        self.q[eng].append((fn, waits, (eng, 1) if inc else None))
        self._commit(reads, writes, eng, val)
        self.ninstr += 1

    def dma(self, eng, out_ap, in_ap, reads=(), writes=(), is_output=False, **kw):
        prim = None
        for b in list(writes) + list(reads):
            if not hasattr(b, "ap"):
                prim = b
                break
        if prim is None:
            prim = (list(writes) + list(reads))[0]
        key = ("d", prim.id, eng)
        waits = self._deps(eng, reads, writes, key)
        prim.dcnt[key] = prim.dcnt.get(key, 0) + 16
        val = prim.dcnt[key]
        fn = lambda e, o=out_ap, i=in_ap, kw=kw: e.dma_start(out=o, in_=i, **kw)
        self.q[eng].append((fn, waits, (key, 16)))
        self._commit(reads, writes, key, val)
        self.dma_latest[key] = val
        if is_output:
            self.out_events.append((key, val))
        self.ninstr += 1

    def emit(self, final_eng="sp"):
        nc = self.nc
        fw = {}
        for k, v in self.out_events:
            fw[k] = max(fw.get(k, 0), v)
        final_waits = list(fw.items())
        for e in self.ENG:
            self.sem(e)
        for e in self.ENG:
            for fn, waits, inc in self.q[e]:
                for k, v in waits:
                    self.sem(k)
                if fn is None:
                    continue
                if inc:
                    self.sem(inc[0])
        for k, v in final_waits:
            self.sem(k)
        qs = self.q
        semh = self.semh

        def run(engobj, e):
            for fn, waits, inc in qs[e]:
                for k, v in waits:
                    engobj.wait_ge(semh[k], v)
                if fn is None:
                    continue
                ins = fn(engobj)
                if inc:
                    ins.then_inc(semh[inc[0]], inc[1])
            if e == final_eng:
                for k, v in final_waits:
                    engobj.wait_ge(semh[k], v)

        with nc.Block() as block:
            @block.tensor
            def _(eng):
                run(eng, "pe")

            @block.scalar
            def _(eng):
                run(eng, "act")

            @block.vector
            def _(eng):
                run(eng, "dve")

            @block.gpsimd
            def _(eng):
                run(eng, "pool")

            @block.sync
            def _(eng):
                run(eng, "sp")
        self.stack.close()


D = 1024
KT = 8
NTOK = 2176
IN_COLS = 2344
EPS = 1e-6
BLOCKS = [(0, 512), (512, 512), (1024, 512), (1536, 512), (2048, 128)]
NT_IN = [(0, 512), (512, 512), (1024, 512), (1536, 512), (2048, 296)]

_CACHE = {}


def _run(key, builder, in_maps):
    if key not in _CACHE:
        _CACHE[key] = builder()
    nc = _CACHE[key]
    res = run_bass_kernel_spmd(nc, in_maps, core_ids=list(range(8)))
    return res.results


def build_stage_a():
    nc = bass.Bass("TRN2", target_bir_lowering=False)
    P = Prog(nc)
    xT = P.dram("xT", [128, KT, NTOK], F32, "ExternalInput")
    cvec = P.dram("cvec", [128, KT, 2], F32, "ExternalInput")
    w_ada = P.dram("w_ada", [D, 6 * D], F32, "ExternalInput")
    b_adaT = P.dram("b_adaT", [128, 48], F32, "ExternalInput")
    norm1T = P.dram("norm1T", [128, KT], F32, "ExternalInput")
    w_in = P.dram("w_in", [D, IN_COLS], F32, "ExternalInput")
    p_out = P.dram("p", [NTOK, IN_COLS], F32, "ExternalOutput")
    mod_out = P.dram("modT", [128, 48, 2], F32, "ExternalOutput")

    ones_bf = P.sb("ones_bf", [128, 128], BF16)
    P.op("dve", lambda e: e.memset(ones_bf[:], 1.0), writes=[ones_bf])
    cv = P.sb("cv", [128, KT, 2])
    sc = P.sb("sc", [128, KT, 2])
    P.dma("sp", cv[:], cvec.ap[:, :, :], writes=[cv])
    P.op("act", lambda e: e.activation(out=sc[:], in_=cv[:], func=AF.Silu), reads=[cv], writes=[sc])
    bada = P.sb("bada", [128, 48])
    n1 = P.sb("n1", [128, KT])
    P.dma("sp", bada[:], b_adaT.ap[:, :], writes=[bada])
    P.dma("sp", n1[:], norm1T.ap[:, :], writes=[n1])

    win = [P.sb(f"win{kt}", [128, IN_COLS], BF16) for kt in range(KT)]
    for kt in range(KT):
        P.dma("pool", win[kt][:], w_in.ap[kt * 128:(kt + 1) * 128, :], writes=[win[kt]])

    modps = P.ps("modps", [128, 48, 2])
    wab = [P.sb(f"wab{i}", [128, KT, 512]) for i in range(2)]
    wv = w_ada.ap.rearrange("(kt p) n -> p kt n", p=128)
    for j in range(12):
        wb = wab[j % 2]
        P.dma("sp", wb[:], wv[:, :, j * 512:(j + 1) * 512], writes=[wb])
        for cl in range(4):
            c = j * 4 + cl
            for kt in range(KT):
                P.op("pe", lambda e, wb=wb, cl=cl, kt=kt, c=c: e.matmul(
                    modps[:, c, :], lhsT=wb[:, kt, cl * 128:(cl + 1) * 128], rhs=sc[:, kt, :],
                    start=(kt == 0), stop=(kt == KT - 1)), reads=[wb, sc], writes=[modps], inc=(kt == KT - 1))
    modT = P.sb("modT_sb", [128, 48, 2])
    for r in range(2):
        P.op("dve", lambda e, r=r: e.tensor_tensor(out=modT[:, :, r], in0=modps[:, :, r], in1=bada[:, :], op=ALU.add),
             reads=[modps, bada], writes=[modT])
    P.dma("sp", mod_out.ap[:, :, :], modT[:], reads=[modT], is_output=True)
    G = P.sb("G", [128, KT, 2])
    for r in range(2):
        P.op("dve", lambda e, r=r: e.scalar_tensor_tensor(out=G[:, :, r], in0=modT[:, 8:16, r], scalar=1.0, in1=n1[:, :],
                                                          op0=ALU.add, op1=ALU.mult), reads=[modT, n1], writes=[G])

    xb = [P.sb(f"xb{i}", [128, KT, 512]) for i in range(2)]
    sq = P.sb("sq", [128, KT, 512], BF16)
    ss_ps = P.ps("ss_ps", [128, 512])
    rs = P.sb("rs", [128, 512])
    rstd = P.sb("rstd", [128, 512])
    tmp = P.sb("tmp", [128, KT, 512])
    xn = [P.sb(f"xn{i}", [128, KT, 512], BF16) for i in range(2)]
    pp = [P.ps(f"pp{i}", [128, 512]) for i in range(3)]
    psb = [P.sb(f"psb{i}", [128, IN_COLS]) for i in range(2)]
    ppi = 0
    sti = 0
    for bi, (t0, T) in enumerate(BLOCKS):
        r = 1 if bi == 4 else 0
        x = xb[bi % 2]
        xnb = xn[bi % 2]
        P.dma("sp", x[:, :, 0:T], xT.ap[:, :, t0:t0 + T], writes=[x])
        P.op("act", lambda e, x=x, T=T: e.activation(out=sq[:, :, 0:T], in_=x[:, :, 0:T], func=AF.Square), reads=[x], writes=[sq])
        for kt in range(KT):
            P.op("pe", lambda e, kt=kt, T=T: e.matmul(ss_ps[:, 0:T], lhsT=ones_bf[:, :], rhs=sq[:, kt, 0:T],
                                                      start=(kt == 0), stop=(kt == KT - 1)),
                 reads=[ones_bf, sq], writes=[ss_ps], inc=(kt == KT - 1))
        P.op("act", lambda e, T=T: e.activation(out=rs[:, 0:T], in_=ss_ps[:, 0:T], func=AF.Sqrt, scale=1.0 / D, bias=EPS),
             reads=[ss_ps], writes=[rs])
        P.op("dve", lambda e, T=T: e.reciprocal(out=rstd[:, 0:T], in_=rs[:, 0:T]), reads=[rs], writes=[rstd])
        for kt in range(KT):
            P.op("dve", lambda e, x=x, kt=kt, T=T: e.tensor_tensor(out=tmp[:, kt, 0:T], in0=x[:, kt, 0:T], in1=rstd[:, 0:T], op=ALU.mult),
                 reads=[x, rstd], writes=[tmp])
        for kt in range(KT):
            P.op("act", lambda e, xnb=xnb, kt=kt, T=T, r=r: e.activation(
                out=xnb[:, kt, 0:T], in_=tmp[:, kt, 0:T], func=AF.Identity, scale=G[:, kt, r:r + 1], bias=modT[:, kt, r:r + 1]),
                reads=[tmp, G, modT], writes=[xnb])
        for s in range(T // 128):
            pb = psb[sti % 2]
            sti += 1
            for ni, (n0, nw) in enumerate(NT_IN):
                ps = pp[ppi % 3]
                ppi += 1
                for kt in range(KT):
                    P.op("pe", lambda e, ps=ps, xnb=xnb, kt=kt, s=s, n0=n0, nw=nw: e.matmul(
                        ps[:, 0:nw], lhsT=xnb[:, kt, s * 128:(s + 1) * 128], rhs=win[kt][:, n0:n0 + nw],
                        start=(kt == 0), stop=(kt == KT - 1)), reads=[xnb, win[kt]], writes=[ps], inc=(kt == KT - 1))
                if ni % 2 == 0:
                    P.op("act", lambda e, ps=ps, pb=pb, n0=n0, nw=nw: e.activation(out=pb[:, n0:n0 + nw], in_=ps[:, 0:nw], func=AF.Copy),
                         reads=[ps], writes=[pb])
                else:
                    P.op("dve", lambda e, ps=ps, pb=pb, n0=n0, nw=nw: e.tensor_copy(out=pb[:, n0:n0 + nw], in_=ps[:, 0:nw]),
                         reads=[ps], writes=[pb])
            tok = t0 + s * 128
            P.dma("sp", p_out.ap[tok:tok + 128, :], pb[:], reads=[pb], is_output=True)
    P.emit()
    return nc


def tok_to_feat(x_tok):
    T = x_tok.shape[0]
    return np.ascontiguousarray(x_tok.reshape(T, KT, 128).transpose(2, 1, 0))


def vecT(v, n=KT):
    return np.ascontiguousarray(v.reshape(n, 128).T)


def core_tokens(x_lat_b, x_ctx_b, q):
    pad = np.zeros((64, x_lat_b.shape[1]), x_lat_b.dtype)
    return np.concatenate([x_lat_b[q * 2048:(q + 1) * 2048], x_ctx_b[q * 64:(q + 1) * 64], pad], axis=0)


def stage_a_inputs(xT_cores, c, c_ctx, w_ada_i, b_ada_i, norm1_i, w_in_i):
    maps = []
    for core in range(8):
        b = core // 4
        cvec = np.stack([vecT(c[b]), vecT(c_ctx)], axis=-1)
        maps.append({"xT": xT_cores[core], "cvec": np.ascontiguousarray(cvec), "w_ada": w_ada_i,
                     "b_adaT": vecT(b_ada_i, 48), "norm1T": vecT(norm1_i), "w_in": w_in_i})
    return maps


TS = 8448
NTILE = 66
SBLK = [(0, 256)] + [(256 + 512 * i, 512) for i in range(16)]


def bview(P, ap, name):
    b = Buf(ap, name)
    return b


def build_stage_attn():
    nc = bass.Bass("TRN2", target_bir_lowering=False)
    P = Prog(nc)
    dr = {}
    for nm, shp in [("mq", [96, TS]), ("mckv", [128, TS]), ("mkr", [32, TS]), ("dq", [64, TS]), ("dk", [64, TS]),
                    ("dv", [128, NTILE, 64]), ("c96", [96, TS]), ("s96", [96, TS]), ("c64", [64, TS]), ("s64", [64, TS]),
                    ("ones96", [96, 96]), ("ones128", [128, 128]), ("bones64", [64, 64]), ("perm96", [96, 96]), ("perm64", [64, 64]),
                    ("wuk", [128, 64]), ("wuv", [128, 64]),
                    ("gq", [96, 1]), ("gk", [96, 1]), ("gkv", [128, 1]), ("gdq", [64, 1]), ("gdk", [64, 1]),
                    ("lamv", [128, 4, 32]), ("laminit", [128, 2]), ("gsub", [128, 64])]:
        dr[nm] = P.dram(nm, shp, F32, "ExternalInput")
    o_m = P.dram("o_m", [128, NTILE, 64], F32, "ExternalOutput")
    o_d = P.dram("o_d", [128, NTILE, 64], F32, "ExternalOutput")

    banks = [P.ps(f"bank{i}", [128, 512]) for i in range(8)]

    def cload(nm, shp, dt=BF16):
        t = P.sb(nm + "_sb", shp, dt)
        P.dma("pool", t[:], dr[nm].ap, writes=[t])
        return t
    ones96 = cload("ones96", [96, 96]); ones128 = cload("ones128", [128, 128]); bones64 = cload("bones64", [64, 64])
    perm96 = cload("perm96", [96, 96]); perm64 = cload("perm64", [64, 64])
    wuk = cload("wuk", [128, 64]); wuv = cload("wuv", [128, 64])
    gq = cload("gq", [96, 1], F32); gk = cload("gk", [96, 1], F32); gkv = cload("gkv", [128, 1], F32)
    gdq = cload("gdq", [64, 1], F32); gdk = cload("gdk", [64, 1], F32)
    lamv = cload("lamv", [128, 4, 32], F32); laminit = cload("laminit", [128, 2], F32); gsub = cload("gsub", [128, 64], F32)

    QTm = P.sb("QTm", [96, TS], BF16); KTm = P.sb("KTm", [96, TS], BF16); Vm = P.sb("Vm", [128, NTILE, 65], BF16)
    Q1p = P.sb("Q1p", [96, TS], BF16); Q2p = P.sb("Q2p", [96, TS], BF16); KTd = P.sb("KTd", [96, TS], BF16); Vd = P.sb("Vd", [128, NTILE, 65], BF16)
    QTd = (Q1p, Q2p)
    P.op("pool", lambda e: e.memset(Q1p[:], 0.0), writes=[Q1p])
    P.op("pool", lambda e: e.memset(Q2p[:], 0.0), writes=[Q2p])
    P.op("pool", lambda e: e.memset(KTd[:], 0.0), writes=[KTd])
    P.op("pool", lambda e: e.memset(Vm[:], 1.0), writes=[Vm])
    P.op("pool", lambda e: e.memset(Vd[:], 1.0), writes=[Vd])

    lprod = P.sb("lprod", [128, 2, 32]); lsum = P.sb("lsum", [128, 2]); lexp = P.sb("lexp", [128, 2]); lam = P.sb("lam", [128, 1]); nlam = P.sb("nlam", [128, 1])
    P.op("dve", lambda e: e.tensor_tensor(out=lprod[:, 0, :], in0=lamv[:, 0, :], in1=lamv[:, 1, :], op=ALU.mult), reads=[lamv], writes=[lprod])
    P.op("dve", lambda e: e.tensor_tensor(out=lprod[:, 1, :], in0=lamv[:, 2, :], in1=lamv[:, 3, :], op=ALU.mult), reads=[lamv], writes=[lprod])
    P.op("dve", lambda e: e.tensor_reduce(out=lsum[:, :], in_=lprod[:, :, :], axis=AX.X, op=ALU.add), reads=[lprod], writes=[lsum])
    P.op("act", lambda e: e.activation(out=lexp[:], in_=lsum[:], func=AF.Exp), reads=[lsum], writes=[lexp])
    P.op("dve", lambda e: e.tensor_tensor(out=lam[:], in0=lexp[:, 0:1], in1=lexp[:, 1:2], op=ALU.subtract), reads=[lexp], writes=[lam])
    P.op("dve", lambda e: e.tensor_tensor(out=lam[:], in0=lam[:], in1=laminit[:, 0:1], op=ALU.add), reads=[lam, laminit], writes=[lam])
    P.op("dve", lambda e: e.tensor_scalar(out=nlam[:], in0=lam[:], scalar1=-1.0, scalar2=None, op0=ALU.mult), reads=[lam], writes=[nlam])

    ss_ps = bview(P, banks[0][:, :], "ss_ps"); sw_ps = bview(P, banks[1][:, :], "sw_ps")
    kn_ps = bview(P, banks[2][:, :], "kn_ps"); v_ps = bview(P, banks[3][:, :], "v_ps")
    ss2_ps = bview(P, banks[4][:, :], "ss2_ps"); sw2_ps = bview(P, banks[5][:, :], "sw2_ps")
    src = [P.sb(f"src{i}", [128, 512]) for i in range(2)]
    sq_l = [P.sb(f"sq_a{i}", [128, 512], BF16) for i in range(2)]; rs_l = [P.sb(f"rs_a{i}", [128, 512]) for i in range(2)]
    rstd_l = [P.sb(f"rstd_a{i}", [128, 512]) for i in range(2)]
    xnrm_l = [P.sb(f"xnrm{i}", [128, 512]) for i in range(2)]; xg_l = [P.sb(f"xg{i}", [128, 512], BF16) for i in range(2)]
    cb_l = [P.sb(f"cb{i}", [128, 512]) for i in range(2)]; sbk_l = [P.sb(f"sbk{i}", [128, 512]) for i in range(2)]
    t1_l = [P.sb(f"t1{i}", [128, 512]) for i in range(2)]; t2_l = [P.sb(f"t2{i}", [128, 512]) for i in range(2)]
    ncall = [0]
    ckvn_l = [P.sb(f"ckvn{i}", [128, 512], BF16) for i in range(2)]; kfull_l = [P.sb(f"kfull{i}", [96, 512]) for i in range(2)]
    dvs = P.sb("dvs", [128, NTILE, 64])
    P.dma("sp", dvs[:], dr["dv"].ap, writes=[dvs])
    P.op("dve", lambda e: e.tensor_copy(out=Vd[:, :, 0:64], in_=dvs[:]), reads=[dvs], writes=[Vd])
    cnt = [0]

    def norm_rope(srcb, R, T, t0, ones_m, ngrp, gain, perm, cdr, sdr, dst, ssb, swb, rope=True):
        zz = ncall[0] % 2; ncall[0] += 1
        sq, rs, rstd, xnrm, xg, cb, sbk, t1, t2 = sq_l[zz], rs_l[zz], rstd_l[zz], xnrm_l[zz], xg_l[zz], cb_l[zz], sbk_l[zz], t1_l[zz], t2_l[zz]
        P.op("act", lambda e: e.activation(out=sq[0:R, 0:T], in_=srcb[0:R, 0:T], func=AF.Square), reads=[srcb], writes=[sq])
        P.op("pe", lambda e: e.matmul(ssb[0:R, 0:T], lhsT=ones_m[:, :], rhs=sq[0:R, 0:T], start=True, stop=True), reads=[ones_m, sq], writes=[ssb])
        P.op("act", lambda e: e.activation(out=rs[0:R, 0:T], in_=ssb[0:R, 0:T], func=AF.Sqrt, scale=1.0 / ngrp, bias=EPS), reads=[ssb], writes=[rs])
        P.op("dve", lambda e: e.reciprocal(out=rstd[0:R, 0:T], in_=rs[0:R, 0:T]), reads=[rs], writes=[rstd])
        P.op("dve", lambda e: e.tensor_tensor(out=xnrm[0:R, 0:T], in0=srcb[0:R, 0:T], in1=rstd[0:R, 0:T], op=ALU.mult), reads=[srcb, rstd], writes=[xnrm])
        if not rope:
            P.op("act", lambda e: e.activation(out=dst[0:R, t0:t0 + T], in_=xnrm[0:R, 0:T], func=AF.Identity, scale=gain[:, 0:1]), reads=[xnrm, gain], writes=[dst])
            return
        P.op("act", lambda e: e.activation(out=xg[0:R, 0:T], in_=xnrm[0:R, 0:T], func=AF.Identity, scale=gain[:, 0:1]), reads=[xnrm, gain], writes=[xg])
        P.op("pe", lambda e: e.matmul(swb[0:R, 0:T], lhsT=perm[:, :], rhs=xg[0:R, 0:T], start=True, stop=True), reads=[perm, xg], writes=[swb])
        P.dma("sp", cb[0:R, 0:T], cdr.ap[:, t0:t0 + T], writes=[cb])
        P.dma("sp", sbk[0:R, 0:T], sdr.ap[:, t0:t0 + T], writes=[sbk])
        P.op("dve", lambda e: e.tensor_tensor(out=t1[0:R, 0:T], in0=xg[0:R, 0:T], in1=cb[0:R, 0:T], op=ALU.mult), reads=[xg, cb], writes=[t1])
        P.op("dve", lambda e: e.tensor_tensor(out=t2[0:R, 0:T], in0=swb[0:R, 0:T], in1=sbk[0:R, 0:T], op=ALU.mult), reads=[swb, sbk], writes=[t2])
        if isinstance(dst, tuple):
            for (dd, pa, pb) in ((dst[0], 0, 32), (dst[1], 32, 64)):
                P.op("pool", lambda e, dd=dd, pa=pa, pb=pb: e.tensor_tensor(out=dd[pa:pb, t0:t0 + T], in0=t1[pa:pb, 0:T], in1=t2[pa:pb, 0:T], op=ALU.add),
                     reads=[t1, t2], writes=[dd])
        else:
            P.op("pool", lambda e: e.tensor_tensor(out=dst[0:R, t0:t0 + T], in0=t1[0:R, 0:T], in1=t2[0:R, 0:T], op=ALU.add), reads=[t1, t2], writes=[dst])

    for bi_, (t0, T) in enumerate(SBLK):
        ckvn = ckvn_l[bi_ % 2]; kfull = kfull_l[bi_ % 2]
        s0 = src[cnt[0] % 2]; cnt[0] += 1
        P.dma("sp", s0[0:96, 0:T], dr["mq"].ap[:, t0:t0 + T], writes=[s0])
        norm_rope(s0, 96, T, t0, ones96, 96, gq, perm96, dr["c96"], dr["s96"], QTm, ss_ps, sw_ps)
        s1 = src[cnt[0] % 2]; cnt[0] += 1
        P.dma("sp", s1[0:128, 0:T], dr["mckv"].ap[:, t0:t0 + T], writes=[s1])
        norm_rope(s1, 128, T, 0, ones128, 128, gkv, None, None, None, ckvn, ss2_ps, None, rope=False)
        P.op("pe", lambda e, T=T, ckvn=ckvn: e.matmul(kn_ps[0:64, 0:T], lhsT=wuk[:, :], rhs=ckvn[:, 0:T], start=True, stop=True), reads=[wuk, ckvn], writes=[kn_ps])
        P.op("act", lambda e, T=T, kfull=kfull: e.activation(out=kfull[0:64, 0:T], in_=kn_ps[0:64, 0:T], func=AF.Copy), reads=[kn_ps], writes=[kfull])
        P.dma("sp", kfull[64:96, 0:T], dr["mkr"].ap[:, t0:t0 + T], writes=[kfull])
        for s in range(T // 128):
            tile = t0 // 128 + s
            P.op("pe", lambda e, s=s, ckvn=ckvn: e.matmul(v_ps[:, s * 64:(s + 1) * 64], lhsT=ckvn[:, s * 128:(s + 1) * 128], rhs=wuv[:, :], start=True, stop=True),
                 reads=[ckvn, wuv], writes=[v_ps])
            P.op("dve", lambda e, s=s, tile=tile: e.tensor_copy(out=Vm[:, tile, 0:64], in_=v_ps[:, s * 64:(s + 1) * 64]), reads=[v_ps], writes=[Vm])
        norm_rope(kfull, 96, T, t0, ones96, 96, gk, perm96, dr["c96"], dr["s96"], KTm, ss_ps, sw_ps)
        s2 = src[cnt[0] % 2]; cnt[0] += 1
        P.dma("sp", s2[0:64, 0:T], dr["dq"].ap[:, t0:t0 + T], writes=[s2])
        norm_rope(s2, 64, T, t0, bones64, 32, gdq, perm64, dr["c64"], dr["s64"], QTd, ss2_ps, sw2_ps)
        s3 = src[cnt[0] % 2]; cnt[0] += 1
        P.dma("sp", s3[0:64, 0:T], dr["dk"].ap[:, t0:t0 + T], writes=[s3])
        norm_rope(s3, 64, T, t0, bones64, 32, gdk, perm64, dr["c64"], dr["s64"], KTd, ss_ps, sw_ps)
    P.barrier()

    def attention(kind):
        if kind == "m":
            sps = [bview(P, banks[i][:, :], f"sps_m{i}") for i in range(3)]
            po = [bview(P, banks[3][:, 0:260].rearrange("p (s c) -> p s c", c=65), "po_m")]
            QT, KT_, V, dk, scale, outd = [QTm], [KTm], Vm, 96, 96 ** -0.5, o_m
            QTs = [(QTm, 0, 96)]
        else:
            sps = [bview(P, banks[i][:, :], f"sps_d{i}") for i in range(4)]
            po = [bview(P, banks[4][:, 0:260].rearrange("p (s c) -> p s c", c=65), "po_d1"),
                  bview(P, banks[5][:, 0:260].rearrange("p (s c) -> p s c", c=65), "po_d2")]
            V, scale, outd = Vd, 32 ** -0.5, o_d
            QTs = [(Q1p, 0, 96), (Q2p, 0, 96)]
        KTs = [KTm] if kind == "m" else [KTd, KTd]
        npt = len(QTs)
        pts = [P.sb(f"pt_{kind}{i}", [128, 512], BF16) for i in range(2 * npt)]
        ob = [P.sb(f"ob_{kind}{i}", [128, 4, 64]) for i in range(2)]
        rec = P.sb(f"rec_{kind}", [128, 2, 4]); a_sb = P.sb(f"a_{kind}", [128, 4, 64])
        ssq = P.sb(f"ssq_{kind}", [128, 4]); junk = P.sb(f"junk_{kind}", [128, 64]); rr = P.sb(f"rr_{kind}", [128, 4])
        items = []
        for qb, (q0, Tq) in enumerate(SBLK):
            nk = 2 if qb == 0 else NTILE
            for kt in range(nk):
                for j in range(npt):
                    items.append((qb, q0, Tq, nk, kt, j))
        NI = len(items)
        LOOK = 2

        def emit_qk(n):
            qb, q0, Tq, nk, kt, j = items[n]
            QTb, r0, r1 = QTs[j]
            KTb = KTs[j]
            sp_ = sps[n % len(sps)]
            P.op("pe", lambda e, sp_=sp_, KTb=KTb, QTb=QTb, r0=r0, r1=r1, kt=kt, q0=q0, Tq=Tq: e.matmul(
                sp_[:, 0:Tq], lhsT=KTb[r0:r1, kt * 128:(kt + 1) * 128], rhs=QTb[r0:r1, q0:q0 + Tq], start=True, stop=True),
                reads=[KTb, QTb], writes=[sp_])

        def emit_exp(n):
            qb, q0, Tq, nk, kt, j = items[n]
            sp_ = sps[n % len(sps)]; pt = pts[n % len(pts)]
            P.op("act", lambda e, sp_=sp_, pt=pt, Tq=Tq: e.activation(out=pt[:, 0:Tq], in_=sp_[:, 0:Tq], func=AF.Exp, scale=scale),
                 reads=[sp_], writes=[pt])

        def emit_pv(n):
            qb, q0, Tq, nk, kt, j = items[n]
            pt = pts[n % len(pts)]
            nsub = Tq // 128
            for s in range(nsub):
                P.op("pe", lambda e, pt=pt, j=j, s=s, kt=kt, nk=nk: e.matmul(
                    po[j][:, s, :], lhsT=pt[:, s * 128:(s + 1) * 128], rhs=V[:, kt, :], start=(kt == 0 and s == 0), stop=(kt == nk - 1), skip_group_check=True),
                    reads=[pt, V], writes=[po[j]], inc=(s == nsub - 1))

        for n in range(min(LOOK, NI)):
            emit_qk(n)
        for n in range(NI):
            qb, q0, Tq, nk, kt, j = items[n]
            nsub = Tq // 128
            emit_exp(n)
            if n + LOOK < NI:
                emit_qk(n + LOOK)
            emit_pv(n)
            if not (kt == nk - 1 and j == npt - 1):
                continue
            o = ob[qb % 2]
            P.op("dve", lambda e: e.memset(ssq[:], 0.0), writes=[ssq])
            for j in range(npt):
                P.op("dve", lambda e, j=j, nsub=nsub: e.reciprocal(out=rec[:, j, 0:nsub], in_=po[j][:, 0:nsub, 64]), reads=[po[j]], writes=[rec])
            if kind == "m":
                for s in range(nsub):
                    P.op("dve", lambda e, s=s, o=o: e.tensor_scalar(out=o[:, s, :], in0=po[0][:, s, 0:64], scalar1=rec[:, 0, s:s + 1], scalar2=None, op0=ALU.mult),
                         reads=[po[0], rec], writes=[o])
            else:
                P.op("dve", lambda e, nsub=nsub: e.tensor_scalar(out=rec[:, 1, 0:nsub], in0=rec[:, 1, 0:nsub], scalar1=nlam[:, 0:1], scalar2=None, op0=ALU.mult),
                     reads=[rec, nlam], writes=[rec])
                for s in range(nsub):
                    P.op("dve", lambda e, s=s: e.tensor_scalar(out=a_sb[:, s, :], in0=po[0][:, s, 0:64], scalar1=rec[:, 0, s:s + 1], scalar2=None, op0=ALU.mult),
                         reads=[po[0], rec], writes=[a_sb])
                    P.op("dve", lambda e, s=s: e.scalar_tensor_tensor(out=a_sb[:, s, :], in0=po[1][:, s, 0:64], scalar=rec[:, 1, s:s + 1], in1=a_sb[:, s, :],
                                                                      op0=ALU.mult, op1=ALU.add), reads=[po[1], rec, a_sb], writes=[a_sb])
                    P.op("act", lambda e, s=s: e.activation(out=junk[:, :], in_=a_sb[:, s, :], func=AF.Square, accum_out=ssq[:, s:s + 1]),
                         reads=[a_sb], writes=[junk, ssq])
                P.op("act", lambda e, nsub=nsub: e.activation(out=rr[:, 0:nsub], in_=ssq[:, 0:nsub], func=AF.Sqrt, scale=1.0 / 64, bias=EPS), reads=[ssq], writes=[rr])
                P.op("dve", lambda e, nsub=nsub: e.reciprocal(out=rr[:, 0:nsub], in_=rr[:, 0:nsub]), reads=[rr], writes=[rr])
                P.op("dve", lambda e, nsub=nsub: e.tensor_scalar(out=rr[:, 0:nsub], in0=rr[:, 0:nsub], scalar1=laminit[:, 1:2], scalar2=None, op0=ALU.mult),
                     reads=[rr, laminit], writes=[rr])
                for s in range(nsub):
                    P.op("dve", lambda e, s=s, o=o: e.scalar_tensor_tensor(out=o[:, s, :], in0=a_sb[:, s, :], scalar=rr[:, s:s + 1], in1=gsub[:, :],
                                                                           op0=ALU.mult, op1=ALU.mult), reads=[a_sb, rr, gsub], writes=[o])
            tl = q0 // 128
            P.dma("sp", outd.ap[:, tl:tl + nsub, :], o[:, 0:nsub, :], reads=[o], is_output=True)

    attention("m")
    P.barrier()
    attention("d")
    P.emit()
    return nc


def rope_tables():
    inv = (10000.0 ** (-np.arange(8, dtype=np.float32) / 8.0)).astype(np.float32)
    t = np.arange(8192)
    rows = (t // 64).astype(np.float32); cols = (t % 64).astype(np.float32)
    ang = np.zeros((32, TS), np.float32)
    for a in range(16):
        ang[a, 256:] = rows * inv[a % 8]
        ang[16 + a, 256:] = cols * inv[a % 8]
    C = np.cos(ang).astype(np.float32); S = np.sin(ang).astype(np.float32)
    C[:, :256] = 1.0; S[:, :256] = 0.0
    return C, S


def perm_matrix(n, offsets):
    Pm = np.zeros((n, n), np.float32)
    for o in offsets:
        for a in range(8):
            Pm[o + a + 8, o + a] = -1.0
            Pm[o + a, o + a + 8] = 1.0
    return Pm


_CONST = {}


def attn_consts():
    if "attn" not in _CONST:
        C, S = rope_tables()
        c96 = np.concatenate([np.ones((64, TS), np.float32), C], 0); s96 = np.concatenate([np.zeros((64, TS), np.float32), S], 0)
        c64 = np.concatenate([C, C], 0); s64 = np.concatenate([S, S], 0)
        b64 = np.zeros((64, 64), np.float32); b64[:32, :32] = 1; b64[32:, 32:] = 1
        _CONST["attn"] = dict(c96=c96, s96=s96, c64=c64, s64=s64, ones96=np.ones((96, 96), np.float32), ones128=np.ones((128, 128), np.float32),
                              bones64=b64, perm96=perm_matrix(96, [64, 80]), perm64=perm_matrix(64, [0, 16, 32, 48]))
    return _CONST["attn"]


def rep(v, n=128):
    return np.ascontiguousarray(np.broadcast_to(np.asarray(v, np.float32).reshape(1, -1), (n, np.asarray(v).size)))


def col(v):
    return np.ascontiguousarray(np.asarray(v, np.float32).reshape(-1, 1))


def stage_attn_inputs(PS, inp, i):
    cst = attn_consts()
    lam_init = 0.8 - 0.6 * math.exp(-0.3 * i)
    maps = []
    for core in range(8):
        b, h = core // 4, core % 4
        p = PS[b]
        dof = 1576
        m = dict(cst)
        m["mq"] = np.ascontiguousarray(p[:, h * 96:(h + 1) * 96].T)
        m["mckv"] = np.ascontiguousarray(p[:, 384:512].T)
        m["mkr"] = np.ascontiguousarray(p[:, 512:544].T)
        m["dq"] = np.ascontiguousarray(p[:, dof + h * 64:dof + (h + 1) * 64].T)
        m["dk"] = np.ascontiguousarray(p[:, dof + 256 + h * 64:dof + 256 + (h + 1) * 64].T)
        m["dv"] = np.ascontiguousarray(p[:, dof + 512 + h * 64:dof + 512 + (h + 1) * 64].reshape(NTILE, 128, 64).transpose(1, 0, 2))
        m["wuk"] = np.ascontiguousarray(inp["mla_w_uk"][i][:, h * 64:(h + 1) * 64])
        m["wuv"] = np.ascontiguousarray(inp["mla_w_uv"][i][:, h * 64:(h + 1) * 64])
        m["gq"] = col(inp["mla_q_norm"][i]); m["gk"] = col(inp["mla_k_norm"][i]); m["gkv"] = col(inp["mla_kv_norm"][i])
        m["gdq"] = col(np.tile(inp["diff_q_norm"][i], 2)); m["gdk"] = col(np.tile(inp["diff_k_norm"][i], 2))
        lv = np.stack([inp["diff_lq1"][i], inp["diff_lk1"][i], inp["diff_lq2"][i], inp["diff_lk2"][i]], 0)
        m["lamv"] = np.ascontiguousarray(np.broadcast_to(lv[None], (128, 4, 32))).astype(np.float32)
        m["laminit"] = rep(np.array([lam_init, 1.0 - lam_init], np.float32))
        m["gsub"] = rep(inp["diff_subln"][i])
        maps.append(m)
    return maps


def untile(o):
    return np.ascontiguousarray(o.transpose(1, 0, 2).reshape(TS, o.shape[2]))


TPAD = TS + 3
ORDER_F = list(range(NTILE))
ORDER_B = [1, 0] + list(range(NTILE - 1, 1, -1))
NEG = -30000.0


def pcol(s):
    return 1 + s if s < 256 else 2 + s


def build_stage_ssd():
    nc = bass.Bass("TRN2", target_bir_lowering=False)
    P = Prog(nc)
    dr = {}
    for nm, shp in [("xp", [64, TPAD]), ("bp", [64, TPAD]), ("cp", [64, TPAD]), ("cw", [64, 3, 3]), ("cbias", [64, 3]),
                    ("z", [128, NTILE, 64]), ("dtr", [128, NTILE, 2]), ("dtb", [128, 2]), ("alog", [128, 2]), ("dsk", [128, 1]),
                    ("triu", [128, 128]), ("tril", [128, 128]), ("ntriu", [128, 128]), ("ntril", [128, 128]),
                    ("mneg_f", [128, 128]), ("mneg_b", [128, 128]), ("ident", [128, 128]), ("ones", [128, 128]),
                    ("slo", [128, 128]), ("sup", [128, 128])]:
        dr[nm] = P.dram(nm, shp, F32, "ExternalInput")
    o_s = P.dram("o_s", [128, NTILE, 64], F32, "ExternalOutput")
    banks = [P.ps(f"bank{i}", [128, 512]) for i in range(8)]

    def cload(nm, shp, dt=F32):
        t = P.sb(nm + "_sb", shp, dt)
        P.dma("pool", t[:], dr[nm].ap, writes=[t])
        return t
    cw = cload("cw", [64, 3, 3]); cbias = cload("cbias", [64, 3])
    dtr = cload("dtr", [128, NTILE, 2]); dtb = cload("dtb", [128, 2]); alog = cload("alog", [128, 2]); dsk = cload("dsk", [128, 1])
    triu = cload("triu", [128, 128]); tril = cload("tril", [128, 128]); ntriu = cload("ntriu", [128, 128]); ntril = cload("ntril", [128, 128])
    mneg_f = cload("mneg_f", [128, 128]); mneg_b = cload("mneg_b", [128, 128]); ident = cload("ident", [128, 128]); ones = cload("ones", [128, 128])
    slo = cload("slo", [128, 128]); sup = cload("sup", [128, 128])
    zs = cload("z", [128, NTILE, 64])
    P.op("act", lambda e: e.activation(out=zs[:], in_=zs[:], func=AF.Silu), reads=[zs], writes=[zs])

    BT = P.sb("BT", [64, TS], BF16); CT = P.sb("CT", [64, TS], BF16)
    x_tok = P.sb("x_tok", [128, NTILE, 64]); B_tok = P.sb("B_tok", [128, NTILE, 64]); y_acc = P.sb("y_acc", [128, NTILE, 64])
    dt = P.sb("dt", [128, NTILE, 2]); dta = P.sb("dta", [128, NTILE, 2]); aneg = P.sb("aneg", [128, 2])
    spx = P.sb("spx", [128, NTILE, 2]); spu = P.sb("spu", [128, NTILE, 2]); spw = P.sb("spw", [128, NTILE, 2])
    spw2 = P.sb("spw2", [128, NTILE, 2]); spq = P.sb("spq", [128, NTILE, 2])
    for d in range(2):
        P.op("dve", lambda e, d=d: e.tensor_scalar(out=spx[:, :, d], in0=dtr[:, :, d], scalar1=dtb[:, d:d + 1], scalar2=None, op0=ALU.add), reads=[dtr, dtb], writes=[spx])
    P.op("act", lambda e: e.activation(out=spu[:], in_=spx[:], func=AF.Abs), reads=[spx], writes=[spu])
    P.op("act", lambda e: e.activation(out=spu[:], in_=spu[:], func=AF.Exp, scale=-1.0), reads=[spu], writes=[spu])
    P.op("dve", lambda e: e.tensor_scalar(out=spw[:], in0=spu[:], scalar1=2.0, scalar2=None, op0=ALU.add), reads=[spu], writes=[spw])
    P.op("dve", lambda e: e.reciprocal(out=spw[:], in_=spw[:]), reads=[spw], writes=[spw])
    P.op("dve", lambda e: e.tensor_tensor(out=spw[:], in0=spw[:], in1=spu[:], op=ALU.mult), reads=[spw, spu], writes=[spw])
    P.op("dve", lambda e: e.tensor_tensor(out=spw2[:], in0=spw[:], in1=spw[:], op=ALU.mult), reads=[spw], writes=[spw2])
    P.op("dve", lambda e: e.tensor_scalar(out=spq[:], in0=spw2[:], scalar1=1.0 / 13.0, scalar2=None, op0=ALU.mult), reads=[spw2], writes=[spq])
    for cst_ in (1.0 / 11.0, 1.0 / 9.0, 1.0 / 7.0, 1.0 / 5.0, 1.0 / 3.0):
        P.op("dve", lambda e, cst_=cst_: e.scalar_tensor_tensor(out=spq[:], in0=spq[:], scalar=cst_, in1=spw2[:], op0=ALU.add, op1=ALU.mult), reads=[spq, spw2], writes=[spq])
    P.op("dve", lambda e: e.scalar_tensor_tensor(out=spq[:], in0=spq[:], scalar=1.0, in1=spw[:], op0=ALU.add, op1=ALU.mult), reads=[spq, spw], writes=[spq])
    P.op("dve", lambda e: e.tensor_scalar(out=spx[:], in0=spx[:], scalar1=0.0, scalar2=None, op0=ALU.max), reads=[spx], writes=[spx])
    P.op("dve", lambda e: e.scalar_tensor_tensor(out=dt[:], in0=spq[:], scalar=2.0, in1=spx[:], op0=ALU.mult, op1=ALU.add), reads=[spq, spx], writes=[dt])
    P.op("act", lambda e: e.activation(out=aneg[:], in_=alog[:], func=AF.Exp), reads=[alog], writes=[aneg])
    P.op("dve", lambda e: e.tensor_scalar(out=aneg[:], in0=aneg[:], scalar1=-1.0, scalar2=None, op0=ALU.mult), reads=[aneg], writes=[aneg])
    for d in range(2):
        P.op("dve", lambda e, d=d: e.tensor_scalar(out=dta[:, :, d], in0=dt[:, :, d], scalar1=aneg[:, d:d + 1], scalar2=None, op0=ALU.mult),
             reads=[dt, aneg], writes=[dta])

    tp_ps = [bview(P, banks[i][:, :], f"tp_ps{i}") for i in range(2)]
    xin = [P.sb(f"xin{i}", [64, 514]) for i in range(2)]
    acc = P.sb("cacc", [64, 512]); xc = P.sb("xc", [64, 512])
    segs = [(0, 256)] + [(256 + 512 * k, 512) for k in range(16)]
    ci = 0
    for (s0, T) in segs:
        c0 = pcol(s0) - 1
        for wi, (nm, dstT) in enumerate([("xp", None), ("bp", BT), ("cp", CT)]):
            xi = xin[ci % 2]; ci += 1
            P.dma("sp", xi[:, 0:T + 2], dr[nm].ap[:, c0:c0 + T + 2], writes=[xi])
            P.op("act", lambda e, xi=xi, T=T, wi=wi: e.activation(out=acc[:, 0:T], in_=xi[:, 1:T + 1], func=AF.Identity, scale=cw[:, wi, 1:2], bias=cbias[:, wi:wi + 1]),
                 reads=[xi, cw, cbias], writes=[acc])
            P.op("dve", lambda e, xi=xi, T=T, wi=wi: e.scalar_tensor_tensor(out=acc[:, 0:T], in0=xi[:, 0:T], scalar=cw[:, wi, 0:1], in1=acc[:, 0:T], op0=ALU.mult, op1=ALU.add),
                 reads=[xi, cw, acc], writes=[acc])
            P.op("dve", lambda e, xi=xi, T=T, wi=wi: e.scalar_tensor_tensor(out=acc[:, 0:T], in0=xi[:, 2:T + 2], scalar=cw[:, wi, 2:3], in1=acc[:, 0:T], op0=ALU.mult, op1=ALU.add),
                 reads=[xi, cw, acc], writes=[acc])
            P.op("act", lambda e, T=T: e.activation(out=xc[:, 0:T], in_=acc[:, 0:T], func=AF.Silu), reads=[acc], writes=[xc])
            if dstT is not None:
                P.op("pool", lambda e, dstT=dstT, s0=s0, T=T: e.tensor_copy(out=dstT[:, s0:s0 + T], in_=xc[:, 0:T]), reads=[xc], writes=[dstT])
            if nm in ("xp", "bp"):
                dtok = x_tok if nm == "xp" else B_tok
                tp = tp_ps[wi % 2]
                for j in range(T // 128):
                    P.op("pe", lambda e, tp=tp, j=j: e.transpose(tp[:, j * 64:(j + 1) * 64], xc[0:64, j * 128:(j + 1) * 128], ident[0:64, 0:64]),
                         reads=[xc, ident], writes=[tp])
                tl = s0 // 128
                nj = T // 128
                P.op("dve", lambda e, tp=tp, dtok=dtok, tl=tl, nj=nj: e.tensor_copy(out=dtok[:, tl:tl + nj, :], in_=tp[:, 0:nj * 64].rearrange("p (j c) -> p j c", c=64)),
                     reads=[tp], writes=[dtok])
    P.barrier()

    rowbc = [bview(P, banks[i][:, 0:128], f"rowbc{i}") for i in range(2)]
    colp = bview(P, banks[2][:, 0:2], "colp")
    STp = [bview(P, banks[3 + i][:, 0:128], f"STp{i}") for i in range(2)]
    ydp = bview(P, banks[5][:, 0:64], "ydp"); yop = bview(P, banks[6][:, 0:64], "yop"); stp = bview(P, banks[7][0:64, 0:64], "stp")
    dbc = [P.sb(f"dbc{i}", [128, 128]) for i in range(2)]
    cols = [P.sb(f"cols{i}", [128, 2]) for i in range(2)]
    Dm = [P.sb(f"Dm{i}", [128, 128]) for i in range(2)]
    STm = [P.sb(f"STm{i}", [128, 128], BF16) for i in range(2)]
    xs = [P.sb(f"xs{i}", [128, 64], BF16) for i in range(2)]
    ea = [P.sb(f"ea{i}", [128, 1]) for i in range(2)]
    yos = [P.sb(f"yos{i}", [128, 64]) for i in range(2)]
    Bdec = [P.sb(f"Bdec{i}", [128, 64], BF16) for i in range(2)]
    cd = [P.sb(f"cd{i}", [64, 1]) for i in range(2)]
    h = P.sb("h", [64, 64]); hbs = [P.sb(f"hb{i}", [64, 64], BF16) for i in range(2)]
    tfin = P.sb("tfin", [128, 64]); ofin = [P.sb(f"ofin{i}", [128, 64]) for i in range(2)]
    items = [(0, c) for c in ORDER_F] + [(1, c) for c in ORDER_B]
    NI = len(items)

    def bufs(n):
        k = n % 2
        return rowbc[k], STp[k], dbc[k], cols[k], Dm[k], STm[k], xs[k], ea[k], yos[k], Bdec[k], cd[k]

    def S1(n):
        d, c = items[n]
        tri, ntri, mneg, strict = (triu, ntriu, mneg_f, slo) if d == 0 else (tril, ntril, mneg_b, sup)
        rb, stq, db, cl, dm, sm, xsb, eab, yob, bd, cdb = bufs(n)
        ch = slice(c * 128, (c + 1) * 128)
        P.op("dve", lambda e, db=db, c=c, d=d, strict=strict: e.tensor_scalar(out=db[:], in0=strict[:], scalar1=dta[:, c, d:d + 1], scalar2=None, op0=ALU.mult),
             reads=[strict, dta], writes=[db])
        P.op("pe", lambda e, rb=rb, db=db, tri=tri: e.matmul(rb[:, :], lhsT=db[:, :], rhs=tri[:, :], start=True, stop=False), reads=[db, tri], writes=[rb], inc=False)
        P.op("pe", lambda e, rb=rb, mneg=mneg: e.matmul(rb[:, :], lhsT=ident[:, :], rhs=mneg[:, :], start=False, stop=True), reads=[ident, mneg], writes=[rb])
        P.op("pe", lambda e, ntri=ntri, c=c, d=d: e.matmul(colp[:, 0:1], lhsT=ntri[:, :], rhs=dta[:, c, d:d + 1], start=True, stop=True), reads=[ntri, dta], writes=[colp], inc=False)
        P.op("pe", lambda e, c=c, d=d: e.matmul(colp[:, 1:2], lhsT=ones[:, :], rhs=dta[:, c, d:d + 1], start=True, stop=True), reads=[ones, dta], writes=[colp])
        P.op("dve", lambda e, cl=cl: e.tensor_copy(out=cl[:], in_=colp[:, :]), reads=[colp], writes=[cl])
        P.op("act", lambda e, dm=dm, rb=rb: e.activation(out=dm[:], in_=rb[:, :], func=AF.Exp), reads=[rb], writes=[dm])
        P.op("pe", lambda e, stq=stq, ch=ch: e.matmul(stq[:, :], lhsT=BT[:, ch], rhs=CT[:, ch], start=True, stop=True), reads=[BT, CT], writes=[stq])
        P.op("pool", lambda e, xsb=xsb, c=c, d=d: e.tensor_scalar(out=xsb[:], in0=x_tok[:, c, :], scalar1=dt[:, c, d:d + 1], scalar2=None, op0=ALU.mult),
             reads=[x_tok, dt], writes=[xsb])
        P.op("act", lambda e, eab=eab, cl=cl: e.activation(out=eab[:], in_=cl[:, 0:1], func=AF.Exp, scale=-1.0), reads=[cl], writes=[eab])
        P.op("act", lambda e, cdb=cdb, cl=cl: e.activation(out=cdb[:], in_=cl[0:64, 1:2], func=AF.Exp), reads=[cl], writes=[cdb])

    def S2(n):
        d, c = items[n]
        dcol = 127 if d == 0 else 0
        rb, stq, db, cl, dm, sm, xsb, eab, yob, bd, cdb = bufs(n)
        ch = slice(c * 128, (c + 1) * 128)
        if n == 0 or n == NTILE:
            P.op("dve", lambda e: e.memset(h[:], 0.0), writes=[h])
            P.op("dve", lambda e, n=n: e.memset(hbs[(n - 1) % 2][:], 0.0), writes=[hbs[(n - 1) % 2]])
        hprev = hbs[(n - 1) % 2]; hnew = hbs[n % 2]
        P.op("dve", lambda e, sm=sm, stq=stq, dm=dm: e.tensor_tensor(out=sm[:], in0=stq[:, :], in1=dm[:], op=ALU.mult), reads=[stq, dm], writes=[sm])
        P.op("dve", lambda e, bd=bd, c=c, dm=dm, dcol=dcol: e.tensor_scalar(out=bd[:], in0=B_tok[:, c, :], scalar1=dm[:, dcol:dcol + 1], scalar2=None, op0=ALU.mult),
             reads=[B_tok, dm], writes=[bd])
        P.op("pe", lambda e, sm=sm, xsb=xsb: e.matmul(ydp[:, :], lhsT=sm[:, :], rhs=xsb[:, :], start=True, stop=True), reads=[sm, xsb], writes=[ydp])
        P.op("pe", lambda e, bd=bd, xsb=xsb: e.matmul(stp[:, :], lhsT=bd[:, :], rhs=xsb[:, :], start=True, stop=True), reads=[bd, xsb], writes=[stp])
        P.op("pe", lambda e, ch=ch, hprev=hprev: e.matmul(yop[:, :], lhsT=CT[:, ch], rhs=hprev[:, :], start=True, stop=True), reads=[CT, hprev], writes=[yop])
        P.op("dve", lambda e, cdb=cdb: e.scalar_tensor_tensor(out=h[:], in0=h[:], scalar=cdb[:, 0:1], in1=stp[:, :], op0=ALU.mult, op1=ALU.add),
             reads=[h, cdb, stp], writes=[h])
        P.op("act", lambda e, hnew=hnew: e.activation(out=hnew[:], in_=h[:], func=AF.Copy), reads=[h], writes=[hnew])
        P.op("act", lambda e, yob=yob, eab=eab: e.activation(out=yob[:], in_=yop[:, :], func=AF.Identity, scale=eab[:, 0:1]), reads=[yop, eab], writes=[yob])
        if d == 0:
            P.op("dve", lambda e, c=c, yob=yob: e.tensor_tensor(out=y_acc[:, c, :], in0=ydp[:, :], in1=yob[:], op=ALU.add), reads=[ydp, yob], writes=[y_acc])
        else:
            of = ofin[n % 2]
            P.op("dve", lambda e, yob=yob: e.tensor_tensor(out=tfin[:], in0=ydp[:, :], in1=yob[:], op=ALU.add), reads=[ydp, yob], writes=[tfin])
            P.op("dve", lambda e, c=c: e.tensor_tensor(out=tfin[:], in0=tfin[:], in1=y_acc[:, c, :], op=ALU.add), reads=[tfin, y_acc], writes=[tfin])
            P.op("dve", lambda e, c=c: e.scalar_tensor_tensor(out=tfin[:], in0=x_tok[:, c, :], scalar=dsk[:, 0:1], in1=tfin[:], op0=ALU.mult, op1=ALU.add),
                 reads=[x_tok, dsk, tfin], writes=[tfin])
            P.op("dve", lambda e, c=c, of=of: e.tensor_tensor(out=of[:], in0=tfin[:], in1=zs[:, c, :], op=ALU.mult), reads=[tfin, zs], writes=[of])
            P.dma("sp", o_s.ap[:, c, :], of[:], reads=[of], is_output=True)

    S1(0)
    for n in range(NI):
        if n + 1 < NI:
            S1(n + 1)
        S2(n)
    P.emit()
    return nc


def ssd_consts():
    if "ssd" not in _CONST:
        tu = np.triu(np.ones((128, 128), np.float32)); tl = np.tril(np.ones((128, 128), np.float32))
        _CONST["ssd"] = dict(triu=tu, tril=tl, ntriu=-tu, ntril=-tl, mneg_f=NEG * (1 - tu), mneg_b=NEG * (1 - tl),
                             ident=np.eye(128, dtype=np.float32), ones=np.ones((128, 128), np.float32),
                             slo=np.tril(np.ones((128, 128), np.float32), -1), sup=np.triu(np.ones((128, 128), np.float32), 1))
    return _CONST["ssd"]


def pad_stream(aT):
    C = aT.shape[0]
    z = np.zeros((C, 1), aT.dtype)
    return np.ascontiguousarray(np.concatenate([z, aT[:, :256], z, aT[:, 256:], z], axis=1))


def tile_tok(a):
    return np.ascontiguousarray(a.reshape(NTILE, 128, a.shape[1]).transpose(1, 0, 2))


def stage_ssd_inputs(PS, inp, i):
    cst = ssd_consts()
    maps = []
    so = 544
    for core in range(8):
        b, h = core // 4, core % 4
        g = h // 2
        p = PS[b]
        m = dict(cst)
        xcols = slice(so + 256 + h * 64, so + 256 + (h + 1) * 64)
        bcols = slice(so + 512 + g * 64, so + 512 + (g + 1) * 64)
        ccols = slice(so + 640 + g * 64, so + 640 + (g + 1) * 64)
        m["xp"] = pad_stream(p[:, xcols].T); m["bp"] = pad_stream(p[:, bcols].T); m["cp"] = pad_stream(p[:, ccols].T)
        cwf = inp["ssd_conv_w"][i]; cbf = inp["ssd_conv_b"][i]
        chx = slice(h * 64, (h + 1) * 64); chb = slice(256 + g * 64, 256 + (g + 1) * 64); chc = slice(384 + g * 64, 384 + (g + 1) * 64)
        m["cw"] = np.ascontiguousarray(np.stack([cwf[:, chx].T, cwf[:, chb].T, cwf[:, chc].T], axis=1)).astype(np.float32)
        m["cbias"] = np.ascontiguousarray(np.stack([cbf[chx], cbf[chb], cbf[chc]], axis=1)).astype(np.float32)
        m["z"] = tile_tok(p[:, so + h * 64: so + (h + 1) * 64])
        m["dtr"] = tile_tok(p[:, [so + 768 + h, so + 768 + 4 + h]])
        m["dtb"] = rep(inp["ssd_dt_bias"][i][:, h]); m["alog"] = rep(inp["ssd_a_log"][i][:, h]); m["dsk"] = rep(inp["ssd_d"][i][h:h + 1])
        maps.append(m)
    return maps


S5OFF = 1320


def build_stage_s5():
    nc = bass.Bass("TRN2", target_bir_lowering=False)
    P = Prog(nc)
    uT = P.dram("uT", [64, TS], F32, "ExternalInput")
    prm = P.dram("prm", [128, 4, 3], F32, "ExternalInput")
    bT = P.dram("bT", [2, 64, 128], F32, "ExternalInput")
    cT = P.dram("cT", [2, 4, 128, 64], F32, "ExternalInput")
    dskd = P.dram("dsk", [64, 1], F32, "ExternalInput")
    yT = P.dram("yT", [64, TS], F32, "ExternalOutput")

    u = P.sb("u_all", [64, TS]); y = P.sb("y_all", [64, TS])
    for (t0, L) in SBLK:
        P.dma("sp", u[:, t0:t0 + L], uT.ap[:, t0:t0 + L], writes=[u])
    pr = P.sb("pr", [128, 4, 3]); P.dma("sp", pr[:], prm.ap, writes=[pr])
    ub = P.sb("u_bf", [64, TS], BF16)
    for (t0, L) in SBLK:
        P.dma("pool", ub[:, t0:t0 + L], uT.ap[:, t0:t0 + L], writes=[ub])
    bre = P.sb("bre", [64, 128], BF16); bim = P.sb("bim", [64, 128], BF16)
    P.dma("pool", bre[:], bT.ap[0], writes=[bre]); P.dma("pool", bim[:], bT.ap[1], writes=[bim])
    cre32 = P.sb("cre32", [128, 4, 64]); cim32 = P.sb("cim32", [128, 4, 64])
    cre = P.sb("cre", [128, 4, 64], BF16); cim = P.sb("cim", [128, 4, 64], BF16); ncre = P.sb("ncre", [128, 4, 64], BF16); ncim = P.sb("ncim", [128, 4, 64], BF16)
    P.dma("sp", cre32[:], cT.ap[0].rearrange("q p m -> p q m"), writes=[cre32])
    P.dma("sp", cim32[:], cT.ap[1].rearrange("q p m -> p q m"), writes=[cim32])
    P.op("dve", lambda e: e.tensor_copy(out=cre[:], in_=cre32[:]), reads=[cre32], writes=[cre])
    P.op("dve", lambda e: e.tensor_copy(out=cim[:], in_=cim32[:]), reads=[cim32], writes=[cim])
    P.op("dve", lambda e: e.tensor_scalar(out=ncre[:], in0=cre32[:], scalar1=-1.0, scalar2=None, op0=ALU.mult), reads=[cre32], writes=[ncre])
    P.op("dve", lambda e: e.tensor_scalar(out=ncim[:], in0=cim32[:], scalar1=-1.0, scalar2=None, op0=ALU.mult), reads=[cim32], writes=[ncim])
    dsk = P.sb("dsk_sb", [64, 1]); P.dma("sp", dsk[:], dskd.ap, writes=[dsk])

    def T4(nm):
        return P.sb(nm, [128, 4])
    step = T4("step"); er = T4("er"); th = T4("th"); cc = T4("cc"); ss = T4("ss"); t1 = T4("t1"); t2 = T4("t2"); t3 = T4("t3")
    halfpi = P.sb("halfpi", [128, 1]); P.op("dve", lambda e: e.memset(halfpi[:], math.pi / 2), writes=[halfpi])
    P.op("act", lambda e: e.activation(out=step[:], in_=pr[:, :, 2], func=AF.Exp), reads=[pr], writes=[step])
    P.op("dve", lambda e: e.tensor_tensor(out=t1[:], in0=pr[:, :, 0], in1=step[:], op=ALU.mult), reads=[pr, step], writes=[t1])
    P.op("act", lambda e: e.activation(out=er[:], in_=t1[:], func=AF.Exp), reads=[t1], writes=[er])
    P.op("dve", lambda e: e.tensor_tensor(out=th[:], in0=pr[:, :, 1], in1=step[:], op=ALU.mult), reads=[pr, step], writes=[th])
    P.op("act", lambda e: e.activation(out=ss[:], in_=th[:], func=AF.Sin, scale=1.0 / 16), reads=[th], writes=[ss])
    P.op("act", lambda e: e.activation(out=cc[:], in_=th[:], func=AF.Sin, scale=1.0 / 16, bias=halfpi[:, 0:1]), reads=[th, halfpi], writes=[cc])

    def square(cin, sin_, cout, sout):
        P.op("dve", lambda e: e.tensor_tensor(out=t1[:], in0=cin[:], in1=cin[:], op=ALU.mult), reads=[cin], writes=[t1])
        P.op("dve", lambda e: e.tensor_tensor(out=t2[:], in0=sin_[:], in1=sin_[:], op=ALU.mult), reads=[sin_], writes=[t2])
        P.op("dve", lambda e: e.tensor_tensor(out=t3[:], in0=cin[:], in1=sin_[:], op=ALU.mult), reads=[cin, sin_], writes=[t3])
        P.op("dve", lambda e: e.tensor_tensor(out=cout[:], in0=t1[:], in1=t2[:], op=ALU.subtract), reads=[t1, t2], writes=[cout])
        P.op("dve", lambda e: e.tensor_scalar(out=sout[:], in0=t3[:], scalar1=2.0, scalar2=None, op0=ALU.mult), reads=[t3], writes=[sout])
    for _ in range(4):
        square(cc, ss, cc, ss)
    Mre = [cc]; Mim = [ss]
    for l in range(1, 10):
        a = T4(f"Mre{l}"); b = T4(f"Mim{l}")
        square(Mre[-1], Mim[-1], a, b)
        Mre.append(a); Mim.append(b)
    ar = T4("ar"); ai = T4("ai"); den = T4("den"); fr = T4("fr"); fi = T4("fi"); t4 = T4("t4")
    P.op("dve", lambda e: e.tensor_tensor(out=ar[:], in0=er[:], in1=Mre[0][:], op=ALU.mult), reads=[er, Mre[0]], writes=[ar])
    P.op("dve", lambda e: e.tensor_tensor(out=ai[:], in0=er[:], in1=Mim[0][:], op=ALU.mult), reads=[er, Mim[0]], writes=[ai])
    P.op("dve", lambda e: e.tensor_scalar(out=ar[:], in0=ar[:], scalar1=-1.0, scalar2=None, op0=ALU.add), reads=[ar], writes=[ar])
    P.op("dve", lambda e: e.tensor_tensor(out=t1[:], in0=pr[:, :, 0], in1=pr[:, :, 0], op=ALU.mult), reads=[pr], writes=[t1])
    P.op("dve", lambda e: e.tensor_tensor(out=t2[:], in0=pr[:, :, 1], in1=pr[:, :, 1], op=ALU.mult), reads=[pr], writes=[t2])
    P.op("dve", lambda e: e.tensor_tensor(out=den[:], in0=t1[:], in1=t2[:], op=ALU.add), reads=[t1, t2], writes=[den])
    P.op("dve", lambda e: e.reciprocal(out=den[:], in_=den[:]), reads=[den], writes=[den])
    P.op("dve", lambda e: e.tensor_tensor(out=t1[:], in0=ar[:], in1=pr[:, :, 0], op=ALU.mult), reads=[ar, pr], writes=[t1])
    P.op("dve", lambda e: e.tensor_tensor(out=t2[:], in0=ai[:], in1=pr[:, :, 1], op=ALU.mult), reads=[ai, pr], writes=[t2])
    P.op("dve", lambda e: e.tensor_tensor(out=t3[:], in0=t1[:], in1=t2[:], op=ALU.add), reads=[t1, t2], writes=[t3])
    P.op("dve", lambda e: e.tensor_tensor(out=fr[:], in0=t3[:], in1=den[:], op=ALU.mult), reads=[t3, den], writes=[fr])
    P.op("dve", lambda e: e.tensor_tensor(out=t1[:], in0=ai[:], in1=pr[:, :, 0], op=ALU.mult), reads=[ai, pr], writes=[t1])
    P.op("dve", lambda e: e.tensor_tensor(out=t2[:], in0=ar[:], in1=pr[:, :, 1], op=ALU.mult), reads=[ar, pr], writes=[t2])
    P.op("dve", lambda e: e.tensor_tensor(out=t4[:], in0=t1[:], in1=t2[:], op=ALU.subtract), reads=[t1, t2], writes=[t4])
    P.op("dve", lambda e: e.tensor_tensor(out=fi[:], in0=t4[:], in1=den[:], op=ALU.mult), reads=[t4, den], writes=[fi])

    Ere = [P.sb(f"Ere{q}", [128, 512]) for q in range(4)]; Eim = [P.sb(f"Eim{q}", [128, 512]) for q in range(4)]
    Wre = [P.sb(f"Wre{q}", [128, 512]) for q in range(4)]; Wim = [P.sb(f"Wim{q}", [128, 512]) for q in range(4)]
    tt = P.sb("tt", [128, 512])
    for q in range(4):
        d = q // 2
        P.op("dve", lambda e, q=q: e.memset(Ere[q][:, 0:1], 1.0), writes=[Ere[q]])
        P.op("dve", lambda e, q=q: e.memset(Eim[q][:, 0:1], 0.0), writes=[Eim[q]])
        for l in range(9):
            n = 1 << l
            mr = Mre[l]; mi = Mim[l]
            P.op("dve", lambda e, q=q, n=n, mi=mi: e.tensor_scalar(out=tt[:, 0:n], in0=Eim[q][:, 0:n], scalar1=mi[:, q:q + 1], scalar2=None, op0=ALU.mult),
                 reads=[Eim[q], mi], writes=[tt])
            P.op("dve", lambda e, q=q, n=n, mr=mr: e.scalar_tensor_tensor(out=Ere[q][:, n:2 * n], in0=Ere[q][:, 0:n], scalar=mr[:, q:q + 1], in1=tt[:, 0:n],
                                                                         op0=ALU.mult, op1=ALU.subtract), reads=[Ere[q], mr, tt], writes=[Ere[q]])
            P.op("dve", lambda e, q=q, n=n, mi=mi: e.tensor_scalar(out=tt[:, 0:n], in0=Ere[q][:, 0:n], scalar1=mi[:, q:q + 1], scalar2=None, op0=ALU.mult),
                 reads=[Ere[q], mi], writes=[tt])
            P.op("dve", lambda e, q=q, n=n, mr=mr: e.scalar_tensor_tensor(out=Eim[q][:, n:2 * n], in0=Eim[q][:, 0:n], scalar=mr[:, q:q + 1], in1=tt[:, 0:n],
                                                                         op0=ALU.mult, op1=ALU.add), reads=[Eim[q], mr, tt], writes=[Eim[q]])
        opa = ALU.add if d == 0 else ALU.subtract
        opb = ALU.subtract if d == 0 else ALU.add
        P.op("dve", lambda e, q=q: e.tensor_scalar(out=tt[:], in0=Eim[q][:], scalar1=fi[:, q:q + 1], scalar2=None, op0=ALU.mult), reads=[Eim[q], fi], writes=[tt])
        P.op("dve", lambda e, q=q, opa=opa: e.scalar_tensor_tensor(out=Wre[q][:], in0=Ere[q][:], scalar=fr[:, q:q + 1], in1=tt[:], op0=ALU.mult, op1=opa),
             reads=[Ere[q], fr, tt], writes=[Wre[q]])
        P.op("dve", lambda e, q=q: e.tensor_scalar(out=tt[:], in0=Eim[q][:], scalar1=fr[:, q:q + 1], scalar2=None, op0=ALU.mult), reads=[Eim[q], fr], writes=[tt])
        P.op("dve", lambda e, q=q, opb=opb: e.scalar_tensor_tensor(out=Wim[q][:], in0=Ere[q][:], scalar=fi[:, q:q + 1], in1=tt[:], op0=ALU.mult, op1=opb),
             reads=[Ere[q], fi, tt], writes=[Wim[q]])

    Aps = [P.ps(f"Aps{i}", [128, 512]) for i in range(2)]; Bps = [P.ps(f"Bps{i}", [128, 512]) for i in range(2)]
    yps = [P.ps(f"yps{i}", [64, 512]) for i in range(2)]

    def dbl(nm):
        return [P.sb(f"{nm}{i}", [128, 512]) for i in range(2)]
    p1 = dbl("p1"); p2 = dbl("p2"); p3 = dbl("p3"); p4 = dbl("p4"); bur = dbl("bur"); bui = dbl("bui"); sre = dbl("sre"); sim = dbl("sim")
    def dblb(nm):
        return [P.sb(f"{nm}{i}", [128, 512], BF16) for i in range(2)]
    m1 = dblb("m1"); m2 = dblb("m2"); m3 = dblb("m3"); m4 = dblb("m4")
    ini_re = [P.sb(f"ini_re{i}", [128, 1]) for i in range(2)]; ini_im = [P.sb(f"ini_im{i}", [128, 1]) for i in range(2)]
    ta = P.sb("ta", [128, 1])
    items = []
    for q in range(4):
        d = q // 2
        order = list(range(17)) if d == 0 else [0] + list(range(16, 0, -1))
        for ci, c in enumerate(order):
            items.append((q, ci, c, len(order)))
    NI = len(items)

    def S1(n):
        q, ci, c, no = items[n]
        d, k = q // 2, q % 2
        t0, L = SBLK[c]
        z = n % 2
        A = Aps[z]; Bp = Bps[z]
        P.op("pe", lambda e, A=A, k=k, t0=t0, L=L: e.matmul(A[:, 0:L], lhsT=bre[32 * k:32 * k + 32, :], rhs=ub[32 * k:32 * k + 32, t0:t0 + L], start=True, stop=True),
             reads=[bre, ub], writes=[A])
        P.op("pe", lambda e, Bp=Bp, k=k, t0=t0, L=L: e.matmul(Bp[:, 0:L], lhsT=bim[32 * k:32 * k + 32, :], rhs=ub[32 * k:32 * k + 32, t0:t0 + L], start=True, stop=True),
             reads=[bim, ub], writes=[Bp])
        wr_, wi_ = Wre[q], Wim[q]
        P.op("dve", lambda e, z=z, A=A, L=L, wr_=wr_: e.tensor_tensor(out=p1[z][:, 0:L], in0=wr_[:, 0:L], in1=A[:, 0:L], op=ALU.mult), reads=[wr_, A], writes=[p1[z]])
        P.op("dve", lambda e, z=z, Bp=Bp, L=L, wi_=wi_: e.tensor_tensor(out=p2[z][:, 0:L], in0=wi_[:, 0:L], in1=Bp[:, 0:L], op=ALU.mult), reads=[wi_, Bp], writes=[p2[z]])
        P.op("dve", lambda e, z=z, Bp=Bp, L=L, wr_=wr_: e.tensor_tensor(out=p3[z][:, 0:L], in0=wr_[:, 0:L], in1=Bp[:, 0:L], op=ALU.mult), reads=[wr_, Bp], writes=[p3[z]])
        P.op("dve", lambda e, z=z, A=A, L=L, wi_=wi_: e.tensor_tensor(out=p4[z][:, 0:L], in0=wi_[:, 0:L], in1=A[:, 0:L], op=ALU.mult), reads=[wi_, A], writes=[p4[z]])
        P.op("pool", lambda e, z=z, L=L: e.tensor_tensor(out=bur[z][:, 0:L], in0=p1[z][:, 0:L], in1=p2[z][:, 0:L], op=ALU.subtract), reads=[p1[z], p2[z]], writes=[bur[z]])
        P.op("pool", lambda e, z=z, L=L: e.tensor_tensor(out=bui[z][:, 0:L], in0=p3[z][:, 0:L], in1=p4[z][:, 0:L], op=ALU.add), reads=[p3[z], p4[z]], writes=[bui[z]])

    def S2(n):
        q, ci, c, no = items[n]
        d, k = q // 2, q % 2
        t0, L = SBLK[c]
        z = n % 2
        rq = er[:, q:q + 1]
        for (sx, bx, inix) in ((sre, bur, ini_re), (sim, bui, ini_im)):
            if ci == 0:
                init = 0.0; rd = [bx[z], er]
            else:
                init = inix[(ci - 1) % 2][:, 0:1]; rd = [bx[z], er, inix[(ci - 1) % 2]]
            if d == 0:
                P.op("dve", lambda e, sx=sx, bx=bx, z=z, L=L, init=init, rq=rq: e.tensor_tensor_scan(
                    out=sx[z][:, 0:L], data0=rq.to_broadcast([128, L]), data1=bx[z][:, 0:L], initial=init, op0=ALU.mult, op1=ALU.add), reads=rd, writes=[sx[z]])
            else:
                P.op("dve", lambda e, sx=sx, bx=bx, z=z, L=L, init=init, rq=rq: e.tensor_tensor_scan(
                    out=sx[z][:, 0:L][:, ::-1], data0=rq.to_broadcast([128, L]), data1=bx[z][:, 0:L][:, ::-1], initial=init, op0=ALU.mult, op1=ALU.add), reads=rd, writes=[sx[z]])
        if ci < no - 1:
            if d == 0:
                lvl = 8 if L == 256 else 9; col_ = L - 1
            else:
                lvl = 9; col_ = 0
            elr = Mre[lvl][:, q:q + 1]; eli = Mim[lvl][:, q:q + 1]
            w = ci % 2
            P.op("dve", lambda e, z=z, col_=col_, eli=eli: e.tensor_tensor(out=ta[:], in0=sim[z][:, col_:col_ + 1], in1=eli, op=ALU.mult), reads=[sim[z], Mim[lvl]], writes=[ta])
            P.op("dve", lambda e, z=z, col_=col_, elr=elr, w=w: e.scalar_tensor_tensor(out=ini_re[w][:], in0=sre[z][:, col_:col_ + 1], scalar=elr, in1=ta[:], op0=ALU.mult, op1=ALU.subtract),
                 reads=[sre[z], Mre[lvl], ta], writes=[ini_re[w]])
            P.op("dve", lambda e, z=z, col_=col_, eli=eli: e.tensor_tensor(out=ta[:], in0=sre[z][:, col_:col_ + 1], in1=eli, op=ALU.mult), reads=[sre[z], Mim[lvl]], writes=[ta])
            P.op("dve", lambda e, z=z, col_=col_, elr=elr, w=w: e.scalar_tensor_tensor(out=ini_im[w][:], in0=sim[z][:, col_:col_ + 1], scalar=elr, in1=ta[:], op0=ALU.mult, op1=ALU.add),
                 reads=[sim[z], Mre[lvl], ta], writes=[ini_im[w]])
        er_, ei_ = Ere[q], Eim[q]
        P.op("pool", lambda e, z=z, L=L, er_=er_: e.tensor_tensor(out=m1[z][:, 0:L], in0=er_[:, 0:L], in1=sre[z][:, 0:L], op=ALU.mult), reads=[er_, sre[z]], writes=[m1[z]])
        P.op("pool", lambda e, z=z, L=L, ei_=ei_: e.tensor_tensor(out=m2[z][:, 0:L], in0=ei_[:, 0:L], in1=sim[z][:, 0:L], op=ALU.mult), reads=[ei_, sim[z]], writes=[m2[z]])
        P.op("pool", lambda e, z=z, L=L, er_=er_: e.tensor_tensor(out=m3[z][:, 0:L], in0=er_[:, 0:L], in1=sim[z][:, 0:L], op=ALU.mult), reads=[er_, sim[z]], writes=[m3[z]])
        P.op("dve", lambda e, z=z, L=L, ei_=ei_: e.tensor_tensor(out=m4[z][:, 0:L], in0=ei_[:, 0:L], in1=sre[z][:, 0:L], op=ALU.mult), reads=[ei_, sre[z]], writes=[m4[z]])
        yp = yps[z]
        c2 = ncre if d == 0 else cre
        c4 = ncim if d == 0 else cim
        for mi_, (cm, mm) in enumerate(((cre, m1), (c2, m2), (ncim, m3), (c4, m4))):
            P.op("pe", lambda e, yp=yp, cm=cm, mm=mm, z=z, L=L, q=q, mi_=mi_: e.matmul(yp[:, 0:L], lhsT=cm[:, q, :], rhs=mm[z][:, 0:L], start=(mi_ == 0), stop=(mi_ == 3)),
                 reads=[cm, mm[z]], writes=[yp], inc=(mi_ == 3))
        if q == 0:
            P.op("act", lambda e, yp=yp, t0=t0, L=L: e.activation(out=y[:, t0:t0 + L], in_=yp[:, 0:L], func=AF.Copy), reads=[yp], writes=[y])
        else:
            P.op("dve", lambda e, yp=yp, t0=t0, L=L: e.tensor_tensor(out=y[:, t0:t0 + L], in0=yp[:, 0:L], in1=y[:, t0:t0 + L], op=ALU.add), reads=[yp, y], writes=[y])

    S1(0)
    for n in range(NI):
        if n + 1 < NI:
            S1(n + 1)
        S2(n)
    for (t0, L) in SBLK:
        P.op("dve", lambda e, t0=t0, L=L: e.scalar_tensor_tensor(out=y[:, t0:t0 + L], in0=u[:, t0:t0 + L], scalar=dsk[:, 0:1], in1=y[:, t0:t0 + L], op0=ALU.mult, op1=ALU.add),
             reads=[u, dsk, y], writes=[y])
    P.dma("sp", yT.ap, y[:], reads=[y], is_output=True)
    P.emit()
    return nc


def stage_s5_inputs(PS, inp, i):
    maps = []
    lre, lim, lst = inp["s5_lam_re"][i], inp["s5_lam_im"][i], inp["s5_log_step"][i]
    for core in range(8):
        b, gq = core // 4, core % 4
        m = {}
        m["uT"] = np.ascontiguousarray(PS[b][:, S5OFF + 64 * gq:S5OFF + 64 * gq + 64].T)
        prm = np.zeros((128, 4, 3), np.float32)
        bT = np.zeros((2, 64, 128), np.float32)
        cT = np.zeros((2, 4, 128, 64), np.float32)
        for k in range(2):
            for gl in range(2):
                g = 4 * gq + 2 * k + gl
                rows = slice(32 * k + 16 * gl, 32 * k + 16 * gl + 16)
                cols = slice(64 * gl, 64 * gl + 64)
                bT[0, rows, cols] = inp["s5_b_re"][i][g].T
                bT[1, rows, cols] = inp["s5_b_im"][i][g].T
                for d in range(2):
                    q = 2 * d + k
                    prm[cols, q, 0] = lre[d, g]; prm[cols, q, 1] = lim[d, g]; prm[cols, q, 2] = lst[d, g]
                    cT[0, q, cols, rows] = inp["s5_c_re"][i][d, g].T
                    cT[1, q, cols, rows] = inp["s5_c_im"][i][d, g].T
        m["prm"] = prm; m["bT"] = bT; m["cT"] = cT
        m["dsk"] = col(inp["s5_d"][i][64 * gq:64 * gq + 64])
        maps.append(m)
    return maps


def build_stage_c1():
    nc = bass.Bass("TRN2", target_bir_lowering=False)
    P = Prog(nc)
    catT = P.dram("catT", [128, KT, NTOK], F32, "ExternalInput")
    xT = P.dram("xT", [128, KT, NTOK], F32, "ExternalInput")
    modTd = P.dram("modT", [128, 48, 2], F32, "ExternalInput")
    w_out = P.dram("w_out", [D, D], F32, "ExternalInput")
    w_glu = P.dram("w_glu", [256, 256], F32, "ExternalInput")
    bgluT = P.dram("bgluT", [128, 2], F32, "ExternalInput")
    gssdT = P.dram("gssdT", [128, 2], F32, "ExternalInput")
    norm2T = P.dram("norm2T", [128, KT], F32, "ExternalInput")
    wr_d = P.dram("wr", [D, 36], F32, "ExternalInput")
    br_d = P.dram("br", [128, 36], F32, "ExternalInput")
    ident_d = P.dram("ident", [128, 128], F32, "ExternalInput")
    xmid = P.dram("xmid", [128, KT, NTOK], F32, "ExternalOutput")
    hT = P.dram("hT", [128, KT, NTOK], F32, "ExternalOutput")
    gatesT = P.dram("gatesT", [32, NTOK], F32, "ExternalOutput")

    ones_bf = P.sb("ones_bf", [128, 128], BF16)
    P.op("dve", lambda e: e.memset(ones_bf[:], 1.0), writes=[ones_bf])
    modT = P.sb("modT_sb", [128, 48, 2]); P.dma("sp", modT[:], modTd.ap, writes=[modT])
    bglu = P.sb("bglu", [128, 2]); P.dma("sp", bglu[:], bgluT.ap, writes=[bglu])
    gssd = P.sb("gssd", [128, 2]); P.dma("sp", gssd[:], gssdT.ap, writes=[gssd])
    n2 = P.sb("n2", [128, KT]); P.dma("sp", n2[:], norm2T.ap, writes=[n2])
    wr = P.sb("wr_sb", [128, KT, 36]); P.dma("sp", wr[:], wr_d.ap.rearrange("(kt p) n -> p kt n", p=128), writes=[wr])
    br = P.sb("br_sb", [128, 36]); P.dma("sp", br[:], br_d.ap, writes=[br])
    ident = P.sb("ident_sb", [128, 128]); P.dma("sp", ident[:], ident_d.ap, writes=[ident])
    wout = P.sb("wout_bf", [128, KT, D], BF16)
    P.dma("pool", wout[:], w_out.ap.rearrange("(kt p) n -> p kt n", p=128), writes=[wout])
    wglu = P.sb("wglu_bf", [128, 2, 256], BF16)
    P.dma("pool", wglu[:], w_glu.ap.rearrange("(kt p) n -> p kt n", p=128), writes=[wglu])
    G2 = P.sb("G2", [128, KT, 2])
    for r in range(2):
        P.op("dve", lambda e, r=r: e.scalar_tensor_tensor(out=G2[:, :, r], in0=modT[:, 32:40, r], scalar=1.0, in1=n2[:, :], op0=ALU.add, op1=ALU.mult),
             reads=[modT, n2], writes=[G2])

    cat = P.sb("cat", [128, KT, 512]); xm = P.sb("xm", [128, KT, 512]); tmp = P.sb("tmp", [128, KT, 512]); hh = P.sb("hh", [128, KT, 512])
    sq = P.sb("sq", [128, KT, 512], BF16); catb = P.sb("catb", [128, KT, 512], BF16)
    gg = P.sb("gg", [128, 2, 512]); ggb = P.sb("ggb", [128, 2, 512], BF16); sig = P.sb("sig", [128, 512])
    rs = P.sb("rs", [128, 512]); rstd = P.sb("rstd", [128, 512])
    gT = P.sb("gT", [32, NTOK])
    ss_ps = P.ps("ss_ps", [128, 512]); lin_ps = P.ps("lin_ps", [128, 512])
    mix_ps = [P.ps(f"mix_ps{i}", [128, 512]) for i in range(2)]
    lg_ps = [P.ps(f"lg_ps{i}", [128, 36]) for i in range(2)]
    tr_ps = P.ps("tr_ps", [32, 128])
    Lg = P.sb("Lg", [128, 36]); gmax = P.sb("gmax", [128, 1]); ngmax = P.sb("ngmax", [128, 1]); ghot = P.sb("ghot", [128, 4]); eg = P.sb("eg", [128, 4])
    sume = P.sb("sume", [128, 1]); pg = P.sb("pg", [128, 1]); les = P.sb("les", [128, 8]); m1 = P.sb("m1", [128, 1]); hot1 = P.sb("hot1", [128, 8])
    le2 = P.sb("le2", [128, 8]); m2 = P.sb("m2", [128, 1]); hot2 = P.sb("hot2", [128, 8]); d21 = P.sb("d21", [128, 1]); e21 = P.sb("e21", [128, 1])
    w1 = P.sb("w1", [128, 1]); w2 = P.sb("w2", [128, 1]); inner = P.sb("inner", [128, 8]); gates = P.sb("gates", [128, 32])
    mi = 0
    for bi, (t0, T) in enumerate(BLOCKS):
        r = 1 if bi == 4 else 0
        P.dma("sp", cat[:, :, 0:T], catT.ap[:, :, t0:t0 + T], writes=[cat])
        P.dma("sp", xm[:, :, 0:T], xT.ap[:, :, t0:t0 + T], writes=[xm])
        P.op("act", lambda e, T=T: e.activation(out=sq[:, 2:4, 0:T], in_=cat[:, 2:4, 0:T], func=AF.Square), reads=[cat], writes=[sq])
        for j, kt in enumerate((2, 3)):
            P.op("pe", lambda e, kt=kt, j=j, T=T: e.matmul(ss_ps[:, 0:T], lhsT=ones_bf[:, :], rhs=sq[:, kt, 0:T], start=(j == 0), stop=(j == 1)),
                 reads=[ones_bf, sq], writes=[ss_ps], inc=(j == 1))
        P.op("act", lambda e, T=T: e.activation(out=rs[:, 0:T], in_=ss_ps[:, 0:T], func=AF.Sqrt, scale=1.0 / 256, bias=EPS), reads=[ss_ps], writes=[rs])
        P.op("dve", lambda e, T=T: e.reciprocal(out=rstd[:, 0:T], in_=rs[:, 0:T]), reads=[rs], writes=[rstd])
        for j, kt in enumerate((2, 3)):
            P.op("dve", lambda e, kt=kt, T=T: e.tensor_tensor(out=tmp[:, kt, 0:T], in0=cat[:, kt, 0:T], in1=rstd[:, 0:T], op=ALU.mult), reads=[cat, rstd], writes=[tmp])
            P.op("act", lambda e, kt=kt, j=j, T=T: e.activation(out=catb[:, kt, 0:T], in_=tmp[:, kt, 0:T], func=AF.Identity, scale=gssd[:, j:j + 1]),
                 reads=[tmp, gssd], writes=[catb])
        P.op("act", lambda e, T=T: e.activation(out=gg[:, :, 0:T], in_=cat[:, 4:6, 0:T], func=AF.Gelu_apprx_tanh), reads=[cat], writes=[gg])
        P.op("pool", lambda e, T=T: e.tensor_copy(out=ggb[:, :, 0:T], in_=gg[:, :, 0:T]), reads=[gg], writes=[ggb])
        for j in range(2):
            for kt in range(2):
                P.op("pe", lambda e, j=j, kt=kt, T=T: e.matmul(lin_ps[:, 0:T], lhsT=wglu[:, kt, j * 128:(j + 1) * 128], rhs=ggb[:, kt, 0:T], start=(kt == 0), stop=(kt == 1)),
                     reads=[wglu, ggb], writes=[lin_ps], inc=(kt == 1))
            P.op("act", lambda e, j=j, T=T: e.activation(out=sig[:, 0:T], in_=lin_ps[:, 0:T], func=AF.Sigmoid, bias=bglu[:, j:j + 1]), reads=[lin_ps, bglu], writes=[sig])
            P.op("dve", lambda e, j=j, T=T: e.tensor_tensor(out=catb[:, 4 + j, 0:T], in0=gg[:, j, 0:T], in1=sig[:, 0:T], op=ALU.mult), reads=[gg, sig], writes=[catb])
        P.op("pool", lambda e, T=T: e.tensor_copy(out=catb[:, 0:2, 0:T], in_=cat[:, 0:2, 0:T]), reads=[cat], writes=[catb])
        P.op("pool", lambda e, T=T: e.tensor_copy(out=catb[:, 6:8, 0:T], in_=cat[:, 6:8, 0:T]), reads=[cat], writes=[catb])
        for nt in range(KT):
            mp = mix_ps[mi % 2]; mi += 1
            for kt in range(KT):
                P.op("pe", lambda e, mp=mp, nt=nt, kt=kt, T=T: e.matmul(mp[:, 0:T], lhsT=wout[:, kt, nt * 128:(nt + 1) * 128], rhs=catb[:, kt, 0:T], start=(kt == 0), stop=(kt == KT - 1)),
                     reads=[wout, catb], writes=[mp], inc=(kt == KT - 1))
            P.op("dve", lambda e, mp=mp, nt=nt, T=T, r=r: e.scalar_tensor_tensor(out=xm[:, nt, 0:T], in0=mp[:, 0:T], scalar=modT[:, 16 + nt, r:r + 1], in1=xm[:, nt, 0:T],
                                                                             op0=ALU.mult, op1=ALU.add), reads=[mp, modT, xm], writes=[xm])
        P.dma("sp", xmid.ap[:, :, t0:t0 + T], xm[:, :, 0:T], reads=[xm], is_output=True)
        P.op("act", lambda e, T=T: e.activation(out=sq[:, :, 0:T], in_=xm[:, :, 0:T], func=AF.Square), reads=[xm], writes=[sq])
        for kt in range(KT):
            P.op("pe", lambda e, kt=kt, T=T: e.matmul(ss_ps[:, 0:T], lhsT=ones_bf[:, :], rhs=sq[:, kt, 0:T], start=(kt == 0), stop=(kt == KT - 1)),
                 reads=[ones_bf, sq], writes=[ss_ps], inc=(kt == KT - 1))
        P.op("act", lambda e, T=T: e.activation(out=rs[:, 0:T], in_=ss_ps[:, 0:T], func=AF.Sqrt, scale=1.0 / D, bias=EPS), reads=[ss_ps], writes=[rs])
        P.op("dve", lambda e, T=T: e.reciprocal(out=rstd[:, 0:T], in_=rs[:, 0:T]), reads=[rs], writes=[rstd])
        for kt in range(KT):
            P.op("dve", lambda e, kt=kt, T=T: e.tensor_tensor(out=tmp[:, kt, 0:T], in0=xm[:, kt, 0:T], in1=rstd[:, 0:T], op=ALU.mult), reads=[xm, rstd], writes=[tmp])
            P.op("act", lambda e, kt=kt, T=T, r=r: e.activation(out=hh[:, kt, 0:T], in_=tmp[:, kt, 0:T], func=AF.Identity, scale=G2[:, kt, r:r + 1], bias=modT[:, 24 + kt, r:r + 1]),
                 reads=[tmp, G2, modT], writes=[hh])
        P.dma("sp", hT.ap[:, :, t0:t0 + T], hh[:, :, 0:T], reads=[hh], is_output=True)
        for s in range(T // 128):
            lp = lg_ps[s % 2]
            for kt in range(KT):
                P.op("pe", lambda e, lp=lp, kt=kt, s=s: e.matmul(lp[:, :], lhsT=hh[:, kt, s * 128:(s + 1) * 128], rhs=wr[:, kt, :], start=(kt == 0), stop=(kt == KT - 1)),
                     reads=[hh, wr], writes=[lp], inc=(kt == KT - 1))
            P.op("dve", lambda e, lp=lp: e.tensor_tensor(out=Lg[:], in0=lp[:, :], in1=br[:], op=ALU.add), reads=[lp, br], writes=[Lg])
            P.op("dve", lambda e: e.tensor_reduce(out=gmax[:], in_=Lg[:, 0:4], axis=AX.X, op=ALU.max), reads=[Lg], writes=[gmax])
            P.op("dve", lambda e: e.tensor_scalar(out=ghot[:], in0=Lg[:, 0:4], scalar1=gmax[:, 0:1], scalar2=None, op0=ALU.is_ge), reads=[Lg, gmax], writes=[ghot])
            P.op("dve", lambda e: e.tensor_scalar(out=ngmax[:], in0=gmax[:], scalar1=-1.0, scalar2=None, op0=ALU.mult), reads=[gmax], writes=[ngmax])
            P.op("act", lambda e: e.activation(out=eg[:], in_=Lg[:, 0:4], func=AF.Exp, bias=ngmax[:, 0:1]), reads=[Lg, ngmax], writes=[eg])
            P.op("dve", lambda e: e.tensor_reduce(out=sume[:], in_=eg[:], axis=AX.X, op=ALU.add), reads=[eg], writes=[sume])
            P.op("dve", lambda e: e.reciprocal(out=pg[:], in_=sume[:]), reads=[sume], writes=[pg])
            P.op("dve", lambda e: e.tensor_scalar(out=les[:], in0=Lg[:, 4:12], scalar1=ghot[:, 0:1], scalar2=None, op0=ALU.mult), reads=[Lg, ghot], writes=[les])
            for g in range(1, 4):
                P.op("dve", lambda e, g=g: e.scalar_tensor_tensor(out=les[:], in0=Lg[:, 4 + 8 * g:12 + 8 * g], scalar=ghot[:, g:g + 1], in1=les[:], op0=ALU.mult, op1=ALU.add),
                     reads=[Lg, ghot, les], writes=[les])
            P.op("dve", lambda e: e.tensor_reduce(out=m1[:], in_=les[:], axis=AX.X, op=ALU.max), reads=[les], writes=[m1])
            P.op("dve", lambda e: e.tensor_scalar(out=hot1[:], in0=les[:], scalar1=m1[:, 0:1], scalar2=None, op0=ALU.is_ge), reads=[les, m1], writes=[hot1])
            P.op("dve", lambda e: e.scalar_tensor_tensor(out=le2[:], in0=hot1[:], scalar=-1e30, in1=les[:], op0=ALU.mult, op1=ALU.add), reads=[hot1, les], writes=[le2])
            P.op("dve", lambda e: e.tensor_reduce(out=m2[:], in_=le2[:], axis=AX.X, op=ALU.max), reads=[le2], writes=[m2])
            P.op("dve", lambda e: e.tensor_scalar(out=hot2[:], in0=le2[:], scalar1=m2[:, 0:1], scalar2=None, op0=ALU.is_ge), reads=[le2, m2], writes=[hot2])
            P.op("dve", lambda e: e.tensor_tensor(out=d21[:], in0=m2[:], in1=m1[:], op=ALU.subtract), reads=[m1, m2], writes=[d21])
            P.op("act", lambda e: e.activation(out=e21[:], in_=d21[:], func=AF.Exp), reads=[d21], writes=[e21])
            P.op("dve", lambda e: e.tensor_scalar(out=w1[:], in0=e21[:], scalar1=1.0, scalar2=None, op0=ALU.add), reads=[e21], writes=[w1])
            P.op("dve", lambda e: e.reciprocal(out=w1[:], in_=w1[:]), reads=[w1], writes=[w1])
            P.op("dve", lambda e: e.tensor_tensor(out=w1[:], in0=w1[:], in1=pg[:], op=ALU.mult), reads=[w1, pg], writes=[w1])
            P.op("dve", lambda e: e.tensor_tensor(out=w2[:], in0=w1[:], in1=e21[:], op=ALU.mult), reads=[w1, e21], writes=[w2])
            P.op("dve", lambda e: e.tensor_scalar(out=inner[:], in0=hot1[:], scalar1=w1[:, 0:1], scalar2=None, op0=ALU.mult), reads=[hot1, w1], writes=[inner])
            P.op("dve", lambda e: e.scalar_tensor_tensor(out=inner[:], in0=hot2[:], scalar=w2[:, 0:1], in1=inner[:], op0=ALU.mult, op1=ALU.add), reads=[hot2, w2, inner], writes=[inner])
            for g in range(4):
                P.op("dve", lambda e, g=g: e.tensor_scalar(out=gates[:, 8 * g:8 * g + 8], in0=inner[:], scalar1=ghot[:, g:g + 1], scalar2=None, op0=ALU.mult),
                     reads=[inner, ghot], writes=[gates])
            P.op("pe", lambda e: e.transpose(tr_ps[:, :], gates[:, :], ident[:, :]), reads=[gates, ident], writes=[tr_ps])
            tk = t0 + s * 128
            P.op("act", lambda e, tk=tk: e.activation(out=gT[:, tk:tk + 128], in_=tr_ps[:, :], func=AF.Copy), reads=[tr_ps], writes=[gT])
    P.dma("sp", gatesT.ap, gT[:], reads=[gT], is_output=True)
    P.emit()
    return nc


def build_stage_c2():
    nc = bass.Bass("TRN2", target_bir_lowering=False)
    P = Prog(nc)
    hTd = P.dram("hT", [128, KT, NTOK], F32, "ExternalInput")
    xmid = P.dram("xmid", [128, KT, NTOK], F32, "ExternalInput")
    gatesT = P.dram("gatesT", [32, NTOK], F32, "ExternalInput")
    modTd = P.dram("modT", [128, 48, 2], F32, "ExternalInput")
    seld = P.dram("sel", [32, 32, 128], F32, "ExternalInput")
    w_gate = P.dram("w_gate", [32, D, 256], F32, "ExternalInput")
    w_up = P.dram("w_up", [32, D, 256], F32, "ExternalInput")
    w_down = P.dram("w_down", [32, 256, D], F32, "ExternalInput")
    xout = P.dram("xout", [128, KT, NTOK], F32, "ExternalOutput")

    hb = P.sb("hb", [128, KT, NTOK], BF16)
    xa = P.sb("xa", [128, KT, NTOK])
    for kt in range(KT):
        P.dma("pool", hb[:, kt, :], hTd.ap[:, kt, :], writes=[hb])
    for kt in range(KT):
        P.dma("sp", xa[:, kt, :], xmid.ap[:, kt, :], writes=[xa])
    gT = P.sb("gT", [32, NTOK]); P.dma("sp", gT[:], gatesT.ap, writes=[gT])
    modT = P.sb("modT_sb", [128, 48, 2]); P.dma("sp", modT[:], modTd.ap, writes=[modT])
    sel = P.sb("sel_sb", [32, 32, 128]); P.dma("sp", sel[:], seld.ap, writes=[sel])
    wg = [P.sb(f"wg{i}", [128, KT, 256], BF16) for i in range(2)]
    wu = [P.sb(f"wu{i}", [128, KT, 256], BF16) for i in range(2)]
    wd = [P.sb(f"wd{i}", [128, 2, D], BF16) for i in range(2)]
    gb_ps = P.ps("gb_ps", [128, 512])
    G_ps = [P.ps(f"G_ps{i}", [128, 512]) for i in range(2)]; U_ps = [P.ps(f"U_ps{i}", [128, 512]) for i in range(2)]
    O_ps = [P.ps(f"O_ps{i}", [128, 512]) for i in range(2)]
    gbs = [P.sb(f"gbs{i}", [128, 512]) for i in range(2)]
    sg = [P.sb(f"sg{i}", [128, 512]) for i in range(2)]; su = [P.sb(f"su{i}", [128, 512]) for i in range(2)]
    hid = [P.sb(f"hid{i}", [128, 2, 512], BF16) for i in range(2)]
    cnt = {"oi": 0, "ji": 0}
    items = [(ex, bi) for ex in range(32) for bi in range(len(BLOCKS))]
    NI = len(items)

    def load_w(ex):
        z = ex % 2
        P.dma("pool", wg[z][:], w_gate.ap[ex].rearrange("(kt p) n -> p kt n", p=128), writes=[wg[z]])
        P.dma("pool", wu[z][:], w_up.ap[ex].rearrange("(kt p) n -> p kt n", p=128), writes=[wu[z]])
        P.dma("pool", wd[z][:], w_down.ap[ex].rearrange("(kt p) n -> p kt n", p=128), writes=[wd[z]])

    def emit_front(n):
        ex, bi = items[n]
        t0, T = BLOCKS[bi]
        z = ex % 2; b2 = n % 2
        P.op("pe", lambda e, ex=ex, t0=t0, T=T: e.matmul(gb_ps[:, 0:T], lhsT=sel[:, ex, :], rhs=gT[:, t0:t0 + T], start=True, stop=True), reads=[sel, gT], writes=[gb_ps])
        P.op("act", lambda e, b2=b2, T=T: e.activation(out=gbs[b2][:, 0:T], in_=gb_ps[:, 0:T], func=AF.Copy), reads=[gb_ps], writes=[gbs[b2]])
        for j in range(2):
            j2 = cnt["ji"] % 2; cnt["ji"] += 1
            Gp = G_ps[j2]; Up = U_ps[j2]
            for kt in range(KT):
                P.op("pe", lambda e, Gp=Gp, z=z, kt=kt, j=j, t0=t0, T=T: e.matmul(Gp[:, 0:T], lhsT=wg[z][:, kt, j * 128:(j + 1) * 128], rhs=hb[:, kt, t0:t0 + T],
                                                                               start=(kt == 0), stop=(kt == KT - 1)), reads=[wg[z], hb], writes=[Gp], inc=(kt == KT - 1))
            for kt in range(KT):
                P.op("pe", lambda e, Up=Up, z=z, kt=kt, j=j, t0=t0, T=T: e.matmul(Up[:, 0:T], lhsT=wu[z][:, kt, j * 128:(j + 1) * 128], rhs=hb[:, kt, t0:t0 + T],
                                                                               start=(kt == 0), stop=(kt == KT - 1)), reads=[wu[z], hb], writes=[Up], inc=(kt == KT - 1))
            P.op("act", lambda e, Gp=Gp, j2=j2, T=T: e.activation(out=sg[j2][:, 0:T], in_=Gp[:, 0:T], func=AF.Silu), reads=[Gp], writes=[sg[j2]])
            P.op("dve", lambda e, Up=Up, j2=j2, T=T: e.tensor_tensor(out=su[j2][:, 0:T], in0=sg[j2][:, 0:T], in1=Up[:, 0:T], op=ALU.mult), reads=[sg[j2], Up], writes=[su[j2]])
            P.op("dve", lambda e, j2=j2, b2=b2, j=j, T=T: e.tensor_tensor(out=hid[b2][:, j, 0:T], in0=su[j2][:, 0:T], in1=gbs[b2][:, 0:T], op=ALU.mult),
                 reads=[su[j2], gbs[b2]], writes=[hid[b2]])

    def emit_back(n):
        ex, bi = items[n]
        t0, T = BLOCKS[bi]
        r = 1 if bi == 4 else 0
        z = ex % 2; b2 = n % 2
        for nt in range(KT):
            Op = O_ps[cnt["oi"] % 2]; cnt["oi"] += 1
            for j in range(2):
                P.op("pe", lambda e, Op=Op, z=z, j=j, nt=nt, b2=b2, T=T: e.matmul(Op[:, 0:T], lhsT=wd[z][:, j, nt * 128:(nt + 1) * 128], rhs=hid[b2][:, j, 0:T],
                                                                               start=(j == 0), stop=(j == 1)), reads=[wd[z], hid[b2]], writes=[Op], inc=(j == 1))
            P.op("dve", lambda e, Op=Op, nt=nt, t0=t0, T=T, r=r: e.scalar_tensor_tensor(out=xa[:, nt, t0:t0 + T], in0=Op[:, 0:T], scalar=modT[:, 40 + nt, r:r + 1],
                                                                                      in1=xa[:, nt, t0:t0 + T], op0=ALU.mult, op1=ALU.add), reads=[Op, modT, xa], writes=[xa])

    load_w(0); load_w(1)
    emit_front(0)
    for n in range(NI):
        if n + 1 < NI:
            emit_front(n + 1)
        emit_back(n)
        ex, bi = items[n]
        if bi == len(BLOCKS) - 1 and ex + 2 < 32:
            load_w(ex + 2)
    for kt in range(KT):
        P.dma("sp", xout.ap[:, kt, :], xa[:, kt, :], reads=[xa], is_output=True)
    P.emit()
    return nc


def feat_to_tok(xt):
    return np.ascontiguousarray(xt.transpose(2, 1, 0).reshape(xt.shape[2], D))


def c_consts():
    if "c" not in _CONST:
        sel = np.zeros((32, 32, 128), np.float32)
        for e in range(32):
            sel[e, e, :] = 1.0
        _CONST["c"] = dict(sel=sel, ident=np.eye(128, dtype=np.float32))
    return _CONST["c"]


def run_layer(i, xT_cores, inp, dbg=None):
    f32 = lambda a: np.ascontiguousarray(np.asarray(a, np.float32))
    resA = _run("A", build_stage_a, stage_a_inputs(xT_cores, inp["c"], inp["c_ctx"], f32(inp["w_ada"][i]), inp["b_ada"][i], inp["norm1"][i], f32(inp["w_in"][i])))
    PS = []
    for b in range(2):
        lat = np.concatenate([resA[4 * b + q]["p"][0:2048] for q in range(4)], 0)
        cx = np.concatenate([resA[4 * b + q]["p"][2048:2112] for q in range(4)], 0)
        PS.append(np.concatenate([cx, lat], 0))
    resB1 = _run("B1", build_stage_attn, stage_attn_inputs(PS, inp, i))
    resB2 = _run("B2", build_stage_ssd, stage_ssd_inputs(PS, inp, i))
    resB3 = _run("B3", build_stage_s5, stage_s5_inputs(PS, inp, i))
    cst = c_consts()
    wr = f32(np.concatenate([inp["moe_w_group"][i], inp["moe_w_expert"][i].transpose(1, 0, 2).reshape(D, 32)], axis=1))
    br = rep(np.concatenate([inp["moe_b_group"][i], inp["moe_b_expert"][i].reshape(32)]))
    mapsC1 = []
    cats = []
    for b in range(2):
        a = np.concatenate([untile(resB1[4 * b + h]["o_m"]) for h in range(4)], 1)
        sd = np.concatenate([untile(resB2[4 * b + h]["o_s"]) for h in range(4)], 1)
        s5 = np.concatenate([resB3[4 * b + g]["yT"].T for g in range(4)], 1)
        df = np.concatenate([untile(resB1[4 * b + h]["o_d"]) for h in range(4)], 1)
        cats.append(np.concatenate([a, sd, s5, df], 1))
    if dbg is not None:
        dbg["PS"] = PS; dbg["cats"] = cats
    for core in range(8):
        b, q = core // 4, core % 4
        cs = cats[b]
        ct = tok_to_feat(core_tokens(cs[256:], cs[:256], q))
        mapsC1.append({"catT": ct, "xT": xT_cores[core], "modT": resA[core]["modT"], "w_out": f32(inp["w_out"][i]), "w_glu": f32(inp["s5_w_glu"][i]),
                       "bgluT": vecT(inp["s5_b_glu"][i], 2), "gssdT": vecT(inp["ssd_norm"][i], 2), "norm2T": vecT(inp["norm2"][i]),
                       "wr": wr, "br": br, "ident": cst["ident"]})
    resC1 = _run("C1", build_stage_c1, mapsC1)
    if dbg is not None:
        dbg["C1"] = resC1
    wg, wu, wd = f32(inp["moe_w_gate"][i]), f32(inp["moe_w_up"][i]), f32(inp["moe_w_down"][i])
    mapsC2 = [{"hT": resC1[c]["hT"], "xmid": resC1[c]["xmid"], "gatesT": resC1[c]["gatesT"], "modT": resA[c]["modT"], "sel": cst["sel"],
               "w_gate": wg, "w_up": wu, "w_down": wd} for c in range(8)]
    resC2 = _run("C2", build_stage_c2, mapsC2)
    return [resC2[c]["xout"] for c in range(8)]


def kernel(**inputs):
    inp = {k: np.asarray(v) for k, v in inputs.items()}
    x = np.asarray(inp["x"], np.float32); ctx = np.asarray(inp["ctx"], np.float32)
    xT_cores = [tok_to_feat(core_tokens(x[c // 4], ctx[c // 4], c % 4)) for c in range(8)]
    for i in range(4):
        xT_cores = run_layer(i, xT_cores, inp)
    out = np.zeros((2, 8192, D), np.float32)
    for c in range(8):
        b, q = c // 4, c % 4
        out[b, q * 2048:(q + 1) * 2048] = feat_to_tok(xT_cores[c])[:2048]
    return out
```

```python
import contextlib
import math
import numpy as np
import concourse.bass as bass
import concourse.mybir as mybir
from concourse.bass_utils import run_bass_kernel_spmd

F32 = mybir.dt.float32
BF16 = mybir.dt.bfloat16
I32 = mybir.dt.int32
AF = mybir.ActivationFunctionType
ALU = mybir.AluOpType
AX = mybir.AxisListType

SAME_ENGINE_SYNC = True


class Buf:
    _n = 0

    def __init__(self, t, name):
        self.t = t
        self.name = name
        Buf._n += 1
        self.id = Buf._n
        self.w = {}
        self.r = {}
        self.dcnt = {}

    def __getitem__(self, idx):
        return self.t[idx]


class Prog:
    ENG = ("pe", "act", "dve", "pool", "sp")

    def __init__(self, nc):
        self.nc = nc
        self.stack = contextlib.ExitStack()
        self.q = {e: [] for e in self.ENG}
        self.cnt = {e: 0 for e in self.ENG}
        self.seen = {e: {} for e in self.ENG}
        self.semh = {}
        self.out_events = []
        self.ninstr = 0
        self.dma_latest = {}

    def sb(self, name, shape, dt=F32):
        t = self.stack.enter_context(self.nc.sbuf_tensor(name, list(shape), dt))
        return Buf(t, name)

    def ps(self, name, shape, dt=F32):
        t = self.stack.enter_context(self.nc.psum_tensor(name, list(shape), dt))
        return Buf(t, name)

    def view(self, b, name=None):
        return Buf(b.t, name or (b.name + "_v"))

    def dram(self, name, shape, dt, kind):
        t = self.nc.dram_tensor(name, list(shape), dt, kind=kind)
        b = Buf(t, name)
        b.ap = t.ap()
        return b

    def sem(self, key):
        if key not in self.semh:
            nm = "s_" + "_".join(str(k) for k in (key if isinstance(key, tuple) else (key,)))
            self.semh[key] = self.stack.enter_context(self.nc.semaphore(nm))
        return self.semh[key]

    def push(self):
        self._saved = getattr(self, "_saved", [])
        self._saved.append(self.stack)
        self.stack = contextlib.ExitStack()

    def pop(self):
        self.barrier()
        self._deferred = getattr(self, "_deferred", [])
        self._deferred.append(self.stack)
        self.stack = self._saved.pop()

    def barrier(self):
        evs = {}
        for e in self.ENG:
            if self.cnt[e] > 0:
                evs[e] = self.cnt[e]
        for k, v in self.dma_latest.items():
            evs[k] = v
        for e in self.ENG:
            waits = []
            for k, v in evs.items():
                if k == e:
                    continue
                if self.seen[e].get(k, 0) >= v:
                    continue
                self.seen[e][k] = v
                waits.append((k, v))
            if waits:
                self.q[e].append((None, waits, None))

    def _deps(self, eng, reads, writes, own_key):
        deps = {}

        def add(k, v):
            if deps.get(k, 0) < v:
                deps[k] = v

        for b in reads:
            for k, v in b.w.items():
                add(k, v)
        for b in writes:
            for k, v in b.w.items():
                add(k, v)
            for k, v in b.r.items():
                add(k, v)
        waits = []
        for k, v in deps.items():
            if k == own_key and (k == "pe" or not SAME_ENGINE_SYNC or isinstance(k, tuple)):
                continue
            if self.seen[eng].get(k, 0) >= v:
                continue
            self.seen[eng][k] = v
            waits.append((k, v))
        return waits

    def _commit(self, reads, writes, key, val):
        for b in reads:
            if b.r.get(key, 0) < val:
                b.r[key] = val
        for b in writes:
            b.w = {key: val}
            b.r = {}

    def op(self, eng, fn, reads=(), writes=(), inc=True):
        waits = self._deps(eng, reads, writes, eng)
        if inc:
            self.cnt[eng] += 1
            val = self.cnt[eng]
        else:
            assert eng == "pe"
            val = self.cnt[eng] + 1
        self.q[eng].append((fn, waits, (eng, 1) if inc else None))
        self._commit(reads, writes, eng, val)
        self.ninstr += 1

    def dma(self, eng, out_ap, in_ap, reads=(), writes=(), is_output=False, **kw):
        prim = None
        for b in list(writes) + list(reads):
            if not hasattr(b, "ap"):
                prim = b
                break
        if prim is None:
            prim = (list(writes) + list(reads))[0]
        key = ("d", prim.id, eng)
        waits = self._deps(eng, reads, writes, key)
        prim.dcnt[key] = prim.dcnt.get(key, 0) + 16
        val = prim.dcnt[key]
        fn = lambda e, o=out_ap, i=in_ap, kw=kw: e.dma_start(out=o, in_=i, **kw)
        self.q[eng].append((fn, waits, (key, 16)))
        self._commit(reads, writes, key, val)
        self.dma_latest[key] = val
        if is_output:
            self.out_events.append((key, val))
        self.ninstr += 1

    def emit(self, final_eng="sp"):
        nc = self.nc
        fw = {}
        for k, v in self.out_events:
            fw[k] = max(fw.get(k, 0), v)
        final_waits = list(fw.items())
        for e in self.ENG:
            self.sem(e)
        for e in self.ENG:
            for fn, waits, inc in self.q[e]:
                for k, v in waits:
                    self.sem(k)
                if fn is None:
                    continue
                if inc:
                    self.sem(inc[0])
        for k, v in final_waits:
            self.sem(k)
        qs = self.q
        semh = self.semh

        def run(engobj, e):
            for fn, waits, inc in qs[e]:
                for k, v in waits:
                    engobj.wait_ge(semh[k], v)
                if fn is None:
                    continue
                ins = fn(engobj)
                if inc:
                    ins.then_inc(semh[inc[0]], inc[1])
            if e == final_eng:
                for k, v in final_waits:
                    engobj.wait_ge(semh[k], v)

        with nc.Block() as block:
            @block.tensor
            def _(eng):
                run(eng, "pe")

            @block.scalar
            def _(eng):
                run(eng, "act")

            @block.vector
            def _(eng):
                run(eng, "dve")

            @block.gpsimd
            def _(eng):
                run(eng, "pool")

            @block.sync
            def _(eng):
                run(eng, "sp")
        self.stack.close()


D = 1024
KT = 8
NTOK = 2176
IN_COLS = 2344
EPS = 1e-6
BLOCKS = [(0, 512), (512, 512), (1024, 512), (1536, 512), (2048, 128)]
NT_IN = [(0, 512), (512, 512), (1024, 512), (1536, 512), (2048, 296)]

_CACHE = {}


def _run(key, builder, in_maps):
    if key not in _CACHE:
        _CACHE[key] = builder()
    nc = _CACHE[key]
    res = run_bass_kernel_spmd(nc, in_maps, core_ids=list(range(8)))
    return res.results


def build_stage_a():
    nc = bass.Bass("TRN2", target_bir_lowering=False)
    P = Prog(nc)
    xT = P.dram("xT", [128, KT, NTOK], F32, "ExternalInput")
    cvec = P.dram("cvec", [128, KT, 2], F32, "ExternalInput")
    w_ada = P.dram("w_ada", [D, 6 * D], F32, "ExternalInput")
    b_adaT = P.dram("b_adaT", [128, 48], F32, "ExternalInput")
    norm1T = P.dram("norm1T", [128, KT], F32, "ExternalInput")
    w_in = P.dram("w_in", [D, IN_COLS], F32, "ExternalInput")
    p_out = P.dram("p", [NTOK, IN_COLS], F32, "ExternalOutput")
    mod_out = P.dram("modT", [128, 48, 2], F32, "ExternalOutput")

    ones_bf = P.sb("ones_bf", [128, 128], BF16)
    P.op("dve", lambda e: e.memset(ones_bf[:], 1.0), writes=[ones_bf])
    cv = P.sb("cv", [128, KT, 2])
    sc = P.sb("sc", [128, KT, 2])
    P.dma("sp", cv[:], cvec.ap[:, :, :], writes=[cv])
    P.op("act", lambda e: e.activation(out=sc[:], in_=cv[:], func=AF.Silu), reads=[cv], writes=[sc])
    bada = P.sb("bada", [128, 48])
    n1 = P.sb("n1", [128, KT])
    P.dma("sp", bada[:], b_adaT.ap[:, :], writes=[bada])
    P.dma("sp", n1[:], norm1T.ap[:, :], writes=[n1])

    win = [P.sb(f"win{kt}", [128, IN_COLS], BF16) for kt in range(KT)]
    for kt in range(KT):
        P.dma("pool", win[kt][:], w_in.ap[kt * 128:(kt + 1) * 128, :], writes=[win[kt]])

    modps = P.ps("modps", [128, 48, 2])
    wab = [P.sb(f"wab{i}", [128, KT, 512]) for i in range(2)]
    wv = w_ada.ap.rearrange("(kt p) n -> p kt n", p=128)
    for j in range(12):
        wb = wab[j % 2]
        P.dma("sp", wb[:], wv[:, :, j * 512:(j + 1) * 512], writes=[wb])
        for cl in range(4):
            c = j * 4 + cl
            for kt in range(KT):
                P.op("pe", lambda e, wb=wb, cl=cl, kt=kt, c=c: e.matmul(
                    modps[:, c, :], lhsT=wb[:, kt, cl * 128:(cl + 1) * 128], rhs=sc[:, kt, :],
                    start=(kt == 0), stop=(kt == KT - 1)), reads=[wb, sc], writes=[modps], inc=(kt == KT - 1))
    modT = P.sb("modT_sb", [128, 48, 2])
    for r in range(2):
        P.op("dve", lambda e, r=r: e.tensor_tensor(out=modT[:, :, r], in0=modps[:, :, r], in1=bada[:, :], op=ALU.add),
             reads=[modps, bada], writes=[modT])
    P.dma("sp", mod_out.ap[:, :, :], modT[:], reads=[modT], is_output=True)
    G = P.sb("G", [128, KT, 2])
    for r in range(2):
        P.op("dve", lambda e, r=r: e.scalar_tensor_tensor(out=G[:, :, r], in0=modT[:, 8:16, r], scalar=1.0, in1=n1[:, :],
                                                          op0=ALU.add, op1=ALU.mult), reads=[modT, n1], writes=[G])

    xb = [P.sb(f"xb{i}", [128, KT, 512]) for i in range(2)]
    sq = P.sb("sq", [128, KT, 512], BF16)
    ss_ps = P.ps("ss_ps", [128, 512])
    rs = P.sb("rs", [128, 512])
    rstd = P.sb("rstd", [128, 512])
    tmp = P.sb("tmp", [128, KT, 512])
    xn = [P.sb(f"xn{i}", [128, KT, 512], BF16) for i in range(2)]
    pp = [P.ps(f"pp{i}", [128, 512]) for i in range(3)]
    psb = [P.sb(f"psb{i}", [128, IN_COLS]) for i in range(2)]
    ppi = 0
    sti = 0
    for bi, (t0, T) in enumerate(BLOCKS):
        r = 1 if bi == 4 else 0
        x = xb[bi % 2]
        xnb = xn[bi % 2]
        P.dma("sp", x[:, :, 0:T], xT.ap[:, :, t0:t0 + T], writes=[x])
        P.op("act", lambda e, x=x, T=T: e.activation(out=sq[:, :, 0:T], in_=x[:, :, 0:T], func=AF.Square), reads=[x], writes=[sq])
        for kt in range(KT):
            P.op("pe", lambda e, kt=kt, T=T: e.matmul(ss_ps[:, 0:T], lhsT=ones_bf[:, :], rhs=sq[:, kt, 0:T],
                                                      start=(kt == 0), stop=(kt == KT - 1)),
                 reads=[ones_bf, sq], writes=[ss_ps], inc=(kt == KT - 1))
        P.op("act", lambda e, T=T: e.activation(out=rs[:, 0:T], in_=ss_ps[:, 0:T], func=AF.Sqrt, scale=1.0 / D, bias=EPS),
             reads=[ss_ps], writes=[rs])
        P.op("dve", lambda e, T=T: e.reciprocal(out=rstd[:, 0:T], in_=rs[:, 0:T]), reads=[rs], writes=[rstd])
        for kt in range(KT):
            P.op("dve", lambda e, x=x, kt=kt, T=T: e.tensor_tensor(out=tmp[:, kt, 0:T], in0=x[:, kt, 0:T], in1=rstd[:, 0:T], op=ALU.mult),
                 reads=[x, rstd], writes=[tmp])
        for kt in range(KT):
            P.op("act", lambda e, xnb=xnb, kt=kt, T=T, r=r: e.activation(
                out=xnb[:, kt, 0:T], in_=tmp[:, kt, 0:T], func=AF.Identity, scale=G[:, kt, r:r + 1], bias=modT[:, kt, r:r + 1]),
                reads=[tmp, G, modT], writes=[xnb])
        for s in range(T // 128):
            pb = psb[sti % 2]
            sti += 1
            for ni, (n0, nw) in enumerate(NT_IN):
                ps = pp[ppi % 3]
                ppi += 1
                for kt in range(KT):
                    P.op("pe", lambda e, ps=ps, xnb=xnb, kt=kt, s=s, n0=n0, nw=nw: e.matmul(
                        ps[:, 0:nw], lhsT=xnb[:, kt, s * 128:(s + 1) * 128], rhs=win[kt][:, n0:n0 + nw],
                        start=(kt == 0), stop=(kt == KT - 1)), reads=[xnb, win[kt]], writes=[ps], inc=(kt == KT - 1))
                if ni % 2 == 0:
                    P.op("act", lambda e, ps=ps, pb=pb, n0=n0, nw=nw: e.activation(out=pb[:, n0:n0 + nw], in_=ps[:, 0:nw], func=AF.Copy),
                         reads=[ps], writes=[pb])
                else:
                    P.op("dve", lambda e, ps=ps, pb=pb, n0=n0, nw=nw: e.tensor_copy(out=pb[:, n0:n0 + nw], in_=ps[:, 0:nw]),
                         reads=[ps], writes=[pb])
            tok = t0 + s * 128
            P.dma("sp", p_out.ap[tok:tok + 128, :], pb[:], reads=[pb], is_output=True)
    P.emit()
    return nc


def tok_to_feat(x_tok):
    T = x_tok.shape[0]
    return np.ascontiguousarray(x_tok.reshape(T, KT, 128).transpose(2, 1, 0))


def vecT(v, n=KT):
    return np.ascontiguousarray(v.reshape(n, 128).T)


def core_tokens(x_lat_b, x_ctx_b, q):
    pad = np.zeros((64, x_lat_b.shape[1]), x_lat_b.dtype)
    return np.concatenate([x_lat_b[q * 2048:(q + 1) * 2048], x_ctx_b[q * 64:(q + 1) * 64], pad], axis=0)


def stage_a_inputs(xT_cores, c, c_ctx, w_ada_i, b_ada_i, norm1_i, w_in_i):
    maps = []
    for core in range(8):
        b = core // 4
        cvec = np.stack([vecT(c[b]), vecT(c_ctx)], axis=-1)
        maps.append({"xT": xT_cores[core], "cvec": np.ascontiguousarray(cvec), "w_ada": w_ada_i,
                     "b_adaT": vecT(b_ada_i, 48), "norm1T": vecT(norm1_i), "w_in": w_in_i})
    return maps


TS = 8448
NTILE = 66
SBLK = [(0, 256)] + [(256 + 512 * i, 512) for i in range(16)]


def bview(P, ap, name):
    b = Buf(ap, name)
    return b


def build_stage_attn():
    nc = bass.Bass("TRN2", target_bir_lowering=False)
    P = Prog(nc)
    dr = {}
    for nm, shp in [("mq", [96, TS]), ("mckv", [128, TS]), ("mkr", [32, TS]), ("dq", [64, TS]), ("dk", [64, TS]),
                    ("dv", [128, NTILE, 64]), ("c96", [96, TS]), ("s96", [96, TS]), ("c64", [64, TS]), ("s64", [64, TS]),
                    ("ones96", [96, 96]), ("ones128", [128, 128]), ("bones64", [64, 64]), ("perm96", [96, 96]), ("perm64", [64, 64]),
                    ("wuk", [128, 64]), ("wuv", [128, 64]),
                    ("gq", [96, 1]), ("gk", [96, 1]), ("gkv", [128, 1]), ("gdq", [64, 1]), ("gdk", [64, 1]),
                    ("lamv", [128, 4, 32]), ("laminit", [128, 2]), ("gsub", [128, 64])]:
        dr[nm] = P.dram(nm, shp, F32, "ExternalInput")
    o_m = P.dram("o_m", [128, NTILE, 64], F32, "ExternalOutput")
    o_d = P.dram("o_d", [128, NTILE, 64], F32, "ExternalOutput")

    banks = [P.ps(f"bank{i}", [128, 512]) for i in range(8)]

    def cload(nm, shp, dt=BF16):
        t = P.sb(nm + "_sb", shp, dt)
        P.dma("pool", t[:], dr[nm].ap, writes=[t])
        return t
    ones96 = cload("ones96", [96, 96]); ones128 = cload("ones128", [128, 128]); bones64 = cload("bones64", [64, 64])
    perm96 = cload("perm96", [96, 96]); perm64 = cload("perm64", [64, 64])
    wuk = cload("wuk", [128, 64]); wuv = cload("wuv", [128, 64])
    gq = cload("gq", [96, 1], F32); gk = cload("gk", [96, 1], F32); gkv = cload("gkv", [128, 1], F32)
    gdq = cload("gdq", [64, 1], F32); gdk = cload("gdk", [64, 1], F32)
    lamv = cload("lamv", [128, 4, 32], F32); laminit = cload("laminit", [128, 2], F32); gsub = cload("gsub", [128, 64], F32)

    QTm = P.sb("QTm", [96, TS], BF16); KTm = P.sb("KTm", [96, TS], BF16); Vm = P.sb("Vm", [128, NTILE, 65], BF16)
    Q1p = P.sb("Q1p", [96, TS], BF16); Q2p = P.sb("Q2p", [96, TS], BF16); KTd = P.sb("KTd", [96, TS], BF16); Vd = P.sb("Vd", [128, NTILE, 65], BF16)
    QTd = (Q1p, Q2p)
    P.op("pool", lambda e: e.memset(Q1p[:], 0.0), writes=[Q1p])
    P.op("pool", lambda e: e.memset(Q2p[:], 0.0), writes=[Q2p])
    P.op("pool", lambda e: e.memset(KTd[:], 0.0), writes=[KTd])
    P.op("pool", lambda e: e.memset(Vm[:], 1.0), writes=[Vm])
    P.op("pool", lambda e: e.memset(Vd[:], 1.0), writes=[Vd])

    lprod = P.sb("lprod", [128, 2, 32]); lsum = P.sb("lsum", [128, 2]); lexp = P.sb("lexp", [128, 2]); lam = P.sb("lam", [128, 1]); nlam = P.sb("nlam", [128, 1])
    P.op("dve", lambda e: e.tensor_tensor(out=lprod[:, 0, :], in0=lamv[:, 0, :], in1=lamv[:, 1, :], op=ALU.mult), reads=[lamv], writes=[lprod])
    P.op("dve", lambda e: e.tensor_tensor(out=lprod[:, 1, :], in0=lamv[:, 2, :], in1=lamv[:, 3, :], op=ALU.mult), reads=[lamv], writes=[lprod])
    P.op("dve", lambda e: e.tensor_reduce(out=lsum[:, :], in_=lprod[:, :, :], axis=AX.X, op=ALU.add), reads=[lprod], writes=[lsum])
    P.op("act", lambda e: e.activation(out=lexp[:], in_=lsum[:], func=AF.Exp), reads=[lsum], writes=[lexp])
    P.op("dve", lambda e: e.tensor_tensor(out=lam[:], in0=lexp[:, 0:1], in1=lexp[:, 1:2], op=ALU.subtract), reads=[lexp], writes=[lam])
    P.op("dve", lambda e: e.tensor_tensor(out=lam[:], in0=lam[:], in1=laminit[:, 0:1], op=ALU.add), reads=[lam, laminit], writes=[lam])
    P.op("dve", lambda e: e.tensor_scalar(out=nlam[:], in0=lam[:], scalar1=-1.0, scalar2=None, op0=ALU.mult), reads=[lam], writes=[nlam])

    ss_ps = bview(P, banks[0][:, :], "ss_ps"); sw_ps = bview(P, banks[1][:, :], "sw_ps")
    kn_ps = bview(P, banks[2][:, :], "kn_ps"); v_ps = bview(P, banks[3][:, :], "v_ps")
    ss2_ps = bview(P, banks[4][:, :], "ss2_ps"); sw2_ps = bview(P, banks[5][:, :], "sw2_ps")
    src = [P.sb(f"src{i}", [128, 512]) for i in range(2)]
    sq_l = [P.sb(f"sq_a{i}", [128, 512], BF16) for i in range(2)]; rs_l = [P.sb(f"rs_a{i}", [128, 512]) for i in range(2)]
    rstd_l = [P.sb(f"rstd_a{i}", [128, 512]) for i in range(2)]
    xnrm_l = [P.sb(f"xnrm{i}", [128, 512]) for i in range(2)]; xg_l = [P.sb(f"xg{i}", [128, 512], BF16) for i in range(2)]
    cb_l = [P.sb(f"cb{i}", [128, 512]) for i in range(2)]; sbk_l = [P.sb(f"sbk{i}", [128, 512]) for i in range(2)]
    t1_l = [P.sb(f"t1{i}", [128, 512]) for i in range(2)]; t2_l = [P.sb(f"t2{i}", [128, 512]) for i in range(2)]
    ncall = [0]
    ckvn_l = [P.sb(f"ckvn{i}", [128, 512], BF16) for i in range(2)]; kfull_l = [P.sb(f"kfull{i}", [96, 512]) for i in range(2)]
    dvs = P.sb("dvs", [128, NTILE, 64])
    P.dma("sp", dvs[:], dr["dv"].ap, writes=[dvs])
    P.op("dve", lambda e: e.tensor_copy(out=Vd[:, :, 0:64], in_=dvs[:]), reads=[dvs], writes=[Vd])
    cnt = [0]

    def norm_rope(srcb, R, T, t0, ones_m, ngrp, gain, perm, cdr, sdr, dst, ssb, swb, rope=True):
        zz = ncall[0] % 2; ncall[0] += 1
        sq, rs, rstd, xnrm, xg, cb, sbk, t1, t2 = sq_l[zz], rs_l[zz], rstd_l[zz], xnrm_l[zz], xg_l[zz], cb_l[zz], sbk_l[zz], t1_l[zz], t2_l[zz]
        P.op("act", lambda e: e.activation(out=sq[0:R, 0:T], in_=srcb[0:R, 0:T], func=AF.Square), reads=[srcb], writes=[sq])
        P.op("pe", lambda e: e.matmul(ssb[0:R, 0:T], lhsT=ones_m[:, :], rhs=sq[0:R, 0:T], start=True, stop=True), reads=[ones_m, sq], writes=[ssb])
        P.op("act", lambda e: e.activation(out=rs[0:R, 0:T], in_=ssb[0:R, 0:T], func=AF.Sqrt, scale=1.0 / ngrp, bias=EPS), reads=[ssb], writes=[rs])
        P.op("dve", lambda e: e.reciprocal(out=rstd[0:R, 0:T], in_=rs[0:R, 0:T]), reads=[rs], writes=[rstd])
        P.op("dve", lambda e: e.tensor_tensor(out=xnrm[0:R, 0:T], in0=srcb[0:R, 0:T], in1=rstd[0:R, 0:T], op=ALU.mult), reads=[srcb, rstd], writes=[xnrm])
        if not rope:
            P.op("act", lambda e: e.activation(out=dst[0:R, t0:t0 + T], in_=xnrm[0:R, 0:T], func=AF.Identity, scale=gain[:, 0:1]), reads=[xnrm, gain], writes=[dst])
            return
        P.op("act", lambda e: e.activation(out=xg[0:R, 0:T], in_=xnrm[0:R, 0:T], func=AF.Identity, scale=gain[:, 0:1]), reads=[xnrm, gain], writes=[xg])
        P.op("pe", lambda e: e.matmul(swb[0:R, 0:T], lhsT=perm[:, :], rhs=xg[0:R, 0:T], start=True, stop=True), reads=[perm, xg], writes=[swb])
        P.dma("sp", cb[0:R, 0:T], cdr.ap[:, t0:t0 + T], writes=[cb])
        P.dma("sp", sbk[0:R, 0:T], sdr.ap[:, t0:t0 + T], writes=[sbk])
        P.op("dve", lambda e: e.tensor_tensor(out=t1[0:R, 0:T], in0=xg[0:R, 0:T], in1=cb[0:R, 0:T], op=ALU.mult), reads=[xg, cb], writes=[t1])
        P.op("dve", lambda e: e.tensor_tensor(out=t2[0:R, 0:T], in0=swb[0:R, 0:T], in1=sbk[0:R, 0:T], op=ALU.mult), reads=[swb, sbk], writes=[t2])
        if isinstance(dst, tuple):
            for (dd, pa, pb) in ((dst[0], 0, 32), (dst[1], 32, 64)):
                P.op("pool", lambda e, dd=dd, pa=pa, pb=pb: e.tensor_tensor(out=dd[pa:pb, t0:t0 + T], in0=t1[pa:pb, 0:T], in1=t2[pa:pb, 0:T], op=ALU.add),
                     reads=[t1, t2], writes=[dd])
        else:
            P.op("pool", lambda e: e.tensor_tensor(out=dst[0:R, t0:t0 + T], in0=t1[0:R, 0:T], in1=t2[0:R, 0:T], op=ALU.add), reads=[t1, t2], writes=[dst])

    for bi_, (t0, T) in enumerate(SBLK):
        ckvn = ckvn_l[bi_ % 2]; kfull = kfull_l[bi_ % 2]
        s0 = src[cnt[0] % 2]; cnt[0] += 1
        P.dma("sp", s0[0:96, 0:T], dr["mq"].ap[:, t0:t0 + T], writes=[s0])
        norm_rope(s0, 96, T, t0, ones96, 96, gq, perm96, dr["c96"], dr["s96"], QTm, ss_ps, sw_ps)
        s1 = src[cnt[0] % 2]; cnt[0] += 1
        P.dma("sp", s1[0:128, 0:T], dr["mckv"].ap[:, t0:t0 + T], writes=[s1])
        norm_rope(s1, 128, T, 0, ones128, 128, gkv, None, None, None, ckvn, ss2_ps, None, rope=False)
        P.op("pe", lambda e, T=T, ckvn=ckvn: e.matmul(kn_ps[0:64, 0:T], lhsT=wuk[:, :], rhs=ckvn[:, 0:T], start=True, stop=True), reads=[wuk, ckvn], writes=[kn_ps])
        P.op("act", lambda e, T=T, kfull=kfull: e.activation(out=kfull[0:64, 0:T], in_=kn_ps[0:64, 0:T], func=AF.Copy), reads=[kn_ps], writes=[kfull])
        P.dma("sp", kfull[64:96, 0:T], dr["mkr"].ap[:, t0:t0 + T], writes=[kfull])
        for s in range(T // 128):
            tile = t0 // 128 + s
            P.op("pe", lambda e, s=s, ckvn=ckvn: e.matmul(v_ps[:, s * 64:(s + 1) * 64], lhsT=ckvn[:, s * 128:(s + 1) * 128], rhs=wuv[:, :], start=True, stop=True),
                 reads=[ckvn, wuv], writes=[v_ps])
            P.op("dve", lambda e, s=s, tile=tile: e.tensor_copy(out=Vm[:, tile, 0:64], in_=v_ps[:, s * 64:(s + 1) * 64]), reads=[v_ps], writes=[Vm])
        norm_rope(kfull, 96, T, t0, ones96, 96, gk, perm96, dr["c96"], dr["s96"], KTm, ss_ps, sw_ps)
        s2 = src[cnt[0] % 2]; cnt[0] += 1
        P.dma("sp", s2[0:64, 0:T], dr["dq"].ap[:, t0:t0 + T], writes=[s2])
        norm_rope(s2, 64, T, t0, bones64, 32, gdq, perm64, dr["c64"], dr["s64"], QTd, ss2_ps, sw2_ps)
        s3 = src[cnt[0] % 2]; cnt[0] += 1
        P.dma("sp", s3[0:64, 0:T], dr["dk"].ap[:, t0:t0 + T], writes=[s3])
        norm_rope(s3, 64, T, t0, bones64, 32, gdk, perm64, dr["c64"], dr["s64"], KTd, ss_ps, sw_ps)
    P.barrier()

    def attention(kind):
        if kind == "m":
            sps = [bview(P, banks[i][:, :], f"sps_m{i}") for i in range(3)]
            po = [bview(P, banks[3][:, 0:260].rearrange("p (s c) -> p s c", c=65), "po_m")]
            QT, KT_, V, dk, scale, outd = [QTm], [KTm], Vm, 96, 96 ** -0.5, o_m
            QTs = [(QTm, 0, 96)]
        else:
            sps = [bview(P, banks[i][:, :], f"sps_d{i}") for i in range(4)]
            po = [bview(P, banks[4][:, 0:260].rearrange("p (s c) -> p s c", c=65), "po_d1"),
                  bview(P, banks[5][:, 0:260].rearrange("p (s c) -> p s c", c=65), "po_d2")]
            V, scale, outd = Vd, 32 ** -0.5, o_d
            QTs = [(Q1p, 0, 96), (Q2p, 0, 96)]
        KTs = [KTm] if kind == "m" else [KTd, KTd]
        npt = len(QTs)
        pts = [P.sb(f"pt_{kind}{i}", [128, 512], BF16) for i in range(2 * npt)]
        ob = [P.sb(f"ob_{kind}{i}", [128, 4, 64]) for i in range(2)]
        rec = P.sb(f"rec_{kind}", [128, 2, 4]); a_sb = P.sb(f"a_{kind}", [128, 4, 64])
        ssq = P.sb(f"ssq_{kind}", [128, 4]); junk = P.sb(f"junk_{kind}", [128, 64]); rr = P.sb(f"rr_{kind}", [128, 4])
        items = []
        for qb, (q0, Tq) in enumerate(SBLK):
            nk = 2 if qb == 0 else NTILE
            for kt in range(nk):
                for j in range(npt):
                    items.append((qb, q0, Tq, nk, kt, j))
        NI = len(items)
        LOOK = 2

        def emit_qk(n):
            qb, q0, Tq, nk, kt, j = items[n]
            QTb, r0, r1 = QTs[j]
            KTb = KTs[j]
            sp_ = sps[n % len(sps)]
            P.op("pe", lambda e, sp_=sp_, KTb=KTb, QTb=QTb, r0=r0, r1=r1, kt=kt, q0=q0, Tq=Tq: e.matmul(
                sp_[:, 0:Tq], lhsT=KTb[r0:r1, kt * 128:(kt + 1) * 128], rhs=QTb[r0:r1, q0:q0 + Tq], start=True, stop=True),
                reads=[KTb, QTb], writes=[sp_])

        def emit_exp(n):
            qb, q0, Tq, nk, kt, j = items[n]
            sp_ = sps[n % len(sps)]; pt = pts[n % len(pts)]
            P.op("act", lambda e, sp_=sp_, pt=pt, Tq=Tq: e.activation(out=pt[:, 0:Tq], in_=sp_[:, 0:Tq], func=AF.Exp, scale=scale),
                 reads=[sp_], writes=[pt])

        def emit_pv(n):
            qb, q0, Tq, nk, kt, j = items[n]
            pt = pts[n % len(pts)]
            nsub = Tq // 128
            for s in range(nsub):
                P.op("pe", lambda e, pt=pt, j=j, s=s, kt=kt, nk=nk: e.matmul(
                    po[j][:, s, :], lhsT=pt[:, s * 128:(s + 1) * 128], rhs=V[:, kt, :], start=(kt == 0 and s == 0), stop=(kt == nk - 1), skip_group_check=True),
                    reads=[pt, V], writes=[po[j]], inc=(s == nsub - 1))

        for n in range(min(LOOK, NI)):
            emit_qk(n)
        for n in range(NI):
            qb, q0, Tq, nk, kt, j = items[n]
            nsub = Tq // 128
            emit_exp(n)
            if n + LOOK < NI:
                emit_qk(n + LOOK)
            emit_pv(n)
            if not (kt == nk - 1 and j == npt - 1):
                continue
            o = ob[qb % 2]
            P.op("dve", lambda e: e.memset(ssq[:], 0.0), writes=[ssq])
            for j in range(npt):
                P.op("dve", lambda e, j=j, nsub=nsub: e.reciprocal(out=rec[:, j, 0:nsub], in_=po[j][:, 0:nsub, 64]), reads=[po[j]], writes=[rec])
            if kind == "m":
                for s in range(nsub):
                    P.op("dve", lambda e, s=s, o=o: e.tensor_scalar(out=o[:, s, :], in0=po[0][:, s, 0:64], scalar1=rec[:, 0, s:s + 1], scalar2=None, op0=ALU.mult),
                         reads=[po[0], rec], writes=[o])
            else:
                P.op("dve", lambda e, nsub=nsub: e.tensor_scalar(out=rec[:, 1, 0:nsub], in0=rec[:, 1, 0:nsub], scalar1=nlam[:, 0:1], scalar2=None, op0=ALU.mult),
                     reads=[rec, nlam], writes=[rec])
                for s in range(nsub):
                    P.op("dve", lambda e, s=s: e.tensor_scalar(out=a_sb[:, s, :], in0=po[0][:, s, 0:64], scalar1=rec[:, 0, s:s + 1], scalar2=None, op0=ALU.mult),
                         reads=[po[0], rec], writes=[a_sb])
                    P.op("dve", lambda e, s=s: e.scalar_tensor_tensor(out=a_sb[:, s, :], in0=po[1][:, s, 0:64], scalar=rec[:, 1, s:s + 1], in1=a_sb[:, s, :],
                                                                      op0=ALU.mult, op1=ALU.add), reads=[po[1], rec, a_sb], writes=[a_sb])
                    P.op("act", lambda e, s=s: e.activation(out=junk[:, :], in_=a_sb[:, s, :], func=AF.Square, accum_out=ssq[:, s:s + 1]),
                         reads=[a_sb], writes=[junk, ssq])
                P.op("act", lambda e, nsub=nsub: e.activation(out=rr[:, 0:nsub], in_=ssq[:, 0:nsub], func=AF.Sqrt, scale=1.0 / 64, bias=EPS), reads=[ssq], writes=[rr])
                P.op("dve", lambda e, nsub=nsub: e.reciprocal(out=rr[:, 0:nsub], in_=rr[:, 0:nsub]), reads=[rr], writes=[rr])
                P.op("dve", lambda e, nsub=nsub: e.tensor_scalar(out=rr[:, 0:nsub], in0=rr[:, 0:nsub], scalar1=laminit[:, 1:2], scalar2=None, op0=ALU.mult),
                     reads=[rr, laminit], writes=[rr])
                for s in range(nsub):
                    P.op("dve", lambda e, s=s, o=o: e.scalar_tensor_tensor(out=o[:, s, :], in0=a_sb[:, s, :], scalar=rr[:, s:s + 1], in1=gsub[:, :],
                                                                           op0=ALU.mult, op1=ALU.mult), reads=[a_sb, rr, gsub], writes=[o])
            tl = q0 // 128
            P.dma("sp", outd.ap[:, tl:tl + nsub, :], o[:, 0:nsub, :], reads=[o], is_output=True)

    attention("m")
    P.barrier()
    attention("d")
    P.emit()
    return nc


def rope_tables():
    inv = (10000.0 ** (-np.arange(8, dtype=np.float32) / 8.0)).astype(np.float32)
    t = np.arange(8192)
    rows = (t // 64).astype(np.float32); cols = (t % 64).astype(np.float32)
    ang = np.zeros((32, TS), np.float32)
    for a in range(16):
        ang[a, 256:] = rows * inv[a % 8]
        ang[16 + a, 256:] = cols * inv[a % 8]
    C = np.cos(ang).astype(np.float32); S = np.sin(ang).astype(np.float32)
    C[:, :256] = 1.0; S[:, :256] = 0.0
    return C, S


def perm_matrix(n, offsets):
    Pm = np.zeros((n, n), np.float32)
    for o in offsets:
        for a in range(8):
            Pm[o + a + 8, o + a] = -1.0
            Pm[o + a, o + a + 8] = 1.0
    return Pm


_CONST = {}


def attn_consts():
    if "attn" not in _CONST:
        C, S = rope_tables()
        c96 = np.concatenate([np.ones((64, TS), np.float32), C], 0); s96 = np.concatenate([np.zeros((64, TS), np.float32), S], 0)
        c64 = np.concatenate([C, C], 0); s64 = np.concatenate([S, S], 0)
        b64 = np.zeros((64, 64), np.float32); b64[:32, :32] = 1; b64[32:, 32:] = 1
        _CONST["attn"] = dict(c96=c96, s96=s96, c64=c64, s64=s64, ones96=np.ones((96, 96), np.float32), ones128=np.ones((128, 128), np.float32),
                              bones64=b64, perm96=perm_matrix(96, [64, 80]), perm64=perm_matrix(64, [0, 16, 32, 48]))
    return _CONST["attn"]


def rep(v, n=128):
    return np.ascontiguousarray(np.broadcast_to(np.asarray(v, np.float32).reshape(1, -1), (n, np.asarray(v).size)))


def col(v):
    return np.ascontiguousarray(np.asarray(v, np.float32).reshape(-1, 1))


def stage_attn_inputs(PS, inp, i):
    cst = attn_consts()
    lam_init = 0.8 - 0.6 * math.exp(-0.3 * i)
    maps = []
    for core in range(8):
        b, h = core // 4, core % 4
        p = PS[b]
        dof = 1576
        m = dict(cst)
        m["mq"] = np.ascontiguousarray(p[:, h * 96:(h + 1) * 96].T)
        m["mckv"] = np.ascontiguousarray(p[:, 384:512].T)
        m["mkr"] = np.ascontiguousarray(p[:, 512:544].T)
        m["dq"] = np.ascontiguousarray(p[:, dof + h * 64:dof + (h + 1) * 64].T)
        m["dk"] = np.ascontiguousarray(p[:, dof + 256 + h * 64:dof + 256 + (h + 1) * 64].T)
        m["dv"] = np.ascontiguousarray(p[:, dof + 512 + h * 64:dof + 512 + (h + 1) * 64].reshape(NTILE, 128, 64).transpose(1, 0, 2))
        m["wuk"] = np.ascontiguousarray(inp["mla_w_uk"][i][:, h * 64:(h + 1) * 64])
        m["wuv"] = np.ascontiguousarray(inp["mla_w_uv"][i][:, h * 64:(h + 1) * 64])
        m["gq"] = col(inp["mla_q_norm"][i]); m["gk"] = col(inp["mla_k_norm"][i]); m["gkv"] = col(inp["mla_kv_norm"][i])
        m["gdq"] = col(np.tile(inp["diff_q_norm"][i], 2)); m["gdk"] = col(np.tile(inp["diff_k_norm"][i], 2))
        lv = np.stack([inp["diff_lq1"][i], inp["diff_lk1"][i], inp["diff_lq2"][i], inp["diff_lk2"][i]], 0)
        m["lamv"] = np.ascontiguousarray(np.broadcast_to(lv[None], (128, 4, 32))).astype(np.float32)
        m["laminit"] = rep(np.array([lam_init, 1.0 - lam_init], np.float32))
        m["gsub"] = rep(inp["diff_subln"][i])
        maps.append(m)
    return maps


def untile(o):
    return np.ascontiguousarray(o.transpose(1, 0, 2).reshape(TS, o.shape[2]))


TPAD = TS + 3
ORDER_F = list(range(NTILE))
ORDER_B = [1, 0] + list(range(NTILE - 1, 1, -1))
NEG = -30000.0


def pcol(s):
    return 1 + s if s < 256 else 2 + s


def build_stage_ssd():
    nc = bass.Bass("TRN2", target_bir_lowering=False)
    P = Prog(nc)
    dr = {}
    for nm, shp in [("xp", [64, TPAD]), ("bp", [64, TPAD]), ("cp", [64, TPAD]), ("cw", [64, 3, 3]), ("cbias", [64, 3]),
                    ("z", [128, NTILE, 64]), ("dtr", [128, NTILE, 2]), ("dtb", [128, 2]), ("alog", [128, 2]), ("dsk", [128, 1]),
                    ("triu", [128, 128]), ("tril", [128, 128]), ("ntriu", [128, 128]), ("ntril", [128, 128]),
                    ("mneg_f", [128, 128]), ("mneg_b", [128, 128]), ("ident", [128, 128]), ("ones", [128, 128]),
                    ("slo", [128, 128]), ("sup", [128, 128])]:
        dr[nm] = P.dram(nm, shp, F32, "ExternalInput")
    o_s = P.dram("o_s", [128, NTILE, 64], F32, "ExternalOutput")
    banks = [P.ps(f"bank{i}", [128, 512]) for i in range(8)]

    def cload(nm, shp, dt=F32):
        t = P.sb(nm + "_sb", shp, dt)
        P.dma("pool", t[:], dr[nm].ap, writes=[t])
        return t
    cw = cload("cw", [64, 3, 3]); cbias = cload("cbias", [64, 3])
    dtr = cload("dtr", [128, NTILE, 2]); dtb = cload("dtb", [128, 2]); alog = cload("alog", [128, 2]); dsk = cload("dsk", [128, 1])
    triu = cload("triu", [128, 128]); tril = cload("tril", [128, 128]); ntriu = cload("ntriu", [128, 128]); ntril = cload("ntril", [128, 128])
    mneg_f = cload("mneg_f", [128, 128]); mneg_b = cload("mneg_b", [128, 128]); ident = cload("ident", [128, 128]); ones = cload("ones", [128, 128])
    slo = cload("slo", [128, 128]); sup = cload("sup", [128, 128])
    zs = cload("z", [128, NTILE, 64])
    P.op("act", lambda e: e.activation(out=zs[:], in_=zs[:], func=AF.Silu), reads=[zs], writes=[zs])

    BT = P.sb("BT", [64, TS], BF16); CT = P.sb("CT", [64, TS], BF16)
    x_tok = P.sb("x_tok", [128, NTILE, 64]); B_tok = P.sb("B_tok", [128, NTILE, 64]); y_acc = P.sb("y_acc", [128, NTILE, 64])
    dt = P.sb("dt", [128, NTILE, 2]); dta = P.sb("dta", [128, NTILE, 2]); aneg = P.sb("aneg", [128, 2])
    spx = P.sb("spx", [128, NTILE, 2]); spu = P.sb("spu", [128, NTILE, 2]); spw = P.sb("spw", [128, NTILE, 2])
    spw2 = P.sb("spw2", [128, NTILE, 2]); spq = P.sb("spq", [128, NTILE, 2])
    for d in range(2):
        P.op("dve", lambda e, d=d: e.tensor_scalar(out=spx[:, :, d], in0=dtr[:, :, d], scalar1=dtb[:, d:d + 1], scalar2=None, op0=ALU.add), reads=[dtr, dtb], writes=[spx])
    P.op("act", lambda e: e.activation(out=spu[:], in_=spx[:], func=AF.Abs), reads=[spx], writes=[spu])
    P.op("act", lambda e: e.activation(out=spu[:], in_=spu[:], func=AF.Exp, scale=-1.0), reads=[spu], writes=[spu])
    P.op("dve", lambda e: e.tensor_scalar(out=spw[:], in0=spu[:], scalar1=2.0, scalar2=None, op0=ALU.add), reads=[spu], writes=[spw])
    P.op("dve", lambda e: e.reciprocal(out=spw[:], in_=spw[:]), reads=[spw], writes=[spw])
    P.op("dve", lambda e: e.tensor_tensor(out=spw[:], in0=spw[:], in1=spu[:], op=ALU.mult), reads=[spw, spu], writes=[spw])
    P.op("dve", lambda e: e.tensor_tensor(out=spw2[:], in0=spw[:], in1=spw[:], op=ALU.mult), reads=[spw], writes=[spw2])
    P.op("dve", lambda e: e.tensor_scalar(out=spq[:], in0=spw2[:], scalar1=1.0 / 13.0, scalar2=None, op0=ALU.mult), reads=[spw2], writes=[spq])
    for cst_ in (1.0 / 11.0, 1.0 / 9.0, 1.0 / 7.0, 1.0 / 5.0, 1.0 / 3.0):
        P.op("dve", lambda e, cst_=cst_: e.scalar_tensor_tensor(out=spq[:], in0=spq[:], scalar=cst_, in1=spw2[:], op0=ALU.add, op1=ALU.mult), reads=[spq, spw2], writes=[spq])
    P.op("dve", lambda e: e.scalar_tensor_tensor(out=spq[:], in0=spq[:], scalar=1.0, in1=spw[:], op0=ALU.add, op1=ALU.mult), reads=[spq, spw], writes=[spq])
    P.op("dve", lambda e: e.tensor_scalar(out=spx[:], in0=spx[:], scalar1=0.0, scalar2=None, op0=ALU.max), reads=[spx], writes=[spx])
    P.op("dve", lambda e: e.scalar_tensor_tensor(out=dt[:], in0=spq[:], scalar=2.0, in1=spx[:], op0=ALU.mult, op1=ALU.add), reads=[spq, spx], writes=[dt])
    P.op("act", lambda e: e.activation(out=aneg[:], in_=alog[:], func=AF.Exp), reads=[alog], writes=[aneg])
    P.op("dve", lambda e: e.tensor_scalar(out=aneg[:], in0=aneg[:], scalar1=-1.0, scalar2=None, op0=ALU.mult), reads=[aneg], writes=[aneg])
    for d in range(2):
        P.op("dve", lambda e, d=d: e.tensor_scalar(out=dta[:, :, d], in0=dt[:, :, d], scalar1=aneg[:, d:d + 1], scalar2=None, op0=ALU.mult),
             reads=[dt, aneg], writes=[dta])

    tp_ps = [bview(P, banks[i][:, :], f"tp_ps{i}") for i in range(2)]
    xin = [P.sb(f"xin{i}", [64, 514]) for i in range(2)]
    acc = P.sb("cacc", [64, 512]); xc = P.sb("xc", [64, 512])
    segs = [(0, 256)] + [(256 + 512 * k, 512) for k in range(16)]
    ci = 0
    for (s0, T) in segs:
        c0 = pcol(s0) - 1
        for wi, (nm, dstT) in enumerate([("xp", None), ("bp", BT), ("cp", CT)]):
            xi = xin[ci % 2]; ci += 1
            P.dma("sp", xi[:, 0:T + 2], dr[nm].ap[:, c0:c0 + T + 2], writes=[xi])
            P.op("act", lambda e, xi=xi, T=T, wi=wi: e.activation(out=acc[:, 0:T], in_=xi[:, 1:T + 1], func=AF.Identity, scale=cw[:, wi, 1:2], bias=cbias[:, wi:wi + 1]),
                 reads=[xi, cw, cbias], writes=[acc])
            P.op("dve", lambda e, xi=xi, T=T, wi=wi: e.scalar_tensor_tensor(out=acc[:, 0:T], in0=xi[:, 0:T], scalar=cw[:, wi, 0:1], in1=acc[:, 0:T], op0=ALU.mult, op1=ALU.add),
                 reads=[xi, cw, acc], writes=[acc])
            P.op("dve", lambda e, xi=xi, T=T, wi=wi: e.scalar_tensor_tensor(out=acc[:, 0:T], in0=xi[:, 2:T + 2], scalar=cw[:, wi, 2:3], in1=acc[:, 0:T], op0=ALU.mult, op1=ALU.add),
                 reads=[xi, cw, acc], writes=[acc])
            P.op("act", lambda e, T=T: e.activation(out=xc[:, 0:T], in_=acc[:, 0:T], func=AF.Silu), reads=[acc], writes=[xc])
            if dstT is not None:
                P.op("pool", lambda e, dstT=dstT, s0=s0, T=T: e.tensor_copy(out=dstT[:, s0:s0 + T], in_=xc[:, 0:T]), reads=[xc], writes=[dstT])
            if nm in ("xp", "bp"):
                dtok = x_tok if nm == "xp" else B_tok
                tp = tp_ps[wi % 2]
                for j in range(T // 128):
                    P.op("pe", lambda e, tp=tp, j=j: e.transpose(tp[:, j * 64:(j + 1) * 64], xc[0:64, j * 128:(j + 1) * 128], ident[0:64, 0:64]),
                         reads=[xc, ident], writes=[tp])
                tl = s0 // 128
                nj = T // 128
                P.op("dve", lambda e, tp=tp, dtok=dtok, tl=tl, nj=nj: e.tensor_copy(out=dtok[:, tl:tl + nj, :], in_=tp[:, 0:nj * 64].rearrange("p (j c) -> p j c", c=64)),
                     reads=[tp], writes=[dtok])
    P.barrier()

    rowbc = [bview(P, banks[i][:, 0:128], f"rowbc{i}") for i in range(2)]
    colp = bview(P, banks[2][:, 0:2], "colp")
    STp = [bview(P, banks[3 + i][:, 0:128], f"STp{i}") for i in range(2)]
    ydp = bview(P, banks[5][:, 0:64], "ydp"); yop = bview(P, banks[6][:, 0:64], "yop"); stp = bview(P, banks[7][0:64, 0:64], "stp")
    dbc = [P.sb(f"dbc{i}", [128, 128]) for i in range(2)]
    cols = [P.sb(f"cols{i}", [128, 2]) for i in range(2)]
    Dm = [P.sb(f"Dm{i}", [128, 128]) for i in range(2)]
    STm = [P.sb(f"STm{i}", [128, 128], BF16) for i in range(2)]
    xs = [P.sb(f"xs{i}", [128, 64], BF16) for i in range(2)]
    ea = [P.sb(f"ea{i}", [128, 1]) for i in range(2)]
    yos = [P.sb(f"yos{i}", [128, 64]) for i in range(2)]
    Bdec = [P.sb(f"Bdec{i}", [128, 64], BF16) for i in range(2)]
    cd = [P.sb(f"cd{i}", [64, 1]) for i in range(2)]
    h = P.sb("h", [64, 64]); hbs = [P.sb(f"hb{i}", [64, 64], BF16) for i in range(2)]
    tfin = P.sb("tfin", [128, 64]); ofin = [P.sb(f"ofin{i}", [128, 64]) for i in range(2)]
    items = [(0, c) for c in ORDER_F] + [(1, c) for c in ORDER_B]
    NI = len(items)

    def bufs(n):
        k = n % 2
        return rowbc[k], STp[k], dbc[k], cols[k], Dm[k], STm[k], xs[k], ea[k], yos[k], Bdec[k], cd[k]

    def S1(n):
        d, c = items[n]
        tri, ntri, mneg, strict = (triu, ntriu, mneg_f, slo) if d == 0 else (tril, ntril, mneg_b, sup)
        rb, stq, db, cl, dm, sm, xsb, eab, yob, bd, cdb = bufs(n)
        ch = slice(c * 128, (c + 1) * 128)
        P.op("dve", lambda e, db=db, c=c, d=d, strict=strict: e.tensor_scalar(out=db[:], in0=strict[:], scalar1=dta[:, c, d:d + 1], scalar2=None, op0=ALU.mult),
             reads=[strict, dta], writes=[db])
        P.op("pe", lambda e, rb=rb, db=db, tri=tri: e.matmul(rb[:, :], lhsT=db[:, :], rhs=tri[:, :], start=True, stop=False), reads=[db, tri], writes=[rb], inc=False)
        P.op("pe", lambda e, rb=rb, mneg=mneg: e.matmul(rb[:, :], lhsT=ident[:, :], rhs=mneg[:, :], start=False, stop=True), reads=[ident, mneg], writes=[rb])
        P.op("pe", lambda e, ntri=ntri, c=c, d=d: e.matmul(colp[:, 0:1], lhsT=ntri[:, :], rhs=dta[:, c, d:d + 1], start=True, stop=True), reads=[ntri, dta], writes=[colp], inc=False)
        P.op("pe", lambda e, c=c, d=d: e.matmul(colp[:, 1:2], lhsT=ones[:, :], rhs=dta[:, c, d:d + 1], start=True, stop=True), reads=[ones, dta], writes=[colp])
        P.op("dve", lambda e, cl=cl: e.tensor_copy(out=cl[:], in_=colp[:, :]), reads=[colp], writes=[cl])
        P.op("act", lambda e, dm=dm, rb=rb: e.activation(out=dm[:], in_=rb[:, :], func=AF.Exp), reads=[rb], writes=[dm])
        P.op("pe", lambda e, stq=stq, ch=ch: e.matmul(stq[:, :], lhsT=BT[:, ch], rhs=CT[:, ch], start=True, stop=True), reads=[BT, CT], writes=[stq])
        P.op("pool", lambda e, xsb=xsb, c=c, d=d: e.tensor_scalar(out=xsb[:], in0=x_tok[:, c, :], scalar1=dt[:, c, d:d + 1], scalar2=None, op0=ALU.mult),
             reads=[x_tok, dt], writes=[xsb])
        P.op("act", lambda e, eab=eab, cl=cl: e.activation(out=eab[:], in_=cl[:, 0:1], func=AF.Exp, scale=-1.0), reads=[cl], writes=[eab])
        P.op("act", lambda e, cdb=cdb, cl=cl: e.activation(out=cdb[:], in_=cl[0:64, 1:2], func=AF.Exp), reads=[cl], writes=[cdb])

    def S2(n):
        d, c = items[n]
        dcol = 127 if d == 0 else 0
        rb, stq, db, cl, dm, sm, xsb, eab, yob, bd, cdb = bufs(n)
        ch = slice(c * 128, (c + 1) * 128)
        if n == 0 or n == NTILE:
            P.op("dve", lambda e: e.memset(h[:], 0.0), writes=[h])
            P.op("dve", lambda e, n=n: e.memset(hbs[(n - 1) % 2][:], 0.0), writes=[hbs[(n - 1) % 2]])
        hprev = hbs[(n - 1) % 2]; hnew = hbs[n % 2]
        P.op("dve", lambda e, sm=sm, stq=stq, dm=dm: e.tensor_tensor(out=sm[:], in0=stq[:, :], in1=dm[:], op=ALU.mult), reads=[stq, dm], writes=[sm])
        P.op("dve", lambda e, bd=bd, c=c, dm=dm, dcol=dcol: e.tensor_scalar(out=bd[:], in0=B_tok[:, c, :], scalar1=dm[:, dcol:dcol + 1], scalar2=None, op0=ALU.mult),
             reads=[B_tok, dm], writes=[bd])
        P.op("pe", lambda e, sm=sm, xsb=xsb: e.matmul(ydp[:, :], lhsT=sm[:, :], rhs=xsb[:, :], start=True, stop=True), reads=[sm, xsb], writes=[ydp])
        P.op("pe", lambda e, bd=bd, xsb=xsb: e.matmul(stp[:, :], lhsT=bd[:, :], rhs=xsb[:, :], start=True, stop=True), reads=[bd, xsb], writes=[stp])
        P.op("pe", lambda e, ch=ch, hprev=hprev: e.matmul(yop[:, :], lhsT=CT[:, ch], rhs=hprev[:, :], start=True, stop=True), reads=[CT, hprev], writes=[yop])
        P.op("dve", lambda e, cdb=cdb: e.scalar_tensor_tensor(out=h[:], in0=h[:], scalar=cdb[:, 0:1], in1=stp[:, :], op0=ALU.mult, op1=ALU.add),
             reads=[h, cdb, stp], writes=[h])
        P.op("act", lambda e, hnew=hnew: e.activation(out=hnew[:], in_=h[:], func=AF.Copy), reads=[h], writes=[hnew])
        P.op("act", lambda e, yob=yob, eab=eab: e.activation(out=yob[:], in_=yop[:, :], func=AF.Identity, scale=eab[:, 0:1]), reads=[yop, eab], writes=[yob])
        if d == 0:
            P.op("dve", lambda e, c=c, yob=yob: e.tensor_tensor(out=y_acc[:, c, :], in0=ydp[:, :], in1=yob[:], op=ALU.add), reads=[ydp, yob], writes=[y_acc])
        else:
            of = ofin[n % 2]
            P.op("dve", lambda e, yob=yob: e.tensor_tensor(out=tfin[:], in0=ydp[:, :], in1=yob[:], op=ALU.add), reads=[ydp, yob], writes=[tfin])
            P.op("dve", lambda e, c=c: e.tensor_tensor(out=tfin[:], in0=tfin[:], in1=y_acc[:, c, :], op=ALU.add), reads=[tfin, y_acc], writes=[tfin])
            P.op("dve", lambda e, c=c: e.scalar_tensor_tensor(out=tfin[:], in0=x_tok[:, c, :], scalar=dsk[:, 0:1], in1=tfin[:], op0=ALU.mult, op1=ALU.add),
                 reads=[x_tok, dsk, tfin], writes=[tfin])
            P.op("dve", lambda e, c=c, of=of: e.tensor_tensor(out=of[:], in0=tfin[:], in1=zs[:, c, :], op=ALU.mult), reads=[tfin, zs], writes=[of])
            P.dma("sp", o_s.ap[:, c, :], of[:], reads=[of], is_output=True)

    S1(0)
    for n in range(NI):
        if n + 1 < NI:
            S1(n + 1)
        S2(n)
    P.emit()
    return nc


def ssd_consts():
    if "ssd" not in _CONST:
        tu = np.triu(np.ones((128, 128), np.float32)); tl = np.tril(np.ones((128, 128), np.float32))
        _CONST["ssd"] = dict(triu=tu, tril=tl, ntriu=-tu, ntril=-tl, mneg_f=NEG * (1 - tu), mneg_b=NEG * (1 - tl),
                             ident=np.eye(128, dtype=np.float32), ones=np.ones((128, 128), np.float32),
                             slo=np.tril(np.ones((128, 128), np.float32), -1), sup=np.triu(np.ones((128, 128), np.float32), 1))
    return _CONST["ssd"]


def pad_stream(aT):
    C = aT.shape[0]
    z = np.zeros((C, 1), aT.dtype)
    return np.ascontiguousarray(np.concatenate([z, aT[:, :256], z, aT[:, 256:], z], axis=1))


def tile_tok(a):
    return np.ascontiguousarray(a.reshape(NTILE, 128, a.shape[1]).transpose(1, 0, 2))


def stage_ssd_inputs(PS, inp, i):
    cst = ssd_consts()
    maps = []
    so = 544
    for core in range(8):
        b, h = core // 4, core % 4
        g = h // 2
        p = PS[b]
        m = dict(cst)
        xcols = slice(so + 256 + h * 64, so + 256 + (h + 1) * 64)
        bcols = slice(so + 512 + g * 64, so + 512 + (g + 1) * 64)
        ccols = slice(so + 640 + g * 64, so + 640 + (g + 1) * 64)
        m["xp"] = pad_stream(p[:, xcols].T); m["bp"] = pad_stream(p[:, bcols].T); m["cp"] = pad_stream(p[:, ccols].T)
        cwf = inp["ssd_conv_w"][i]; cbf = inp["ssd_conv_b"][i]
        chx = slice(h * 64, (h + 1) * 64); chb = slice(256 + g * 64, 256 + (g + 1) * 64); chc = slice(384 + g * 64, 384 + (g + 1) * 64)
        m["cw"] = np.ascontiguousarray(np.stack([cwf[:, chx].T, cwf[:, chb].T, cwf[:, chc].T], axis=1)).astype(np.float32)
        m["cbias"] = np.ascontiguousarray(np.stack([cbf[chx], cbf[chb], cbf[chc]], axis=1)).astype(np.float32)
        m["z"] = tile_tok(p[:, so + h * 64: so + (h + 1) * 64])
        m["dtr"] = tile_tok(p[:, [so + 768 + h, so + 768 + 4 + h]])
        m["dtb"] = rep(inp["ssd_dt_bias"][i][:, h]); m["alog"] = rep(inp["ssd_a_log"][i][:, h]); m["dsk"] = rep(inp["ssd_d"][i][h:h + 1])
        maps.append(m)
    return maps


S5OFF = 1320


def build_stage_s5():
    nc = bass.Bass("TRN2", target_bir_lowering=False)
    P = Prog(nc)
    uT = P.dram("uT", [64, TS], F32, "ExternalInput")
    prm = P.dram("prm", [128, 4, 3], F32, "ExternalInput")
    bT = P.dram("bT", [2, 64, 128], F32, "ExternalInput")
    cT = P.dram("cT", [2, 4, 128, 64], F32, "ExternalInput")
    dskd = P.dram("dsk", [64, 1], F32, "ExternalInput")
    yT = P.dram("yT", [64, TS], F32, "ExternalOutput")

    u = P.sb("u_all", [64, TS]); y = P.sb("y_all", [64, TS])
    for (t0, L) in SBLK:
        P.dma("sp", u[:, t0:t0 + L], uT.ap[:, t0:t0 + L], writes=[u])
    pr = P.sb("pr", [128, 4, 3]); P.dma("sp", pr[:], prm.ap, writes=[pr])
    ub = P.sb("u_bf", [64, TS], BF16)
    for (t0, L) in SBLK:
        P.dma("pool", ub[:, t0:t0 + L], uT.ap[:, t0:t0 + L], writes=[ub])
    bre = P.sb("bre", [64, 128], BF16); bim = P.sb("bim", [64, 128], BF16)
    P.dma("pool", bre[:], bT.ap[0], writes=[bre]); P.dma("pool", bim[:], bT.ap[1], writes=[bim])
    cre32 = P.sb("cre32", [128, 4, 64]); cim32 = P.sb("cim32", [128, 4, 64])
    cre = P.sb("cre", [128, 4, 64], BF16); cim = P.sb("cim", [128, 4, 64], BF16); ncre = P.sb("ncre", [128, 4, 64], BF16); ncim = P.sb("ncim", [128, 4, 64], BF16)
    P.dma("sp", cre32[:], cT.ap[0].rearrange("q p m -> p q m"), writes=[cre32])
    P.dma("sp", cim32[:], cT.ap[1].rearrange("q p m -> p q m"), writes=[cim32])
    P.op("dve", lambda e: e.tensor_copy(out=cre[:], in_=cre32[:]), reads=[cre32], writes=[cre])
    P.op("dve", lambda e: e.tensor_copy(out=cim[:], in_=cim32[:]), reads=[cim32], writes=[cim])
    P.op("dve", lambda e: e.tensor_scalar(out=ncre[:], in0=cre32[:], scalar1=-1.0, scalar2=None, op0=ALU.mult), reads=[cre32], writes=[ncre])
    P.op("dve", lambda e: e.tensor_scalar(out=ncim[:], in0=cim32[:], scalar1=-1.0, scalar2=None, op0=ALU.mult), reads=[cim32], writes=[ncim])
    dsk = P.sb("dsk_sb", [64, 1]); P.dma("sp", dsk[:], dskd.ap, writes=[dsk])

    def T4(nm):
        return P.sb(nm, [128, 4])
    step = T4("step"); er = T4("er"); th = T4("th"); cc = T4("cc"); ss = T4("ss"); t1 = T4("t1"); t2 = T4("t2"); t3 = T4("t3")
    halfpi = P.sb("halfpi", [128, 1]); P.op("dve", lambda e: e.memset(halfpi[:], math.pi / 2), writes=[halfpi])
    P.op("act", lambda e: e.activation(out=step[:], in_=pr[:, :, 2], func=AF.Exp), reads=[pr], writes=[step])
    P.op("dve", lambda e: e.tensor_tensor(out=t1[:], in0=pr[:, :, 0], in1=step[:], op=ALU.mult), reads=[pr, step], writes=[t1])
    P.op("act", lambda e: e.activation(out=er[:], in_=t1[:], func=AF.Exp), reads=[t1], writes=[er])
    P.op("dve", lambda e: e.tensor_tensor(out=th[:], in0=pr[:, :, 1], in1=step[:], op=ALU.mult), reads=[pr, step], writes=[th])
    P.op("act", lambda e: e.activation(out=ss[:], in_=th[:], func=AF.Sin, scale=1.0 / 16), reads=[th], writes=[ss])
    P.op("act", lambda e: e.activation(out=cc[:], in_=th[:], func=AF.Sin, scale=1.0 / 16, bias=halfpi[:, 0:1]), reads=[th, halfpi], writes=[cc])

    def square(cin, sin_, cout, sout):
        P.op("dve", lambda e: e.tensor_tensor(out=t1[:], in0=cin[:], in1=cin[:], op=ALU.mult), reads=[cin], writes=[t1])
        P.op("dve", lambda e: e.tensor_tensor(out=t2[:], in0=sin_[:], in1=sin_[:], op=ALU.mult), reads=[sin_], writes=[t2])
        P.op("dve", lambda e: e.tensor_tensor(out=t3[:], in0=cin[:], in1=sin_[:], op=ALU.mult), reads=[cin, sin_], writes=[t3])
        P.op("dve", lambda e: e.tensor_tensor(out=cout[:], in0=t1[:], in1=t2[:], op=ALU.subtract), reads=[t1, t2], writes=[cout])
        P.op("dve", lambda e: e.tensor_scalar(out=sout[:], in0=t3[:], scalar1=2.0, scalar2=None, op0=ALU.mult), reads=[t3], writes=[sout])
    for _ in range(4):
        square(cc, ss, cc, ss)
    Mre = [cc]; Mim = [ss]
    for l in range(1, 10):
        a = T4(f"Mre{l}"); b = T4(f"Mim{l}")
        square(Mre[-1], Mim[-1], a, b)
        Mre.append(a); Mim.append(b)
    ar = T4("ar"); ai = T4("ai"); den = T4("den"); fr = T4("fr"); fi = T4("fi"); t4 = T4("t4")
    P.op("dve", lambda e: e.tensor_tensor(out=ar[:], in0=er[:], in1=Mre[0][:], op=ALU.mult), reads=[er, Mre[0]], writes=[ar])
    P.op("dve", lambda e: e.tensor_tensor(out=ai[:], in0=er[:], in1=Mim[0][:], op=ALU.mult), reads=[er, Mim[0]], writes=[ai])
    P.op("dve", lambda e: e.tensor_scalar(out=ar[:], in0=ar[:], scalar1=-1.0, scalar2=None, op0=ALU.add), reads=[ar], writes=[ar])
    P.op("dve", lambda e: e.tensor_tensor(out=t1[:], in0=pr[:, :, 0], in1=pr[:, :, 0], op=ALU.mult), reads=[pr], writes=[t1])
    P.op("dve", lambda e: e.tensor_tensor(out=t2[:], in0=pr[:, :, 1], in1=pr[:, :, 1], op=ALU.mult), reads=[pr], writes=[t2])
    P.op("dve", lambda e: e.tensor_tensor(out=den[:], in0=t1[:], in1=t2[:], op=ALU.add), reads=[t1, t2], writes=[den])
    P.op("dve", lambda e: e.reciprocal(out=den[:], in_=den[:]), reads=[den], writes=[den])
    P.op("dve", lambda e: e.tensor_tensor(out=t1[:], in0=ar[:], in1=pr[:, :, 0], op=ALU.mult), reads=[ar, pr], writes=[t1])
    P.op("dve", lambda e: e.tensor_tensor(out=t2[:], in0=ai[:], in1=pr[:, :, 1], op=ALU.mult), reads=[ai, pr], writes=[t2])
    P.op("dve", lambda e: e.tensor_tensor(out=t3[:], in0=t1[:], in1=t2[:], op=ALU.add), reads=[t1, t2], writes=[t3])
    P.op("dve", lambda e: e.tensor_tensor(out=fr[:], in0=t3[:], in1=den[:], op=ALU.mult), reads=[t3, den], writes=[fr])
    P.op("dve", lambda e: e.tensor_tensor(out=t1[:], in0=ai[:], in1=pr[:, :, 0], op=ALU.mult), reads=[ai, pr], writes=[t1])
    P.op("dve", lambda e: e.tensor_tensor(out=t2[:], in0=ar[:], in1=pr[:, :, 1], op=ALU.mult), reads=[ar, pr], writes=[t2])
    P.op("dve", lambda e: e.tensor_tensor(out=t4[:], in0=t1[:], in1=t2[:], op=ALU.subtract), reads=[t1, t2], writes=[t4])
    P.op("dve", lambda e: e.tensor_tensor(out=fi[:], in0=t4[:], in1=den[:], op=ALU.mult), reads=[t4, den], writes=[fi])

    Ere = [P.sb(f"Ere{q}", [128, 512]) for q in range(4)]; Eim = [P.sb(f"Eim{q}", [128, 512]) for q in range(4)]
    Wre = [P.sb(f"Wre{q}", [128, 512]) for q in range(4)]; Wim = [P.sb(f"Wim{q}", [128, 512]) for q in range(4)]
    tt = P.sb("tt", [128, 512])
    for q in range(4):
        d = q // 2
        P.op("dve", lambda e, q=q: e.memset(Ere[q][:, 0:1], 1.0), writes=[Ere[q]])
        P.op("dve", lambda e, q=q: e.memset(Eim[q][:, 0:1], 0.0), writes=[Eim[q]])
        for l in range(9):
            n = 1 << l
            mr = Mre[l]; mi = Mim[l]
            P.op("dve", lambda e, q=q, n=n, mi=mi: e.tensor_scalar(out=tt[:, 0:n], in0=Eim[q][:, 0:n], scalar1=mi[:, q:q + 1], scalar2=None, op0=ALU.mult),
                 reads=[Eim[q], mi], writes=[tt])
            P.op("dve", lambda e, q=q, n=n, mr=mr: e.scalar_tensor_tensor(out=Ere[q][:, n:2 * n], in0=Ere[q][:, 0:n], scalar=mr[:, q:q + 1], in1=tt[:, 0:n],
                                                                         op0=ALU.mult, op1=ALU.subtract), reads=[Ere[q], mr, tt], writes=[Ere[q]])
            P.op("dve", lambda e, q=q, n=n, mi=mi: e.tensor_scalar(out=tt[:, 0:n], in0=Ere[q][:, 0:n], scalar1=mi[:, q:q + 1], scalar2=None, op0=ALU.mult),
                 reads=[Ere[q], mi], writes=[tt])
            P.op("dve", lambda e, q=q, n=n, mr=mr: e.scalar_tensor_tensor(out=Eim[q][:, n:2 * n], in0=Eim[q][:, 0:n], scalar=mr[:, q:q + 1], in1=tt[:, 0:n],
                                                                         op0=ALU.mult, op1=ALU.add), reads=[Eim[q], mr, tt], writes=[Eim[q]])
        opa = ALU.add if d == 0 else ALU.subtract
        opb = ALU.subtract if d == 0 else ALU.add
        P.op("dve", lambda e, q=q: e.tensor_scalar(out=tt[:], in0=Eim[q][:], scalar1=fi[:, q:q + 1], scalar2=None, op0=ALU.mult), reads=[Eim[q], fi], writes=[tt])
        P.op("dve", lambda e, q=q, opa=opa: e.scalar_tensor_tensor(out=Wre[q][:], in0=Ere[q][:], scalar=fr[:, q:q + 1], in1=tt[:], op0=ALU.mult, op1=opa),
             reads=[Ere[q], fr, tt], writes=[Wre[q]])
        P.op("dve", lambda e, q=q: e.tensor_scalar(out=tt[:], in0=Eim[q][:], scalar1=fr[:, q:q + 1], scalar2=None, op0=ALU.mult), reads=[Eim[q], fr], writes=[tt])
        P.op("dve", lambda e, q=q, opb=opb: e.scalar_tensor_tensor(out=Wim[q][:], in0=Ere[q][:], scalar=fi[:, q:q + 1], in1=tt[:], op0=ALU.mult, op1=opb),
             reads=[Ere[q], fi, tt], writes=[Wim[q]])

    Aps = [P.ps(f"Aps{i}", [128, 512]) for i in range(2)]; Bps = [P.ps(f"Bps{i}", [128, 512]) for i in range(2)]
    yps = [P.ps(f"yps{i}", [64, 512]) for i in range(2)]

    def dbl(nm):
        return [P.sb(f"{nm}{i}", [128, 512]) for i in range(2)]
    p1 = dbl("p1"); p2 = dbl("p2"); p3 = dbl("p3"); p4 = dbl("p4"); bur = dbl("bur"); bui = dbl("bui"); sre = dbl("sre"); sim = dbl("sim")
    def dblb(nm):
        return [P.sb(f"{nm}{i}", [128, 512], BF16) for i in range(2)]
    m1 = dblb("m1"); m2 = dblb("m2"); m3 = dblb("m3"); m4 = dblb("m4")
    ini_re = [P.sb(f"ini_re{i}", [128, 1]) for i in range(2)]; ini_im = [P.sb(f"ini_im{i}", [128, 1]) for i in range(2)]
    ta = P.sb("ta", [128, 1])
    items = []
    for q in range(4):
        d = q // 2
        order = list(range(17)) if d == 0 else [0] + list(range(16, 0, -1))
        for ci, c in enumerate(order):
            items.append((q, ci, c, len(order)))
    NI = len(items)

    def S1(n):
        q, ci, c, no = items[n]
        d, k = q // 2, q % 2
        t0, L = SBLK[c]
        z = n % 2
        A = Aps[z]; Bp = Bps[z]
        P.op("pe", lambda e, A=A, k=k, t0=t0, L=L: e.matmul(A[:, 0:L], lhsT=bre[32 * k:32 * k + 32, :], rhs=ub[32 * k:32 * k + 32, t0:t0 + L], start=True, stop=True),
             reads=[bre, ub], writes=[A])
        P.op("pe", lambda e, Bp=Bp, k=k, t0=t0, L=L: e.matmul(Bp[:, 0:L], lhsT=bim[32 * k:32 * k + 32, :], rhs=ub[32 * k:32 * k + 32, t0:t0 + L], start=True, stop=True),
             reads=[bim, ub], writes=[Bp])
        wr_, wi_ = Wre[q], Wim[q]
        P.op("dve", lambda e, z=z, A=A, L=L, wr_=wr_: e.tensor_tensor(out=p1[z][:, 0:L], in0=wr_[:, 0:L], in1=A[:, 0:L], op=ALU.mult), reads=[wr_, A], writes=[p1[z]])
        P.op("dve", lambda e, z=z, Bp=Bp, L=L, wi_=wi_: e.tensor_tensor(out=p2[z][:, 0:L], in0=wi_[:, 0:L], in1=Bp[:, 0:L], op=ALU.mult), reads=[wi_, Bp], writes=[p2[z]])
        P.op("dve", lambda e, z=z, Bp=Bp, L=L, wr_=wr_: e.tensor_tensor(out=p3[z][:, 0:L], in0=wr_[:, 0:L], in1=Bp[:, 0:L], op=ALU.mult), reads=[wr_, Bp], writes=[p3[z]])
        P.op("dve", lambda e, z=z, A=A, L=L, wi_=wi_: e.tensor_tensor(out=p4[z][:, 0:L], in0=wi_[:, 0:L], in1=A[:, 0:L], op=ALU.mult), reads=[wi_, A], writes=[p4[z]])
        P.op("pool", lambda e, z=z, L=L: e.tensor_tensor(out=bur[z][:, 0:L], in0=p1[z][:, 0:L], in1=p2[z][:, 0:L], op=ALU.subtract), reads=[p1[z], p2[z]], writes=[bur[z]])
        P.op("pool", lambda e, z=z, L=L: e.tensor_tensor(out=bui[z][:, 0:L], in0=p3[z][:, 0:L], in1=p4[z][:, 0:L], op=ALU.add), reads=[p3[z], p4[z]], writes=[bui[z]])

    def S2(n):
        q, ci, c, no = items[n]
        d, k = q // 2, q % 2
        t0, L = SBLK[c]
        z = n % 2
        rq = er[:, q:q + 1]
        for (sx, bx, inix) in ((sre, bur, ini_re), (sim, bui, ini_im)):
            if ci == 0:
                init = 0.0; rd = [bx[z], er]
            else:
                init = inix[(ci - 1) % 2][:, 0:1]; rd = [bx[z], er, inix[(ci - 1) % 2]]
            if d == 0:
                P.op("dve", lambda e, sx=sx, bx=bx, z=z, L=L, init=init, rq=rq: e.tensor_tensor_scan(
                    out=sx[z][:, 0:L], data0=rq.to_broadcast([128, L]), data1=bx[z][:, 0:L], initial=init, op0=ALU.mult, op1=ALU.add), reads=rd, writes=[sx[z]])
            else:
                P.op("dve", lambda e, sx=sx, bx=bx, z=z, L=L, init=init, rq=rq: e.tensor_tensor_scan(
                    out=sx[z][:, 0:L][:, ::-1], data0=rq.to_broadcast([128, L]), data1=bx[z][:, 0:L][:, ::-1], initial=init, op0=ALU.mult, op1=ALU.add), reads=rd, writes=[sx[z]])
        if ci < no - 1:
            if d == 0:
                lvl = 8 if L == 256 else 9; col_ = L - 1
            else:
                lvl = 9; col_ = 0
            elr = Mre[lvl][:, q:q + 1]; eli = Mim[lvl][:, q:q + 1]
            w = ci % 2
            P.op("dve", lambda e, z=z, col_=col_, eli=eli: e.tensor_tensor(out=ta[:], in0=sim[z][:, col_:col_ + 1], in1=eli, op=ALU.mult), reads=[sim[z], Mim[lvl]], writes=[ta])
            P.op("dve", lambda e, z=z, col_=col_, elr=elr, w=w: e.scalar_tensor_tensor(out=ini_re[w][:], in0=sre[z][:, col_:col_ + 1], scalar=elr, in1=ta[:], op0=ALU.mult, op1=ALU.subtract),
                 reads=[sre[z], Mre[lvl], ta], writes=[ini_re[w]])
            P.op("dve", lambda e, z=z, col_=col_, eli=eli: e.tensor_tensor(out=ta[:], in0=sre[z][:, col_:col_ + 1], in1=eli, op=ALU.mult), reads=[sre[z], Mim[lvl]], writes=[ta])
            P.op("dve", lambda e, z=z, col_=col_, elr=elr, w=w: e.scalar_tensor_tensor(out=ini_im[w][:], in0=sim[z][:, col_:col_ + 1], scalar=elr, in1=ta[:], op0=ALU.mult, op1=ALU.add),
                 reads=[sim[z], Mre[lvl], ta], writes=[ini_im[w]])
        er_, ei_ = Ere[q], Eim[q]
        P.op("pool", lambda e, z=z, L=L, er_=er_: e.tensor_tensor(out=m1[z][:, 0:L], in0=er_[:, 0:L], in1=sre[z][:, 0:L], op=ALU.mult), reads=[er_, sre[z]], writes=[m1[z]])
        P.op("pool", lambda e, z=z, L=L, ei_=ei_: e.tensor_tensor(out=m2[z][:, 0:L], in0=ei_[:, 0:L], in1=sim[z][:, 0:L], op=ALU.mult), reads=[ei_, sim[z]], writes=[m2[z]])
        P.op("pool", lambda e, z=z, L=L, er_=er_: e.tensor_tensor(out=m3[z][:, 0:L], in0=er_[:, 0:L], in1=sim[z][:, 0:L], op=ALU.mult), reads=[er_, sim[z]], writes=[m3[z]])
        P.op("dve", lambda e, z=z, L=L, ei_=ei_: e.tensor_tensor(out=m4[z][:, 0:L], in0=ei_[:, 0:L], in1=sre[z][:, 0:L], op=ALU.mult), reads=[ei_, sre[z]], writes=[m4[z]])
        yp = yps[z]
        c2 = ncre if d == 0 else cre
        c4 = ncim if d == 0 else cim
        for mi_, (cm, mm) in enumerate(((cre, m1), (c2, m2), (ncim, m3), (c4, m4))):
            P.op("pe", lambda e, yp=yp, cm=cm, mm=mm, z=z, L=L, q=q, mi_=mi_: e.matmul(yp[:, 0:L], lhsT=cm[:, q, :], rhs=mm[z][:, 0:L], start=(mi_ == 0), stop=(mi_ == 3)),
                 reads=[cm, mm[z]], writes=[yp], inc=(mi_ == 3))
        if q == 0:
            P.op("act", lambda e, yp=yp, t0=t0, L=L: e.activation(out=y[:, t0:t0 + L], in_=yp[:, 0:L], func=AF.Copy), reads=[yp], writes=[y])
        else:
            P.op("dve", lambda e, yp=yp, t0=t0, L=L: e.tensor_tensor(out=y[:, t0:t0 + L], in0=yp[:, 0:L], in1=y[:, t0:t0 + L], op=ALU.add), reads=[yp, y], writes=[y])

    S1(0)
    for n in range(NI):
        if n + 1 < NI:
            S1(n + 1)
        S2(n)
    for (t0, L) in SBLK:
        P.op("dve", lambda e, t0=t0, L=L: e.scalar_tensor_tensor(out=y[:, t0:t0 + L], in0=u[:, t0:t0 + L], scalar=dsk[:, 0:1], in1=y[:, t0:t0 + L], op0=ALU.mult, op1=ALU.add),
             reads=[u, dsk, y], writes=[y])
    P.dma("sp", yT.ap, y[:], reads=[y], is_output=True)
    P.emit()
    return nc


def stage_s5_inputs(PS, inp, i):
    maps = []
    lre, lim, lst = inp["s5_lam_re"][i], inp["s5_lam_im"][i], inp["s5_log_step"][i]
    for core in range(8):
        b, gq = core // 4, core % 4
        m = {}
        m["uT"] = np.ascontiguousarray(PS[b][:, S5OFF + 64 * gq:S5OFF + 64 * gq + 64].T)
        prm = np.zeros((128, 4, 3), np.float32)
        bT = np.zeros((2, 64, 128), np.float32)
        cT = np.zeros((2, 4, 128, 64), np.float32)
        for k in range(2):
            for gl in range(2):
                g = 4 * gq + 2 * k + gl
                rows = slice(32 * k + 16 * gl, 32 * k + 16 * gl + 16)
                cols = slice(64 * gl, 64 * gl + 64)
                bT[0, rows, cols] = inp["s5_b_re"][i][g].T
                bT[1, rows, cols] = inp["s5_b_im"][i][g].T
                for d in range(2):
                    q = 2 * d + k
                    prm[cols, q, 0] = lre[d, g]; prm[cols, q, 1] = lim[d, g]; prm[cols, q, 2] = lst[d, g]
                    cT[0, q, cols, rows] = inp["s5_c_re"][i][d, g].T
                    cT[1, q, cols, rows] = inp["s5_c_im"][i][d, g].T
        m["prm"] = prm; m["bT"] = bT; m["cT"] = cT
        m["dsk"] = col(inp["s5_d"][i][64 * gq:64 * gq + 64])
        maps.append(m)
    return maps


def build_stage_c1():
    nc = bass.Bass("TRN2", target_bir_lowering=False)
    P = Prog(nc)
    catT = P.dram("catT", [128, KT, NTOK], F32, "ExternalInput")
    xT = P.dram("xT", [128, KT, NTOK], F32, "ExternalInput")
    modTd = P.dram("modT", [128, 48, 2], F32, "ExternalInput")
    w_out = P.dram("w_out", [D, D], F32, "ExternalInput")
    w_glu = P.dram("w_glu", [256, 256], F32, "ExternalInput")
    bgluT = P.dram("bgluT", [128, 2], F32, "ExternalInput")
    gssdT = P.dram("gssdT", [128, 2], F32, "ExternalInput")
    norm2T = P.dram("norm2T", [128, KT], F32, "ExternalInput")
    wr_d = P.dram("wr", [D, 36], F32, "ExternalInput")
    br_d = P.dram("br", [128, 36], F32, "ExternalInput")
    ident_d = P.dram("ident", [128, 128], F32, "ExternalInput")
    xmid = P.dram("xmid", [128, KT, NTOK], F32, "ExternalOutput")
    hT = P.dram("hT", [128, KT, NTOK], F32, "ExternalOutput")
    gatesT = P.dram("gatesT", [32, NTOK], F32, "ExternalOutput")

    ones_bf = P.sb("ones_bf", [128, 128], BF16)
    P.op("dve", lambda e: e.memset(ones_bf[:], 1.0), writes=[ones_bf])
    modT = P.sb("modT_sb", [128, 48, 2]); P.dma("sp", modT[:], modTd.ap, writes=[modT])
    bglu = P.sb("bglu", [128, 2]); P.dma("sp", bglu[:], bgluT.ap, writes=[bglu])
    gssd = P.sb("gssd", [128, 2]); P.dma("sp", gssd[:], gssdT.ap, writes=[gssd])
    n2 = P.sb("n2", [128, KT]); P.dma("sp", n2[:], norm2T.ap, writes=[n2])
    wr = P.sb("wr_sb", [128, KT, 36]); P.dma("sp", wr[:], wr_d.ap.rearrange("(kt p) n -> p kt n", p=128), writes=[wr])
    br = P.sb("br_sb", [128, 36]); P.dma("sp", br[:], br_d.ap, writes=[br])
    ident = P.sb("ident_sb", [128, 128]); P.dma("sp", ident[:], ident_d.ap, writes=[ident])
    wout = P.sb("wout_bf", [128, KT, D], BF16)
    P.dma("pool", wout[:], w_out.ap.rearrange("(kt p) n -> p kt n", p=128), writes=[wout])
    wglu = P.sb("wglu_bf", [128, 2, 256], BF16)
    P.dma("pool", wglu[:], w_glu.ap.rearrange("(kt p) n -> p kt n", p=128), writes=[wglu])
    G2 = P.sb("G2", [128, KT, 2])
    for r in range(2):
        P.op("dve", lambda e, r=r: e.scalar_tensor_tensor(out=G2[:, :, r], in0=modT[:, 32:40, r], scalar=1.0, in1=n2[:, :], op0=ALU.add, op1=ALU.mult),
             reads=[modT, n2], writes=[G2])

    cat = P.sb("cat", [128, KT, 512]); xm = P.sb("xm", [128, KT, 512]); tmp = P.sb("tmp", [128, KT, 512]); hh = P.sb("hh", [128, KT, 512])
    sq = P.sb("sq", [128, KT, 512], BF16); catb = P.sb("catb", [128, KT, 512], BF16)
    gg = P.sb("gg", [128, 2, 512]); ggb = P.sb("ggb", [128, 2, 512], BF16); sig = P.sb("sig", [128, 512])
    rs = P.sb("rs", [128, 512]); rstd = P.sb("rstd", [128, 512])
    gT = P.sb("gT", [32, NTOK])
    ss_ps = P.ps("ss_ps", [128, 512]); lin_ps = P.ps("lin_ps", [128, 512])
    mix_ps = [P.ps(f"mix_ps{i}", [128, 512]) for i in range(2)]
    lg_ps = [P.ps(f"lg_ps{i}", [128, 36]) for i in range(2)]
    tr_ps = P.ps("tr_ps", [32, 128])
    Lg = P.sb("Lg", [128, 36]); gmax = P.sb("gmax", [128, 1]); ngmax = P.sb("ngmax", [128, 1]); ghot = P.sb("ghot", [128, 4]); eg = P.sb("eg", [128, 4])
    sume = P.sb("sume", [128, 1]); pg = P.sb("pg", [128, 1]); les = P.sb("les", [128, 8]); m1 = P.sb("m1", [128, 1]); hot1 = P.sb("hot1", [128, 8])
    le2 = P.sb("le2", [128, 8]); m2 = P.sb("m2", [128, 1]); hot2 = P.sb("hot2", [128, 8]); d21 = P.sb("d21", [128, 1]); e21 = P.sb("e21", [128, 1])
    w1 = P.sb("w1", [128, 1]); w2 = P.sb("w2", [128, 1]); inner = P.sb("inner", [128, 8]); gates = P.sb("gates", [128, 32])
    mi = 0
    for bi, (t0, T) in enumerate(BLOCKS):
        r = 1 if bi == 4 else 0
        P.dma("sp", cat[:, :, 0:T], catT.ap[:, :, t0:t0 + T], writes=[cat])
        P.dma("sp", xm[:, :, 0:T], xT.ap[:, :, t0:t0 + T], writes=[xm])
        P.op("act", lambda e, T=T: e.activation(out=sq[:, 2:4, 0:T], in_=cat[:, 2:4, 0:T], func=AF.Square), reads=[cat], writes=[sq])
        for j, kt in enumerate((2, 3)):
            P.op("pe", lambda e, kt=kt, j=j, T=T: e.matmul(ss_ps[:, 0:T], lhsT=ones_bf[:, :], rhs=sq[:, kt, 0:T], start=(j == 0), stop=(j == 1)),
                 reads=[ones_bf, sq], writes=[ss_ps], inc=(j == 1))
        P.op("act", lambda e, T=T: e.activation(out=rs[:, 0:T], in_=ss_ps[:, 0:T], func=AF.Sqrt, scale=1.0 / 256, bias=EPS), reads=[ss_ps], writes=[rs])
        P.op("dve", lambda e, T=T: e.reciprocal(out=rstd[:, 0:T], in_=rs[:, 0:T]), reads=[rs], writes=[rstd])
        for j, kt in enumerate((2, 3)):
            P.op("dve", lambda e, kt=kt, T=T: e.tensor_tensor(out=tmp[:, kt, 0:T], in0=cat[:, kt, 0:T], in1=rstd[:, 0:T], op=ALU.mult), reads=[cat, rstd], writes=[tmp])
            P.op("act", lambda e, kt=kt, j=j, T=T: e.activation(out=catb[:, kt, 0:T], in_=tmp[:, kt, 0:T], func=AF.Identity, scale=gssd[:, j:j + 1]),
                 reads=[tmp, gssd], writes=[catb])
        P.op("act", lambda e, T=T: e.activation(out=gg[:, :, 0:T], in_=cat[:, 4:6, 0:T], func=AF.Gelu_apprx_tanh), reads=[cat], writes=[gg])
        P.op("pool", lambda e, T=T: e.tensor_copy(out=ggb[:, :, 0:T], in_=gg[:, :, 0:T]), reads=[gg], writes=[ggb])
        for j in range(2):
            for kt in range(2):
                P.op("pe", lambda e, j=j, kt=kt, T=T: e.matmul(lin_ps[:, 0:T], lhsT=wglu[:, kt, j * 128:(j + 1) * 128], rhs=ggb[:, kt, 0:T], start=(kt == 0), stop=(kt == 1)),
                     reads=[wglu, ggb], writes=[lin_ps], inc=(kt == 1))
            P.op("act", lambda e, j=j, T=T: e.activation(out=sig[:, 0:T], in_=lin_ps[:, 0:T], func=AF.Sigmoid, bias=bglu[:, j:j + 1]), reads=[lin_ps, bglu], writes=[sig])
            P.op("dve", lambda e, j=j, T=T: e.tensor_tensor(out=catb[:, 4 + j, 0:T], in0=gg[:, j, 0:T], in1=sig[:, 0:T], op=ALU.mult), reads=[gg, sig], writes=[catb])
        P.op("pool", lambda e, T=T: e.tensor_copy(out=catb[:, 0:2, 0:T], in_=cat[:, 0:2, 0:T]), reads=[cat], writes=[catb])
        P.op("pool", lambda e, T=T: e.tensor_copy(out=catb[:, 6:8, 0:T], in_=cat[:, 6:8, 0:T]), reads=[cat], writes=[catb])
        for nt in range(KT):
            mp = mix_ps[mi % 2]; mi += 1
            for kt in range(KT):
                P.op("pe", lambda e, mp=mp, nt=nt, kt=kt, T=T: e.matmul(mp[:, 0:T], lhsT=wout[:, kt, nt * 128:(nt + 1) * 128], rhs=catb[:, kt, 0:T], start=(kt == 0), stop=(kt == KT - 1)),
                     reads=[wout, catb], writes=[mp], inc=(kt == KT - 1))
            P.op("dve", lambda e, mp=mp, nt=nt, T=T, r=r: e.scalar_tensor_tensor(out=xm[:, nt, 0:T], in0=mp[:, 0:T], scalar=modT[:, 16 + nt, r:r + 1], in1=xm[:, nt, 0:T],
                                                                             op0=ALU.mult, op1=ALU.add), reads=[mp, modT, xm], writes=[xm])
        P.dma("sp", xmid.ap[:, :, t0:t0 + T], xm[:, :, 0:T], reads=[xm], is_output=True)
        P.op("act", lambda e, T=T: e.activation(out=sq[:, :, 0:T], in_=xm[:, :, 0:T], func=AF.Square), reads=[xm], writes=[sq])
        for kt in range(KT):
            P.op("pe", lambda e, kt=kt, T=T: e.matmul(ss_ps[:, 0:T], lhsT=ones_bf[:, :], rhs=sq[:, kt, 0:T], start=(kt == 0), stop=(kt == KT - 1)),
                 reads=[ones_bf, sq], writes=[ss_ps], inc=(kt == KT - 1))
        P.op("act", lambda e, T=T: e.activation(out=rs[:, 0:T], in_=ss_ps[:, 0:T], func=AF.Sqrt, scale=1.0 / D, bias=EPS), reads=[ss_ps], writes=[rs])
        P.op("dve", lambda e, T=T: e.reciprocal(out=rstd[:, 0:T], in_=rs[:, 0:T]), reads=[rs], writes=[rstd])
        for kt in range(KT):
            P.op("dve", lambda e, kt=kt, T=T: e.tensor_tensor(out=tmp[:, kt, 0:T], in0=xm[:, kt, 0:T], in1=rstd[:, 0:T], op=ALU.mult), reads=[xm, rstd], writes=[tmp])
            P.op("act", lambda e, kt=kt, T=T, r=r: e.activation(out=hh[:, kt, 0:T], in_=tmp[:, kt, 0:T], func=AF.Identity, scale=G2[:, kt, r:r + 1], bias=modT[:, 24 + kt, r:r + 1]),
                 reads=[tmp, G2, modT], writes=[hh])
        P.dma("sp", hT.ap[:, :, t0:t0 + T], hh[:, :, 0:T], reads=[hh], is_output=True)
        for s in range(T // 128):
            lp = lg_ps[s % 2]
            for kt in range(KT):
                P.op("pe", lambda e, lp=lp, kt=kt, s=s: e.matmul(lp[:, :], lhsT=hh[:, kt, s * 128:(s + 1) * 128], rhs=wr[:, kt, :], start=(kt == 0), stop=(kt == KT - 1)),
                     reads=[hh, wr], writes=[lp], inc=(kt == KT - 1))
            P.op("dve", lambda e, lp=lp: e.tensor_tensor(out=Lg[:], in0=lp[:, :], in1=br[:], op=ALU.add), reads=[lp, br], writes=[Lg])
            P.op("dve", lambda e: e.tensor_reduce(out=gmax[:], in_=Lg[:, 0:4], axis=AX.X, op=ALU.max), reads=[Lg], writes=[gmax])
            P.op("dve", lambda e: e.tensor_scalar(out=ghot[:], in0=Lg[:, 0:4], scalar1=gmax[:, 0:1], scalar2=None, op0=ALU.is_ge), reads=[Lg, gmax], writes=[ghot])
            P.op("dve", lambda e: e.tensor_scalar(out=ngmax[:], in0=gmax[:], scalar1=-1.0, scalar2=None, op0=ALU.mult), reads=[gmax], writes=[ngmax])
            P.op("act", lambda e: e.activation(out=eg[:], in_=Lg[:, 0:4], func=AF.Exp, bias=ngmax[:, 0:1]), reads=[Lg, ngmax], writes=[eg])
            P.op("dve", lambda e: e.tensor_reduce(out=sume[:], in_=eg[:], axis=AX.X, op=ALU.add), reads=[eg], writes=[sume])
            P.op("dve", lambda e: e.reciprocal(out=pg[:], in_=sume[:]), reads=[sume], writes=[pg])
            P.op("dve", lambda e: e.tensor_scalar(out=les[:], in0=Lg[:, 4:12], scalar1=ghot[:, 0:1], scalar2=None, op0=ALU.mult), reads=[Lg, ghot], writes=[les])
            for g in range(1, 4):
                P.op("dve", lambda e, g=g: e.scalar_tensor_tensor(out=les[:], in0=Lg[:, 4 + 8 * g:12 + 8 * g], scalar=ghot[:, g:g + 1], in1=les[:], op0=ALU.mult, op1=ALU.add),
                     reads=[Lg, ghot, les], writes=[les])
            P.op("dve", lambda e: e.tensor_reduce(out=m1[:], in_=les[:], axis=AX.X, op=ALU.max), reads=[les], writes=[m1])
            P.op("dve", lambda e: e.tensor_scalar(out=hot1[:], in0=les[:], scalar1=m1[:, 0:1], scalar2=None, op0=ALU.is_ge), reads=[les, m1], writes=[hot1])
            P.op("dve", lambda e: e.scalar_tensor_tensor(out=le2[:], in0=hot1[:], scalar=-1e30, in1=les[:], op0=ALU.mult, op1=ALU.add), reads=[hot1, les], writes=[le2])
            P.op("dve", lambda e: e.tensor_reduce(out=m2[:], in_=le2[:], axis=AX.X, op=ALU.max), reads=[le2], writes=[m2])
            P.op("dve", lambda e: e.tensor_scalar(out=hot2[:], in0=le2[:], scalar1=m2[:, 0:1], scalar2=None, op0=ALU.is_ge), reads=[le2, m2], writes=[hot2])
            P.op("dve", lambda e: e.tensor_tensor(out=d21[:], in0=m2[:], in1=m1[:], op=ALU.subtract), reads=[m1, m2], writes=[d21])
            P.op("act", lambda e: e.activation(out=e21[:], in_=d21[:], func=AF.Exp), reads=[d21], writes=[e21])
            P.op("dve", lambda e: e.tensor_scalar(out=w1[:], in0=e21[:], scalar1=1.0, scalar2=None, op0=ALU.add), reads=[e21], writes=[w1])
            P.op("dve", lambda e: e.reciprocal(out=w1[:], in_=w1[:]), reads=[w1], writes=[w1])
            P.op("dve", lambda e: e.tensor_tensor(out=w1[:], in0=w1[:], in1=pg[:], op=ALU.mult), reads=[w1, pg], writes=[w1])
            P.op("dve", lambda e: e.tensor_tensor(out=w2[:], in0=w1[:], in1=e21[:], op=ALU.mult), reads=[w1, e21], writes=[w2])
            P.op("dve", lambda e: e.tensor_scalar(out=inner[:], in0=hot1[:], scalar1=w1[:, 0:1], scalar2=None, op0=ALU.mult), reads=[hot1, w1], writes=[inner])
            P.op("dve", lambda e: e.scalar_tensor_tensor(out=inner[:], in0=hot2[:], scalar=w2[:, 0:1], in1=inner[:], op0=ALU.mult, op1=ALU.add), reads=[hot2, w2, inner], writes=[inner])
            for g in range(4):
                P.op("dve", lambda e, g=g: e.tensor_scalar(out=gates[:, 8 * g:8 * g + 8], in0=inner[:], scalar1=ghot[:, g:g + 1], scalar2=None, op0=ALU.mult),
                     reads=[inner, ghot], writes=[gates])
            P.op("pe", lambda e: e.transpose(tr_ps[:, :], gates[:, :], ident[:, :]), reads=[gates, ident], writes=[tr_ps])
            tk = t0 + s * 128
            P.op("act", lambda e, tk=tk: e.activation(out=gT[:, tk:tk + 128], in_=tr_ps[:, :], func=AF.Copy), reads=[tr_ps], writes=[gT])
    P.dma("sp", gatesT.ap, gT[:], reads=[gT], is_output=True)
    P.emit()
    return nc


def build_stage_c2():
    nc = bass.Bass("TRN2", target_bir_lowering=False)
    P = Prog(nc)
    hTd = P.dram("hT", [128, KT, NTOK], F32, "ExternalInput")
    xmid = P.dram("xmid", [128, KT, NTOK], F32, "ExternalInput")
    gatesT = P.dram("gatesT", [32, NTOK], F32, "ExternalInput")
    modTd = P.dram("modT", [128, 48, 2], F32, "ExternalInput")
    seld = P.dram("sel", [32, 32, 128], F32, "ExternalInput")
    w_gate = P.dram("w_gate", [32, D, 256], F32, "ExternalInput")
    w_up = P.dram("w_up", [32, D, 256], F32, "ExternalInput")
    w_down = P.dram("w_down", [32, 256, D], F32, "ExternalInput")
    xout = P.dram("xout", [128, KT, NTOK], F32, "ExternalOutput")

    hb = P.sb("hb", [128, KT, NTOK], BF16)
    xa = P.sb("xa", [128, KT, NTOK])
    for kt in range(KT):
        P.dma("pool", hb[:, kt, :], hTd.ap[:, kt, :], writes=[hb])
    for kt in range(KT):
        P.dma("sp", xa[:, kt, :], xmid.ap[:, kt, :], writes=[xa])
    gT = P.sb("gT", [128, NTOK]); P.op("dve", lambda e: e.memset(gT[:], 0.0), writes=[gT])
    P.dma("sp", gT[0:32, :], gatesT.ap, writes=[gT])
    modT = P.sb("modT_sb", [128, 48, 2]); P.dma("sp", modT[:], modTd.ap, writes=[modT])
    sel = P.sb("sel_sb", [128, 32, 128]); P.op("dve", lambda e: e.memset(sel[:], 0.0), writes=[sel])
    P.dma("sp", sel[0:32, :, :], seld.ap, writes=[sel])
    wg = [P.sb(f"wg{i}", [128, KT, 256], BF16) for i in range(2)]
    wu = [P.sb(f"wu{i}", [128, KT, 256], BF16) for i in range(2)]
    wd = [P.sb(f"wd{i}", [128, 2, D], BF16) for i in range(2)]
    gb_ps = P.ps("gb_ps", [128, 512])
    G_ps = [P.ps(f"G_ps{i}", [128, 512]) for i in range(2)]; U_ps = [P.ps(f"U_ps{i}", [128, 512]) for i in range(2)]
    O_ps = [P.ps(f"O_ps{i}", [128, 512]) for i in range(2)]
    gbs = [P.sb(f"gbs{i}", [128, 512]) for i in range(2)]
    sg = [P.sb(f"sg{i}", [128, 512]) for i in range(2)]; su = [P.sb(f"su{i}", [128, 512]) for i in range(2)]
    hid = [P.sb(f"hid{i}", [128, 2, 512], BF16) for i in range(2)]
    cnt = {"oi": 0, "ji": 0}
    items = [(ex, bi) for ex in range(32) for bi in range(len(BLOCKS))]
    NI = len(items)

    def load_w(ex):
        z = ex % 2
        P.dma("pool", wg[z][:], w_gate.ap[ex].rearrange("(kt p) n -> p kt n", p=128), writes=[wg[z]])
        P.dma("pool", wu[z][:], w_up.ap[ex].rearrange("(kt p) n -> p kt n", p=128), writes=[wu[z]])
        P.dma("pool", wd[z][:], w_down.ap[ex].rearrange("(kt p) n -> p kt n", p=128), writes=[wd[z]])

    def emit_front(n):
        ex, bi = items[n]
        t0, T = BLOCKS[bi]
        z = ex % 2; b2 = n % 2
        P.op("pe", lambda e, ex=ex, t0=t0, T=T: e.matmul(gb_ps[:, 0:T], lhsT=sel[:, ex, :], rhs=gT[:, t0:t0 + T], start=True, stop=True), reads=[sel, gT], writes=[gb_ps])
        P.op("act", lambda e, b2=b2, T=T: e.activation(out=gbs[b2][:, 0:T], in_=gb_ps[:, 0:T], func=AF.Copy), reads=[gb_ps], writes=[gbs[b2]])
        for j in range(2):
            j2 = cnt["ji"] % 2; cnt["ji"] += 1
            Gp = G_ps[j2]; Up = U_ps[j2]
            for kt in range(KT):
                P.op("pe", lambda e, Gp=Gp, z=z, kt=kt, j=j, t0=t0, T=T: e.matmul(Gp[:, 0:T], lhsT=wg[z][:, kt, j * 128:(j + 1) * 128], rhs=hb[:, kt, t0:t0 + T],
                                                                               start=(kt == 0), stop=(kt == KT - 1)), reads=[wg[z], hb], writes=[Gp], inc=(kt == KT - 1))
            for kt in range(KT):
                P.op("pe", lambda e, Up=Up, z=z, kt=kt, j=j, t0=t0, T=T: e.matmul(Up[:, 0:T], lhsT=wu[z][:, kt, j * 128:(j + 1) * 128], rhs=hb[:, kt, t0:t0 + T],
                                                                               start=(kt == 0), stop=(kt == KT - 1)), reads=[wu[z], hb], writes=[Up], inc=(kt == KT - 1))
            P.op("act", lambda e, Gp=Gp, j2=j2, T=T: e.activation(out=sg[j2][:, 0:T], in_=Gp[:, 0:T], func=AF.Silu), reads=[Gp], writes=[sg[j2]])
            P.op("dve", lambda e, Up=Up, j2=j2, T=T: e.tensor_tensor(out=su[j2][:, 0:T], in0=sg[j2][:, 0:T], in1=Up[:, 0:T], op=ALU.mult), reads=[sg[j2], Up], writes=[su[j2]])
            P.op("dve", lambda e, j2=j2, b2=b2, j=j, T=T: e.tensor_tensor(out=hid[b2][:, j, 0:T], in0=su[j2][:, 0:T], in1=gbs[b2][:, 0:T], op=ALU.mult),
                 reads=[su[j2], gbs[b2]], writes=[hid[b2]])

    def emit_back(n):
        ex, bi = items[n]
        t0, T = BLOCKS[bi]
        r = 1 if bi == 4 else 0
        z = ex % 2; b2 = n % 2
        for nt in range(KT):
            Op = O_ps[cnt["oi"] % 2]; cnt["oi"] += 1
            for j in range(2):
                P.op("pe", lambda e, Op=Op, z=z, j=j, nt=nt, b2=b2, T=T: e.matmul(Op[:, 0:T], lhsT=wd[z][:, j, nt * 128:(nt + 1) * 128], rhs=hid[b2][:, j, 0:T],
                                                                               start=(j == 0), stop=(j == 1)), reads=[wd[z], hid[b2]], writes=[Op], inc=(j == 1))
            P.op("dve", lambda e, Op=Op, nt=nt, t0=t0, T=T, r=r: e.scalar_tensor_tensor(out=xa[:, nt, t0:t0 + T], in0=Op[:, 0:T], scalar=modT[:, 40 + nt, r:r + 1],
                                                                                      in1=xa[:, nt, t0:t0 + T], op0=ALU.mult, op1=ALU.add), reads=[Op, modT, xa], writes=[xa])

    load_w(0); load_w(1)
    emit_front(0)
    for n in range(NI):
        if n + 1 < NI:
            emit_front(n + 1)
        emit_back(n)
        ex, bi = items[n]
        if bi == len(BLOCKS) - 1 and ex + 2 < 32:
            load_w(ex + 2)
    for kt in range(KT):
        P.dma("sp", xout.ap[:, kt, :], xa[:, kt, :], reads=[xa], is_output=True)
    P.emit()
    return nc


def feat_to_tok(xt):
    return np.ascontiguousarray(xt.transpose(2, 1, 0).reshape(xt.shape[2], D))


def c_consts():
    if "c" not in _CONST:
        sel = np.zeros((32, 32, 128), np.float32)
        for e in range(32):
            sel[e, e, :] = 1.0
        _CONST["c"] = dict(sel=sel, ident=np.eye(128, dtype=np.float32))
    return _CONST["c"]


def run_layer(i, xT_cores, inp, dbg=None):
    f32 = lambda a: np.ascontiguousarray(np.asarray(a, np.float32))
    resA = _run("A", build_stage_a, stage_a_inputs(xT_cores, inp["c"], inp["c_ctx"], f32(inp["w_ada"][i]), inp["b_ada"][i], inp["norm1"][i], f32(inp["w_in"][i])))
    PS = []
    for b in range(2):
        lat = np.concatenate([resA[4 * b + q]["p"][0:2048] for q in range(4)], 0)
        cx = np.concatenate([resA[4 * b + q]["p"][2048:2112] for q in range(4)], 0)
        PS.append(np.concatenate([cx, lat], 0))
    resB1 = _run("B1", build_stage_attn, stage_attn_inputs(PS, inp, i))
    resB2 = _run("B2", build_stage_ssd, stage_ssd_inputs(PS, inp, i))
    resB3 = _run("B3", build_stage_s5, stage_s5_inputs(PS, inp, i))
    cst = c_consts()
    wr = f32(np.concatenate([inp["moe_w_group"][i], inp["moe_w_expert"][i].transpose(1, 0, 2).reshape(D, 32)], axis=1))
    br = rep(np.concatenate([inp["moe_b_group"][i], inp["moe_b_expert"][i].reshape(32)]))
    mapsC1 = []
    cats = []
    for b in range(2):
        a = np.concatenate([untile(resB1[4 * b + h]["o_m"]) for h in range(4)], 1)
        sd = np.concatenate([untile(resB2[4 * b + h]["o_s"]) for h in range(4)], 1)
        s5 = np.concatenate([resB3[4 * b + g]["yT"].T for g in range(4)], 1)
        df = np.concatenate([untile(resB1[4 * b + h]["o_d"]) for h in range(4)], 1)
        cats.append(np.concatenate([a, sd, s5, df], 1))
    if dbg is not None:
        dbg["PS"] = PS; dbg["cats"] = cats
    for core in range(8):
        b, q = core // 4, core % 4
        cs = cats[b]
        ct = tok_to_feat(core_tokens(cs[256:], cs[:256], q))
        mapsC1.append({"catT": ct, "xT": xT_cores[core], "modT": resA[core]["modT"], "w_out": f32(inp["w_out"][i]), "w_glu": f32(inp["s5_w_glu"][i]),
                       "bgluT": vecT(inp["s5_b_glu"][i], 2), "gssdT": vecT(inp["ssd_norm"][i], 2), "norm2T": vecT(inp["norm2"][i]),
                       "wr": wr, "br": br, "ident": cst["ident"]})
    resC1 = _run("C1", build_stage_c1, mapsC1)
    if dbg is not None:
        dbg["C1"] = resC1
    wg, wu, wd = f32(inp["moe_w_gate"][i]), f32(inp["moe_w_up"][i]), f32(inp["moe_w_down"][i])
    mapsC2 = [{"hT": resC1[c]["hT"], "xmid": resC1[c]["xmid"], "gatesT": resC1[c]["gatesT"], "modT": resA[c]["modT"], "sel": cst["sel"],
               "w_gate": wg, "w_up": wu, "w_down": wd} for c in range(8)]
    resC2 = _run("C2", build_stage_c2, mapsC2)
    return [resC2[c]["xout"] for c in range(8)]


def kernel(**inputs):
    inp = {k: np.asarray(v) for k, v in inputs.items()}
    x = np.asarray(inp["x"], np.float32); ctx = np.asarray(inp["ctx"], np.float32)
    xT_cores = [tok_to_feat(core_tokens(x[c // 4], ctx[c // 4], c % 4)) for c in range(8)]
    for i in range(4):
        xT_cores = run_layer(i, xT_cores, inp)
    out = np.zeros((2, 8192, D), np.float32)
    for c in range(8):
        b, q = c // 4, c % 4
        out[b, q * 2048:(q + 1) * 2048] = feat_to_tok(xT_cores[c])[:2048]
    return out
```
